# Optimizing a Trainium2 kernel written in Bass

```python
import jax, jax.numpy as jnp
from jax import lax
import numpy as np

D_MODEL = 1024
BATCH = 8
SEQ = 2048
DEPTH = 4

CHUNK = 64
Q_BLOCK = 128
N_MIXERS = 3
NORM_EPS = 1e-6

RW_HEAD = 64
RW_HEADS = D_MODEL // RW_HEAD
DECAY_LORA = max(32, round(1.8 * D_MODEL ** 0.5 / 32) * 32)
AAA_LORA = max(32, round(1.8 * D_MODEL ** 0.5 / 32) * 32)
MV_LORA = max(32, round(1.3 * D_MODEL ** 0.5 / 32) * 32)
GATE_LORA = max(32, round(0.6 * D_MODEL ** 0.8 / 32) * 32)
GN_EPS = 64e-5

MLA_HEADS = 16
MLA_NOPE = 64
MLA_ROPE = 32
MLA_V = 64
MLA_Q_LORA = 768
MLA_KV_LORA = 256
MLA_SCALE = (MLA_NOPE + MLA_ROPE) ** -0.5
ROPE_BASE = 10000.0

FOX_HEADS = 16
FOX_HEAD = D_MODEL // FOX_HEADS
FOX_SCALE = FOX_HEAD ** -0.5

D_FF = 2816
N_EXPERTS = 8
TOP_K = 2
D_FF_EXPERT = 2816

N_A = (DEPTH + 2) // 3
N_B = (DEPTH + 1) // 3
N_C = DEPTH // 3
N_VRES = max(N_A - 1, 0)
N_DENSE = (DEPTH + 1) // 2
N_MOE = DEPTH // 2

kernel_name = 'hybrid_rwkv7_mla_fox_moe_adaln_trunk'


def rms_norm(x, g):
    xf = x.astype(jnp.float32)
    y = xf * lax.rsqrt(jnp.mean(xf * xf, axis=-1, keepdims=True) + NORM_EPS)
    return (y * g.astype(jnp.float32)).astype(x.dtype)


def ada_modulate(h, shift, scale):
    return h * (1 + scale[:, None, :]) + shift[:, None, :]


def apply_rope(x, positions):
    half = x.shape[-1] // 2
    inv_freq = ROPE_BASE ** (-jnp.arange(half, dtype=jnp.float32) / half)
    ang = positions.astype(jnp.float32)[..., None] * inv_freq
    cos = jnp.cos(ang)[:, :, None, :]
    sin = jnp.sin(ang)[:, :, None, :]
    xf = x.astype(jnp.float32)
    x1, x2 = xf[..., :half], xf[..., half:]
    return jnp.concatenate([x1 * cos - x2 * sin, x2 * cos + x1 * sin], axis=-1).astype(x.dtype)


def block_attention(q, k, v, per_frame, cum_log_f=None):
    seq = q.shape[1]
    outs = []
    for blk in range(seq // Q_BLOCK):
        q0, q1 = blk * Q_BLOCK, (blk + 1) * Q_BLOCK
        logits = jnp.einsum('bqhd,bkhd->bhqk', q[:, q0:q1], k[:, :q1]).astype(jnp.float32)
        t = jnp.arange(q0, q1)[:, None]
        s = jnp.arange(q1)[None, :]
        allowed = (s <= t) if per_frame else (s // CHUNK <= t // CHUNK)
        if cum_log_f is not None:
            logits = logits + cum_log_f[:, :, q0:q1, None] - cum_log_f[:, :, None, :q1]
        logits = jnp.where(allowed, logits, -jnp.inf)
        probs = jax.nn.softmax(logits, axis=-1).astype(v.dtype)
        outs.append(jnp.einsum('bhqk,bkhd->bqhd', probs, v[:, :q1]))
    return jnp.concatenate(outs, axis=1)


def rwkv7_time_mix(h, v_first, vres, mu, w_rkv, w_o, w0, w1, w2, a0, a1, a2, g1, g2,
                   k_k, k_a, r_k, lnx_g, lnx_b):
    B, S, D = h.shape
    H, N = RW_HEADS, RW_HEAD
    f32 = jnp.float32
    prev = jnp.pad(h, ((0, 0), (1, 0), (0, 0)))[:, :-1]
    delta = prev - h
    xr, xw, xk, xv, xa, xg = [h + delta * mu[n] for n in range(6)]
    r = xr @ w_rkv[0]
    k = xk @ w_rkv[1]
    v = xv @ w_rkv[2]
    w_raw = (w0 + jnp.tanh(xw @ w1) @ w2).astype(f32)
    log_w = -jax.nn.softplus(-w_raw) - 0.5
    decay = jnp.exp(-jnp.exp(log_w))
    if vres is None:
        v_first = v
    else:
        v0, v1, v2 = vres
        v = v + (v_first - v) * jax.nn.sigmoid(v0 + (xv @ v1) @ v2)
    a = jax.nn.sigmoid(a0 + (xa @ a1) @ a2)
    g = jax.nn.sigmoid(xg @ g1) @ g2
    split = lambda t: t.reshape(B, S, H, N)
    kk = split(k * k_k).astype(f32)
    kk = kk / jnp.maximum(jnp.sqrt(jnp.sum(kk * kk, axis=-1, keepdims=True)), 1e-12)
    k = k * (1 + (a - 1) * k_a)
    r4, k4, v4, a4, w4 = split(r), split(k), split(v), split(a), split(decay)

    def step(state, inp):
        r_t, w_t, k_t, v_t, kk_t, a_t = inp
        sa = jnp.einsum('bhvk,bhk->bhv', state, -kk_t)
        state = (state * w_t[:, :, None, :]
                 + sa[..., None] * (kk_t * a_t)[:, :, None, :]
                 + v_t[..., None] * k_t[:, :, None, :])
        return state, jnp.einsum('bhvk,bhk->bhv', state, r_t)

    to_time = lambda t: jnp.moveaxis(t.astype(f32), 1, 0)
    xs = (to_time(r4), to_time(w4), to_time(k4), to_time(v4), to_time(kk), to_time(a4))
    state0 = jnp.zeros((B, H, N, N), f32)
    _, y = lax.scan(step, state0, xs)
    y = jnp.moveaxis(y, 0, 1)
    mean = jnp.mean(y, axis=-1, keepdims=True)
    var = jnp.var(y, axis=-1, keepdims=True)
    y = ((y - mean) * lax.rsqrt(var + GN_EPS)).reshape(B, S, D) * lnx_g + lnx_b
    bonus = jnp.sum(r4 * k4 * r_k, axis=-1, keepdims=True) * v4
    y = (y + bonus.reshape(B, S, D)).astype(h.dtype)
    return (y * g) @ w_o, v_first


def mla_mix(h, positions, w_down, q_norm_g, kv_norm_g, w_uq, w_ukv, w_o):
    B, S, _ = h.shape
    down = h @ w_down
    c_q = rms_norm(down[..., :MLA_Q_LORA], q_norm_g)
    c_kv = rms_norm(down[..., MLA_Q_LORA:MLA_Q_LORA + MLA_KV_LORA], kv_norm_g)
    k_rope = apply_rope(down[..., MLA_Q_LORA + MLA_KV_LORA:][:, :, None, :], positions)
    q = (c_q @ w_uq).reshape(B, S, MLA_HEADS, MLA_NOPE + MLA_ROPE)
    q = jnp.concatenate([q[..., :MLA_NOPE], apply_rope(q[..., MLA_NOPE:], positions)], axis=-1) * MLA_SCALE
    kv = (c_kv @ w_ukv).reshape(B, S, MLA_HEADS, MLA_NOPE + MLA_V)
    k = jnp.concatenate([kv[..., :MLA_NOPE],
                         jnp.broadcast_to(k_rope, (B, S, MLA_HEADS, MLA_ROPE))], axis=-1)
    o = block_attention(q, k, kv[..., MLA_NOPE:], per_frame=False)
    return o.reshape(B, S, MLA_HEADS * MLA_V) @ w_o


def fox_mix(h, w_in, b_f, q_norm_g, k_norm_g, w_o):
    B, S, D = h.shape
    proj = h @ w_in
    q, k, v, f_logit, o_gate = jnp.split(proj, [D, 2 * D, 3 * D, 3 * D + FOX_HEADS], axis=-1)
    heads = lambda t: t.reshape(B, S, FOX_HEADS, FOX_HEAD)
    q = rms_norm(heads(q), q_norm_g) * FOX_SCALE
    k = rms_norm(heads(k), k_norm_g)
    log_f = jax.nn.log_sigmoid((f_logit + b_f).astype(jnp.float32))
    cum_log_f = jnp.transpose(lax.cumsum(log_f, axis=1), (0, 2, 1))
    o = block_attention(q, k, heads(v), per_frame=True, cum_log_f=cum_log_f)
    o = o.reshape(B, S, D) * jax.nn.sigmoid(o_gate)
    return o @ w_o


def swiglu(h, w_gate_up, w_down):
    gate, up = jnp.split(h @ w_gate_up, 2, axis=-1)
    return (jax.nn.silu(gate) * up) @ w_down


def moe_swiglu(h, w_router, b_router, w_gate_up, w_down):
    logits = (h @ w_router + b_router).astype(jnp.float32)
    top_vals, top_idx = lax.top_k(logits, TOP_K)
    top_w = jax.nn.softmax(top_vals, axis=-1)
    combine = jnp.sum(jax.nn.one_hot(top_idx, N_EXPERTS, dtype=jnp.float32) * top_w[..., None], axis=-2)
    out = jnp.zeros_like(h)
    for e in range(N_EXPERTS):
        out = out + combine[..., e:e + 1].astype(h.dtype) * swiglu(h, w_gate_up[e], w_down[e])
    return out


def setup_inputs(seed: int = 0) -> dict:
    key = jax.random.key(seed)
    ks = iter(jax.random.split(key, 64))
    D = D_MODEL

    def normal(shape, scale):
        return jax.random.normal(next(ks), shape, jnp.float32) * scale

    def gain(shape):
        return 1.0 + normal(shape, 0.02)

    x = normal((BATCH, SEQ, D), 1.0)
    c = normal((BATCH, D), 1.0)
    positions = (jax.random.randint(next(ks), (BATCH, 1), 0, 1024, dtype=jnp.int32)
                 + jnp.arange(SEQ, dtype=jnp.int32)[None, :])
    return {
        'x': x,
        'c': c,
        'positions': positions,
        'ada_w': normal((DEPTH, D, 6 * D), 0.5 * D ** -0.5),
        'ada_b': normal((DEPTH, 6 * D), 0.02),
        'norm_mix_g': gain((DEPTH, D)),
        'norm_ffn_g': gain((DEPTH, D)),
        'final_norm_g': gain((D,)),
        'rw_mu': jax.random.uniform(next(ks), (N_A, 6, D), jnp.float32),
        'rw_w_rkv': normal((N_A, 3, D, D), D ** -0.5),
        'rw_w_o': normal((N_A, D, D), D ** -0.5),
        'rw_w0': jax.random.uniform(next(ks), (N_A, D), jnp.float32, -6.5, -1.5),
        'rw_w1': normal((N_A, D, DECAY_LORA), D ** -0.5),
        'rw_w2': normal((N_A, DECAY_LORA, D), 0.1 * DECAY_LORA ** -0.5),
        'rw_a0': normal((N_A, D), 0.1),
        'rw_a1': normal((N_A, D, AAA_LORA), D ** -0.5),
        'rw_a2': normal((N_A, AAA_LORA, D), 0.1 * AAA_LORA ** -0.5),
        'rw_g1': normal((N_A, D, GATE_LORA), D ** -0.5),
        'rw_g2': normal((N_A, GATE_LORA, D), GATE_LORA ** -0.5),
        'rw_k_k': 0.85 + normal((N_A, D), 0.05),
        'rw_k_a': 1.0 + normal((N_A, D), 0.05),
        'rw_r_k': normal((N_A, RW_HEADS, RW_HEAD), 0.1),
        'rw_lnx_g': gain((N_A, D)),
        'rw_lnx_b': normal((N_A, D), 0.02),
        'rw_v0': normal((N_VRES, D), 0.1),
        'rw_v1': normal((N_VRES, D, MV_LORA), D ** -0.5),
        'rw_v2': normal((N_VRES, MV_LORA, D), 0.1 * MV_LORA ** -0.5),
        'mla_w_down': normal((N_B, D, MLA_Q_LORA + MLA_KV_LORA + MLA_ROPE), D ** -0.5),
        'mla_q_norm_g': gain((N_B, MLA_Q_LORA)),
        'mla_kv_norm_g': gain((N_B, MLA_KV_LORA)),
        'mla_w_uq': normal((N_B, MLA_Q_LORA, MLA_HEADS * (MLA_NOPE + MLA_ROPE)), MLA_Q_LORA ** -0.5),
        'mla_w_ukv': normal((N_B, MLA_KV_LORA, MLA_HEADS * (MLA_NOPE + MLA_V)), MLA_KV_LORA ** -0.5),
        'mla_w_o': normal((N_B, MLA_HEADS * MLA_V, D), (MLA_HEADS * MLA_V) ** -0.5),
        'fox_w_in': normal((N_C, D, 4 * D + FOX_HEADS), D ** -0.5),
        'fox_b_f': 3.0 + normal((N_C, FOX_HEADS), 0.5),
        'fox_q_norm_g': gain((N_C, FOX_HEAD)),
        'fox_k_norm_g': gain((N_C, FOX_HEAD)),
        'fox_w_o': normal((N_C, D, D), D ** -0.5),
        'ffn_w_gate_up': normal((N_DENSE, D, 2 * D_FF), D ** -0.5),
        'ffn_w_down': normal((N_DENSE, D_FF, D), D_FF ** -0.5),
        'moe_w_router': normal((N_MOE, D, N_EXPERTS), D ** -0.5),
        'moe_b_router': normal((N_MOE, N_EXPERTS), 0.01),
        'moe_w_gate_up': normal((N_MOE, N_EXPERTS, D, 2 * D_FF_EXPERT), D ** -0.5),
        'moe_w_down': normal((N_MOE, N_EXPERTS, D_FF_EXPERT, D), D_FF_EXPERT ** -0.5),
    }


def reference(x, c, positions, ada_w, ada_b, norm_mix_g, norm_ffn_g, final_norm_g,
              rw_mu, rw_w_rkv, rw_w_o, rw_w0, rw_w1, rw_w2, rw_a0, rw_a1, rw_a2, rw_g1, rw_g2,
              rw_k_k, rw_k_a, rw_r_k, rw_lnx_g, rw_lnx_b, rw_v0, rw_v1, rw_v2,
              mla_w_down, mla_q_norm_g, mla_kv_norm_g, mla_w_uq, mla_w_ukv, mla_w_o,
              fox_w_in, fox_b_f, fox_q_norm_g, fox_k_norm_g, fox_w_o,
              ffn_w_gate_up, ffn_w_down,
              moe_w_router, moe_b_router, moe_w_gate_up, moe_w_down):
    cond = jax.nn.silu(c)
    v_first = None
    for i in range(DEPTH):
        mod = cond @ ada_w[i] + ada_b[i]
        sh_m, sc_m, g_m, sh_f, sc_f, g_f = jnp.split(mod, 6, axis=-1)
        h = ada_modulate(rms_norm(x, norm_mix_g[i]), sh_m, sc_m)
        kind, j = i % N_MIXERS, i // N_MIXERS
        if kind == 0:
            vres = None if j == 0 else (rw_v0[j - 1], rw_v1[j - 1], rw_v2[j - 1])
            y, v_first = rwkv7_time_mix(h, v_first, vres, rw_mu[j], rw_w_rkv[j], rw_w_o[j],
                                        rw_w0[j], rw_w1[j], rw_w2[j], rw_a0[j], rw_a1[j], rw_a2[j],
                                        rw_g1[j], rw_g2[j], rw_k_k[j], rw_k_a[j], rw_r_k[j],
                                        rw_lnx_g[j], rw_lnx_b[j])
        elif kind == 1:
            y = mla_mix(h, positions, mla_w_down[j], mla_q_norm_g[j], mla_kv_norm_g[j],
                        mla_w_uq[j], mla_w_ukv[j], mla_w_o[j])
        else:
            y = fox_mix(h, fox_w_in[j], fox_b_f[j], fox_q_norm_g[j], fox_k_norm_g[j], fox_w_o[j])
        x = x + g_m[:, None, :] * y
        h = ada_modulate(rms_norm(x, norm_ffn_g[i]), sh_f, sc_f)
        if i % 2 == 0:
            y = swiglu(h, ffn_w_gate_up[i // 2], ffn_w_down[i // 2])
        else:
            y = moe_swiglu(h, moe_w_router[i // 2], moe_b_router[i // 2],
                           moe_w_gate_up[i // 2], moe_w_down[i // 2])
        x = x + g_f[:, None, :] * y
    return rms_norm(x, final_norm_g)
```

```python
import contextlib
import math
import numpy as np
import concourse.bass as bass
import concourse.mybir as mybir
from concourse.bass_utils import run_bass_kernel_spmd

F32 = mybir.dt.float32
BF16 = mybir.dt.bfloat16
I32 = mybir.dt.int32
AF = mybir.ActivationFunctionType
ALU = mybir.AluOpType
AX = mybir.AxisListType

S = 2048
D = 1024
DC = 8
NL = 4
DFF = 2816
FC = 22
NE = 8
EPS = 1e-6
NEG = -30000.0
SB_BASE = 16512
SB_TOP = 229344


class Dep:
    __slots__ = ("w", "r")

    def __init__(self):
        self.w = None
        self.r = {}


class Sched:
    def __init__(self, nc, n_dma_sems=24):
        self.nc = nc
        self.eng = {"pe": nc.tensor, "dve": nc.vector, "act": nc.scalar,
                    "pool": nc.gpsimd, "sp": nc.sync}
        self.sem = {e: nc.alloc_semaphore(name=f"s_{e}") for e in self.eng}
        self.cnt = {e: 0 for e in self.eng}
        self.dsem = [nc.alloc_semaphore(name=f"d_{i}") for i in range(n_dma_sems)]
        self.dcnt = [0] * n_dma_sems
        self.dlast = [None] * n_dma_sems
        self.dnext = 0
        self.seen = {e: {} for e in self.eng}
        self.nins = 0

    def _wait(self, e, key, val):
        if self.seen[e].get(key, 0) >= val:
            return
        sem = self.sem[key[1]] if key[0] == "e" else self.dsem[key[1]]
        self.eng[e].wait_ge(sem, val)
        self.seen[e][key] = val

    def _deps(self, e, reads, writes):
        need = {}
        for d in reads:
            if d.w is not None:
                k, v = d.w
                if need.get(k, 0) < v:
                    need[k] = v
        for d in writes:
            if d.w is not None:
                k, v = d.w
                if need.get(k, 0) < v:
                    need[k] = v
            for k, v in d.r.items():
                if need.get(k, 0) < v:
                    need[k] = v
        for k, v in need.items():
            self._wait(e, k, v)

    def _commit(self, tok, reads, writes):
        k, v = tok
        for d in reads:
            if d.r.get(k, 0) < v:
                d.r[k] = v
        for d in writes:
            d.w = tok
            d.r = {}

    def op(self, e, fn, reads=(), writes=()):
        self._deps(e, reads, writes)
        ins = fn(self.eng[e])
        self.cnt[e] += 1
        ins.then_inc(self.sem[e], 1)
        tok = (("e", e), self.cnt[e])
        self._commit(tok, reads, writes)
        self.nins += 1
        return tok

    def mm(self, out, lhsT, rhs, start, stop, reads=(), out_dep=None, inc=None):
        e = "pe"
        if inc is None:
            inc = stop
        self._deps(e, reads, [])
        if start and out_dep is not None:
            need = dict(out_dep.r)
            if out_dep.w is not None and out_dep.w[0] != ("e", "pe"):
                k, v = out_dep.w
                if need.get(k, 0) < v:
                    need[k] = v
            for k, v in need.items():
                self._wait(e, k, v)
        ins = self.eng[e].matmul(out, lhsT, rhs, start=start, stop=stop)
        if inc:
            self.cnt[e] += 1
            ins.then_inc(self.sem[e], 1)
            tok = (("e", e), self.cnt[e])
        else:
            tok = (("e", e), self.cnt[e] + 1)
        self._commit(tok, reads, [out_dep] if stop else [])
        self.nins += 1
        return tok

    def dma(self, q, out, in_, reads=(), writes=(), **kw):
        i = self.dnext
        self.dnext = (i + 1) % len(self.dsem)
        self._deps(q, reads, writes)
        if self.dlast[i] is not None:
            self._wait(q, *self.dlast[i])
        ins = self.eng[q].dma_start(out=out, in_=in_, **kw)
        self.dcnt[i] += 16
        ins.then_inc(self.dsem[i], 16)
        tok = (("d", i), self.dcnt[i])
        self.dlast[i] = tok
        self._commit(tok, reads, writes)
        self.nins += 1
        return tok

    def barrier(self, engines=None):
        engines = engines or list(self.eng)
        for e in engines:
            for f in self.eng:
                if self.cnt[f] > 0:
                    self._wait(e, ("e", f), self.cnt[f])
            for t in self.dlast:
                if t is not None:
                    self._wait(e, *t)


class Stream:
    def __init__(self, bufs, items, load_fn):
        self.bufs = bufs
        self.items = items
        self.load_fn = load_fn
        self.n = 0
        self.issued = 0

    def get(self):
        nb = len(self.bufs)
        while self.issued < len(self.items) and self.issued < self.n + nb:
            t, d = self.bufs[self.issued % nb]
            self.load_fn(self.items[self.issued], t, d)
            self.issued += 1
        t, d = self.bufs[self.n % nb]
        self.n += 1
        return t, d


def _fm(v):
    v = np.asarray(v, np.float32).reshape(-1)
    n = v.size // 128
    return np.ascontiguousarray(v.reshape(n, 128).T)


class Table:
    def __init__(self):
        self.cols = {}
        self.parts = []
        self.n = 0

    def add(self, name, arr):
        arr = np.asarray(arr, np.float32)
        assert arr.shape[0] == 128, (name, arr.shape)
        self.cols[name] = (self.n, arr.shape[1])
        self.parts.append(arr)
        self.n += arr.shape[1]

    def build(self):
        return np.ascontiguousarray(np.concatenate(self.parts, axis=1))


def _pad128(a):
    out = np.zeros((128, a.shape[1]), np.float32)
    out[: a.shape[0]] = a
    return out


def const_table():
    t = Table()
    p = np.arange(128)
    t.add("ident", np.eye(128, dtype=np.float32))
    t.add("ones", np.ones((128, 128), np.float32))
    t.add("bd", (p[:, None] // 64 == p[None, :] // 64).astype(np.float32))
    t.add("identh", (p[:, None] % 64 == np.arange(64)[None, :]).astype(np.float32))
    j = np.arange(64)[:, None]
    tt = np.arange(64)[None, :]
    t.add("mask_ar", _pad128(np.concatenate([(j < tt), (j <= tt)], axis=1).astype(np.float32)))
    t.add("maskT", _pad128((tt < j).astype(np.float32)))
    qi = p[:, None]
    kj = p[None, :]
    t.add("cmask", np.where(kj <= qi, 0.0, NEG).astype(np.float32))
    t.add("mmask", np.where((kj // 64) <= (qi // 64), 0.0, NEG).astype(np.float32))
    t.add("invf", (10000.0 ** (-(p % 16).astype(np.float64) / 16.0)).astype(np.float32)[:, None])
    sel8 = np.zeros((128, 8 * 128), np.float32)
    for e in range(8):
        sel8[e, e * 128:(e + 1) * 128] = 1.0
    t.add("sel8", sel8)
    sel16 = np.zeros((128, 16 * 128), np.float32)
    for e in range(16):
        sel16[e, e * 128:(e + 1) * 128] = 1.0
    t.add("sel16", sel16)
    return t


def param_table(inp, b):
    t = Table()
    t.add("c", _fm(inp["c"][b]))
    for i in range(NL):
        t.add(f"adab{i}", _fm(inp["ada_b"][i]))
        t.add(f"nmg{i}", _fm(inp["norm_mix_g"][i]))
        t.add(f"nfg{i}", _fm(inp["norm_ffn_g"][i]))
    t.add("fng", _fm(inp["final_norm_g"]))
    for j in range(2):
        t.add(f"mu{j}", _fm(inp["rw_mu"][j]))
        for nm in ("w0", "a0", "k_k", "k_a", "r_k", "lnx_g", "lnx_b"):
            t.add(f"{nm}{j}", _fm(inp["rw_" + nm][j]))
    t.add("v0", _fm(inp["rw_v0"][0]))
    t.add("mqg", _fm(inp["mla_q_norm_g"][0]))
    t.add("mkvg", _fm(inp["mla_kv_norm_g"][0]))
    t.add("fbf", _pad128(np.asarray(inp["fox_b_f"][0], np.float32).reshape(16, 1)))
    t.add("fqg", np.tile(np.asarray(inp["fox_q_norm_g"][0], np.float32).reshape(64, 1), (2, 1)))
    t.add("fkg", np.tile(np.asarray(inp["fox_k_norm_g"][0], np.float32).reshape(64, 1), (2, 1)))
    for j in range(2):
        t.add(f"mbr{j}", np.tile(np.asarray(inp["moe_b_router"][j], np.float32).reshape(1, 8), (128, 1)))
        wr = np.asarray(inp["moe_w_router"][j], np.float32)
        t.add(f"mwr{j}", np.ascontiguousarray(wr.reshape(8, 128, 8).transpose(1, 0, 2).reshape(128, 64)))
    return t


_CT = const_table()


W_SHAPES = {
    "ada_w": [4, 1024, 6144],
    "rw_w_rkv": [2, 3, 1024, 1024], "rw_w_o": [2, 1024, 1024],
    "rw_w1": [2, 1024, 64], "rw_w2": [2, 64, 1024],
    "rw_a1": [2, 1024, 64], "rw_a2": [2, 64, 1024],
    "rw_g1": [2, 1024, 160], "rw_g2": [2, 160, 1024],
    "rw_v1": [1, 1024, 32], "rw_v2": [1, 32, 1024],
    "mla_w_down": [1, 1024, 1056], "mla_w_uq": [1, 768, 1536],
    "mla_w_ukv": [1, 256, 2048], "mla_w_o": [1, 1024, 1024],
    "fox_w_in": [1, 1024, 4112], "fox_w_o": [1, 1024, 1024],
    "ffn_w_gate_up": [2, 1024, 5632], "ffn_w_down": [2, 2816, 1024],
    "moe_w_gate_up": [2, 8, 1024, 5632], "moe_w_down": [2, 8, 2816, 1024],
}


class Prog:
    def __init__(self, pcols, npcols, plan=None, x_in_dbg=False):
        self.plan = plan
        nc = bass.Bass("TRN2", target_bir_lowering=False)
        self.nc = nc
        self.s = Sched(nc)
        self.pcols = pcols
        self.ccols = _CT.cols
        d = {}
        d["xT"] = nc.dram_tensor("xT", [D, S], F32, kind="ExternalInput").ap()
        d["pos"] = nc.dram_tensor("pos", [1, S], I32, kind="ExternalInput").ap()
        d["ptab"] = nc.dram_tensor("ptab", [128, npcols], F32, kind="ExternalInput").ap()
        d["ctab"] = nc.dram_tensor("ctab", [128, _CT.n], F32, kind="ExternalInput").ap()
        for k, shp in W_SHAPES.items():
            d[k] = nc.dram_tensor(k, shp, F32, kind="ExternalInput").ap()
        d["outT"] = nc.dram_tensor("outT", [D, S], F32, kind="ExternalOutput").ap()
        self.d = d
        self.sb_ptr = SB_BASE
        self.uid = 0
        self.X = self.salloc("X", [128, DC, S], F32)
        self.Xd = [[Dep() for _ in range(4)] for _ in range(DC)]
        self.pt = self.salloc("pt", [128, npcols], F32)
        self.ptd = Dep()
        nct = self.ccols["sel8"][0]
        self.ct = self.salloc("ct", [128, nct], F32)
        self.ctd = Dep()
        self.identb = self.salloc("identb", [128, 128], BF16)
        self.cmaskb = self.salloc("cmaskb", [128, 128], BF16)
        self.mmaskb = self.salloc("mmaskb", [128, 128], BF16)
        self.cbd = Dep()
        self.mod = self.salloc("mod", [128, NL * 48], F32)
        self.modd = Dep()
        self.amod = self.salloc("amod", [128, NL * 16 + 8], F32)
        self.amodd = Dep()
        self.cond = self.salloc("cond", [128, 8], F32)
        self.epsc = self.salloc("epsc", [128, 4], F32)
        self.s.op("dve", lambda e: e.memset(self.epsc[:, 0:1], EPS))
        self.s.op("dve", lambda e: e.memset(self.epsc[:, 1:2], 64e-5))
        self.s.op("dve", lambda e: e.memset(self.epsc[:, 2:3], 1.0))
        self.s.op("dve", lambda e: e.memset(self.epsc[:, 3:4], 0.0))
        self.condd = Dep()
        self.psA = nc.alloc_psum_tensor("psA", [128, 2048], F32)
        self.psB = [nc.alloc_psum_tensor(f"psB{i}", [128, 512], F32) for i in range(4)]
        self.bd_ = [Dep() for _ in range(8)]
        self.rr = 0

    def bank(self, i):
        if i < 4:
            return self.psA[:, i * 512:(i + 1) * 512], self.bd_[i]
        return self.psB[i - 4][:, :], self.bd_[i]

    def P(self, name, a=0, b=None):
        st, n = self.pcols[name]
        b = n if b is None else b
        return self.pt[:, st + a:st + b]

    def C(self, name, a=0, b=None, rows=None):
        st, n = self.ccols[name]
        b = n if b is None else b
        if rows is None:
            return self.ct[:, st + a:st + b]
        return self.ct[rows[0]:rows[1], st + a:st + b]

    def modc(self, i, w, c0=0, c1=8):
        return self.mod[:, i * 48 + w * 8 + c0:i * 48 + w * 8 + c1]

    def salloc(self, name, shape, dt, at=None):
        nbytes = int(np.prod(shape[1:])) * (2 if dt == BF16 else 4)
        nbytes = (nbytes + 31) // 32 * 32
        if at is None:
            off = self.sb_ptr
            self.sb_ptr += nbytes
            assert self.sb_ptr <= SB_TOP, f"SBUF overflow allocating {name}: {self.sb_ptr} > {SB_TOP}"
        else:
            off = at
        self.uid += 1
        t = self.nc.alloc_sbuf_tensor_at(f"{name}_{self.uid}", list(shape), dt, offset=off)
        return t

    @contextlib.contextmanager
    def phase(self):
        mark = self.sb_ptr
        yield self.salloc
        self.s.barrier()
        self.sb_ptr = mark

    def prelude(self, layers):
        s, nc, d = self.s, self.nc, self.d
        s.dma("sp", self.pt[:, :], d["ptab"][:, :], writes=[self.ptd])
        s.dma("sp", self.ct[:, :], d["ctab"][:, 0:self.ccols["sel8"][0]], writes=[self.ctd])
        xv = d["xT"].rearrange("(c p) s -> p c s", p=128)
        for c in range(DC):
            s.dma("sp" if c % 2 == 0 else "act", self.X[:, c, :], xv[:, c, :], writes=self.Xd[c])
        s.op("dve", lambda e: e.tensor_copy(self.identb[:, :], self.C("ident")), reads=[self.ctd], writes=[self.cbd])
        s.op("dve", lambda e: e.tensor_copy(self.cmaskb[:, :], self.C("cmask")), reads=[self.ctd], writes=[self.cbd])
        s.op("dve", lambda e: e.tensor_copy(self.mmaskb[:, :], self.C("mmask")), reads=[self.ctd], writes=[self.cbd])
        s.op("act", lambda e: e.activation(out=self.cond[:, :], in_=self.P("c"), func=AF.Silu),
             reads=[self.ptd], writes=[self.condd])
        with self.phase() as alloc:
            NB = 768
            bufs = [(alloc(f"adaw{i}", [128, 8, NB], F32), Dep()) for i in range(2)]
            items = [(i, cb) for i in layers for cb in range(8)]

            def load(it, t, dep):
                i, cb = it
                src = d["ada_w"][i].rearrange("(kc p) n -> p kc n", p=128)
                s.dma("sp", t[:, :, :], src[:, :, cb * NB:(cb + 1) * NB], writes=[dep])
            st = Stream(bufs, items, load)
            pb, pbd = self.bank(4)
            for i in layers:
                for cb in range(8):
                    t, dep = st.get()
                    for oc in range(6):
                        col = cb * 6 + oc
                        for kc in range(8):
                            s.mm(pb[:, col:col + 1], t[:, kc, oc * 128:(oc + 1) * 128], self.cond[:, kc:kc + 1],
                                 kc == 0, kc == 7, reads=[dep, self.condd], out_dep=pbd)
                s.op("dve", lambda e: e.tensor_tensor(out=self.mod[:, i * 48:(i + 1) * 48], in0=pb[:, 0:48],
                                                      in1=self.P(f"adab{i}"), op=ALU.add),
                     reads=[pbd, self.ptd], writes=[self.modd])
                s.op("dve", lambda e: e.scalar_tensor_tensor(out=self.amod[:, i * 16:i * 16 + 8], in0=self.modc(i, 1), scalar=1.0,
                                                             in1=self.P(f"nmg{i}"), op0=ALU.add, op1=ALU.mult),
                     reads=[self.modd, self.ptd], writes=[self.amodd])
                s.op("dve", lambda e: e.scalar_tensor_tensor(out=self.amod[:, i * 16 + 8:i * 16 + 16], in0=self.modc(i, 4), scalar=1.0,
                                                             in1=self.P(f"nfg{i}"), op0=ALU.add, op1=ALU.mult),
                     reads=[self.modd, self.ptd], writes=[self.amodd])
            s.op("dve", lambda e: e.memset(self.amod[:, NL * 16:NL * 16 + 8], 0.0), writes=[self.amodd])

    def norm_tmps(self, alloc):
        return {"sq": [(alloc(f"sq{i}", [128, 512], F32), Dep()) for i in range(2)],
                "tmp": [(alloc(f"nt{i}", [128, 512], F32), Dep()) for i in range(2)],
                "rstd": (alloc("rstd", [128, 512], F32), Dep())}

    def norm_mod(self, nt, A, B, t0, nblk, out_fn):
        s = self.s
        sq, tmp = nt["sq"], nt["tmp"]
        rstd, rstdd = nt["rstd"]
        pb, pbd = self.bank(7)
        for tb in range(nblk):
            tsl = slice(t0 + tb * 512, t0 + (tb + 1) * 512)
            xb = (t0 // 512) + tb
            for c in range(DC):
                q, qd = sq[c % 2]
                s.op("act", lambda e: e.activation(out=q[:, :], in_=self.X[:, c, tsl], func=AF.Square, scale=1.0 / 32.0),
                     reads=[self.Xd[c][xb]], writes=[qd])
                s.mm(pb, self.C("ones"), q[:, :], c == 0, c == DC - 1, reads=[qd, self.ctd], out_dep=pbd, inc=True)
            s.op("act", lambda e: e.activation(out=rstd[:, :], in_=pb, func=AF.Sqrt, bias=self.epsc[:, 0:1]),
                 reads=[pbd], writes=[rstdd])
            s.op("dve", lambda e: e.reciprocal(out=rstd[:, :], in_=rstd[:, :]), reads=[rstdd], writes=[rstdd])
            for c in range(DC):
                t, td = tmp[c % 2]
                s.op("pool", lambda e: e.tensor_tensor(out=t[:, :], in0=self.X[:, c, tsl], in1=rstd[:, :], op=ALU.mult),
                     reads=[self.Xd[c][xb], rstdd], writes=[td])
                out_fn(c, tb, t, td, A[:, c:c + 1], B[:, c:c + 1])

    def ffn_expert(self, hT, hTd, act, actd, gu_stream, wd_stream, gf, t0, sgb, cb=None):
        s = self.s
        k = 0
        for g in range(FC // 2):
            wt, wdep = gu_stream.get()
            for j in range(2):
                fc = g * 2 + j
                for sb in range(2):
                    bg, bgd = self.bank(0 + (k % 2))
                    bu, bud = self.bank(2 + (k % 2))
                    tsl = slice(sb * 512, (sb + 1) * 512)
                    for kc in range(DC):
                        s.mm(bg, wt[:, kc, 0, j * 128:(j + 1) * 128], hT[:, kc, tsl], kc == 0, kc == DC - 1,
                             reads=[wdep, hTd[sb]], out_dep=bgd)
                    for kc in range(DC):
                        s.mm(bu, wt[:, kc, 1, j * 128:(j + 1) * 128], hT[:, kc, tsl], kc == 0, kc == DC - 1,
                             reads=[wdep, hTd[sb]], out_dep=bud)
                    sg, sgd = sgb[k % 2]
                    s.op("act", lambda e: e.activation(out=sg[:, :], in_=bg, func=AF.Silu), reads=[bgd], writes=[sgd])
                    if cb is not None:
                        cbt, cbdep = cb[sb]
                        s.op("pool", lambda e: e.tensor_tensor(out=sg[:, :], in0=sg[:, :], in1=cbt[:, :], op=ALU.mult),
                             reads=[cbdep, sgd], writes=[sgd])
                    s.op("dve", lambda e: e.tensor_tensor(out=act[:, fc, tsl], in0=sg[:, :], in1=bu, op=ALU.mult),
                         reads=[sgd, bud], writes=[actd[fc][sb]])
                    k += 1
        for dc in range(DC):
            wdt, wddep = wd_stream.get()
            for sb in range(2):
                bo, bod = self.bank(4 + ((dc * 2 + sb) % 2))
                tsl = slice(sb * 512, (sb + 1) * 512)
                for fc in range(FC):
                    s.mm(bo, wdt[:, fc, :], act[:, fc, tsl], fc == 0, fc == FC - 1,
                         reads=[wddep, actd[fc][sb]], out_dep=bod)
                xsl = slice(t0 + sb * 512, t0 + (sb + 1) * 512)
                xb = (t0 // 512) + sb
                s.op("dve", lambda e: e.scalar_tensor_tensor(out=self.X[:, dc, xsl], in0=bo, scalar=gf[:, dc:dc + 1],
                                                             in1=self.X[:, dc, xsl], op0=ALU.mult, op1=ALU.add),
                     reads=[bod, self.modd, self.Xd[dc][xb]], writes=[self.Xd[dc][xb]])

    def ffn_layer(self, i):
        s, d = self.s, self.d
        moe = (i % 2 == 1)
        li = i // 2
        A = self.amod[:, i * 16 + 8:i * 16 + 16]
        B = self.modc(i, 3)
        gf = self.modc(i, 5)
        with self.phase() as alloc:
            hT = alloc("f_hT", [128, DC, 1024], BF16)
            hTd = [Dep(), Dep()]
            act_off = self.sb_ptr
            act = alloc("f_act", [128, FC, 1024], BF16)
            nt = self.norm_tmps(alloc)
            actd = [[Dep(), Dep()] for _ in range(FC)]
            gub = [(alloc(f"f_gu{k}", [128, DC, 2, 256], BF16), Dep()) for k in range(3)]
            wdb = [(alloc(f"f_wd{k}", [128, FC, 128], BF16), Dep()) for k in range(2)]
            sgb = [(alloc(f"f_sg{k}", [128, 512], F32), Dep()) for k in range(2)]
            experts = list(range(NE)) if moe else [None]

            def wgu_ap(e):
                return d["moe_w_gate_up"][li, e] if moe else d["ffn_w_gate_up"][li]

            def wd_ap(e):
                return d["moe_w_down"][li, e] if moe else d["ffn_w_down"][li]

            def load_gu(it, t, dep):
                e, g = it
                src = wgu_ap(e).rearrange("(kc p) n -> p kc n", p=128)
                s.dma("pool", t[:, :, 0, :], src[:, :, g * 256:(g + 1) * 256], writes=[dep])
                s.dma("pool", t[:, :, 1, :], src[:, :, DFF + g * 256:DFF + (g + 1) * 256], writes=[dep])

            def load_wd(it, t, dep):
                e, dc = it
                src = wd_ap(e).rearrange("(fc p) n -> p fc n", p=128)
                s.dma("pool", t[:, :, :], src[:, :, dc * 128:(dc + 1) * 128], writes=[dep])

            gu_items = [(e, g) for _ in range(2) for e in experts for g in range(FC // 2)]
            wd_items = [(e, dc) for _ in range(2) for e in experts for dc in range(DC)]
            gu_stream = Stream(gub, gu_items, load_gu)
            wd_stream = Stream(wdb, wd_items, load_wd)
            if moe:
                h32 = self.salloc("f_h32", [128, DC, 1024], F32, at=act_off)
                h32d = Dep()
                combT = alloc("f_combT", [8, 1024], F32)
                combTd = Dep()
                sel8 = alloc("f_sel8", [8, 8 * 128], F32)
                sel8d = Dep()
                st8 = self.ccols["sel8"][0]
                s.dma("sp", sel8[:, :], d["ctab"][0:8, st8:st8 + 1024], writes=[sel8d])
                cbb = [(alloc(f"f_cb{k}", [128, 512], F32), Dep()) for k in range(4)]
                rt = {nm: (alloc(f"f_rt_{nm}", [128, 8, 8], F32), Dep()) for nm in ("lg", "z", "m", "z2", "ez")}
                rs = {nm: (alloc(f"f_rs_{nm}", [128, 8], F32), Dep()) for nm in ("m1", "m2", "ss")}
            for ps_ in range(2):
                t0 = ps_ * 1024

                def out_fn(c, tb, t, td, Ac, Bc):
                    s.op("act", lambda e: e.activation(out=hT[:, c, tb * 512:(tb + 1) * 512], in_=t[:, :], func=AF.Identity,
                                                       bias=Bc, scale=Ac),
                         reads=[td, self.amodd, self.modd], writes=[hTd[tb]])
                    if moe:
                        s.op("act", lambda e: e.activation(out=h32[:, c, tb * 512:(tb + 1) * 512], in_=t[:, :], func=AF.Identity, bias=Bc, scale=Ac),
                             reads=[td, self.amodd, self.modd], writes=[h32d])
                if moe:
                    s.barrier()
                self.norm_mod(nt, A, B, t0, 2, out_fn)
                if moe:
                    self.router(li, h32, h32d, combT, combTd, rt, rs)
                    s.barrier()
                for e in experts:
                    cb = None
                    if moe:
                        cb = []
                        for sb in range(2):
                            cbt, cbdep = cbb[(e * 2 + sb) % 4]
                            pb, pbd = self.bank(6)
                            s.mm(pb, sel8[0:8, e * 128:(e + 1) * 128], combT[0:8, sb * 512:(sb + 1) * 512], True, True,
                                 reads=[sel8d, combTd], out_dep=pbd)
                            s.op("act", lambda e_: e_.activation(out=cbt[:, :], in_=pb, func=AF.Copy), reads=[pbd], writes=[cbdep])
                            cb.append((cbt, cbdep))
                    self.ffn_expert(hT, hTd, act, actd, gu_stream, wd_stream, gf, t0, sgb, cb)

    def router(self, li, h32, h32d, combT, combTd, rt, rs):
        s = self.s
        G = 8
        pb, pbd = self.bank(6)
        mwr = self.P(f"mwr{li}")
        lg, lgd = rt["lg"]; z, zd = rt["z"]; m, md = rt["m"]; z2, z2d = rt["z2"]; ez, ezd = rt["ez"]
        m1, m1d = rs["m1"]; m2, m2d = rs["m2"]; ss, ssd = rs["ss"]
        for g in range(G):
            for c in range(DC):
                s.mm(pb[:, g * 8:(g + 1) * 8], h32[:, c, g * 128:(g + 1) * 128], mwr[:, c * 8:(c + 1) * 8], c == 0, c == DC - 1,
                     reads=[h32d, self.ptd], out_dep=pbd)
        bc = lambda t: t[:, :].unsqueeze(2).broadcast_to([128, G, 8])
        pv = pb[:, 0:G * 8].rearrange("p (g e) -> p g e", e=8)
        s.op("dve", lambda e: e.tensor_tensor(out=lg[:, :, :], in0=pv, in1=self.P(f"mbr{li}").unsqueeze(1).broadcast_to([128, G, 8]), op=ALU.add),
             reads=[pbd, self.ptd], writes=[lgd])
        s.op("dve", lambda e: e.reduce_max(out=m1[:, :], in_=lg[:, :, :], axis=AX.X), reads=[lgd], writes=[m1d])
        s.op("dve", lambda e: e.tensor_tensor(out=z[:, :, :], in0=lg[:, :, :], in1=bc(m1), op=ALU.subtract), reads=[lgd, m1d], writes=[zd])
        s.op("dve", lambda e: e.tensor_single_scalar(out=m[:, :, :], in_=z[:, :, :], scalar=0.0, op=ALU.is_ge), reads=[zd], writes=[md])
        s.op("dve", lambda e: e.scalar_tensor_tensor(out=z2[:, :, :], in0=m[:, :, :], scalar=-1e30, in1=z[:, :, :], op0=ALU.mult, op1=ALU.add),
             reads=[md, zd], writes=[z2d])
        s.op("dve", lambda e: e.reduce_max(out=m2[:, :], in_=z2[:, :, :], axis=AX.X), reads=[z2d], writes=[m2d])
        s.op("dve", lambda e: e.tensor_tensor(out=m[:, :, :], in0=z[:, :, :], in1=bc(m2), op=ALU.is_ge), reads=[zd, m2d, md], writes=[md])
        s.op("act", lambda e: e.activation(out=ez[:, :, :], in_=z[:, :, :], func=AF.Exp), reads=[zd], writes=[ezd])
        s.op("dve", lambda e: e.tensor_tensor(out=ez[:, :, :], in0=ez[:, :, :], in1=m[:, :, :], op=ALU.mult), reads=[ezd, md], writes=[ezd])
        s.op("dve", lambda e: e.reduce_sum(out=ss[:, :], in_=ez[:, :, :], axis=AX.X), reads=[ezd], writes=[ssd])
        s.op("dve", lambda e: e.reciprocal(out=ss[:, :], in_=ss[:, :]), reads=[ssd], writes=[ssd])
        s.op("dve", lambda e: e.tensor_tensor(out=ez[:, :, :], in0=ez[:, :, :], in1=bc(ss), op=ALU.mult), reads=[ezd, ssd], writes=[ezd])
        for half in range(2):
            pt_, ptd_ = self.bank(7 if half == 0 else 5)
            for gg in range(4):
                g = half * 4 + gg
                s.mm(pt_[0:8, gg * 128:(gg + 1) * 128], ez[:, g, :], self.C("ident"), True, gg == 3, reads=[ezd, self.ctd], out_dep=ptd_)
            s.op("dve", lambda e: e.tensor_copy(out=combT[0:8, half * 512:(half + 1) * 512], in_=pt_[0:8, 0:512]), reads=[ptd_], writes=[combTd])

    def final(self, do_norm=True):
        s, d = self.s, self.d
        ov = d["outT"].rearrange("(c p) s -> p c s", p=128)
        with self.phase() as alloc:
            if not do_norm:
                for c in range(DC):
                    s.dma("sp", ov[:, c, :], self.X[:, c, :], reads=self.Xd[c])
                return
            ob = [(alloc(f"ob{k}", [128, 512], F32), Dep()) for k in range(3)]
            cnt = [0]

            def out_fn(c, tb, t, td, Ac, Bc):
                o, od = ob[cnt[0] % 3]
                cnt[0] += 1
                s.op("act", lambda e: e.activation(out=o[:, :], in_=t[:, :], func=AF.Identity, bias=Bc, scale=Ac),
                     reads=[td, self.amodd, self.ptd], writes=[od])
                s.dma("sp", ov[:, c, tb * 512:(tb + 1) * 512], o[:, :], reads=[od])
            self.norm_mod(self.norm_tmps(alloc), self.P("fng"), self.amod[:, NL * 16:NL * 16 + 8], 0, 4, out_fn)

    def run_plan(self, plan, final_norm=True):
        layers = sorted({i for _, i in plan})
        self.prelude(layers)
        for kind, i in plan:
            if kind == "ffn":
                self.ffn_layer(i)
            elif kind == "mix":
                self.mix_layer(i)
        self.final(final_norm)
        self.s.barrier(["sp"])

    def mix_layer(self, i):
        kind = i % 3
        if kind == 0:
            self.rwkv_layer(i)
        elif kind == 1:
            self.mla_layer(i)
        else:
            self.fox_layer(i)


FULL_PLAN = [(k, i) for i in range(NL) for k in ("mix", "ffn")]


def build(pcols, npcols, plan=None, final_norm=True):
    plan = FULL_PLAN if plan is None else plan
    p = Prog(pcols, npcols)
    p.run_plan(plan, final_norm)
    return p.nc


def make_in_maps(inp, x_override=None):
    ctab = _CT.build()
    maps = []
    pcols = None
    wts = {k: np.ascontiguousarray(np.asarray(inp[k], np.float32)) for k in W_SHAPES}
    for b in range(8):
        pt = param_table(inp, b)
        pcols = pt.cols
        x = np.asarray(inp["x"][b] if x_override is None else x_override[b], np.float32)
        m = {"xT": np.ascontiguousarray(x.T),
             "pos": np.ascontiguousarray(np.asarray(inp["positions"][b], np.int32).reshape(1, S)),
             "ptab": pt.build(), "ctab": ctab}
        m.update(wts)
        maps.append(m)
    return maps, pcols, maps[0]["ptab"].shape[1]


def kernel(**inputs):
    maps, pcols, npc = make_in_maps(inputs)
    nc = build(pcols, npc)
    res = run_bass_kernel_spmd(nc, maps, core_ids=list(range(8)))
    out = np.stack([np.ascontiguousarray(r["outT"].T) for r in res.results], axis=0)
    return out.astype(np.float32)


def _attn_methods():
    def out_proj(self, alloc, oT, oTd, w_ap, gm):
        s = self.s
        wob = [(alloc(f"wo{k}", [128, DC, 128], BF16), Dep()) for k in range(2)]
        src = w_ap.rearrange("(kc p) n -> p kc n", p=128)

        def load(m, t, dep):
            s.dma("pool", t[:, :, :], src[:, :, m * 128:(m + 1) * 128], writes=[dep])
        st = Stream(wob, list(range(DC)), load)
        k = 0
        for m in range(DC):
            wt, wd = st.get()
            for tb in range(4):
                pb, pbd = self.bank(4 + k % 2)
                k += 1
                tsl = slice(tb * 512, (tb + 1) * 512)
                for kc in range(DC):
                    s.mm(pb, wt[:, kc, :], oT[:, kc, tsl], kc == 0, kc == DC - 1, reads=[wd, oTd[tb]], out_dep=pbd)
                s.op("dve", lambda e: e.scalar_tensor_tensor(out=self.X[:, m, tsl], in0=pb, scalar=gm[:, m:m + 1],
                                                             in1=self.X[:, m, tsl], op0=ALU.mult, op1=ALU.add),
                     reads=[pbd, self.modd, self.Xd[m][tb]], writes=[self.Xd[m][tb]])

    def attn_bufs(self, alloc, nP=1, nPT=1):
        return {"Pb": [(alloc(f"a_P{k}", [128, S], BF16), Dep()) for k in range(nP)] * (2 // nP),
                "PT": [(alloc(f"a_PT{k}", [128, 16, 128], BF16), Dep()) for k in range(nPT)] * (2 // nPT),
                "dg": [(alloc(f"a_dg{k}", [128, 128], BF16), Dep()) for k in range(2)],
                "st": [(alloc(f"a_st{k}", [128, 8], F32), Dep()) for k in range(2)],
                "raw": [(alloc(f"a_raw{k}", [128, S], F32), Dep()) for k in range(2)]}

    def attention_pair(self, ab, score_fn, Vt, Vtd, maskb, o_evac):
        s = self.s
        items = [(qb, par) for qb in range(16) for par in range(2)]
        segrr = [0]

        seg_banks = {}

        def a_pe(it):
            qb, par = items[it]
            nk = (qb + 1) * 128
            nseg = (nk + 511) // 512
            seg_banks[it] = []
            for sg in range(nseg):
                k0 = sg * 512
                n = min(512, nk - k0)
                bk = segrr[0]
                segrr[0] = (segrr[0] + 1) % 4
                pb, pbd = self.bank(bk)
                score_fn(par, qb, k0, n, pb[:, 0:n], pbd)
                if sg == nseg - 1:
                    s.mm(pb[:, n - 128:n], self.identb[:, :], maskb[:, :], False, True, reads=[self.cbd], out_dep=pbd)
                else:
                    pbd.w = (("e", "pe"), s.cnt["pe"])
                    pbd.r = {}
                seg_banks[it].append((pb, pbd, k0, n))

        def a_ev(it):
            raw, rawd = ab["raw"][it % 2]
            st, std = ab["st"][it % 2]
            for sg, (pb, pbd, k0, n) in enumerate(seg_banks.pop(it)):
                s.op("act", lambda e: e.activation(out=raw[:, k0:k0 + n], in_=pb[:, 0:n], func=AF.Copy), reads=[pbd], writes=[rawd])
                s.op("dve", lambda e: e.reduce_max(out=st[:, sg:sg + 1], in_=raw[:, k0:k0 + n], axis=AX.X), reads=[rawd], writes=[std])

        def b_soft(it):
            qb, par = items[it]
            nk = (qb + 1) * 128
            nseg = (nk + 511) // 512
            raw, rawd = ab["raw"][it % 2]
            st, std = ab["st"][it % 2]
            Pb, Pbd = ab["Pb"][it % 2]
            dg, dgd = ab["dg"][it % 2]
            if nseg > 1:
                s.op("dve", lambda e: e.reduce_max(out=st[:, 4:5], in_=st[:, 0:nseg], axis=AX.X), reads=[std], writes=[std])
                mcol = st[:, 4:5]
            else:
                mcol = st[:, 0:1]
            s.op("dve", lambda e: e.tensor_scalar(out=st[:, 5:6], in0=mcol, scalar1=-1.0, scalar2=None, op0=ALU.mult), reads=[std], writes=[std])
            s.op("act", lambda e: e.activation(out=Pb[:, 0:nk], in_=raw[:, 0:nk], func=AF.Exp, bias=st[:, 5:6], accum_out=st[:, 6:7]),
                 reads=[rawd, std], writes=[Pbd, std])
            s.op("dve", lambda e: e.reciprocal(out=st[:, 7:8], in_=st[:, 6:7]), reads=[std], writes=[std])
            s.op("dve", lambda e: e.tensor_scalar(out=dg[:, :], in0=self.C("ident"), scalar1=st[:, 7:8], scalar2=None, op0=ALU.mult),
                 reads=[std, self.ctd], writes=[dgd])

        def b_pe(it):
            qb, par = items[it]
            nkb = qb + 1
            Pb, Pbd = ab["Pb"][it % 2]
            PT, PTd = ab["PT"][it % 2]
            dg, dgd = ab["dg"][it % 2]
            for g4 in range((nkb + 3) // 4):
                pb, pbd = self.bank(4 + g4 % 2)
                nj = min(4, nkb - g4 * 4)
                for j in range(nj):
                    kb = g4 * 4 + j
                    s.mm(pb[:, j * 128:(j + 1) * 128], Pb[:, kb * 128:(kb + 1) * 128], dg[:, :], True, j == nj - 1,
                         reads=[Pbd, dgd], out_dep=pbd)
                src = pb[:, 0:nj * 128]
                dst = PT[:, g4 * 4:g4 * 4 + nj, :]
                s.op("dve", lambda e: e.tensor_copy(out=dst, in_=src), reads=[pbd], writes=[PTd])
            ob, obd = self.bank(6 + (it % 2))
            for kb in range(nkb):
                s.mm(ob[:, 0:128], Vt[:, kb, :], PT[:, kb, :], kb == 0, kb == nkb - 1, reads=[Vtd, PTd], out_dep=obd)
            o_evac(par, qb, ob[par * 64:(par + 1) * 64, 0:128], obd)

        a_pe(0)
        a_ev(0)
        for it in range(len(items)):
            if it + 1 < len(items):
                a_pe(it + 1)
            b_soft(it)
            if it + 1 < len(items):
                a_ev(it + 1)
            b_pe(it)

    def rope_tables(self, cosT, sinT, tabd):
        s, d = self.s, self.d
        with self.phase() as alloc:
            posi = alloc("r_posi", [64, S], I32)
            ang = alloc("r_ang", [64, S], F32)
            y = alloc("r_y", [64, S], F32)
            yi = alloc("r_yi", [64, S], I32)
            r = alloc("r_r", [64, S], F32)
            mk = alloc("r_mk", [64, S], F32)
            dd = Dep()
            s.dma("sp", posi[:, :], d["pos"][0:1, :].partition_broadcast(64), writes=[dd])
            s.op("dve", lambda e: e.tensor_copy(out=ang[:, :], in_=posi[:, :]), reads=[dd], writes=[dd])
            s.op("dve", lambda e: e.tensor_scalar(out=ang[:, :], in0=ang[:, :], scalar1=self.C("invf", rows=(0, 64)), scalar2=None,
                                                  op0=ALU.mult), reads=[dd, self.ctd], writes=[dd])
            TWO_PI = 2.0 * math.pi
            for tab, shift in ((sinT, 0.0), (cosT, math.pi / 2.0)):
                s.op("dve", lambda e: e.tensor_scalar(out=y[:, :], in0=ang[:, :], scalar1=shift, scalar2=1.0 / TWO_PI,
                                                      op0=ALU.add, op1=ALU.mult), reads=[dd], writes=[dd])
                s.op("dve", lambda e: e.tensor_copy(out=yi[:, :], in_=y[:, :]), reads=[dd], writes=[dd])
                s.op("dve", lambda e: e.tensor_copy(out=y[:, :], in_=yi[:, :]), reads=[dd], writes=[dd])
                s.op("dve", lambda e: e.scalar_tensor_tensor(out=r[:, :], in0=y[:, :], scalar=-TWO_PI, in1=ang[:, :],
                                                             op0=ALU.mult, op1=ALU.add), reads=[dd], writes=[dd])
                if shift != 0.0:
                    s.op("dve", lambda e: e.tensor_scalar(out=r[:, :], in0=r[:, :], scalar1=shift, scalar2=None, op0=ALU.add),
                         reads=[dd], writes=[dd])
                s.op("dve", lambda e: e.tensor_single_scalar(out=mk[:, :], in_=r[:, :], scalar=math.pi, op=ALU.is_gt), reads=[dd], writes=[dd])
                s.op("dve", lambda e: e.scalar_tensor_tensor(out=r[:, :], in0=mk[:, :], scalar=-TWO_PI, in1=r[:, :],
                                                             op0=ALU.mult, op1=ALU.add), reads=[dd], writes=[dd])
                s.op("dve", lambda e: e.tensor_single_scalar(out=mk[:, :], in_=r[:, :], scalar=-math.pi, op=ALU.is_lt), reads=[dd], writes=[dd])
                s.op("dve", lambda e: e.scalar_tensor_tensor(out=r[:, :], in0=mk[:, :], scalar=TWO_PI, in1=r[:, :],
                                                             op0=ALU.mult, op1=ALU.add), reads=[dd], writes=[dd])
                s.op("dve", lambda e: e.tensor_scalar(out=r[:, :], in0=r[:, :], scalar1=3.14159, scalar2=-3.14159, op0=ALU.min, op1=ALU.max),
                     reads=[dd], writes=[dd])
                s.op("act", lambda e: e.activation(out=tab[:, :], in_=r[:, :], func=AF.Sin), reads=[dd], writes=[tabd])

    def mla_layer(self, i):
        s, d = self.s, self.d
        A = self.amod[:, i * 16:i * 16 + 8]
        B = self.modc(i, 0)
        gm = self.modc(i, 2)
        SC = float((64 + 32) ** -0.5)
        wdown = d["mla_w_down"][0].rearrange("(kc p) n -> p kc n", p=128)
        wuq = d["mla_w_uq"][0].rearrange("(kc p) (h dd) -> p kc h dd", p=128, dd=96)
        wukv = d["mla_w_ukv"][0].rearrange("(kc p) (h dd) -> p kc h dd", p=128, dd=128)
        with self.phase() as alloc:
            cq = alloc("m_cq", [128, 6, S], BF16)
            ckv = alloc("m_ckv", [128, 2, S], BF16)
            kr = alloc("m_kr", [64, S], BF16)
            cqd = [Dep() for _ in range(4)]
            ckvd = [Dep() for _ in range(4)]
            krd = [Dep() for _ in range(4)]
            cosT = alloc("m_cos", [64, S], F32)
            sinT = alloc("m_sin", [64, S], F32)
            tabd = Dep()
            self.rope_tables(cosT, sinT, tabd)
            with self.phase() as a1:
                hT = a1("m_hT", [128, DC, 1024], BF16)
                hTd = [Dep(), Dep()]
                nt = self.norm_tmps(a1)
                wdn = a1("m_wdn", [128, DC, 1024], BF16)
                wdnd = Dep()
                wkr = a1("m_wkr", [128, DC, 128], BF16)
                wkrd = Dep()
                raw = a1("m_raw", [128, 8, 256], F32)
                rawd = [Dep() for _ in range(8)]
                sqt = [(a1(f"m_sq{k}", [128, 256], F32), Dep()) for k in range(2)]
                rq = a1("m_rq", [128, 256], F32)
                rkv = a1("m_rkv", [128, 256], F32)
                rqd, rkvd = Dep(), Dep()
                t1 = a1("m_t1", [64, 256], F32)
                t2 = a1("m_t2", [64, 256], F32)
                t1d, t2d = Dep(), Dep()
                for h2 in range(2):
                    s.dma("pool", wdn[:, :, h2 * 512:(h2 + 1) * 512], wdown[:, :, h2 * 512:(h2 + 1) * 512], writes=[wdnd])
                s.dma("pool", wkr[:, :, 0:32], wdown[:, :, 1024:1056], writes=[wkrd])
                s.dma("pool", wkr[:, :, 32:64], wdown[:, :, 1024:1056], writes=[wkrd])
                s.op("dve", lambda e: e.tensor_scalar(out=wkr[:, :, 64:80], in0=wkr[:, :, 16:32], scalar1=-1.0, scalar2=None, op0=ALU.mult),
                     reads=[wkrd], writes=[wkrd])
                s.op("dve", lambda e: e.tensor_copy(out=wkr[:, :, 80:96], in_=wkr[:, :, 0:16]), reads=[wkrd], writes=[wkrd])
                s.op("dve", lambda e: e.tensor_copy(out=wkr[:, :, 96:128], in_=wkr[:, :, 64:96]), reads=[wkrd], writes=[wkrd])
                for ps_ in range(2):
                    def out_fn(c, tb, t, td, Ac, Bc):
                        s.op("act", lambda e: e.activation(out=hT[:, c, tb * 512:(tb + 1) * 512], in_=t[:, :], func=AF.Identity,
                                                           bias=Bc, scale=Ac),
                             reads=[td, self.amodd, self.modd], writes=[hTd[tb]])
                    self.norm_mod(nt, A, B, ps_ * 1024, 2, out_fn)
                    for blk in range(4):
                        tok0 = ps_ * 1024 + blk * 256
                        tsl = slice(tok0, tok0 + 256)
                        hsl = slice(blk * 256, (blk + 1) * 256)
                        hd = hTd[blk // 2]
                        dq = tok0 // 512
                        bq, bqd = self.bank(2)
                        bkv, bkvd = self.bank(3)
                        for m in range(8):
                            pb, pbd = self.bank(m % 2)
                            for kc in range(DC):
                                s.mm(pb[:, 0:256], wdn[:, kc, m * 128:(m + 1) * 128], hT[:, kc, hsl], kc == 0, kc == DC - 1,
                                     reads=[wdnd, hd], out_dep=pbd)
                            s.op("act", lambda e: e.activation(out=raw[:, m, :], in_=pb[:, 0:256], func=AF.Copy), reads=[pbd], writes=[rawd[m]])
                            q, qd = sqt[m % 2]
                            s.op("pool", lambda e: e.tensor_tensor(out=q[:, :], in0=raw[:, m, :], in1=raw[:, m, :], op=ALU.mult),
                                 reads=[rawd[m]], writes=[qd])
                            if m < 6:
                                s.mm(bq[:, 0:256], self.C("ones"), q[:, :], m == 0, m == 5, reads=[qd, self.ctd], out_dep=bqd, inc=True)
                            else:
                                s.mm(bkv[:, 0:256], self.C("ones"), q[:, :], m == 6, m == 7, reads=[qd, self.ctd], out_dep=bkvd, inc=True)
                        s.op("act", lambda e: e.activation(out=rq[:, :], in_=bq[:, 0:256], func=AF.Sqrt, bias=self.epsc[:, 0:1], scale=1.0 / 768.0),
                             reads=[bqd], writes=[rqd])
                        s.op("dve", lambda e: e.reciprocal(out=rq[:, :], in_=rq[:, :]), reads=[rqd], writes=[rqd])
                        s.op("act", lambda e: e.activation(out=rkv[:, :], in_=bkv[:, 0:256], func=AF.Sqrt, bias=self.epsc[:, 0:1], scale=1.0 / 256.0),
                             reads=[bkvd], writes=[rkvd])
                        s.op("dve", lambda e: e.reciprocal(out=rkv[:, :], in_=rkv[:, :]), reads=[rkvd], writes=[rkvd])
                        for m in range(8):
                            if m < 6:
                                s.op("dve", lambda e: e.scalar_tensor_tensor(out=cq[:, m, tsl], in0=raw[:, m, :], scalar=self.P("mqg", m, m + 1),
                                                                             in1=rq[:, :], op0=ALU.mult, op1=ALU.mult),
                                     reads=[rawd[m], rqd, self.ptd], writes=[cqd[dq]])
                            else:
                                s.op("dve", lambda e: e.scalar_tensor_tensor(out=ckv[:, m - 6, tsl], in0=raw[:, m, :], scalar=self.P("mkvg", m - 6, m - 5),
                                                                             in1=rkv[:, :], op0=ALU.mult, op1=ALU.mult),
                                     reads=[rawd[m], rkvd, self.ptd], writes=[ckvd[dq]])
                        bx, bxd = self.bank(4)
                        br, brd = self.bank(5)
                        for kc in range(DC):
                            s.mm(bx[0:64, 0:256], wkr[:, kc, 0:64], hT[:, kc, hsl], kc == 0, kc == DC - 1, reads=[wkrd, hd], out_dep=bxd)
                        for kc in range(DC):
                            s.mm(br[0:64, 0:256], wkr[:, kc, 64:128], hT[:, kc, hsl], kc == 0, kc == DC - 1, reads=[wkrd, hd], out_dep=brd)
                        s.op("dve", lambda e: e.tensor_tensor(out=t1[:, :], in0=bx[0:64, 0:256], in1=cosT[:, tsl], op=ALU.mult),
                             reads=[bxd, tabd], writes=[t1d])
                        s.op("dve", lambda e: e.tensor_tensor(out=t2[:, :], in0=br[0:64, 0:256], in1=sinT[:, tsl], op=ALU.mult),
                             reads=[brd, tabd], writes=[t2d])
                        s.op("pool", lambda e: e.tensor_tensor(out=kr[:, tsl], in0=t1[:, :], in1=t2[:, :], op=ALU.add),
                             reads=[t1d, t2d], writes=[krd[dq]])
            with self.phase() as a2:
                oT = a2("m_oT", [128, DC, S], BF16)
                oTd = [Dep() for _ in range(4)]
                with self.phase() as a3:
                    ab = self.attn_bufs(a3)
                    wqn = [(a3(f"m_wqn{k}", [128, 6, 128], BF16), Dep()) for k in range(1)]
                    wqr = [(a3(f"m_wqr{k}", [128, 6, 64], BF16), Dep()) for k in range(1)]
                    wqo = [(a3(f"m_wqo{k}", [128, 6, 64], BF16), Dep()) for k in range(1)]
                    wkn = [(a3(f"m_wkn{k}", [128, 2, 128], BF16), Dep()) for k in range(1)]
                    wv = [(a3(f"m_wv{k}", [128, 2, 128], BF16), Dep()) for k in range(1)]
                    qn = a3("m_qn", [128, S], BF16)
                    kn = a3("m_kn", [128, S], BF16)
                    qr = a3("m_qr", [64, S], BF16)
                    Vt = a3("m_Vt", [128, 16, 128], BF16)
                    qnd, knd, qrd, Vtd = Dep(), Dep(), Dep(), Dep()
                    t1 = a3("m_u1", [64, 512], F32)
                    t2 = a3("m_u2", [64, 512], F32)
                    t1d, t2d = Dep(), Dep()
                    for hp in range(8):
                        k2 = 0
                        (wqn_t, wqn_d), (wqr_t, wqr_d), (wqo_t, wqo_d) = wqn[k2], wqr[k2], wqo[k2]
                        (wkn_t, wkn_d), (wv_t, wv_d) = wkn[k2], wv[k2]
                        for hh in range(2):
                            s.dma("pool", wqn_t[:, :, hh * 64:(hh + 1) * 64], wuq[:, :, 2 * hp + hh, 0:64], writes=[wqn_d])
                            s.dma("pool", wqr_t[:, :, hh * 32:(hh + 1) * 32], wuq[:, :, 2 * hp + hh, 64:96], writes=[wqr_d])
                            s.dma("pool", wkn_t[:, :, hh * 64:(hh + 1) * 64], wukv[:, :, 2 * hp + hh, 0:64], writes=[wkn_d])
                            s.dma("pool", wv_t[:, :, hh * 64:(hh + 1) * 64], wukv[:, :, 2 * hp + hh, 64:128], writes=[wv_d])
                        wr4 = wqr_t[:, :, :].rearrange("p k (h e) -> p k h e", h=2)
                        wo4 = wqo_t[:, :, :].rearrange("p k (h e) -> p k h e", h=2)
                        s.op("dve", lambda e: e.tensor_scalar(out=wo4[:, :, :, 0:16], in0=wr4[:, :, :, 16:32], scalar1=-1.0, scalar2=None, op0=ALU.mult),
                             reads=[wqr_d], writes=[wqo_d])
                        s.op("dve", lambda e: e.tensor_copy(out=wo4[:, :, :, 16:32], in_=wr4[:, :, :, 0:16]), reads=[wqr_d], writes=[wqo_d])
                        for tb in range(4):
                            tsl = slice(tb * 512, (tb + 1) * 512)
                            pb, pbd = self.bank(4)
                            for kc in range(6):
                                s.mm(pb, wqn_t[:, kc, :], cq[:, kc, tsl], kc == 0, kc == 5, reads=[wqn_d, cqd[tb]], out_dep=pbd)
                            s.op("act", lambda e: e.activation(out=qn[:, tsl], in_=pb, func=AF.Copy, scale=SC), reads=[pbd], writes=[qnd])
                            pk, pkd = self.bank(5)
                            for kc in range(2):
                                s.mm(pk, wkn_t[:, kc, :], ckv[:, kc, tsl], kc == 0, kc == 1, reads=[wkn_d, ckvd[tb]], out_dep=pkd)
                            s.op("act", lambda e: e.activation(out=kn[:, tsl], in_=pk, func=AF.Copy), reads=[pkd], writes=[knd])
                            bx, bxd = self.bank(6)
                            br, brd = self.bank(7)
                            for kc in range(6):
                                s.mm(bx[0:64, :], wqr_t[:, kc, :], cq[:, kc, tsl], kc == 0, kc == 5, reads=[wqr_d, cqd[tb]], out_dep=bxd)
                            for kc in range(6):
                                s.mm(br[0:64, :], wqo_t[:, kc, :], cq[:, kc, tsl], kc == 0, kc == 5, reads=[wqo_d, cqd[tb]], out_dep=brd)
                            s.op("dve", lambda e: e.scalar_tensor_tensor(out=t1[:, :], in0=bx[0:64, :], scalar=SC, in1=cosT[:, tsl],
                                                                         op0=ALU.mult, op1=ALU.mult), reads=[bxd, tabd], writes=[t1d])
                            s.op("dve", lambda e: e.scalar_tensor_tensor(out=t2[:, :], in0=br[0:64, :], scalar=SC, in1=sinT[:, tsl],
                                                                         op0=ALU.mult, op1=ALU.mult), reads=[brd, tabd], writes=[t2d])
                            s.op("pool", lambda e: e.tensor_tensor(out=qr[:, tsl], in0=t1[:, :], in1=t2[:, :], op=ALU.add),
                                 reads=[t1d, t2d], writes=[qrd])
                        for g in range(4):
                            pb, pbd = self.bank(4 + g % 2)
                            for j in range(4):
                                t16 = g * 4 + j
                                for kc in range(2):
                                    s.mm(pb[:, j * 128:(j + 1) * 128], ckv[:, kc, t16 * 128:(t16 + 1) * 128], wv_t[:, kc, :],
                                         kc == 0, (kc == 1 and j == 3), reads=[wv_d, ckvd[t16 // 4]], out_dep=pbd)
                            s.op("act", lambda e: e.activation(out=Vt[:, g * 4:(g + 1) * 4, :], in_=pb, func=AF.Copy), reads=[pbd], writes=[Vtd])

                        def score_fn(par, qb, k0, n, out_ap, od):
                            ps = slice(par * 64, (par + 1) * 64)
                            rs = slice(par * 32, (par + 1) * 32)
                            qsl = slice(qb * 128, (qb + 1) * 128)
                            s.mm(out_ap, qn[ps, qsl], kn[ps, k0:k0 + n], True, False, reads=[qnd, knd], out_dep=od)
                            s.mm(out_ap, qr[rs, qsl], kr[rs, k0:k0 + n], False, False, reads=[qrd] + krd, out_dep=od, inc=True)

                        def o_evac(par, qb, src, srcd):
                            ps = slice(par * 64, (par + 1) * 64)
                            s.op("act", lambda e: e.activation(out=oT[ps, hp, qb * 128:(qb + 1) * 128], in_=src, func=AF.Copy),
                                 reads=[srcd], writes=[oTd[qb // 4]])
                        self.attention_pair(ab, score_fn, Vt, Vtd, self.mmaskb, o_evac)
                self.out_proj(a2, oT, oTd, d["mla_w_o"][0], gm)

    Prog.out_proj = out_proj
    Prog.attn_bufs = attn_bufs
    Prog.attention_pair = attention_pair
    Prog.rope_tables = rope_tables
    Prog.mla_layer = mla_layer


_attn_methods()


def _fox_methods():
    def fox_layer(self, i):
        s, d = self.s, self.d
        A = self.amod[:, i * 16:i * 16 + 8]
        B = self.modc(i, 0)
        gm = self.modc(i, 2)
        FSC = float(64 ** -0.5)
        win = d["fox_w_in"][0].rearrange("(kc p) n -> p kc n", p=128)
        with self.phase() as alloc:
            hT = alloc("x_hT", [128, DC, S], BF16)
            hTd = [Dep() for _ in range(4)]
            oT = alloc("x_oT", [128, DC, S], BF16)
            oTd = [Dep() for _ in range(4)]
            negF = alloc("x_negF", [16, S], F32)
            negFd = Dep()
            gsc = alloc("x_gsc", [128, 2], F32)
            gscd = Dep()
            s.op("dve", lambda e: e.tensor_scalar(out=gsc[:, 0:1], in0=self.P("fqg"), scalar1=FSC, scalar2=None, op0=ALU.mult),
                 reads=[self.ptd], writes=[gscd])
            s.op("dve", lambda e: e.tensor_copy(out=gsc[:, 1:2], in_=self.P("fkg")), reads=[self.ptd], writes=[gscd])
            with self.phase() as a1:
                nt = self.norm_tmps(a1)

                def out_fn(c, tb, t, td, Ac, Bc):
                    s.op("act", lambda e: e.activation(out=hT[:, c, tb * 512:(tb + 1) * 512], in_=t[:, :], func=AF.Identity,
                                                       bias=Bc, scale=Ac),
                         reads=[td, self.amodd, self.modd], writes=[hTd[tb]])
                self.norm_mod(nt, A, B, 0, 4, out_fn)
                wf = a1("x_wf", [128, DC, 16], BF16)
                wfd = Dep()
                s.dma("pool", wf[:, :, :], win[:, :, 3072:3088], writes=[wfd])
                z = a1("x_z", [16, 512], F32)
                az = a1("x_az", [16, 512], F32)
                lf = a1("x_lf", [16, 512], F32)
                ones16 = a1("x_ones", [16, 512], F32)
                Fc = a1("x_F", [16, S], F32)
                zd = Dep()
                s.op("dve", lambda e: e.memset(ones16[:, :], 1.0), writes=[zd])
                for tb in range(4):
                    tsl = slice(tb * 512, (tb + 1) * 512)
                    pb, pbd = self.bank(4)
                    for kc in range(DC):
                        s.mm(pb[0:16, :], wf[:, kc, :], hT[:, kc, tsl], kc == 0, kc == DC - 1, reads=[wfd, hTd[tb]], out_dep=pbd)
                    s.op("act", lambda e: e.activation(out=z[:, :], in_=pb[0:16, :], func=AF.Identity, bias=self.P("fbf")[0:16, :]),
                         reads=[pbd, self.ptd, zd], writes=[zd])
                    s.op("act", lambda e: e.activation(out=az[:, :], in_=z[:, :], func=AF.Abs), reads=[zd], writes=[zd])
                    s.op("act", lambda e: e.activation(out=az[:, :], in_=az[:, :], func=AF.Exp, scale=-1.0), reads=[zd], writes=[zd])
                    s.op("act", lambda e: e.activation(out=az[:, :], in_=az[:, :], func=AF.Ln, bias=self.epsc[0:16, 2:3]), reads=[zd], writes=[zd])
                    s.op("dve", lambda e: e.tensor_scalar(out=lf[:, :], in0=z[:, :], scalar1=0.0, scalar2=None, op0=ALU.min), reads=[zd], writes=[zd])
                    s.op("dve", lambda e: e.tensor_tensor(out=lf[:, :], in0=lf[:, :], in1=az[:, :], op=ALU.subtract), reads=[zd], writes=[zd])
                    init = 0.0 if tb == 0 else Fc[:, tb * 512 - 1:tb * 512]
                    s.op("dve", lambda e: e.tensor_tensor_scan(Fc[:, tsl], ones16[:, :], lf[:, :], init, ALU.mult, ALU.add),
                         reads=[zd], writes=[zd])
                s.op("dve", lambda e: e.tensor_scalar(out=negF[:, :], in0=Fc[:, :], scalar1=-1.0, scalar2=None, op0=ALU.mult),
                     reads=[zd], writes=[negFd])
            with self.phase() as a3:
                ab = self.attn_bufs(a3)
                wq = [(a3(f"x_wq{k}", [128, DC, 128], BF16), Dep()) for k in range(1)]
                wk = [(a3(f"x_wk{k}", [128, DC, 128], BF16), Dep()) for k in range(1)]
                wv = [(a3(f"x_wv{k}", [128, DC, 128], BF16), Dep()) for k in range(1)]
                wg = [(a3(f"x_wg{k}", [128, DC, 128], BF16), Dep()) for k in range(1)]
                selp = [(a3(f"x_sel{k}", [16, 256], F32), Dep()) for k in range(2)]
                qn = a3("x_qn", [128, S], BF16)
                kn = a3("x_kn", [128, S], BF16)
                og = a3("x_og", [128, S], BF16)
                Vt = a3("x_Vt", [128, 16, 128], BF16)
                qnd, knd, ogd, Vtd = Dep(), Dep(), Dep(), Dep()
                raw = [(a3(f"x_raw{k}", [128, 512], F32), Dep()) for k in range(1)] * 2
                sq = [(a3(f"x_sq{k}", [128, 512], F32), Dep()) for k in range(1)] * 2
                rs_ = [(a3(f"x_rs{k}", [128, 512], F32), Dep()) for k in range(1)] * 2
                st16 = self.ccols["sel16"][0]
                for hp in range(8):
                    k2 = 0
                    (wq_t, wq_d), (wk_t, wk_d), (wv_t, wv_d), (wg_t, wg_d) = wq[k2], wk[k2], wv[k2], wg[k2]
                    sel_t, sel_d = selp[hp % 2]
                    s.dma("pool", wq_t[:, :, :], win[:, :, hp * 128:(hp + 1) * 128], writes=[wq_d])
                    s.dma("pool", wk_t[:, :, :], win[:, :, 1024 + hp * 128:1024 + (hp + 1) * 128], writes=[wk_d])
                    s.dma("pool", wv_t[:, :, :], win[:, :, 2048 + hp * 128:2048 + (hp + 1) * 128], writes=[wv_d])
                    s.dma("pool", wg_t[:, :, :], win[:, :, 3088 + hp * 128:3088 + (hp + 1) * 128], writes=[wg_d])
                    s.dma("sp", sel_t[:, :], d["ctab"][0:16, st16 + hp * 256:st16 + (hp + 1) * 256], writes=[sel_d])
                    for tb in range(4):
                        tsl = slice(tb * 512, (tb + 1) * 512)
                        for which, (w_t, w_d), dst, dstd, gcol in ((0, (wq_t, wq_d), qn, qnd, 0), (1, (wk_t, wk_d), kn, knd, 1)):
                            pb, pbd = self.bank(4 + which)
                            for kc in range(DC):
                                s.mm(pb, w_t[:, kc, :], hT[:, kc, tsl], kc == 0, kc == DC - 1, reads=[w_d, hTd[tb]], out_dep=pbd)
                            r_t, r_d = raw[which]
                            q_t, q_d = sq[which]
                            rr_t, rr_d = rs_[which]
                            s.op("act", lambda e: e.activation(out=r_t[:, :], in_=pb, func=AF.Copy), reads=[pbd], writes=[r_d])
                            s.op("pool", lambda e: e.tensor_tensor(out=q_t[:, :], in0=r_t[:, :], in1=r_t[:, :], op=ALU.mult), reads=[r_d], writes=[q_d])
                            p2, p2d = self.bank(6 + which)
                            s.mm(p2, self.C("bd"), q_t[:, :], True, True, reads=[q_d, self.ctd], out_dep=p2d)
                            s.op("act", lambda e: e.activation(out=rr_t[:, :], in_=p2, func=AF.Sqrt, bias=self.epsc[:, 0:1], scale=1.0 / 64.0),
                                 reads=[p2d], writes=[rr_d])
                            s.op("dve", lambda e: e.reciprocal(out=rr_t[:, :], in_=rr_t[:, :]), reads=[rr_d], writes=[rr_d])
                            s.op("dve", lambda e: e.scalar_tensor_tensor(out=dst[:, tsl], in0=r_t[:, :], scalar=gsc[:, gcol:gcol + 1],
                                                                         in1=rr_t[:, :], op0=ALU.mult, op1=ALU.mult),
                                 reads=[r_d, rr_d, gscd], writes=[dstd])
                        pg, pgd = self.bank(4)
                        for kc in range(DC):
                            s.mm(pg, wg_t[:, kc, :], hT[:, kc, tsl], kc == 0, kc == DC - 1, reads=[wg_d, hTd[tb]], out_dep=pgd)
                        s.op("act", lambda e: e.activation(out=og[:, tsl], in_=pg, func=AF.Sigmoid), reads=[pgd], writes=[ogd])
                    for g in range(4):
                        pb, pbd = self.bank(4 + g % 2)
                        for j in range(4):
                            t16 = g * 4 + j
                            for kc in range(DC):
                                s.mm(pb[:, j * 128:(j + 1) * 128], hT[:, kc, t16 * 128:(t16 + 1) * 128], wv_t[:, kc, :],
                                     kc == 0, (kc == DC - 1 and j == 3), reads=[wv_d, hTd[t16 // 4]], out_dep=pbd)
                        s.op("act", lambda e: e.activation(out=Vt[:, g * 4:(g + 1) * 4, :], in_=pb, func=AF.Copy), reads=[pbd], writes=[Vtd])

                    def score_fn(par, qb, k0, n, out_ap, od):
                        ps = slice(par * 64, (par + 1) * 64)
                        qsl = slice(qb * 128, (qb + 1) * 128)
                        s.mm(out_ap, qn[ps, qsl], kn[ps, k0:k0 + n], True, False, reads=[qnd, knd], out_dep=od)
                        s.mm(out_ap, sel_t[0:16, par * 128:(par + 1) * 128], negF[0:16, k0:k0 + n], False, False,
                             reads=[sel_d, negFd], out_dep=od, inc=True)

                    def o_evac(par, qb, src, srcd):
                        ps = slice(par * 64, (par + 1) * 64)
                        qsl = slice(qb * 128, (qb + 1) * 128)
                        s.op("dve", lambda e: e.tensor_tensor(out=oT[ps, hp, qsl], in0=src, in1=og[ps, qsl], op=ALU.mult),
                             reads=[srcd, ogd], writes=[oTd[qb // 4]])
                    self.attention_pair(ab, score_fn, Vt, Vtd, self.cmaskb, o_evac)
            self.out_proj(alloc, oT, oTd, d["fox_w_o"][0], gm)

    Prog.fox_layer = fox_layer


_fox_methods()


def _rwkv_methods():
    def rw_scratch(self):
        if not hasattr(self, "_scr"):
            nc = self.nc
            self._scr = {nm: nc.dram_tensor(f"scr_{nm}", [D, S], F32).ap() for nm in ("r", "k", "v", "sg", "a", "g", "vf", "xs")}
            self._scr["yg"] = nc.dram_tensor("scr_yg", [D, S], BF16).ap()
        return self._scr

    def rwkv_layer(self, i):
        j = i // 3
        lvl = getattr(self, "dbg_rw", 3)
        self.rwkv_pass1(i, j)
        if lvl >= 2:
            self.rwkv_pass2(i, j)
        if lvl >= 3:
            self.rwkv_pass3(i, j)

    def rwkv_pass1(self, i, j):
        s, d = self.s, self.d
        scr = self.rw_scratch()
        A = self.amod[:, i * 16:i * 16 + 8]
        B = self.modc(i, 0)
        fmv = lambda ap: ap.rearrange("(c p) n -> p c n", p=128)
        with self.phase() as alloc:
            hT = alloc("w_hT", [128, DC, S + 1], BF16)
            hTd = [Dep() for _ in range(4)]
            h0d = Dep()
            s.op("dve", lambda e: e.memset(hT[:, :, 0:1], 0.0), writes=[h0d])
            nt = self.norm_tmps(alloc)

            def out_fn(c, tb, t, td, Ac, Bc):
                s.op("act", lambda e: e.activation(out=hT[:, c, 1 + tb * 512:1 + (tb + 1) * 512], in_=t[:, :], func=AF.Identity,
                                                   bias=Bc, scale=Ac),
                     reads=[td, self.amodd, self.modd], writes=[hTd[tb]])
            self.norm_mod(nt, A, B, 0, 4, out_fn)
            omu = alloc("w_omu", [128, 48], F32)
            omud = Dep()
            s.op("dve", lambda e: e.tensor_scalar(out=omu[:, :], in0=self.P(f"mu{j}"), scalar1=-1.0, scalar2=1.0, op0=ALU.mult, op1=ALU.add),
                 reads=[self.ptd], writes=[omud])
            xn = [(alloc(f"w_xn{k}", [128, DC, 512], BF16), Dep()) for k in range(2)]
            tmpx = [(alloc(f"w_tx{k}", [128, 512], F32), Dep()) for k in range(4)]
            wbig = [(alloc(f"w_big{k}", [128, DC, 1024], BF16), Dep()) for k in range(2)]
            w1 = alloc("w_w1", [128, DC, 64], BF16)
            a1 = alloc("w_a1", [128, DC, 64], BF16)
            g1 = alloc("w_g1", [128, DC, 160], BF16)
            w2 = alloc("w_w2", [64, D], BF16)
            a2 = alloc("w_a2", [64, D], BF16)
            g2a = alloc("w_g2a", [128, D], BF16)
            g2b = alloc("w_g2b", [32, D], BF16)
            lwd = Dep()
            kcv = lambda ap: ap.rearrange("(kc p) n -> p kc n", p=128)
            s.dma("pool", w1[:, :, :], kcv(d["rw_w1"][j]), writes=[lwd])
            s.dma("pool", a1[:, :, :], kcv(d["rw_a1"][j]), writes=[lwd])
            s.dma("pool", g1[:, :, :], kcv(d["rw_g1"][j]), writes=[lwd])
            s.dma("pool", w2[:, :], d["rw_w2"][j], writes=[lwd])
            s.dma("pool", a2[:, :], d["rw_a2"][j], writes=[lwd])
            s.dma("pool", g2a[:, :], d["rw_g2"][j][0:128, :], writes=[lwd])
            s.dma("pool", g2b[:, :], d["rw_g2"][j][128:160, :], writes=[lwd])
            vres = (j > 0)
            if vres:
                v1 = alloc("w_v1", [128, DC, 32], BF16)
                v2 = alloc("w_v2", [32, D], BF16)
                s.dma("pool", v1[:, :, :], kcv(d["rw_v1"][j - 1]), writes=[lwd])
                s.dma("pool", v2[:, :], d["rw_v2"][j - 1], writes=[lwd])
                vgt = alloc("w_vgt", [128, DC, 512], BF16)
                vgd = Dep()
                vft = [(alloc(f"w_vf{k}", [128, 512], F32), Dep()) for k in range(2)]
            stage = [(alloc(f"w_st{k}", [128, 512], F32), Dep()) for k in range(3)]
            t1 = alloc("w_t1", [128, 512], BF16)
            t1b = alloc("w_t1b", [32, 512], BF16)
            t1d = Dep()
            stc = [0]

            def emit_out(pb, pbd, func, bias, dst, m, tb, post=None):
                st, std = stage[stc[0] % 3]
                stc[0] += 1
                kw = {} if bias is None else {"bias": bias}
                s.op("act", lambda e: e.activation(out=st[:, :], in_=pb, func=func, **kw), reads=[pbd, self.ptd], writes=[std])
                if post is not None:
                    post(st, std)
                s.dma("sp", fmv(dst)[:, m, tb * 512:(tb + 1) * 512], st[:, :], reads=[std])

            big_items = [0, 1, 2]

            def load_big(which, t, dep):
                src = kcv(d["rw_w_rkv"][j, which])
                for h2 in range(2):
                    s.dma("pool", t[:, :, h2 * 512:(h2 + 1) * 512], src[:, :, h2 * 512:(h2 + 1) * 512], writes=[dep])
            bst = Stream(wbig, big_items, load_big)
            xc = [0]

            def make_xn(n, tb):
                x_t, x_d = xn[xc[0] % 2]
                xc[0] += 1
                for c in range(DC):
                    tx, txd = tmpx[c % 4]
                    col = n * 8 + c
                    s.op("act", lambda e: e.activation(out=tx[:, :], in_=hT[:, c, tb * 512:tb * 512 + 512], func=AF.Copy,
                                                       scale=self.P(f"mu{j}", col, col + 1)),
                         reads=[hTd[tb], hTd[max(tb - 1, 0)], h0d, self.ptd], writes=[txd])
                    s.op("dve", lambda e: e.scalar_tensor_tensor(out=x_t[:, c, :], in0=hT[:, c, 1 + tb * 512:1 + tb * 512 + 512],
                                                               scalar=omu[:, col:col + 1], in1=tx[:, :], op0=ALU.mult, op1=ALU.add),
                         reads=[hTd[tb], omud, txd], writes=[x_d])
                return x_t, x_d

            def big_proj(x_t, x_d, wt, wd, m):
                pb, pbd = self.bank(m % 4)
                for kc in range(DC):
                    s.mm(pb, wt[:, kc, m * 128:(m + 1) * 128], x_t[:, kc, :], kc == 0, kc == DC - 1, reads=[wd, x_d], out_dep=pbd)
                return pb, pbd

            def lora(x_t, x_d, wa, ncols, func1, wb, m_bias_name, func2, dst, tb, wb2=None):
                pb, pbd = self.bank(4)
                n1 = min(ncols, 128)
                for kc in range(DC):
                    s.mm(pb[0:n1, :], wa[:, kc, 0:n1], x_t[:, kc, :], kc == 0, kc == DC - 1, reads=[lwd, x_d], out_dep=pbd)
                s.op("act", lambda e: e.activation(out=t1[0:n1, :], in_=pb[0:n1, :], func=func1), reads=[pbd], writes=[t1d])
                if ncols > 128:
                    pb2, pb2d = self.bank(5)
                    for kc in range(DC):
                        s.mm(pb2[0:32, :], wa[:, kc, 128:160], x_t[:, kc, :], kc == 0, kc == DC - 1, reads=[lwd, x_d], out_dep=pb2d)
                    s.op("act", lambda e: e.activation(out=t1b[0:32, :], in_=pb2[0:32, :], func=func1), reads=[pb2d], writes=[t1d])
                for m in range(DC):
                    po, pod = self.bank(m % 4)
                    s.mm(po, wb[0:n1, m * 128:(m + 1) * 128], t1[0:n1, :], True, wb2 is None, reads=[lwd, t1d], out_dep=pod)
                    if wb2 is not None:
                        s.mm(po, wb2[0:32, m * 128:(m + 1) * 128], t1b[0:32, :], False, True, reads=[lwd, t1d], out_dep=pod)
                    bias = None if m_bias_name is None else self.P(m_bias_name, m, m + 1)
                    if dst is None:
                        s.op("act", lambda e: e.activation(out=vgt[:, m, :], in_=po, func=func2, bias=bias), reads=[pod, self.ptd], writes=[vgd])
                    else:
                        emit_out(po, pod, func2, bias, dst, m, tb)

            for n in range(6):
                if n in (0, 2, 3):
                    wt, wd = bst.get()
                for tb in range(4):
                    x_t, x_d = make_xn(n, tb)
                    if n == 0:
                        for m in range(DC):
                            pb, pbd = big_proj(x_t, x_d, wt, wd, m)
                            emit_out(pb, pbd, AF.Copy, None, scr["r"], m, tb)
                    elif n == 2:
                        for m in range(DC):
                            pb, pbd = big_proj(x_t, x_d, wt, wd, m)
                            emit_out(pb, pbd, AF.Copy, None, scr["k"], m, tb)
                    elif n == 3:
                        if vres:
                            lora(x_t, x_d, v1, 32, AF.Copy, v2, "v0", AF.Sigmoid, None, tb)
                        for m in range(DC):
                            pb, pbd = big_proj(x_t, x_d, wt, wd, m)
                            if not vres:
                                emit_out(pb, pbd, AF.Copy, None, scr["vf"], m, tb)
                            else:
                                vf, vfd = vft[m % 2]
                                s.dma("sp", vf[:, :], fmv(scr["vf"])[:, m, tb * 512:(tb + 1) * 512], writes=[vfd])

                                def post(st, std, m=m, vf=vf, vfd=vfd):
                                    s.op("dve", lambda e: e.tensor_tensor(out=vf[:, :], in0=vf[:, :], in1=st[:, :], op=ALU.subtract),
                                         reads=[vfd, std], writes=[vfd])
                                    s.op("pool", lambda e: e.tensor_tensor(out=vf[:, :], in0=vf[:, :], in1=vgt[:, m, :], op=ALU.mult),
                                         reads=[vfd, vgd], writes=[vfd])
                                    s.op("dve", lambda e: e.tensor_tensor(out=st[:, :], in0=st[:, :], in1=vf[:, :], op=ALU.add),
                                         reads=[vfd, std], writes=[std])
                                emit_out(pb, pbd, AF.Copy, None, scr["v"], m, tb, post=post)
                    elif n == 1:
                        lora(x_t, x_d, w1, 64, AF.Tanh, w2, f"w0{j}", AF.Sigmoid, scr["sg"], tb)
                    elif n == 4:
                        lora(x_t, x_d, a1, 64, AF.Copy, a2, f"a0{j}", AF.Sigmoid, scr["a"], tb)
                    else:
                        lora(x_t, x_d, g1, 160, AF.Sigmoid, g2a, None, AF.Copy, scr["g"], tb, wb2=g2b)

    def rwkv_pass3(self, i, j):
        s, d = self.s, self.d
        scr = self.rw_scratch()
        gm = self.modc(i, 2)
        with self.phase() as alloc:
            YG = alloc("w_YG", [128, DC, S], BF16)
            YGd = [Dep() for _ in range(4)]
            src = scr["yg"].rearrange("(c p) n -> p c n", p=128)
            for tb in range(4):
                s.dma("sp", YG[:, :, tb * 512:(tb + 1) * 512], src[:, :, tb * 512:(tb + 1) * 512], writes=[YGd[tb]])
            self.out_proj(alloc, YG, YGd, d["rw_w_o"][j], gm)

    Prog.rw_scratch = rw_scratch
    Prog.rwkv_layer = rwkv_layer
    Prog.rwkv_pass1 = rwkv_pass1
    Prog.rwkv_pass3 = rwkv_pass3


_rwkv_methods()


def _rwkv2_methods():
    def rwkv_pass2(self, i, j):
        s, d = self.s, self.d
        scr = self.rw_scratch()
        fmv = lambda ap: ap.rearrange("(c p) n -> p c n", p=128)
        vsrc = scr["v"] if j > 0 else scr["vf"]
        xs = fmv(scr["xs"])
        for c in range(DC):
            s.dma("sp", xs[:, c, :], self.X[:, c, :], reads=self.Xd[c])
        s.barrier()
        xptr = [SB_BASE]

        def xalloc(name, shape, dt):
            nbytes = (int(np.prod(shape[1:])) * (2 if dt == BF16 else 4) + 31) // 32 * 32
            off = xptr[0]
            if off + nbytes > SB_BASE + DC * S * 4:
                return self.salloc(name, shape, dt)
            xptr[0] += nbytes
            return self.salloc(name, shape, dt, at=off)

        with self.phase() as alloc:
            rmask = alloc("p_rmask", [128, 8, 64], F32)
            cd = Dep()
            s.op("dve", lambda e: e.memset(rmask[:, :, :], 1.0), writes=[cd])
            s.op("dve", lambda e: e.memset(rmask[:, :, 0:1], 0.0), writes=[cd])
            omka = alloc("p_omka", [128, 8], F32)
            s.op("dve", lambda e: e.tensor_scalar(out=omka[:, :], in0=self.P(f"k_a{j}"), scalar1=-1.0, scalar2=1.0, op0=ALU.mult, op1=ALU.add),
                 reads=[self.ptd], writes=[cd])
            tiny = alloc("p_tiny", [128, 1], F32)
            s.op("dve", lambda e: e.memset(tiny[:, :], 1e-24), writes=[cd])
            Sf = alloc("p_Sf", [64, 16, 64], F32)
            Sb = alloc("p_Sb", [64, 16, 64], BF16)
            Sfd = [Dep() for _ in range(4)]
            Sbd = [Dep() for _ in range(4)]
            s.op("dve", lambda e: e.memset(Sf[:, :, :], 0.0), writes=Sfd)
            s.op("dve", lambda e: e.memset(Sb[:, :, :], 0.0), writes=Sbd)
            bc8 = lambda nm: self.P(nm).unsqueeze(2).broadcast_to([128, 8, 64])
            names = ("r", "k", "v", "sg", "a", "g")
            srcs = {"r": scr["r"], "k": scr["k"], "v": vsrc, "sg": scr["sg"], "a": scr["a"], "g": scr["g"]}
            Lb = [{nm: xalloc(f"p_L{nm}{k}", [128, 8, 128], F32) for nm in names} for k in range(2)]
            Ld = [Dep(), Dep()]

            def load_sb(sb, bufs, dep):
                for nm in names:
                    s.dma("sp", bufs[nm][:, :, :], fmv(srcs[nm])[:, :, sb * 128:(sb + 1) * 128], writes=[dep])
            lst = Stream([(Lb[0], Ld[0]), (Lb[1], Ld[1])], list(range(16)), load_sb)
            def T(nm, dt=F32, shape=(128, 8, 64)):
                return xalloc("p_" + nm, list(shape), dt)
            prep = []
            inter_names = ("lw", "cum", "cumE", "e1", "e2", "e3", "e4", "kkr", "sqk", "rn", "kk", "ta", "k2", "beta", "rk")
            inter = {nm: T(nm) for nm in inter_names}
            inter_d = {nm: Dep() for nm in inter_names}
            for k in range(2):
                pt_ = dict(inter)
                pt_.update({nm: T(f"{nm}{k}") for nm in ("bonus", "dg")})
                pt_["PL"] = xalloc(f"p_PL{k}", [128, 8], F32)
                pt_["PLs"] = xalloc(f"p_PLs{k}", [128, 8, 2], F32)
                pt_["PLh"] = xalloc(f"p_PLh{k}", [64, 16], F32)
                s.op("dve", lambda e: e.memset(pt_["PLs"][:, :, :], 0.0), writes=[cd])
                pt_["AR"] = T(f"AR{k}", BF16, (128, 8, 128))
                for nm in ("Bt", "Kt", "Bh", "Kh", "vb"):
                    pt_[nm] = T(f"{nm}{k}", BF16)
                pt_["d"] = {nm: Dep() for nm in ("bonus", "dg", "PL", "PLs", "PLh", "AR", "Bt", "Kt", "Bh", "Kh", "vb")}
                pt_["d"].update(inter_d)
                prep.append(pt_)
            tmaj = []
            for k in range(2):
                tm = {"AX": alloc(f"p_AX{k}", [64, 16, 128], BF16), "B": alloc(f"p_tmB{k}", [64, 1024], BF16),
                      "K": alloc(f"p_tmK{k}", [64, 1024], BF16), "V": alloc(f"p_tmV{k}", [64, 1024], BF16)}
                tm["d"] = {"AXa": Dep(), "AXx": [Dep() for _ in range(4)], "B": Dep(), "K": Dep(), "V": Dep()}
                tmaj.append(tm)
            grp = []
            for k in range(2):
                gb = {"Mbm": alloc(f"p_Mbm{k}", [64, 4, 128], F32), "Mbr": alloc(f"p_Mbr{k}", [64, 4, 64], BF16),
                      "Mkb": alloc(f"p_Mkb{k}", [64, 4, 128], BF16), "ATt": alloc(f"p_ATt{k}", [64, 4, 64], BF16),
                      "AT": alloc(f"p_AT{k}", [64, 4, 128], BF16), "Tb": alloc(f"p_Tb{k}", [64, 4, 64], BF16),
                      "AU": alloc(f"p_AU{k}", [64, 4, 128], BF16), "Rh": alloc(f"p_Rh{k}", [64, 4, 64], BF16),
                      "Phi": alloc(f"p_Phi{k}", [64, 4, 64], F32), "Sn": alloc(f"p_Sn{k}", [64, 4, 64], F32)}
                gb["d"] = {nm: Dep() for nm in ("Mbm", "Mbr", "Mkb", "ATt", "AT", "Tb", "AU", "Rh", "Phi", "Sn")}
                grp.append(gb)
            Yall = [(alloc(f"p_Y{k}", [64, 16, 64], F32), [Dep() for _ in range(4)]) for k in range(2)]
            Yc = alloc("p_Yc", [64, 16, 64], F32)
            Ysq = alloc("p_Ysq", [64, 16, 64], F32)
            yst = alloc("p_yst", [64, 64], F32)
            ycd, ysqd, ystd = Dep(), Dep(), Dep()
            yf = alloc("p_yf", [128, 8, 64], F32)
            yfd = Dep()
            ygs = [(alloc(f"p_ygs{k}", [128, 8, 256], BF16), Dep()) for k in range(2)]
            gnb = alloc("p_gnb", [64, 1], F32)
            s.op("dve", lambda e: e.memset(gnb[:, :], 64e-5), writes=[cd])
            rrb = [4]

            def nb():
                b = rrb[0]
                rrb[0] = 4 + (rrb[0] - 4 + 1) % 4
                return self.bank(b)
            ident = self.C("ident")
            mask_ar = self.C("mask_ar", rows=(0, 64)).unsqueeze(1).broadcast_to([64, 4, 128])
            maskT = self.C("maskT", rows=(0, 64)).unsqueeze(1).broadcast_to([64, 4, 64])
            id64b = self.C("ident", 0, 64, rows=(0, 64)).unsqueeze(1).broadcast_to([64, 4, 64])
            identh_b = self.C("identh").unsqueeze(1).broadcast_to([128, 8, 64])
            v4 = lambda ap, n: ap.rearrange("p (h c) -> p h c", c=n)

            Lcur = [None]
            Rodd2 = [(alloc(f"p_Rodd{k}", [64, 8, 64], F32), Dep()) for k in range(2)]
            gq2 = [(alloc(f"p_gq{k}", [128, 8, 64], F32), Dep()) for k in range(2)]
            nch = getattr(self, "dbg_nch", 32)

            def serial_pre(c):
                if False:
                    yield
                sbk, ch = c // 2, c % 2
                if ch == 0:
                    Lcur[0] = lst.get()
                L, Ldep = Lcur[0]
                csl = slice(ch * 64, (ch + 1) * 64)
                Rodd_c, Roddd_c = Rodd2[c % 2]
                gq_c, gqd_c = gq2[c % 2]
                P_ = prep[c % 2]
                pd = P_["d"]
                Lr, Lk, Lv, Lsg, La, Lg = (L[nm][:, :, csl] for nm in names)

                def op(eng, fn, reads, writes):
                    s.op(eng, fn, reads=[(pd[x] if isinstance(x, str) else x) for x in reads],
                         writes=[(pd[x] if isinstance(x, str) else x) for x in writes])
                op("dve", lambda e: e.tensor_scalar(out=P_["lw"][:, :, :], in0=Lsg, scalar1=-0.6065306597126334, scalar2=None, op0=ALU.mult),
                   [Ldep], ["lw"])
                op("dve", lambda e: e.tensor_tensor_scan(P_["cum"][:, :, :].rearrange("p a b -> p (a b)"), rmask[:, :, :].rearrange("p a b -> p (a b)"),
                                                         P_["lw"][:, :, :].rearrange("p a b -> p (a b)"), 0.0, ALU.mult, ALU.add),
                   ["lw", cd], ["cum"])
                op("pool", lambda e: e.tensor_tensor(out=P_["cumE"][:, :, :], in0=P_["cum"][:, :, :], in1=P_["lw"][:, :, :], op=ALU.subtract),
                   ["cum", "lw"], ["cumE"])
                op("act", lambda e: e.activation(out=P_["e1"][:, :, :], in_=P_["cumE"][:, :, :], func=AF.Exp), ["cumE"], ["e1"])
                op("act", lambda e: e.activation(out=P_["e2"][:, :, :], in_=P_["cum"][:, :, :], func=AF.Exp), ["cum"], ["e2"])
                op("act", lambda e: e.activation(out=P_["e3"][:, :, :], in_=P_["cum"][:, :, :], func=AF.Exp, scale=-1.0), ["cum"], ["e3"])
                op("dve", lambda e: e.tensor_tensor(out=P_["e4"][:, :, :], in0=P_["cum"][:, :, :],
                                                    in1=P_["cum"][:, :, 63:64].broadcast_to([128, 8, 64]), op=ALU.subtract), ["cum"], ["e4"])
                op("act", lambda e: e.activation(out=P_["e4"][:, :, :], in_=P_["e4"][:, :, :], func=AF.Exp, scale=-1.0), ["e4"], ["e4"])
                op("act", lambda e: e.activation(out=P_["PL"][:, :].unsqueeze(2), in_=P_["cum"][:, :, 63:64], func=AF.Exp), ["cum"], ["PL"])
                op("pool", lambda e: e.tensor_copy(out=P_["PLs"][0:64, :, 0:1], in_=P_["PL"][0:64, :].unsqueeze(2)), ["PL", cd], ["PLs"])
                op("pool", lambda e: e.tensor_copy(out=P_["PLs"][64:128, :, 1:2], in_=P_["PL"][64:128, :].unsqueeze(2)), ["PL", cd], ["PLs"])
                bpl, bpld = self.bank(3)
                s.mm(bpl[0:64, 0:16], self.C("identh"), P_["PLs"][:, :, :].rearrange("p a b -> p (a b)"), True, True, reads=[pd["PLs"], self.ctd], out_dep=bpld)
                op("dve", lambda e: e.tensor_copy(out=P_["PLh"][:, :], in_=bpl[0:64, 0:16]), [bpld], ["PLh"])
                yield
                op("dve", lambda e: e.tensor_tensor(out=P_["kkr"][:, :, :], in0=Lk, in1=bc8(f"k_k{j}"), op=ALU.mult), [Ldep, self.ptd], ["kkr"])
                op("act", lambda e: e.activation(out=P_["sqk"][:, :, :], in_=P_["kkr"][:, :, :], func=AF.Square), ["kkr"], ["sqk"])
                yield
                bs, bsd = self.bank(2)
                for hp in range(8):
                    s.mm(bs[:, hp * 64:(hp + 1) * 64], self.C("bd"), P_["sqk"][:, hp, :], True, hp == 7, reads=[pd["sqk"], self.ctd], out_dep=bsd)
                op("act", lambda e: e.activation(out=P_["rn"][:, :, :], in_=v4(bs, 64), func=AF.Sqrt, bias=tiny[:, 0:1]), [bsd, cd], ["rn"])
                op("dve", lambda e: e.reciprocal(out=P_["rn"][:, :, :], in_=P_["rn"][:, :, :]), ["rn"], ["rn"])
                op("dve", lambda e: e.tensor_tensor(out=P_["kk"][:, :, :], in0=P_["kkr"][:, :, :], in1=P_["rn"][:, :, :], op=ALU.mult), ["kkr", "rn"], ["kk"])
                yield
                op("pool", lambda e: e.tensor_tensor(out=P_["ta"][:, :, :], in0=La, in1=bc8(f"k_a{j}"), op=ALU.mult), [Ldep, self.ptd], ["ta"])
                op("pool", lambda e: e.tensor_tensor(out=P_["ta"][:, :, :], in0=P_["ta"][:, :, :], in1=omka[:, :].unsqueeze(2).broadcast_to([128, 8, 64]),
                                                     op=ALU.add), ["ta", cd], ["ta"])
                op("dve", lambda e: e.tensor_tensor(out=P_["k2"][:, :, :], in0=Lk, in1=P_["ta"][:, :, :], op=ALU.mult), [Ldep, "ta"], ["k2"])
                op("pool", lambda e: e.tensor_tensor(out=P_["beta"][:, :, :], in0=P_["kk"][:, :, :], in1=La, op=ALU.mult), ["kk", Ldep], ["beta"])
                yield
                op("dve", lambda e: e.scalar_tensor_tensor(out=P_["AR"][:, :, 0:64], in0=P_["kk"][:, :, :], scalar=-1.0, in1=P_["e1"][:, :, :],
                                                           op0=ALU.mult, op1=ALU.mult), ["kk", "e1"], ["AR"])
                op("pool", lambda e: e.tensor_tensor(out=P_["AR"][:, :, 64:128], in0=Lr, in1=P_["e2"][:, :, :], op=ALU.mult), [Ldep, "e2"], ["AR"])
                op("dve", lambda e: e.tensor_tensor(out=P_["Bt"][:, :, :], in0=P_["beta"][:, :, :], in1=P_["e3"][:, :, :], op=ALU.mult), ["beta", "e3"], ["Bt"])
                op("pool", lambda e: e.tensor_tensor(out=P_["Kt"][:, :, :], in0=P_["k2"][:, :, :], in1=P_["e3"][:, :, :], op=ALU.mult), ["k2", "e3"], ["Kt"])
                op("dve", lambda e: e.tensor_tensor(out=P_["Bh"][:, :, :], in0=P_["beta"][:, :, :], in1=P_["e4"][:, :, :], op=ALU.mult), ["beta", "e4"], ["Bh"])
                op("pool", lambda e: e.tensor_tensor(out=P_["Kh"][:, :, :], in0=P_["k2"][:, :, :], in1=P_["e4"][:, :, :], op=ALU.mult), ["k2", "e4"], ["Kh"])
                yield
                s.op("act", lambda e: e.activation(out=gq_c[:, :, :], in_=Lg, func=AF.Copy), reads=[Ldep], writes=[gqd_c])
                op("act", lambda e: e.activation(out=P_["vb"][:, :, :], in_=Lv, func=AF.Copy), [Ldep], ["vb"])
                op("pool", lambda e: e.tensor_tensor(out=P_["rk"][:, :, :], in0=Lr, in1=P_["k2"][:, :, :], op=ALU.mult), [Ldep, "k2"], ["rk"])
                op("pool", lambda e: e.tensor_tensor(out=P_["rk"][:, :, :], in0=P_["rk"][:, :, :], in1=bc8(f"r_k{j}"), op=ALU.mult), ["rk", self.ptd], ["rk"])
                yield
                bb, bbd = self.bank(3)
                for hp in range(8):
                    s.mm(bb[:, hp * 64:(hp + 1) * 64], self.C("bd"), P_["rk"][:, hp, :], True, hp == 7, reads=[pd["rk"], self.ctd], out_dep=bbd)
                op("dve", lambda e: e.tensor_tensor(out=P_["bonus"][:, :, :], in0=v4(bb, 64), in1=Lv, op=ALU.mult), [bbd, Ldep], ["bonus"])
                tm = tmaj[c % 2]
                td_ = tm["d"]
                for ti, (srcf, sdep, dst, ddeps) in enumerate((
                        (lambda hp: P_["AR"][:, hp, 0:64], "AR", tm["AX"][:, :, 0:64], [td_["AXa"]]),
                        (lambda hp: P_["Bh"][:, hp, :], "Bh", tm["B"][:, :].rearrange("p (h c) -> p h c", c=64), [td_["B"]]),
                        (lambda hp: P_["Kh"][:, hp, :], "Kh", tm["K"][:, :].rearrange("p (h c) -> p h c", c=64), [td_["K"]]),
                        (lambda hp: P_["vb"][:, hp, :], "vb", tm["V"][:, :].rearrange("p (h c) -> p h c", c=64), [td_["V"]]))):
                    yield
                    half = 1024
                    b0 = 2
                    for hp in range(8):
                        s.mm(self.psA[0:64, half + hp * 128:half + (hp + 1) * 128], srcf(hp), self.identb[:, :], True, hp % 4 == 3,
                             reads=[pd[sdep], self.cbd], out_dep=self.bd_[b0 + hp // 4])
                    src = self.psA[0:64, half:half + 1024].rearrange("p (h c) -> p h c", c=64)
                    if ti % 2 == 0:
                        s.op("act", lambda e: e.activation(out=dst, in_=src, func=AF.Copy), reads=[self.bd_[b0], self.bd_[b0 + 1]], writes=ddeps)
                    else:
                        s.op("dve", lambda e: e.tensor_copy(out=dst, in_=src), reads=[self.bd_[b0], self.bd_[b0 + 1]], writes=ddeps)
                yield
                bro, brod = self.bank(2)
                for hp in range(8):
                    s.mm(bro[0:64, hp * 64:(hp + 1) * 64], self.identb[64:128, 64:128], P_["AR"][64:128, hp, 64:128], True, hp == 7,
                         reads=[self.cbd, pd["AR"]], out_dep=brod)
                s.op("act", lambda e: e.activation(out=Rodd_c[:, :, :], in_=v4(bro[0:64, :], 64), func=AF.Copy), reads=[brod], writes=[Roddd_c])

            def make_groups(c):
                sbk, ch = c // 2, c % 2
                P_ = prep[c % 2]
                pd = P_["d"]
                tm = tmaj[c % 2]
                td_ = tm["d"]
                Yt, Ytd = Yall[c % 2]

                def op(eng, fn, reads, writes):
                    s.op(eng, fn, reads=[(pd[x] if isinstance(x, str) else x) for x in reads],
                         writes=[(pd[x] if isinstance(x, str) else x) for x in writes])
                def gsteps(g, mybanks):
                    rr_ = [0]

                    def nb():
                        b_ = mybanks[rr_[0] % len(mybanks)]
                        rr_[0] += 1
                        return self.bank(b_)
                    G = grp[g % 2]
                    gd = G["d"]
                    hb = (g // 2) * 8 + (g % 2)
                    heads = [(hb + 2 * hh, (hb + 2 * hh) // 2, hb % 2) for hh in range(4)]
                    hs = slice(hb, hb + 7, 2)
                    hpsl = slice(hb // 2, hb // 2 + 4)
                    gpar = hb % 2
                    b1, b1d = nb()
                    b2, b2d = nb()
                    b3, b3d = nb()
                    for hh, (h, hp, par) in enumerate(heads):
                        ps = slice(par * 64, (par + 1) * 64)
                        s.mm(b1[0:64, hh * 128:(hh + 1) * 128], P_["Bt"][ps, hp, :], P_["AR"][ps, hp, :], True, hh == 3, reads=[pd["Bt"], pd["AR"]], out_dep=b1d)
                    for hh, (h, hp, par) in enumerate(heads):
                        ps = slice(par * 64, (par + 1) * 64)
                        s.mm(b2[0:64, hh * 128:(hh + 1) * 128], P_["Kt"][ps, hp, :], P_["AR"][ps, hp, :], True, hh == 3, reads=[pd["Kt"], pd["AR"]], out_dep=b2d)
                    for hh, (h, hp, par) in enumerate(heads):
                        ps = slice(par * 64, (par + 1) * 64)
                        s.mm(b3[0:64, hh * 64:(hh + 1) * 64], P_["AR"][ps, hp, 0:64], P_["Bt"][ps, hp, :], True, hh == 3, reads=[pd["Bt"], pd["AR"]], out_dep=b3d)
                    if getattr(self, 'dbg_m', 9) < 1:
                        return
                    s.op("dve", lambda e: e.tensor_tensor(out=G["Mbm"][:, :, :], in0=v4(b1[0:64, :], 128), in1=mask_ar, op=ALU.mult),
                         reads=[b1d, self.ctd], writes=[gd["Mbm"]])
                    s.op("dve", lambda e: e.tensor_tensor(out=G["Mkb"][:, :, :], in0=v4(b2[0:64, :], 128), in1=mask_ar, op=ALU.mult),
                         reads=[b2d, self.ctd], writes=[gd["Mkb"]])
                    s.op("dve", lambda e: e.tensor_tensor(out=G["ATt"][:, :, :], in0=v4(b3[0:64, 0:256], 64), in1=maskT, op=ALU.mult),
                         reads=[b3d, self.ctd], writes=[gd["ATt"]])
                    if getattr(self, 'dbg_m', 9) < 2:
                        return
                    s.op("act", lambda e: e.activation(out=G["AT"][:, :, 0:64], in_=G["Mbm"][:, :, 0:64], func=AF.Copy), reads=[gd["Mbm"]], writes=[gd["AT"]])
                    s.op("pool", lambda e: e.tensor_tensor(out=G["AT"][:, :, 64:128], in0=G["Mbm"][:, :, 0:64], in1=id64b, op=ALU.add),
                         reads=[gd["Mbm"], self.ctd], writes=[gd["AT"]])
                    s.op("act", lambda e: e.activation(out=G["Mbr"][:, :, :], in_=G["Mbm"][:, :, 64:128], func=AF.Copy), reads=[gd["Mbm"]], writes=[gd["Mbr"]])
                    if getattr(self, 'dbg_sub', 9) < 2:
                        return
                    yield
                    for rnd in range(6):
                        if rnd > 0:
                            yield
                        bA, bAd = nb()
                        if rnd == 0:
                            bB, bBd = nb()
                            for hh in range(4):
                                s.mm(bA[0:64, hh * 64:(hh + 1) * 64], G["ATt"][:, hh, :], G["AT"][:, hh, 0:64], True, hh == 3, reads=[gd["ATt"], gd["AT"]], out_dep=bAd)
                            for hh in range(4):
                                s.mm(bB[0:64, hh * 64:(hh + 1) * 64], G["AT"][:, hh, 0:64], G["ATt"][:, hh, :], True, hh == 3, reads=[gd["ATt"], gd["AT"]], out_dep=bBd)
                            s.op("act", lambda e: e.activation(out=G["AT"][:, :, 0:64], in_=v4(bA[0:64, 0:256], 64), func=AF.Copy), reads=[bAd], writes=[gd["AT"]])
                            s.op("act", lambda e: e.activation(out=G["ATt"][:, :, :], in_=v4(bB[0:64, 0:256], 64), func=AF.Copy), reads=[bBd], writes=[gd["ATt"]])
                        elif rnd < 5:
                            bB, bBd = nb()
                            for hh in range(4):
                                s.mm(bA[0:64, hh * 128:(hh + 1) * 128], G["ATt"][:, hh, :], G["AT"][:, hh, :], True, hh == 3, reads=[gd["ATt"], gd["AT"]], out_dep=bAd)
                            for hh in range(4):
                                s.mm(bB[0:64, hh * 64:(hh + 1) * 64], G["AT"][:, hh, 0:64], G["ATt"][:, hh, :], True, hh == 3, reads=[gd["ATt"], gd["AT"]], out_dep=bBd)
                            bA4 = v4(bA[0:64, :], 128)
                            s.op("dve", lambda e: e.tensor_tensor(out=G["AT"][:, :, 64:128], in0=G["AT"][:, :, 64:128], in1=bA4[:, :, 64:128], op=ALU.add),
                                 reads=[bAd, gd["AT"]], writes=[gd["AT"]])
                            s.op("dve", lambda e: e.tensor_copy(out=G["AT"][:, :, 0:64], in_=bA4[:, :, 0:64]), reads=[bAd, gd["AT"]], writes=[gd["AT"]])
                            s.op("act", lambda e: e.activation(out=G["ATt"][:, :, :], in_=v4(bB[0:64, 0:256], 64), func=AF.Copy), reads=[bBd], writes=[gd["ATt"]])
                        else:
                            for hh in range(4):
                                s.mm(bA[0:64, hh * 64:(hh + 1) * 64], G["ATt"][:, hh, :], G["AT"][:, hh, 64:128], True, hh == 3, reads=[gd["ATt"], gd["AT"]], out_dep=bAd)
                            s.op("dve", lambda e: e.tensor_tensor(out=G["Tb"][:, :, :], in0=G["AT"][:, :, 64:128], in1=v4(bA[0:64, 0:256], 64), op=ALU.add),
                                 reads=[bAd, gd["AT"]], writes=[gd["Tb"]])
                    if getattr(self, 'dbg_sub', 9) < 3:
                        return
                    yield
                    bx, bxd = nb()
                    for hh, (h, hp, par) in enumerate(heads):
                        s.mm(bx[0:64, hh * 64:(hh + 1) * 64], G["Mkb"][:, hh, 0:64], tm["V"][:, h * 64:(h + 1) * 64], True, hh == 3,
                             reads=[gd["Mkb"], td_["V"]], out_dep=bxd)
                    s.op("act", lambda e: e.activation(out=tm["AX"][:, hs, 64:128], in_=v4(bx[0:64, 0:256], 64), func=AF.Copy),
                         reads=[bxd], writes=[td_["AXx"][g]])
                    yield
                    bu, bud = nb()
                    for hh, (h, hp, par) in enumerate(heads):
                        s.mm(bu[0:64, hh * 128:(hh + 1) * 128], G["Tb"][:, hh, :], tm["AX"][:, h, :], True, hh == 3,
                             reads=[gd["Tb"], td_["AXa"], td_["AXx"][g]], out_dep=bud)
                    s.op("act", lambda e: e.activation(out=G["AU"][:, :, :], in_=v4(bu[0:64, :], 128), func=AF.Copy), reads=[bud], writes=[gd["AU"]])
                    if getattr(self, 'dbg_sub', 9) < 4:
                        return
                    yield
                    br_, brd = nb()
                    bp, bpd = nb()
                    for hh, (h, hp, par) in enumerate(heads):
                        ps = slice(par * 64, (par + 1) * 64)
                        s.mm(br_[0:64, hh * 64:(hh + 1) * 64], G["AU"][:, hh, 0:64], G["Mbr"][:, hh, :], True, hh == 3, reads=[gd["AU"], gd["Mbr"]], out_dep=brd)
                    for hh, (h, hp, par) in enumerate(heads):
                        ps = slice(par * 64, (par + 1) * 64)
                        s.mm(bp[0:64, hh * 64:(hh + 1) * 64], G["AU"][:, hh, 0:64], tm["B"][:, h * 64:(h + 1) * 64], True, hh == 3, reads=[gd["AU"], td_["B"]], out_dep=bpd)
                    rsrc = P_["AR"][0:64, hpsl, 64:128] if gpar == 0 else Rodd2[c % 2][0][:, hpsl, :]
                    s.op("dve", lambda e: e.tensor_tensor(out=G["Rh"][:, :, :], in0=v4(br_[0:64, 0:256], 64), in1=rsrc, op=ALU.add),
                         reads=[brd, pd["AR"], Rodd2[c % 2][1]], writes=[gd["Rh"]])
                    s.op("pool", lambda e: e.tensor_tensor(out=G["Phi"][:, :, :], in0=id64b, in1=P_["PLh"][:, hs].unsqueeze(2).broadcast_to([64, 4, 64]),
                                                           op=ALU.mult), reads=[pd["PLh"], self.ctd], writes=[gd["Phi"]])
                    s.op("dve", lambda e: e.tensor_tensor(out=G["Phi"][:, :, :], in0=G["Phi"][:, :, :], in1=v4(bp[0:64, 0:256], 64), op=ALU.add),
                         reads=[bpd, gd["Phi"]], writes=[gd["Phi"]])
                    if getattr(self, 'dbg_sub', 9) < 5:
                        return
                    yield
                    by, byd = nb()
                    for hh, (h, hp, par) in enumerate(heads):
                        o_ = by[0:64, hh * 64:(hh + 1) * 64]
                        s.mm(o_, G["Mbr"][:, hh, :], G["AU"][:, hh, 64:128], True, False, reads=[gd["Mbr"], gd["AU"]], out_dep=byd)
                        s.mm(o_, G["Mkb"][:, hh, 64:128], tm["V"][:, h * 64:(h + 1) * 64], False, False, reads=[gd["Mkb"], td_["V"]], out_dep=byd)
                        s.mm(o_, G["Rh"][:, hh, :], Sb[:, h, :], False, hh == 3, reads=[gd["Rh"], Sbd[g]], out_dep=byd)
                    s.op("act", lambda e: e.activation(out=Yt[:, hs, :], in_=v4(by[0:64, 0:256], 64), func=AF.Copy), reads=[byd], writes=[Ytd[g]])
                    if getattr(self, 'dbg_sub', 9) < 6:
                        return
                    yield
                    bn, bnd = nb()
                    bn2, bn2d = nb()
                    for hh, (h, hp, par) in enumerate(heads):
                        o_ = bn[0:64, hh * 64:(hh + 1) * 64]
                        s.mm(o_, tm["B"][:, h * 64:(h + 1) * 64], G["AU"][:, hh, 64:128], True, False, reads=[td_["B"], gd["AU"]], out_dep=bnd)
                        s.mm(o_, tm["K"][:, h * 64:(h + 1) * 64], tm["V"][:, h * 64:(h + 1) * 64], False, hh == 3, reads=[td_["K"], td_["V"]], out_dep=bnd)
                    for hh, (h, hp, par) in enumerate(heads):
                        s.mm(bn2[0:64, hh * 64:(hh + 1) * 64], G["Phi"][:, hh, :], Sf[:, h, :], True, hh == 3, reads=[gd["Phi"], Sfd[g]], out_dep=bn2d)
                    s.op("act", lambda e: e.activation(out=G["Sn"][:, :, :], in_=v4(bn[0:64, 0:256], 64), func=AF.Copy), reads=[bnd], writes=[gd["Sn"]])
                    s.op("dve", lambda e: e.tensor_tensor(out=Sf[:, hs, :], in0=G["Sn"][:, :, :], in1=v4(bn2[0:64, 0:256], 64), op=ALU.add),
                         reads=[bn2d, gd["Sn"]], writes=[Sfd[g]])
                    s.op("act", lambda e: e.activation(out=Sb[:, hs, :], in_=Sf[:, hs, :], func=AF.Copy), reads=[Sfd[g]], writes=[Sbd[g]])

                return gsteps

            def serial_out(c):
                if False:
                    yield
                sbk, ch = c // 2, c % 2
                P_ = prep[c % 2]
                pd = P_["d"]
                tm = tmaj[c % 2]
                td_ = tm["d"]
                Yt, Ytd = Yall[c % 2]

                def op(eng, fn, reads, writes):
                    s.op(eng, fn, reads=[(pd[x] if isinstance(x, str) else x) for x in reads],
                         writes=[(pd[x] if isinstance(x, str) else x) for x in writes])
                s.op("dve", lambda e: e.reduce_sum(out=yst[:, 0:16], in_=Yt[:, :, :], axis=AX.X), reads=Ytd, writes=[ystd])
                s.op("dve", lambda e: e.tensor_scalar(out=yst[:, 0:16], in0=yst[:, 0:16], scalar1=1.0 / 64.0, scalar2=None, op0=ALU.mult), reads=[ystd], writes=[ystd])
                s.op("pool", lambda e: e.tensor_tensor(out=Yc[:, :, :], in0=Yt[:, :, :], in1=yst[:, 0:16].unsqueeze(2).broadcast_to([64, 16, 64]), op=ALU.subtract),
                     reads=Ytd + [ystd], writes=[ycd])
                yield
                s.op("act", lambda e: e.activation(out=Ysq[:, :, :], in_=Yc[:, :, :], func=AF.Square), reads=[ycd], writes=[ysqd])
                s.op("dve", lambda e: e.reduce_sum(out=yst[:, 16:32], in_=Ysq[:, :, :], axis=AX.X), reads=[ysqd, ystd], writes=[ystd])
                s.op("act", lambda e: e.activation(out=yst[:, 16:32], in_=yst[:, 16:32], func=AF.Sqrt, bias=gnb[:, 0:1], scale=1.0 / 64.0), reads=[ystd, cd], writes=[ystd])
                s.op("dve", lambda e: e.reciprocal(out=yst[:, 16:32], in_=yst[:, 16:32]), reads=[ystd], writes=[ystd])
                s.op("pool", lambda e: e.tensor_tensor(out=Yc[:, :, :], in0=Yc[:, :, :], in1=yst[:, 16:32].unsqueeze(2).broadcast_to([64, 16, 64]), op=ALU.mult),
                     reads=[ycd, ystd, ysqd], writes=[ycd])
                yield
                bo, bod = self.bank(2)
                Ycf = Yc[:, :, :].rearrange("p a b -> p (a b)")
                for hp in range(8):
                    s.mm(bo[:, hp * 64:(hp + 1) * 64], Ycf[:, hp * 128:(hp + 1) * 128], ident[0:64, 0:64], True, hp == 7, reads=[ycd, self.ctd], out_dep=bod)
                s.op("dve", lambda e: e.tensor_tensor(out=yf[:, :, :], in0=v4(bo, 64), in1=bc8(f"lnx_g{j}"), op=ALU.mult), reads=[bod, self.ptd], writes=[yfd])
                s.op("pool", lambda e: e.tensor_tensor(out=yf[:, :, :], in0=yf[:, :, :], in1=bc8(f"lnx_b{j}"), op=ALU.add), reads=[yfd, self.ptd], writes=[yfd])
                s.op("pool", lambda e: e.tensor_tensor(out=yf[:, :, :], in0=yf[:, :, :], in1=P_["bonus"][:, :, :], op=ALU.add), reads=[yfd, pd["bonus"]], writes=[yfd])
                yield
                yg_t, yg_d = ygs[(c // 4) % 2]
                s.op("dve", lambda e: e.tensor_tensor(out=yg_t[:, :, (c % 4) * 64:(c % 4 + 1) * 64], in0=yf[:, :, :], in1=gq2[c % 2][0][:, :, :], op=ALU.mult),
                     reads=[yfd, gq2[c % 2][1]], writes=[yg_d])
                if c % 4 == 3:
                    c0 = (c // 4) * 256
                    s.dma("sp", fmv(scr["yg"])[:, :, c0:c0 + 256], yg_t[:, :, :], reads=[yg_d])

            def chain(*gens):
                for g_ in gens:
                    if g_ is not None:
                        yield from g_

            for _ in serial_pre(0):
                pass
            for c in range(nch):
                ser = chain(serial_out(c - 1) if c > 0 else None, serial_pre(c + 1) if c + 1 < nch else None)
                gsteps = make_groups(c)
                ser_live = True
                for pair in ((0, 1), (2, 3)):
                    live = [gsteps(pair[0], [4, 5, 6]), gsteps(pair[1], [7, 0, 1])]
                    while live:
                        for gg in list(live):
                            try:
                                next(gg)
                            except StopIteration:
                                live.remove(gg)
                        if ser_live:
                            try:
                                next(ser)
                            except StopIteration:
                                ser_live = False
                if ser_live:
                    for _ in ser:
                        pass
            for _ in serial_out(nch - 1):
                pass
        self.Xd = [[Dep() for _ in range(4)] for _ in range(DC)]
        for c in range(DC):
            s.dma("sp", self.X[:, c, :], xs[:, c, :], writes=self.Xd[c])

    Prog.rwkv_pass2 = rwkv_pass2


_rwkv2_methods()
```

```python
import contextlib
import math
import numpy as np
import concourse.bass as bass
import concourse.mybir as mybir
from concourse.bass_utils import run_bass_kernel_spmd

F32 = mybir.dt.float32
BF16 = mybir.dt.bfloat16
I32 = mybir.dt.int32
AF = mybir.ActivationFunctionType
ALU = mybir.AluOpType
AX = mybir.AxisListType

S = 2048
D = 1024
DC = 8
NL = 4
DFF = 2816
FC = 22
NE = 8
EPS = 1e-6
NEG = -30000.0
SB_BASE = 16512
SB_TOP = 229344


class Dep:
    __slots__ = ("w", "r")

    def __init__(self):
        self.w = None
        self.r = {}


class Sched:
    def __init__(self, nc, n_dma_sems=24):
        self.nc = nc
        self.eng = {"pe": nc.tensor, "dve": nc.vector, "act": nc.scalar,
                    "pool": nc.gpsimd, "sp": nc.sync}
        self.sem = {e: nc.alloc_semaphore(name=f"s_{e}") for e in self.eng}
        self.cnt = {e: 0 for e in self.eng}
        self.dsem = [nc.alloc_semaphore(name=f"d_{i}") for i in range(n_dma_sems)]
        self.dcnt = [0] * n_dma_sems
        self.dlast = [None] * n_dma_sems
        self.dnext = 0
        self.seen = {e: {} for e in self.eng}
        self.nins = 0

    def _wait(self, e, key, val):
        if self.seen[e].get(key, 0) >= val:
            return
        sem = self.sem[key[1]] if key[0] == "e" else self.dsem[key[1]]
        self.eng[e].wait_ge(sem, val)
        self.seen[e][key] = val

    def _deps(self, e, reads, writes):
        need = {}
        for d in reads:
            if d.w is not None:
                k, v = d.w
                if need.get(k, 0) < v:
                    need[k] = v
        for d in writes:
            if d.w is not None:
                k, v = d.w
                if need.get(k, 0) < v:
                    need[k] = v
            for k, v in d.r.items():
                if need.get(k, 0) < v:
                    need[k] = v
        for k, v in need.items():
            self._wait(e, k, v)

    def _commit(self, tok, reads, writes):
        k, v = tok
        for d in reads:
            if d.r.get(k, 0) < v:
                d.r[k] = v
        for d in writes:
            d.w = tok
            d.r = {}

    def op(self, e, fn, reads=(), writes=()):
        self._deps(e, reads, writes)
        ins = fn(self.eng[e])
        self.cnt[e] += 1
        ins.then_inc(self.sem[e], 1)
        tok = (("e", e), self.cnt[e])
        self._commit(tok, reads, writes)
        self.nins += 1
        return tok

    def mm(self, out, lhsT, rhs, start, stop, reads=(), out_dep=None, inc=None):
        e = "pe"
        if inc is None:
            inc = stop
        self._deps(e, reads, [])
        if start and out_dep is not None:
            need = dict(out_dep.r)
            if out_dep.w is not None and out_dep.w[0] != ("e", "pe"):
                k, v = out_dep.w
                if need.get(k, 0) < v:
                    need[k] = v
            for k, v in need.items():
                self._wait(e, k, v)
        ins = self.eng[e].matmul(out, lhsT, rhs, start=start, stop=stop)
        if inc:
            self.cnt[e] += 1
            ins.then_inc(self.sem[e], 1)
            tok = (("e", e), self.cnt[e])
        else:
            tok = (("e", e), self.cnt[e] + 1)
        self._commit(tok, reads, [out_dep] if stop else [])
        self.nins += 1
        return tok

    def dma(self, q, out, in_, reads=(), writes=(), **kw):
        i = self.dnext
        self.dnext = (i + 1) % len(self.dsem)
        self._deps(q, reads, writes)
        if self.dlast[i] is not None:
            self._wait(q, *self.dlast[i])
        ins = self.eng[q].dma_start(out=out, in_=in_, **kw)
        self.dcnt[i] += 16
        ins.then_inc(self.dsem[i], 16)
        tok = (("d", i), self.dcnt[i])
        self.dlast[i] = tok
        self._commit(tok, reads, writes)
        self.nins += 1
        return tok

    def barrier(self, engines=None):
        engines = engines or list(self.eng)
        for e in engines:
            for f in self.eng:
                if self.cnt[f] > 0:
                    self._wait(e, ("e", f), self.cnt[f])
            for t in self.dlast:
                if t is not None:
                    self._wait(e, *t)


class Stream:
    def __init__(self, bufs, items, load_fn):
        self.bufs = bufs
        self.items = items
        self.load_fn = load_fn
        self.n = 0
        self.issued = 0

    def get(self):
        nb = len(self.bufs)
        while self.issued < len(self.items) and self.issued < self.n + nb:
            t, d = self.bufs[self.issued % nb]
            self.load_fn(self.items[self.issued], t, d)
            self.issued += 1
        t, d = self.bufs[self.n % nb]
        self.n += 1
        return t, d


def _fm(v):
    v = np.asarray(v, np.float32).reshape(-1)
    n = v.size // 128
    return np.ascontiguousarray(v.reshape(n, 128).T)


class Table:
    def __init__(self):
        self.cols = {}
        self.parts = []
        self.n = 0

    def add(self, name, arr):
        arr = np.asarray(arr, np.float32)
        assert arr.shape[0] == 128, (name, arr.shape)
        self.cols[name] = (self.n, arr.shape[1])
        self.parts.append(arr)
        self.n += arr.shape[1]

    def build(self):
        return np.ascontiguousarray(np.concatenate(self.parts, axis=1))


def _pad128(a):
    out = np.zeros((128, a.shape[1]), np.float32)
    out[: a.shape[0]] = a
    return out


def const_table():
    t = Table()
    p = np.arange(128)
    t.add("ident", np.eye(128, dtype=np.float32))
    t.add("ones", np.ones((128, 128), np.float32))
    t.add("bd", (p[:, None] // 64 == p[None, :] // 64).astype(np.float32))
    t.add("identh", (p[:, None] % 64 == np.arange(64)[None, :]).astype(np.float32))
    j = np.arange(64)[:, None]
    tt = np.arange(64)[None, :]
    t.add("mask_ar", _pad128(np.concatenate([(j < tt), (j <= tt)], axis=1).astype(np.float32)))
    t.add("maskT", _pad128((tt < j).astype(np.float32)))
    qi = p[:, None]
    kj = p[None, :]
    t.add("cmask", np.where(kj <= qi, 0.0, NEG).astype(np.float32))
    t.add("mmask", np.where((kj // 64) <= (qi // 64), 0.0, NEG).astype(np.float32))
    t.add("invf", (10000.0 ** (-(p % 16).astype(np.float64) / 16.0)).astype(np.float32)[:, None])
    sel8 = np.zeros((128, 8 * 128), np.float32)
    for e in range(8):
        sel8[e, e * 128:(e + 1) * 128] = 1.0
    t.add("sel8", sel8)
    sel16 = np.zeros((128, 16 * 128), np.float32)
    for e in range(16):
        sel16[e, e * 128:(e + 1) * 128] = 1.0
    t.add("sel16", sel16)
    return t


def param_table(inp, b):
    t = Table()
    t.add("c", _fm(inp["c"][b]))
    for i in range(NL):
        t.add(f"adab{i}", _fm(inp["ada_b"][i]))
        t.add(f"nmg{i}", _fm(inp["norm_mix_g"][i]))
        t.add(f"nfg{i}", _fm(inp["norm_ffn_g"][i]))
    t.add("fng", _fm(inp["final_norm_g"]))
    for j in range(2):
        t.add(f"mu{j}", _fm(inp["rw_mu"][j]))
        for nm in ("w0", "a0", "k_k", "k_a", "r_k", "lnx_g", "lnx_b"):
            t.add(f"{nm}{j}", _fm(inp["rw_" + nm][j]))
    t.add("v0", _fm(inp["rw_v0"][0]))
    t.add("mqg", _fm(inp["mla_q_norm_g"][0]))
    t.add("mkvg", _fm(inp["mla_kv_norm_g"][0]))
    t.add("fbf", _pad128(np.asarray(inp["fox_b_f"][0], np.float32).reshape(16, 1)))
    t.add("fqg", np.tile(np.asarray(inp["fox_q_norm_g"][0], np.float32).reshape(64, 1), (2, 1)))
    t.add("fkg", np.tile(np.asarray(inp["fox_k_norm_g"][0], np.float32).reshape(64, 1), (2, 1)))
    for j in range(2):
        t.add(f"mbr{j}", np.tile(np.asarray(inp["moe_b_router"][j], np.float32).reshape(1, 8), (128, 1)))
        wr = np.asarray(inp["moe_w_router"][j], np.float32)
        t.add(f"mwr{j}", np.ascontiguousarray(wr.reshape(8, 128, 8).transpose(1, 0, 2).reshape(128, 64)))
    return t


_CT = const_table()


W_SHAPES = {
    "ada_w": [4, 1024, 6144],
    "rw_w_rkv": [2, 3, 1024, 1024], "rw_w_o": [2, 1024, 1024],
    "rw_w1": [2, 1024, 64], "rw_w2": [2, 64, 1024],
    "rw_a1": [2, 1024, 64], "rw_a2": [2, 64, 1024],
    "rw_g1": [2, 1024, 160], "rw_g2": [2, 160, 1024],
    "rw_v1": [1, 1024, 32], "rw_v2": [1, 32, 1024],
    "mla_w_down": [1, 1024, 1056], "mla_w_uq": [1, 768, 1536],
    "mla_w_ukv": [1, 256, 2048], "mla_w_o": [1, 1024, 1024],
    "fox_w_in": [1, 1024, 4112], "fox_w_o": [1, 1024, 1024],
    "ffn_w_gate_up": [2, 1024, 5632], "ffn_w_down": [2, 2816, 1024],
    "moe_w_gate_up": [2, 8, 1024, 5632], "moe_w_down": [2, 8, 2816, 1024],
}


class Prog:
    def __init__(self, pcols, npcols, plan=None, x_in_dbg=False):
        self.plan = plan
        nc = bass.Bass("TRN2", target_bir_lowering=False)
        self.nc = nc
        self.s = Sched(nc)
        self.pcols = pcols
        self.ccols = _CT.cols
        d = {}
        d["xT"] = nc.dram_tensor("xT", [D, S], F32, kind="ExternalInput").ap()
        d["pos"] = nc.dram_tensor("pos", [1, S], I32, kind="ExternalInput").ap()
        d["ptab"] = nc.dram_tensor("ptab", [128, npcols], F32, kind="ExternalInput").ap()
        d["ctab"] = nc.dram_tensor("ctab", [128, _CT.n], F32, kind="ExternalInput").ap()
        for k, shp in W_SHAPES.items():
            d[k] = nc.dram_tensor(k, shp, F32, kind="ExternalInput").ap()
        d["outT"] = nc.dram_tensor("outT", [D, S], F32, kind="ExternalOutput").ap()
        self.d = d
        self.sb_ptr = SB_BASE
        self.uid = 0
        self.X = self.salloc("X", [128, DC, S], F32)
        self.Xd = [[Dep() for _ in range(4)] for _ in range(DC)]
        self.pt = self.salloc("pt", [128, npcols], F32)
        self.ptd = Dep()
        nct = self.ccols["sel8"][0]
        self.ct = self.salloc("ct", [128, nct], F32)
        self.ctd = Dep()
        self.identb = self.salloc("identb", [128, 128], BF16)
        self.cmaskb = self.salloc("cmaskb", [128, 128], BF16)
        self.onesb = self.salloc("onesb", [128, 128], BF16)
        self.mmaskb = self.salloc("mmaskb", [128, 128], BF16)
        self.cbd = Dep()
        self.mod = self.salloc("mod", [128, NL * 48], F32)
        self.modd = Dep()
        self.amod = self.salloc("amod", [128, NL * 16 + 8], F32)
        self.amodd = Dep()
        self.cond = self.salloc("cond", [128, 8], F32)
        self.epsc = self.salloc("epsc", [128, 4], F32)
        self.s.op("dve", lambda e: e.memset(self.epsc[:, 0:1], EPS))
        self.s.op("dve", lambda e: e.memset(self.epsc[:, 1:2], 64e-5))
        self.s.op("dve", lambda e: e.memset(self.epsc[:, 2:3], 1.0))
        self.s.op("dve", lambda e: e.memset(self.epsc[:, 3:4], 0.0))
        self.condd = Dep()
        self.psA = nc.alloc_psum_tensor("psA", [128, 2048], F32)
        self.psB = [nc.alloc_psum_tensor(f"psB{i}", [128, 512], F32) for i in range(4)]
        self.bd_ = [Dep() for _ in range(8)]
        self.rr = 0

    def bank(self, i):
        if i < 4:
            return self.psA[:, i * 512:(i + 1) * 512], self.bd_[i]
        return self.psB[i - 4][:, :], self.bd_[i]

    def P(self, name, a=0, b=None):
        st, n = self.pcols[name]
        b = n if b is None else b
        return self.pt[:, st + a:st + b]

    def C(self, name, a=0, b=None, rows=None):
        st, n = self.ccols[name]
        b = n if b is None else b
        if rows is None:
            return self.ct[:, st + a:st + b]
        return self.ct[rows[0]:rows[1], st + a:st + b]

    def modc(self, i, w, c0=0, c1=8):
        return self.mod[:, i * 48 + w * 8 + c0:i * 48 + w * 8 + c1]

    def salloc(self, name, shape, dt, at=None):
        nbytes = int(np.prod(shape[1:])) * (2 if dt == BF16 else 4)
        nbytes = (nbytes + 31) // 32 * 32
        if at is None:
            off = self.sb_ptr
            self.sb_ptr += nbytes
            assert self.sb_ptr <= SB_TOP, f"SBUF overflow allocating {name}: {self.sb_ptr} > {SB_TOP}"
        else:
            off = at
        self.uid += 1
        t = self.nc.alloc_sbuf_tensor_at(f"{name}_{self.uid}", list(shape), dt, offset=off)
        return t

    @contextlib.contextmanager
    def phase(self):
        mark = self.sb_ptr
        yield self.salloc
        self.s.barrier()
        self.sb_ptr = mark

    def prelude(self, layers):
        s, nc, d = self.s, self.nc, self.d
        s.dma("sp", self.pt[:, :], d["ptab"][:, :], writes=[self.ptd])
        s.dma("sp", self.ct[:, :], d["ctab"][:, 0:self.ccols["sel8"][0]], writes=[self.ctd])
        xv = d["xT"].rearrange("(c p) s -> p c s", p=128)
        for c in range(DC):
            s.dma("sp" if c % 2 == 0 else "act", self.X[:, c, :], xv[:, c, :], writes=self.Xd[c])
        s.op("dve", lambda e: e.tensor_copy(self.identb[:, :], self.C("ident")), reads=[self.ctd], writes=[self.cbd])
        s.op("dve", lambda e: e.tensor_copy(self.cmaskb[:, :], self.C("cmask")), reads=[self.ctd], writes=[self.cbd])
        s.op("dve", lambda e: e.tensor_copy(self.mmaskb[:, :], self.C("mmask")), reads=[self.ctd], writes=[self.cbd])
        s.op("dve", lambda e: e.memset(self.onesb[:, :], 1.0), writes=[self.cbd])
        s.op("act", lambda e: e.activation(out=self.cond[:, :], in_=self.P("c"), func=AF.Silu),
             reads=[self.ptd], writes=[self.condd])
        with self.phase() as alloc:
            NB = 768
            bufs = [(alloc(f"adaw{i}", [128, 8, NB], F32), Dep()) for i in range(2)]
            items = [(i, cb) for i in layers for cb in range(8)]

            def load(it, t, dep):
                i, cb = it
                src = d["ada_w"][i].rearrange("(kc p) n -> p kc n", p=128)
                s.dma("sp", t[:, :, :], src[:, :, cb * NB:(cb + 1) * NB], writes=[dep])
            st = Stream(bufs, items, load)
            pb, pbd = self.bank(4)
            for i in layers:
                for cb in range(8):
                    t, dep = st.get()
                    for oc in range(6):
                        col = cb * 6 + oc
                        for kc in range(8):
                            s.mm(pb[:, col:col + 1], t[:, kc, oc * 128:(oc + 1) * 128], self.cond[:, kc:kc + 1],
                                 kc == 0, kc == 7, reads=[dep, self.condd], out_dep=pbd)
                s.op("dve", lambda e: e.tensor_tensor(out=self.mod[:, i * 48:(i + 1) * 48], in0=pb[:, 0:48],
                                                      in1=self.P(f"adab{i}"), op=ALU.add),
                     reads=[pbd, self.ptd], writes=[self.modd])
                s.op("dve", lambda e: e.scalar_tensor_tensor(out=self.amod[:, i * 16:i * 16 + 8], in0=self.modc(i, 1), scalar=1.0,
                                                             in1=self.P(f"nmg{i}"), op0=ALU.add, op1=ALU.mult),
                     reads=[self.modd, self.ptd], writes=[self.amodd])
                s.op("dve", lambda e: e.scalar_tensor_tensor(out=self.amod[:, i * 16 + 8:i * 16 + 16], in0=self.modc(i, 4), scalar=1.0,
                                                             in1=self.P(f"nfg{i}"), op0=ALU.add, op1=ALU.mult),
                     reads=[self.modd, self.ptd], writes=[self.amodd])
            s.op("dve", lambda e: e.memset(self.amod[:, NL * 16:NL * 16 + 8], 0.0), writes=[self.amodd])

    def norm_tmps(self, alloc):
        return {"sq": [(alloc(f"sq{i}", [128, 512], BF16), Dep()) for i in range(2)],
                "tmp": [(alloc(f"nt{i}", [128, 512], F32), Dep()) for i in range(2)],
                "rstd": (alloc("rstd", [128, 512], F32), Dep())}

    def norm_mod(self, nt, A, B, t0, nblk, out_fn):
        s = self.s
        sq, tmp = nt["sq"], nt["tmp"]
        rstd, rstdd = nt["rstd"]
        pb, pbd = self.bank(7)
        for tb in range(nblk):
            tsl = slice(t0 + tb * 512, t0 + (tb + 1) * 512)
            xb = (t0 // 512) + tb
            for c in range(DC):
                q, qd = sq[c % 2]
                s.op("act", lambda e: e.activation(out=q[:, :], in_=self.X[:, c, tsl], func=AF.Square, scale=1.0 / 32.0),
                     reads=[self.Xd[c][xb]], writes=[qd])
                s.mm(pb, self.onesb[:, :], q[:, :], c == 0, c == DC - 1, reads=[qd, self.cbd], out_dep=pbd, inc=True)
            s.op("act", lambda e: e.activation(out=rstd[:, :], in_=pb, func=AF.Sqrt, bias=self.epsc[:, 0:1]),
                 reads=[pbd], writes=[rstdd])
            s.op("dve", lambda e: e.reciprocal(out=rstd[:, :], in_=rstd[:, :]), reads=[rstdd], writes=[rstdd])
            for c in range(DC):
                t, td = tmp[c % 2]
                s.op("pool", lambda e: e.tensor_tensor(out=t[:, :], in0=self.X[:, c, tsl], in1=rstd[:, :], op=ALU.mult),
                     reads=[self.Xd[c][xb], rstdd], writes=[td])
                out_fn(c, tb, t, td, A[:, c:c + 1], B[:, c:c + 1])

    def ffn_expert(self, hT, hTd, act, actd, gu_stream, wd_stream, gf, t0, sgb, cb=None):
        s = self.s
        k = 0
        for g in range(FC // 2):
            wt, wdep = gu_stream.get()
            for j in range(2):
                fc = g * 2 + j
                for sb in range(2):
                    bg, bgd = self.bank(0 + (k % 2))
                    bu, bud = self.bank(2 + (k % 2))
                    tsl = slice(sb * 512, (sb + 1) * 512)
                    for kc in range(DC):
                        s.mm(bg, wt[:, kc, 0, j * 128:(j + 1) * 128], hT[:, kc, tsl], kc == 0, kc == DC - 1,
                             reads=[wdep, hTd[sb]], out_dep=bgd)
                    for kc in range(DC):
                        s.mm(bu, wt[:, kc, 1, j * 128:(j + 1) * 128], hT[:, kc, tsl], kc == 0, kc == DC - 1,
                             reads=[wdep, hTd[sb]], out_dep=bud)
                    sg, sgd = sgb[k % 2]
                    s.op("act", lambda e: e.activation(out=sg[:, :], in_=bg, func=AF.Silu), reads=[bgd], writes=[sgd])
                    if cb is not None:
                        cbt, cbdep = cb[sb]
                        s.op("pool", lambda e: e.tensor_tensor(out=sg[:, :], in0=sg[:, :], in1=cbt[:, :], op=ALU.mult),
                             reads=[cbdep, sgd], writes=[sgd])
                    s.op("dve", lambda e: e.tensor_tensor(out=act[:, fc, tsl], in0=sg[:, :], in1=bu, op=ALU.mult),
                         reads=[sgd, bud], writes=[actd[fc][sb]])
                    k += 1
        for dc in range(DC):
            wdt, wddep = wd_stream.get()
            for sb in range(2):
                bo, bod = self.bank(4 + ((dc * 2 + sb) % 2))
                tsl = slice(sb * 512, (sb + 1) * 512)
                for fc in range(FC):
                    s.mm(bo, wdt[:, fc, :], act[:, fc, tsl], fc == 0, fc == FC - 1,
                         reads=[wddep, actd[fc][sb]], out_dep=bod)
                xsl = slice(t0 + sb * 512, t0 + (sb + 1) * 512)
                xb = (t0 // 512) + sb
                s.op("dve", lambda e: e.scalar_tensor_tensor(out=self.X[:, dc, xsl], in0=bo, scalar=gf[:, dc:dc + 1],
                                                             in1=self.X[:, dc, xsl], op0=ALU.mult, op1=ALU.add),
                     reads=[bod, self.modd, self.Xd[dc][xb]], writes=[self.Xd[dc][xb]])

    def ffn_layer(self, i):
        s, d = self.s, self.d
        moe = (i % 2 == 1)
        li = i // 2
        A = self.amod[:, i * 16 + 8:i * 16 + 16]
        B = self.modc(i, 3)
        gf = self.modc(i, 5)
        with self.phase() as alloc:
            hT = alloc("f_hT", [128, DC, 1024], BF16)
            hTd = [Dep(), Dep()]
            act_off = self.sb_ptr
            act = alloc("f_act", [128, FC, 1024], BF16)
            nt = self.norm_tmps(alloc)
            actd = [[Dep(), Dep()] for _ in range(FC)]
            gub = [(alloc(f"f_gu{k}", [128, DC, 2, 256], BF16), Dep()) for k in range(3)]
            wdb = [(alloc(f"f_wd{k}", [128, FC, 128], BF16), Dep()) for k in range(2)]
            sgb = [(alloc(f"f_sg{k}", [128, 512], F32), Dep()) for k in range(2)]
            experts = list(range(NE)) if moe else [None]

            def wgu_ap(e):
                return d["moe_w_gate_up"][li, e] if moe else d["ffn_w_gate_up"][li]

            def wd_ap(e):
                return d["moe_w_down"][li, e] if moe else d["ffn_w_down"][li]

            def load_gu(it, t, dep):
                e, g = it
                src = wgu_ap(e).rearrange("(kc p) n -> p kc n", p=128)
                s.dma("pool", t[:, :, 0, :], src[:, :, g * 256:(g + 1) * 256], writes=[dep])
                s.dma("pool", t[:, :, 1, :], src[:, :, DFF + g * 256:DFF + (g + 1) * 256], writes=[dep])

            def load_wd(it, t, dep):
                e, dc = it
                src = wd_ap(e).rearrange("(fc p) n -> p fc n", p=128)
                s.dma("pool", t[:, :, :], src[:, :, dc * 128:(dc + 1) * 128], writes=[dep])

            gu_items = [(e, g) for _ in range(2) for e in experts for g in range(FC // 2)]
            wd_items = [(e, dc) for _ in range(2) for e in experts for dc in range(DC)]
            gu_stream = Stream(gub, gu_items, load_gu)
            wd_stream = Stream(wdb, wd_items, load_wd)
            if moe:
                h32 = self.salloc("f_h32", [128, DC, 1024], F32, at=act_off)
                h32d = Dep()
                combT = alloc("f_combT", [8, 1024], F32)
                combTd = Dep()
                sel8 = alloc("f_sel8", [8, 8 * 128], F32)
                sel8d = Dep()
                st8 = self.ccols["sel8"][0]
                s.dma("sp", sel8[:, :], d["ctab"][0:8, st8:st8 + 1024], writes=[sel8d])
                cbb = [(alloc(f"f_cb{k}", [128, 512], F32), Dep()) for k in range(4)]
                rt = {nm: (alloc(f"f_rt_{nm}", [128, 8, 8], F32), Dep()) for nm in ("lg", "z", "m", "z2", "ez")}
                rs = {nm: (alloc(f"f_rs_{nm}", [128, 8], F32), Dep()) for nm in ("m1", "m2", "ss")}
            for ps_ in range(2):
                t0 = ps_ * 1024

                def out_fn(c, tb, t, td, Ac, Bc):
                    s.op("act", lambda e: e.activation(out=hT[:, c, tb * 512:(tb + 1) * 512], in_=t[:, :], func=AF.Identity,
                                                       bias=Bc, scale=Ac),
                         reads=[td, self.amodd, self.modd], writes=[hTd[tb]])
                    if moe:
                        s.op("act", lambda e: e.activation(out=h32[:, c, tb * 512:(tb + 1) * 512], in_=t[:, :], func=AF.Identity, bias=Bc, scale=Ac),
                             reads=[td, self.amodd, self.modd], writes=[h32d])
                if moe:
                    s.barrier()
                self.norm_mod(nt, A, B, t0, 2, out_fn)
                if moe:
                    self.router(li, h32, h32d, combT, combTd, rt, rs)
                    s.barrier()
                for e in experts:
                    cb = None
                    if moe:
                        cb = []
                        for sb in range(2):
                            cbt, cbdep = cbb[(e * 2 + sb) % 4]
                            pb, pbd = self.bank(6)
                            s.mm(pb, sel8[0:8, e * 128:(e + 1) * 128], combT[0:8, sb * 512:(sb + 1) * 512], True, True,
                                 reads=[sel8d, combTd], out_dep=pbd)
                            s.op("act", lambda e_: e_.activation(out=cbt[:, :], in_=pb, func=AF.Copy), reads=[pbd], writes=[cbdep])
                            cb.append((cbt, cbdep))
                    self.ffn_expert(hT, hTd, act, actd, gu_stream, wd_stream, gf, t0, sgb, cb)

    def router(self, li, h32, h32d, combT, combTd, rt, rs):
        s = self.s
        G = 8
        pb, pbd = self.bank(6)
        mwr = self.P(f"mwr{li}")
        lg, lgd = rt["lg"]; z, zd = rt["z"]; m, md = rt["m"]; z2, z2d = rt["z2"]; ez, ezd = rt["ez"]
        m1, m1d = rs["m1"]; m2, m2d = rs["m2"]; ss, ssd = rs["ss"]
        for g in range(G):
            for c in range(DC):
                s.mm(pb[:, g * 8:(g + 1) * 8], h32[:, c, g * 128:(g + 1) * 128], mwr[:, c * 8:(c + 1) * 8], c == 0, c == DC - 1,
                     reads=[h32d, self.ptd], out_dep=pbd)
        bc = lambda t: t[:, :].unsqueeze(2).broadcast_to([128, G, 8])
        pv = pb[:, 0:G * 8].rearrange("p (g e) -> p g e", e=8)
        s.op("dve", lambda e: e.tensor_tensor(out=lg[:, :, :], in0=pv, in1=self.P(f"mbr{li}").unsqueeze(1).broadcast_to([128, G, 8]), op=ALU.add),
             reads=[pbd, self.ptd], writes=[lgd])
        s.op("dve", lambda e: e.reduce_max(out=m1[:, :], in_=lg[:, :, :], axis=AX.X), reads=[lgd], writes=[m1d])
        s.op("dve", lambda e: e.tensor_tensor(out=z[:, :, :], in0=lg[:, :, :], in1=bc(m1), op=ALU.subtract), reads=[lgd, m1d], writes=[zd])
        s.op("dve", lambda e: e.tensor_single_scalar(out=m[:, :, :], in_=z[:, :, :], scalar=0.0, op=ALU.is_ge), reads=[zd], writes=[md])
        s.op("dve", lambda e: e.scalar_tensor_tensor(out=z2[:, :, :], in0=m[:, :, :], scalar=-1e30, in1=z[:, :, :], op0=ALU.mult, op1=ALU.add),
             reads=[md, zd], writes=[z2d])
        s.op("dve", lambda e: e.reduce_max(out=m2[:, :], in_=z2[:, :, :], axis=AX.X), reads=[z2d], writes=[m2d])
        s.op("dve", lambda e: e.tensor_tensor(out=m[:, :, :], in0=z[:, :, :], in1=bc(m2), op=ALU.is_ge), reads=[zd, m2d, md], writes=[md])
        s.op("act", lambda e: e.activation(out=ez[:, :, :], in_=z[:, :, :], func=AF.Exp), reads=[zd], writes=[ezd])
        s.op("dve", lambda e: e.tensor_tensor(out=ez[:, :, :], in0=ez[:, :, :], in1=m[:, :, :], op=ALU.mult), reads=[ezd, md], writes=[ezd])
        s.op("dve", lambda e: e.reduce_sum(out=ss[:, :], in_=ez[:, :, :], axis=AX.X), reads=[ezd], writes=[ssd])
        s.op("dve", lambda e: e.reciprocal(out=ss[:, :], in_=ss[:, :]), reads=[ssd], writes=[ssd])
        s.op("dve", lambda e: e.tensor_tensor(out=ez[:, :, :], in0=ez[:, :, :], in1=bc(ss), op=ALU.mult), reads=[ezd, ssd], writes=[ezd])
        for half in range(2):
            pt_, ptd_ = self.bank(7 if half == 0 else 5)
            for gg in range(4):
                g = half * 4 + gg
                s.mm(pt_[0:8, gg * 128:(gg + 1) * 128], ez[:, g, :], self.C("ident"), True, gg == 3, reads=[ezd, self.ctd], out_dep=ptd_)
            s.op("dve", lambda e: e.tensor_copy(out=combT[0:8, half * 512:(half + 1) * 512], in_=pt_[0:8, 0:512]), reads=[ptd_], writes=[combTd])

    def final(self, do_norm=True):
        s, d = self.s, self.d
        ov = d["outT"].rearrange("(c p) s -> p c s", p=128)
        with self.phase() as alloc:
            if not do_norm:
                for c in range(DC):
                    s.dma("sp", ov[:, c, :], self.X[:, c, :], reads=self.Xd[c])
                return
            ob = [(alloc(f"ob{k}", [128, 512], F32), Dep()) for k in range(3)]
            cnt = [0]

            def out_fn(c, tb, t, td, Ac, Bc):
                o, od = ob[cnt[0] % 3]
                cnt[0] += 1
                s.op("act", lambda e: e.activation(out=o[:, :], in_=t[:, :], func=AF.Identity, bias=Bc, scale=Ac),
                     reads=[td, self.amodd, self.ptd], writes=[od])
                s.dma("sp", ov[:, c, tb * 512:(tb + 1) * 512], o[:, :], reads=[od])
            self.norm_mod(self.norm_tmps(alloc), self.P("fng"), self.amod[:, NL * 16:NL * 16 + 8], 0, 4, out_fn)

    def run_plan(self, plan, final_norm=True):
        layers = sorted({i for _, i in plan})
        self.prelude(layers)
        for kind, i in plan:
            if kind == "ffn":
                self.ffn_layer(i)
            elif kind == "mix":
                self.mix_layer(i)
        self.final(final_norm)
        self.s.barrier(["sp"])

    def mix_layer(self, i):
        kind = i % 3
        if kind == 0:
            self.rwkv_layer(i)
        elif kind == 1:
            self.mla_layer(i)
        else:
            self.fox_layer(i)


FULL_PLAN = [(k, i) for i in range(NL) for k in ("mix", "ffn")]


def build(pcols, npcols, plan=None, final_norm=True):
    plan = FULL_PLAN if plan is None else plan
    p = Prog(pcols, npcols)
    p.run_plan(plan, final_norm)
    return p.nc


def make_in_maps(inp, x_override=None):
    ctab = _CT.build()
    maps = []
    pcols = None
    wts = {k: np.ascontiguousarray(np.asarray(inp[k], np.float32)) for k in W_SHAPES}
    for b in range(8):
        pt = param_table(inp, b)
        pcols = pt.cols
        x = np.asarray(inp["x"][b] if x_override is None else x_override[b], np.float32)
        m = {"xT": np.ascontiguousarray(x.T),
             "pos": np.ascontiguousarray(np.asarray(inp["positions"][b], np.int32).reshape(1, S)),
             "ptab": pt.build(), "ctab": ctab}
        m.update(wts)
        maps.append(m)
    return maps, pcols, maps[0]["ptab"].shape[1]


def kernel(**inputs):
    maps, pcols, npc = make_in_maps(inputs)
    nc = build(pcols, npc)
    res = run_bass_kernel_spmd(nc, maps, core_ids=list(range(8)))
    out = np.stack([np.ascontiguousarray(r["outT"].T) for r in res.results], axis=0)
    return out.astype(np.float32)


def _attn_methods():
    def out_proj(self, alloc, oT, oTd, w_ap, gm):
        s = self.s
        wob = [(alloc(f"wo{k}", [128, DC, 128], BF16), Dep()) for k in range(2)]
        src = w_ap.rearrange("(kc p) n -> p kc n", p=128)

        def load(m, t, dep):
            s.dma("pool", t[:, :, :], src[:, :, m * 128:(m + 1) * 128], writes=[dep])
        st = Stream(wob, list(range(DC)), load)
        k = 0
        for m in range(DC):
            wt, wd = st.get()
            for tb in range(4):
                pb, pbd = self.bank(4 + k % 2)
                k += 1
                tsl = slice(tb * 512, (tb + 1) * 512)
                for kc in range(DC):
                    s.mm(pb, wt[:, kc, :], oT[:, kc, tsl], kc == 0, kc == DC - 1, reads=[wd, oTd[tb]], out_dep=pbd)
                s.op("dve", lambda e: e.scalar_tensor_tensor(out=self.X[:, m, tsl], in0=pb, scalar=gm[:, m:m + 1],
                                                             in1=self.X[:, m, tsl], op0=ALU.mult, op1=ALU.add),
                     reads=[pbd, self.modd, self.Xd[m][tb]], writes=[self.Xd[m][tb]])

    def attn_bufs(self, alloc, nP=1, nPT=1):
        return {"Pb": [(alloc(f"a_P{k}", [128, S], BF16), Dep()) for k in range(nP)] * (2 // nP),
                "PT": [(alloc(f"a_PT{k}", [128, 16, 128], BF16), Dep()) for k in range(nPT)] * (2 // nPT),
                "dg": [(alloc(f"a_dg{k}", [128, 128], BF16), Dep()) for k in range(2)],
                "st": [(alloc(f"a_st{k}", [128, 8], F32), Dep()) for k in range(2)],
                "raw": [(alloc(f"a_raw{k}", [128, S], F32), Dep()) for k in range(2)]}

    def attention_pair(self, ab, score_fn, Vt, Vtd, maskb, o_evac):
        s = self.s
        items = [(qb, par) for qb in range(16) for par in range(2)]
        segrr = [0]

        seg_banks = {}

        def a_pe(it):
            qb, par = items[it]
            nk = (qb + 1) * 128
            nseg = (nk + 511) // 512
            seg_banks[it] = []
            for sg in range(nseg):
                k0 = sg * 512
                n = min(512, nk - k0)
                bk = segrr[0]
                segrr[0] = (segrr[0] + 1) % 4
                pb, pbd = self.bank(bk)
                score_fn(par, qb, k0, n, pb[:, 0:n], pbd)
                if sg == nseg - 1:
                    s.mm(pb[:, n - 128:n], self.identb[:, :], maskb[:, :], False, True, reads=[self.cbd], out_dep=pbd)
                else:
                    pbd.w = (("e", "pe"), s.cnt["pe"])
                    pbd.r = {}
                seg_banks[it].append((pb, pbd, k0, n))

        def a_ev(it):
            raw, rawd = ab["raw"][it % 2]
            st, std = ab["st"][it % 2]
            for sg, (pb, pbd, k0, n) in enumerate(seg_banks.pop(it)):
                s.op("act", lambda e: e.activation(out=raw[:, k0:k0 + n], in_=pb[:, 0:n], func=AF.Copy), reads=[pbd], writes=[rawd])
                s.op("dve", lambda e: e.reduce_max(out=st[:, sg:sg + 1], in_=raw[:, k0:k0 + n], axis=AX.X), reads=[rawd], writes=[std])

        def b_soft(it):
            qb, par = items[it]
            nk = (qb + 1) * 128
            nseg = (nk + 511) // 512
            raw, rawd = ab["raw"][it % 2]
            st, std = ab["st"][it % 2]
            Pb, Pbd = ab["Pb"][it % 2]
            dg, dgd = ab["dg"][it % 2]
            if nseg > 1:
                s.op("dve", lambda e: e.reduce_max(out=st[:, 4:5], in_=st[:, 0:nseg], axis=AX.X), reads=[std], writes=[std])
                mcol = st[:, 4:5]
            else:
                mcol = st[:, 0:1]
            s.op("dve", lambda e: e.tensor_scalar(out=st[:, 5:6], in0=mcol, scalar1=-1.0, scalar2=None, op0=ALU.mult), reads=[std], writes=[std])
            s.op("act", lambda e: e.activation(out=Pb[:, 0:nk], in_=raw[:, 0:nk], func=AF.Exp, bias=st[:, 5:6], accum_out=st[:, 6:7]),
                 reads=[rawd, std], writes=[Pbd, std])
            s.op("dve", lambda e: e.reciprocal(out=st[:, 7:8], in_=st[:, 6:7]), reads=[std], writes=[std])
            s.op("dve", lambda e: e.tensor_scalar(out=dg[:, :], in0=self.C("ident"), scalar1=st[:, 7:8], scalar2=None, op0=ALU.mult),
                 reads=[std, self.ctd], writes=[dgd])

        def b_pe(it):
            qb, par = items[it]
            nkb = qb + 1
            Pb, Pbd = ab["Pb"][it % 2]
            PT, PTd = ab["PT"][it % 2]
            dg, dgd = ab["dg"][it % 2]
            for g4 in range((nkb + 3) // 4):
                pb, pbd = self.bank(4 + g4 % 2)
                nj = min(4, nkb - g4 * 4)
                for j in range(nj):
                    kb = g4 * 4 + j
                    s.mm(pb[:, j * 128:(j + 1) * 128], Pb[:, kb * 128:(kb + 1) * 128], dg[:, :], True, j == nj - 1,
                         reads=[Pbd, dgd], out_dep=pbd)
                src = pb[:, 0:nj * 128]
                dst = PT[:, g4 * 4:g4 * 4 + nj, :]
                s.op("dve", lambda e: e.tensor_copy(out=dst, in_=src), reads=[pbd], writes=[PTd])
            ob, obd = self.bank(6 + (it % 2))
            for kb in range(nkb):
                s.mm(ob[:, 0:128], Vt[:, kb, :], PT[:, kb, :], kb == 0, kb == nkb - 1, reads=[Vtd, PTd], out_dep=obd)
            o_evac(par, qb, ob[par * 64:(par + 1) * 64, 0:128], obd)

        a_pe(0)
        a_ev(0)
        for it in range(len(items)):
            if it + 1 < len(items):
                a_pe(it + 1)
            b_soft(it)
            if it + 1 < len(items):
                a_ev(it + 1)
            b_pe(it)

    def rope_tables(self, cosT, sinT, tabd):
        s, d = self.s, self.d
        with self.phase() as alloc:
            posi = alloc("r_posi", [64, S], I32)
            ang = alloc("r_ang", [64, S], F32)
            y = alloc("r_y", [64, S], F32)
            yi = alloc("r_yi", [64, S], I32)
            r = alloc("r_r", [64, S], F32)
            mk = alloc("r_mk", [64, S], F32)
            dd = Dep()
            s.dma("sp", posi[:, :], d["pos"][0:1, :].partition_broadcast(64), writes=[dd])
            s.op("dve", lambda e: e.tensor_copy(out=ang[:, :], in_=posi[:, :]), reads=[dd], writes=[dd])
            s.op("dve", lambda e: e.tensor_scalar(out=ang[:, :], in0=ang[:, :], scalar1=self.C("invf", rows=(0, 64)), scalar2=None,
                                                  op0=ALU.mult), reads=[dd, self.ctd], writes=[dd])
            TWO_PI = 2.0 * math.pi
            for tab, shift in ((sinT, 0.0), (cosT, math.pi / 2.0)):
                s.op("dve", lambda e: e.tensor_scalar(out=y[:, :], in0=ang[:, :], scalar1=shift, scalar2=1.0 / TWO_PI,
                                                      op0=ALU.add, op1=ALU.mult), reads=[dd], writes=[dd])
                s.op("dve", lambda e: e.tensor_copy(out=yi[:, :], in_=y[:, :]), reads=[dd], writes=[dd])
                s.op("dve", lambda e: e.tensor_copy(out=y[:, :], in_=yi[:, :]), reads=[dd], writes=[dd])
                s.op("dve", lambda e: e.scalar_tensor_tensor(out=r[:, :], in0=y[:, :], scalar=-TWO_PI, in1=ang[:, :],
                                                             op0=ALU.mult, op1=ALU.add), reads=[dd], writes=[dd])
                if shift != 0.0:
                    s.op("dve", lambda e: e.tensor_scalar(out=r[:, :], in0=r[:, :], scalar1=shift, scalar2=None, op0=ALU.add),
                         reads=[dd], writes=[dd])
                s.op("dve", lambda e: e.tensor_single_scalar(out=mk[:, :], in_=r[:, :], scalar=math.pi, op=ALU.is_gt), reads=[dd], writes=[dd])
                s.op("dve", lambda e: e.scalar_tensor_tensor(out=r[:, :], in0=mk[:, :], scalar=-TWO_PI, in1=r[:, :],
                                                             op0=ALU.mult, op1=ALU.add), reads=[dd], writes=[dd])
                s.op("dve", lambda e: e.tensor_single_scalar(out=mk[:, :], in_=r[:, :], scalar=-math.pi, op=ALU.is_lt), reads=[dd], writes=[dd])
                s.op("dve", lambda e: e.scalar_tensor_tensor(out=r[:, :], in0=mk[:, :], scalar=TWO_PI, in1=r[:, :],
                                                             op0=ALU.mult, op1=ALU.add), reads=[dd], writes=[dd])
                s.op("dve", lambda e: e.tensor_scalar(out=r[:, :], in0=r[:, :], scalar1=3.14159, scalar2=-3.14159, op0=ALU.min, op1=ALU.max),
                     reads=[dd], writes=[dd])
                s.op("act", lambda e: e.activation(out=tab[:, :], in_=r[:, :], func=AF.Sin), reads=[dd], writes=[tabd])

    def mla_layer(self, i):
        s, d = self.s, self.d
        A = self.amod[:, i * 16:i * 16 + 8]
        B = self.modc(i, 0)
        gm = self.modc(i, 2)
        SC = float((64 + 32) ** -0.5)
        wdown = d["mla_w_down"][0].rearrange("(kc p) n -> p kc n", p=128)
        wuq = d["mla_w_uq"][0].rearrange("(kc p) (h dd) -> p kc h dd", p=128, dd=96)
        wukv = d["mla_w_ukv"][0].rearrange("(kc p) (h dd) -> p kc h dd", p=128, dd=128)
        with self.phase() as alloc:
            cq = alloc("m_cq", [128, 6, S], BF16)
            ckv = alloc("m_ckv", [128, 2, S], BF16)
            kr = alloc("m_kr", [64, S], BF16)
            cqd = [Dep() for _ in range(4)]
            ckvd = [Dep() for _ in range(4)]
            krd = [Dep() for _ in range(4)]
            cosT = alloc("m_cos", [64, S], F32)
            sinT = alloc("m_sin", [64, S], F32)
            tabd = Dep()
            self.rope_tables(cosT, sinT, tabd)
            with self.phase() as a1:
                hT = a1("m_hT", [128, DC, 1024], BF16)
                hTd = [Dep(), Dep()]
                nt = self.norm_tmps(a1)
                wdn = a1("m_wdn", [128, DC, 1024], BF16)
                wdnd = Dep()
                wkr = a1("m_wkr", [128, DC, 128], BF16)
                wkrd = Dep()
                raw = a1("m_raw", [128, 8, 256], F32)
                rawd = [Dep() for _ in range(8)]
                sqt = [(a1(f"m_sq{k}", [128, 256], F32), Dep()) for k in range(2)]
                rq = a1("m_rq", [128, 256], F32)
                rkv = a1("m_rkv", [128, 256], F32)
                rqd, rkvd = Dep(), Dep()
                t1 = a1("m_t1", [64, 256], F32)
                t2 = a1("m_t2", [64, 256], F32)
                t1d, t2d = Dep(), Dep()
                for h2 in range(2):
                    s.dma("pool", wdn[:, :, h2 * 512:(h2 + 1) * 512], wdown[:, :, h2 * 512:(h2 + 1) * 512], writes=[wdnd])
                s.dma("pool", wkr[:, :, 0:32], wdown[:, :, 1024:1056], writes=[wkrd])
                s.dma("pool", wkr[:, :, 32:64], wdown[:, :, 1024:1056], writes=[wkrd])
                s.op("dve", lambda e: e.tensor_scalar(out=wkr[:, :, 64:80], in0=wkr[:, :, 16:32], scalar1=-1.0, scalar2=None, op0=ALU.mult),
                     reads=[wkrd], writes=[wkrd])
                s.op("dve", lambda e: e.tensor_copy(out=wkr[:, :, 80:96], in_=wkr[:, :, 0:16]), reads=[wkrd], writes=[wkrd])
                s.op("dve", lambda e: e.tensor_copy(out=wkr[:, :, 96:128], in_=wkr[:, :, 64:96]), reads=[wkrd], writes=[wkrd])
                for ps_ in range(2):
                    def out_fn(c, tb, t, td, Ac, Bc):
                        s.op("act", lambda e: e.activation(out=hT[:, c, tb * 512:(tb + 1) * 512], in_=t[:, :], func=AF.Identity,
                                                           bias=Bc, scale=Ac),
                             reads=[td, self.amodd, self.modd], writes=[hTd[tb]])
                    self.norm_mod(nt, A, B, ps_ * 1024, 2, out_fn)
                    for blk in range(4):
                        tok0 = ps_ * 1024 + blk * 256
                        tsl = slice(tok0, tok0 + 256)
                        hsl = slice(blk * 256, (blk + 1) * 256)
                        hd = hTd[blk // 2]
                        dq = tok0 // 512
                        bq, bqd = self.bank(2)
                        bkv, bkvd = self.bank(3)
                        for m in range(8):
                            pb, pbd = self.bank(m % 2)
                            for kc in range(DC):
                                s.mm(pb[:, 0:256], wdn[:, kc, m * 128:(m + 1) * 128], hT[:, kc, hsl], kc == 0, kc == DC - 1,
                                     reads=[wdnd, hd], out_dep=pbd)
                            s.op("act", lambda e: e.activation(out=raw[:, m, :], in_=pb[:, 0:256], func=AF.Copy), reads=[pbd], writes=[rawd[m]])
                            q, qd = sqt[m % 2]
                            s.op("pool", lambda e: e.tensor_tensor(out=q[:, :], in0=raw[:, m, :], in1=raw[:, m, :], op=ALU.mult),
                                 reads=[rawd[m]], writes=[qd])
                            if m < 6:
                                s.mm(bq[:, 0:256], self.C("ones"), q[:, :], m == 0, m == 5, reads=[qd, self.ctd], out_dep=bqd, inc=True)
                            else:
                                s.mm(bkv[:, 0:256], self.C("ones"), q[:, :], m == 6, m == 7, reads=[qd, self.ctd], out_dep=bkvd, inc=True)
                        s.op("act", lambda e: e.activation(out=rq[:, :], in_=bq[:, 0:256], func=AF.Sqrt, bias=self.epsc[:, 0:1], scale=1.0 / 768.0),
                             reads=[bqd], writes=[rqd])
                        s.op("dve", lambda e: e.reciprocal(out=rq[:, :], in_=rq[:, :]), reads=[rqd], writes=[rqd])
                        s.op("act", lambda e: e.activation(out=rkv[:, :], in_=bkv[:, 0:256], func=AF.Sqrt, bias=self.epsc[:, 0:1], scale=1.0 / 256.0),
                             reads=[bkvd], writes=[rkvd])
                        s.op("dve", lambda e: e.reciprocal(out=rkv[:, :], in_=rkv[:, :]), reads=[rkvd], writes=[rkvd])
                        for m in range(8):
                            if m < 6:
                                s.op("dve", lambda e: e.scalar_tensor_tensor(out=cq[:, m, tsl], in0=raw[:, m, :], scalar=self.P("mqg", m, m + 1),
                                                                             in1=rq[:, :], op0=ALU.mult, op1=ALU.mult),
                                     reads=[rawd[m], rqd, self.ptd], writes=[cqd[dq]])
                            else:
                                s.op("dve", lambda e: e.scalar_tensor_tensor(out=ckv[:, m - 6, tsl], in0=raw[:, m, :], scalar=self.P("mkvg", m - 6, m - 5),
                                                                             in1=rkv[:, :], op0=ALU.mult, op1=ALU.mult),
                                     reads=[rawd[m], rkvd, self.ptd], writes=[ckvd[dq]])
                        bx, bxd = self.bank(4)
                        br, brd = self.bank(5)
                        for kc in range(DC):
                            s.mm(bx[0:64, 0:256], wkr[:, kc, 0:64], hT[:, kc, hsl], kc == 0, kc == DC - 1, reads=[wkrd, hd], out_dep=bxd)
                        for kc in range(DC):
                            s.mm(br[0:64, 0:256], wkr[:, kc, 64:128], hT[:, kc, hsl], kc == 0, kc == DC - 1, reads=[wkrd, hd], out_dep=brd)
                        s.op("dve", lambda e: e.tensor_tensor(out=t1[:, :], in0=bx[0:64, 0:256], in1=cosT[:, tsl], op=ALU.mult),
                             reads=[bxd, tabd], writes=[t1d])
                        s.op("dve", lambda e: e.tensor_tensor(out=t2[:, :], in0=br[0:64, 0:256], in1=sinT[:, tsl], op=ALU.mult),
                             reads=[brd, tabd], writes=[t2d])
                        s.op("pool", lambda e: e.tensor_tensor(out=kr[:, tsl], in0=t1[:, :], in1=t2[:, :], op=ALU.add),
                             reads=[t1d, t2d], writes=[krd[dq]])
            with self.phase() as a2:
                oT = a2("m_oT", [128, DC, S], BF16)
                oTd = [Dep() for _ in range(4)]
                with self.phase() as a3:
                    ab = self.attn_bufs(a3)
                    wqn = [(a3(f"m_wqn{k}", [128, 6, 128], BF16), Dep()) for k in range(1)]
                    wqr = [(a3(f"m_wqr{k}", [128, 6, 64], BF16), Dep()) for k in range(1)]
                    wqo = [(a3(f"m_wqo{k}", [128, 6, 64], BF16), Dep()) for k in range(1)]
                    wkn = [(a3(f"m_wkn{k}", [128, 2, 128], BF16), Dep()) for k in range(1)]
                    wv = [(a3(f"m_wv{k}", [128, 2, 128], BF16), Dep()) for k in range(1)]
                    qn = a3("m_qn", [128, S], BF16)
                    kn = a3("m_kn", [128, S], BF16)
                    qr = a3("m_qr", [64, S], BF16)
                    Vt = a3("m_Vt", [128, 16, 128], BF16)
                    qnd, knd, qrd, Vtd = Dep(), Dep(), Dep(), Dep()
                    t1 = a3("m_u1", [64, 512], F32)
                    t2 = a3("m_u2", [64, 512], F32)
                    t1d, t2d = Dep(), Dep()
                    for hp in range(8):
                        k2 = 0
                        (wqn_t, wqn_d), (wqr_t, wqr_d), (wqo_t, wqo_d) = wqn[k2], wqr[k2], wqo[k2]
                        (wkn_t, wkn_d), (wv_t, wv_d) = wkn[k2], wv[k2]
                        for hh in range(2):
                            s.dma("pool", wqn_t[:, :, hh * 64:(hh + 1) * 64], wuq[:, :, 2 * hp + hh, 0:64], writes=[wqn_d])
                            s.dma("pool", wqr_t[:, :, hh * 32:(hh + 1) * 32], wuq[:, :, 2 * hp + hh, 64:96], writes=[wqr_d])
                            s.dma("pool", wkn_t[:, :, hh * 64:(hh + 1) * 64], wukv[:, :, 2 * hp + hh, 0:64], writes=[wkn_d])
                            s.dma("pool", wv_t[:, :, hh * 64:(hh + 1) * 64], wukv[:, :, 2 * hp + hh, 64:128], writes=[wv_d])
                        wr4 = wqr_t[:, :, :].rearrange("p k (h e) -> p k h e", h=2)
                        wo4 = wqo_t[:, :, :].rearrange("p k (h e) -> p k h e", h=2)
                        s.op("dve", lambda e: e.tensor_scalar(out=wo4[:, :, :, 0:16], in0=wr4[:, :, :, 16:32], scalar1=-1.0, scalar2=None, op0=ALU.mult),
                             reads=[wqr_d], writes=[wqo_d])
                        s.op("dve", lambda e: e.tensor_copy(out=wo4[:, :, :, 16:32], in_=wr4[:, :, :, 0:16]), reads=[wqr_d], writes=[wqo_d])
                        for tb in range(4):
                            tsl = slice(tb * 512, (tb + 1) * 512)
                            pb, pbd = self.bank(4)
                            for kc in range(6):
                                s.mm(pb, wqn_t[:, kc, :], cq[:, kc, tsl], kc == 0, kc == 5, reads=[wqn_d, cqd[tb]], out_dep=pbd)
                            s.op("act", lambda e: e.activation(out=qn[:, tsl], in_=pb, func=AF.Copy, scale=SC), reads=[pbd], writes=[qnd])
                            pk, pkd = self.bank(5)
                            for kc in range(2):
                                s.mm(pk, wkn_t[:, kc, :], ckv[:, kc, tsl], kc == 0, kc == 1, reads=[wkn_d, ckvd[tb]], out_dep=pkd)
                            s.op("act", lambda e: e.activation(out=kn[:, tsl], in_=pk, func=AF.Copy), reads=[pkd], writes=[knd])
                            bx, bxd = self.bank(6)
                            br, brd = self.bank(7)
                            for kc in range(6):
                                s.mm(bx[0:64, :], wqr_t[:, kc, :], cq[:, kc, tsl], kc == 0, kc == 5, reads=[wqr_d, cqd[tb]], out_dep=bxd)
                            for kc in range(6):
                                s.mm(br[0:64, :], wqo_t[:, kc, :], cq[:, kc, tsl], kc == 0, kc == 5, reads=[wqo_d, cqd[tb]], out_dep=brd)
                            s.op("dve", lambda e: e.scalar_tensor_tensor(out=t1[:, :], in0=bx[0:64, :], scalar=SC, in1=cosT[:, tsl],
                                                                         op0=ALU.mult, op1=ALU.mult), reads=[bxd, tabd], writes=[t1d])
                            s.op("dve", lambda e: e.scalar_tensor_tensor(out=t2[:, :], in0=br[0:64, :], scalar=SC, in1=sinT[:, tsl],
                                                                         op0=ALU.mult, op1=ALU.mult), reads=[brd, tabd], writes=[t2d])
                            s.op("pool", lambda e: e.tensor_tensor(out=qr[:, tsl], in0=t1[:, :], in1=t2[:, :], op=ALU.add),
                                 reads=[t1d, t2d], writes=[qrd])
                        for g in range(4):
                            pb, pbd = self.bank(4 + g % 2)
                            for j in range(4):
                                t16 = g * 4 + j
                                for kc in range(2):
                                    s.mm(pb[:, j * 128:(j + 1) * 128], ckv[:, kc, t16 * 128:(t16 + 1) * 128], wv_t[:, kc, :],
                                         kc == 0, (kc == 1 and j == 3), reads=[wv_d, ckvd[t16 // 4]], out_dep=pbd)
                            s.op("act", lambda e: e.activation(out=Vt[:, g * 4:(g + 1) * 4, :], in_=pb, func=AF.Copy), reads=[pbd], writes=[Vtd])

                        def score_fn(par, qb, k0, n, out_ap, od):
                            ps = slice(par * 64, (par + 1) * 64)
                            rs = slice(par * 32, (par + 1) * 32)
                            qsl = slice(qb * 128, (qb + 1) * 128)
                            s.mm(out_ap, qn[ps, qsl], kn[ps, k0:k0 + n], True, False, reads=[qnd, knd], out_dep=od)
                            s.mm(out_ap, qr[rs, qsl], kr[rs, k0:k0 + n], False, False, reads=[qrd] + krd, out_dep=od, inc=True)

                        def o_evac(par, qb, src, srcd):
                            ps = slice(par * 64, (par + 1) * 64)
                            s.op("act", lambda e: e.activation(out=oT[ps, hp, qb * 128:(qb + 1) * 128], in_=src, func=AF.Copy),
                                 reads=[srcd], writes=[oTd[qb // 4]])
                        self.attention_pair(ab, score_fn, Vt, Vtd, self.mmaskb, o_evac)
                self.out_proj(a2, oT, oTd, d["mla_w_o"][0], gm)

    Prog.out_proj = out_proj
    Prog.attn_bufs = attn_bufs
    Prog.attention_pair = attention_pair
    Prog.rope_tables = rope_tables
    Prog.mla_layer = mla_layer


_attn_methods()


def _fox_methods():
    def fox_layer(self, i):
        s, d = self.s, self.d
        A = self.amod[:, i * 16:i * 16 + 8]
        B = self.modc(i, 0)
        gm = self.modc(i, 2)
        FSC = float(64 ** -0.5)
        win = d["fox_w_in"][0].rearrange("(kc p) n -> p kc n", p=128)
        with self.phase() as alloc:
            hT = alloc("x_hT", [128, DC, S], BF16)
            hTd = [Dep() for _ in range(4)]
            oT = alloc("x_oT", [128, DC, S], BF16)
            oTd = [Dep() for _ in range(4)]
            nFh = alloc("x_nFh", [16, S], BF16)
            nFl = alloc("x_nFl", [16, S], BF16)
            negFd = Dep()
            gsc = alloc("x_gsc", [128, 2], F32)
            gscd = Dep()
            s.op("dve", lambda e: e.tensor_scalar(out=gsc[:, 0:1], in0=self.P("fqg"), scalar1=FSC, scalar2=None, op0=ALU.mult),
                 reads=[self.ptd], writes=[gscd])
            s.op("dve", lambda e: e.tensor_copy(out=gsc[:, 1:2], in_=self.P("fkg")), reads=[self.ptd], writes=[gscd])
            with self.phase() as a1:
                nt = self.norm_tmps(a1)

                def out_fn(c, tb, t, td, Ac, Bc):
                    s.op("act", lambda e: e.activation(out=hT[:, c, tb * 512:(tb + 1) * 512], in_=t[:, :], func=AF.Identity,
                                                       bias=Bc, scale=Ac),
                         reads=[td, self.amodd, self.modd], writes=[hTd[tb]])
                self.norm_mod(nt, A, B, 0, 4, out_fn)
                wf = a1("x_wf", [128, DC, 16], BF16)
                wfd = Dep()
                s.dma("pool", wf[:, :, :], win[:, :, 3072:3088], writes=[wfd])
                z = a1("x_z", [16, 512], F32)
                az = a1("x_az", [16, 512], F32)
                lf = a1("x_lf", [16, 512], F32)
                ones16 = a1("x_ones", [16, 512], F32)
                Fc = a1("x_F", [16, S], F32)
                zd = Dep()
                s.op("dve", lambda e: e.memset(ones16[:, :], 1.0), writes=[zd])
                for tb in range(4):
                    tsl = slice(tb * 512, (tb + 1) * 512)
                    pb, pbd = self.bank(4)
                    for kc in range(DC):
                        s.mm(pb[0:16, :], wf[:, kc, :], hT[:, kc, tsl], kc == 0, kc == DC - 1, reads=[wfd, hTd[tb]], out_dep=pbd)
                    s.op("act", lambda e: e.activation(out=z[:, :], in_=pb[0:16, :], func=AF.Identity, bias=self.P("fbf")[0:16, :]),
                         reads=[pbd, self.ptd, zd], writes=[zd])
                    s.op("act", lambda e: e.activation(out=az[:, :], in_=z[:, :], func=AF.Abs), reads=[zd], writes=[zd])
                    s.op("act", lambda e: e.activation(out=az[:, :], in_=az[:, :], func=AF.Exp, scale=-1.0), reads=[zd], writes=[zd])
                    s.op("act", lambda e: e.activation(out=az[:, :], in_=az[:, :], func=AF.Ln, bias=self.epsc[0:16, 2:3]), reads=[zd], writes=[zd])
                    s.op("dve", lambda e: e.tensor_scalar(out=lf[:, :], in0=z[:, :], scalar1=0.0, scalar2=None, op0=ALU.min), reads=[zd], writes=[zd])
                    s.op("dve", lambda e: e.tensor_tensor(out=lf[:, :], in0=lf[:, :], in1=az[:, :], op=ALU.subtract), reads=[zd], writes=[zd])
                    init = 0.0 if tb == 0 else Fc[:, tb * 512 - 1:tb * 512]
                    s.op("dve", lambda e: e.tensor_tensor_scan(Fc[:, tsl], ones16[:, :], lf[:, :], init, ALU.mult, ALU.add),
                         reads=[zd], writes=[zd])
                negF = a1("x_negF", [16, S], F32)
                nF32 = a1("x_nF32", [16, S], F32)
                s.op("dve", lambda e: e.tensor_scalar(out=negF[:, :], in0=Fc[:, :], scalar1=-1.0, scalar2=None, op0=ALU.mult),
                     reads=[zd], writes=[zd])
                s.op("dve", lambda e: e.tensor_copy(out=nFh[:, :], in_=negF[:, :]), reads=[zd], writes=[negFd])
                s.op("dve", lambda e: e.tensor_copy(out=nF32[:, :], in_=nFh[:, :]), reads=[negFd, zd], writes=[zd])
                s.op("dve", lambda e: e.tensor_tensor(out=nFl[:, :], in0=negF[:, :], in1=nF32[:, :], op=ALU.subtract), reads=[zd], writes=[negFd])
            with self.phase() as a3:
                ab = self.attn_bufs(a3)
                wq = [(a3(f"x_wq{k}", [128, DC, 128], BF16), Dep()) for k in range(1)]
                wk = [(a3(f"x_wk{k}", [128, DC, 128], BF16), Dep()) for k in range(1)]
                wv = [(a3(f"x_wv{k}", [128, DC, 128], BF16), Dep()) for k in range(1)]
                wg = [(a3(f"x_wg{k}", [128, DC, 128], BF16), Dep()) for k in range(1)]
                selp = [(a3(f"x_sel{k}", [16, 256], BF16), Dep()) for k in range(2)]
                qn = a3("x_qn", [128, S], BF16)
                kn = a3("x_kn", [128, S], BF16)
                og = a3("x_og", [128, S], BF16)
                Vt = a3("x_Vt", [128, 16, 128], BF16)
                qnd, knd, ogd, Vtd = Dep(), Dep(), Dep(), Dep()
                raw = [(a3(f"x_raw{k}", [128, 512], F32), Dep()) for k in range(2)]
                sq = [(a3(f"x_sq{k}", [128, 512], F32), Dep()) for k in range(2)]
                rs_ = [(a3(f"x_rs{k}", [128, 512], F32), Dep()) for k in range(2)]
                st16 = self.ccols["sel16"][0]
                for hp in range(8):
                    k2 = 0
                    (wq_t, wq_d), (wk_t, wk_d), (wv_t, wv_d), (wg_t, wg_d) = wq[k2], wk[k2], wv[k2], wg[k2]
                    sel_t, sel_d = selp[hp % 2]
                    s.dma("pool", wq_t[:, :, :], win[:, :, hp * 128:(hp + 1) * 128], writes=[wq_d])
                    s.dma("pool", wk_t[:, :, :], win[:, :, 1024 + hp * 128:1024 + (hp + 1) * 128], writes=[wk_d])
                    s.dma("pool", wv_t[:, :, :], win[:, :, 2048 + hp * 128:2048 + (hp + 1) * 128], writes=[wv_d])
                    s.dma("pool", wg_t[:, :, :], win[:, :, 3088 + hp * 128:3088 + (hp + 1) * 128], writes=[wg_d])
                    s.dma("pool", sel_t[:, :], d["ctab"][0:16, st16 + hp * 256:st16 + (hp + 1) * 256], writes=[sel_d])
                    for tb in range(4):
                        tsl = slice(tb * 512, (tb + 1) * 512)
                        for which, (w_t, w_d), dst, dstd, gcol in ((0, (wq_t, wq_d), qn, qnd, 0), (1, (wk_t, wk_d), kn, knd, 1)):
                            pb, pbd = self.bank(4 + which)
                            for kc in range(DC):
                                s.mm(pb, w_t[:, kc, :], hT[:, kc, tsl], kc == 0, kc == DC - 1, reads=[w_d, hTd[tb]], out_dep=pbd)
                            r_t, r_d = raw[which]
                            q_t, q_d = sq[which]
                            rr_t, rr_d = rs_[which]
                            s.op("act", lambda e: e.activation(out=r_t[:, :], in_=pb, func=AF.Copy), reads=[pbd], writes=[r_d])
                            s.op("pool", lambda e: e.tensor_tensor(out=q_t[:, :], in0=r_t[:, :], in1=r_t[:, :], op=ALU.mult), reads=[r_d], writes=[q_d])
                            p2, p2d = self.bank(6 + which)
                            s.mm(p2, self.C("bd"), q_t[:, :], True, True, reads=[q_d, self.ctd], out_dep=p2d)
                            s.op("act", lambda e: e.activation(out=rr_t[:, :], in_=p2, func=AF.Sqrt, bias=self.epsc[:, 0:1], scale=1.0 / 64.0),
                                 reads=[p2d], writes=[rr_d])
                            s.op("dve", lambda e: e.reciprocal(out=rr_t[:, :], in_=rr_t[:, :]), reads=[rr_d], writes=[rr_d])
                            s.op("dve", lambda e: e.scalar_tensor_tensor(out=dst[:, tsl], in0=r_t[:, :], scalar=gsc[:, gcol:gcol + 1],
                                                                         in1=rr_t[:, :], op0=ALU.mult, op1=ALU.mult),
                                 reads=[r_d, rr_d, gscd], writes=[dstd])
                        pg, pgd = self.bank(4)
                        for kc in range(DC):
                            s.mm(pg, wg_t[:, kc, :], hT[:, kc, tsl], kc == 0, kc == DC - 1, reads=[wg_d, hTd[tb]], out_dep=pgd)
                        s.op("act", lambda e: e.activation(out=og[:, tsl], in_=pg, func=AF.Sigmoid), reads=[pgd], writes=[ogd])
                    for g in range(4):
                        pb, pbd = self.bank(4 + g % 2)
                        for j in range(4):
                            t16 = g * 4 + j
                            for kc in range(DC):
                                s.mm(pb[:, j * 128:(j + 1) * 128], hT[:, kc, t16 * 128:(t16 + 1) * 128], wv_t[:, kc, :],
                                     kc == 0, (kc == DC - 1 and j == 3), reads=[wv_d, hTd[t16 // 4]], out_dep=pbd)
                        s.op("act", lambda e: e.activation(out=Vt[:, g * 4:(g + 1) * 4, :], in_=pb, func=AF.Copy), reads=[pbd], writes=[Vtd])

                    def score_fn(par, qb, k0, n, out_ap, od):
                        ps = slice(par * 64, (par + 1) * 64)
                        qsl = slice(qb * 128, (qb + 1) * 128)
                        s.mm(out_ap, qn[ps, qsl], kn[ps, k0:k0 + n], True, False, reads=[qnd, knd], out_dep=od)
                        s.mm(out_ap, sel_t[0:16, par * 128:(par + 1) * 128], nFh[0:16, k0:k0 + n], False, False,
                             reads=[sel_d, negFd], out_dep=od)
                        s.mm(out_ap, sel_t[0:16, par * 128:(par + 1) * 128], nFl[0:16, k0:k0 + n], False, False,
                             reads=[sel_d, negFd], out_dep=od, inc=True)

                    def o_evac(par, qb, src, srcd):
                        ps = slice(par * 64, (par + 1) * 64)
                        qsl = slice(qb * 128, (qb + 1) * 128)
                        s.op("dve", lambda e: e.tensor_tensor(out=oT[ps, hp, qsl], in0=src, in1=og[ps, qsl], op=ALU.mult),
                             reads=[srcd, ogd], writes=[oTd[qb // 4]])
                    self.attention_pair(ab, score_fn, Vt, Vtd, self.cmaskb, o_evac)
            self.out_proj(alloc, oT, oTd, d["fox_w_o"][0], gm)

    Prog.fox_layer = fox_layer


_fox_methods()


def _rwkv_methods():
    def rw_scratch(self):
        if not hasattr(self, "_scr"):
            nc = self.nc
            self._scr = {nm: nc.dram_tensor(f"scr_{nm}", [D, S], F32).ap() for nm in ("r", "k", "v", "sg", "a", "g", "vf", "xs")}
            self._scr["yg"] = nc.dram_tensor("scr_yg", [D, S], BF16).ap()
        return self._scr

    def rwkv_layer(self, i):
        j = i // 3
        lvl = getattr(self, "dbg_rw", 3)
        self.rwkv_pass1(i, j)
        if lvl >= 2:
            self.rwkv_pass2(i, j)
        if lvl >= 3:
            self.rwkv_pass3(i, j)

    def rwkv_pass1(self, i, j):
        s, d = self.s, self.d
        scr = self.rw_scratch()
        A = self.amod[:, i * 16:i * 16 + 8]
        B = self.modc(i, 0)
        fmv = lambda ap: ap.rearrange("(c p) n -> p c n", p=128)
        with self.phase() as alloc:
            hT = alloc("w_hT", [128, DC, S + 1], BF16)
            hTd = [Dep() for _ in range(4)]
            h0d = Dep()
            s.op("dve", lambda e: e.memset(hT[:, :, 0:1], 0.0), writes=[h0d])
            nt = self.norm_tmps(alloc)

            def out_fn(c, tb, t, td, Ac, Bc):
                s.op("act", lambda e: e.activation(out=hT[:, c, 1 + tb * 512:1 + (tb + 1) * 512], in_=t[:, :], func=AF.Identity,
                                                   bias=Bc, scale=Ac),
                     reads=[td, self.amodd, self.modd], writes=[hTd[tb]])
            self.norm_mod(nt, A, B, 0, 4, out_fn)
            omu = alloc("w_omu", [128, 48], F32)
            omud = Dep()
            s.op("dve", lambda e: e.tensor_scalar(out=omu[:, :], in0=self.P(f"mu{j}"), scalar1=-1.0, scalar2=1.0, op0=ALU.mult, op1=ALU.add),
                 reads=[self.ptd], writes=[omud])
            xn = [(alloc(f"w_xn{k}", [128, DC, 512], BF16), Dep()) for k in range(2)]
            tmpx = [(alloc(f"w_tx{k}", [128, 512], F32), Dep()) for k in range(4)]
            wbig = [(alloc(f"w_big{k}", [128, DC, 1024], BF16), Dep()) for k in range(2)]
            w1 = alloc("w_w1", [128, DC, 64], BF16)
            a1 = alloc("w_a1", [128, DC, 64], BF16)
            g1 = alloc("w_g1", [128, DC, 160], BF16)
            w2 = alloc("w_w2", [64, D], BF16)
            a2 = alloc("w_a2", [64, D], BF16)
            g2a = alloc("w_g2a", [128, D], BF16)
            g2b = alloc("w_g2b", [32, D], BF16)
            lwd = Dep()
            kcv = lambda ap: ap.rearrange("(kc p) n -> p kc n", p=128)
            s.dma("pool", w1[:, :, :], kcv(d["rw_w1"][j]), writes=[lwd])
            s.dma("pool", a1[:, :, :], kcv(d["rw_a1"][j]), writes=[lwd])
            s.dma("pool", g1[:, :, :], kcv(d["rw_g1"][j]), writes=[lwd])
            s.dma("pool", w2[:, :], d["rw_w2"][j], writes=[lwd])
            s.dma("pool", a2[:, :], d["rw_a2"][j], writes=[lwd])
            s.dma("pool", g2a[:, :], d["rw_g2"][j][0:128, :], writes=[lwd])
            s.dma("pool", g2b[:, :], d["rw_g2"][j][128:160, :], writes=[lwd])
            vres = (j > 0)
            if vres:
                v1 = alloc("w_v1", [128, DC, 32], BF16)
                v2 = alloc("w_v2", [32, D], BF16)
                s.dma("pool", v1[:, :, :], kcv(d["rw_v1"][j - 1]), writes=[lwd])
                s.dma("pool", v2[:, :], d["rw_v2"][j - 1], writes=[lwd])
                vgt = alloc("w_vgt", [128, DC, 512], BF16)
                vgd = Dep()
                vft = [(alloc(f"w_vf{k}", [128, 512], F32), Dep()) for k in range(2)]
            stage = [(alloc(f"w_st{k}", [128, 512], F32), Dep()) for k in range(3)]
            t1 = alloc("w_t1", [128, 512], BF16)
            t1b = alloc("w_t1b", [32, 512], BF16)
            t1d = Dep()
            stc = [0]

            def emit_out(pb, pbd, func, bias, dst, m, tb, post=None):
                st, std = stage[stc[0] % 3]
                stc[0] += 1
                kw = {} if bias is None else {"bias": bias}
                s.op("act", lambda e: e.activation(out=st[:, :], in_=pb, func=func, **kw), reads=[pbd, self.ptd], writes=[std])
                if post is not None:
                    post(st, std)
                s.dma("sp", fmv(dst)[:, m, tb * 512:(tb + 1) * 512], st[:, :], reads=[std])

            big_items = [0, 1, 2]

            def load_big(which, t, dep):
                src = kcv(d["rw_w_rkv"][j, which])
                for h2 in range(2):
                    s.dma("pool", t[:, :, h2 * 512:(h2 + 1) * 512], src[:, :, h2 * 512:(h2 + 1) * 512], writes=[dep])
            bst = Stream(wbig, big_items, load_big)
            xc = [0]

            def make_xn(n, tb):
                x_t, x_d = xn[xc[0] % 2]
                xc[0] += 1
                for c in range(DC):
                    tx, txd = tmpx[c % 4]
                    col = n * 8 + c
                    s.op("act", lambda e: e.activation(out=tx[:, :], in_=hT[:, c, tb * 512:tb * 512 + 512], func=AF.Copy,
                                                       scale=self.P(f"mu{j}", col, col + 1)),
                         reads=[hTd[tb], hTd[max(tb - 1, 0)], h0d, self.ptd], writes=[txd])
                    s.op("dve", lambda e: e.scalar_tensor_tensor(out=x_t[:, c, :], in0=hT[:, c, 1 + tb * 512:1 + tb * 512 + 512],
                                                               scalar=omu[:, col:col + 1], in1=tx[:, :], op0=ALU.mult, op1=ALU.add),
                         reads=[hTd[tb], omud, txd], writes=[x_d])
                return x_t, x_d

            def big_proj(x_t, x_d, wt, wd, m):
                pb, pbd = self.bank(m % 4)
                for kc in range(DC):
                    s.mm(pb, wt[:, kc, m * 128:(m + 1) * 128], x_t[:, kc, :], kc == 0, kc == DC - 1, reads=[wd, x_d], out_dep=pbd)
                return pb, pbd

            def lora(x_t, x_d, wa, ncols, func1, wb, m_bias_name, func2, dst, tb, wb2=None):
                pb, pbd = self.bank(4)
                n1 = min(ncols, 128)
                for kc in range(DC):
                    s.mm(pb[0:n1, :], wa[:, kc, 0:n1], x_t[:, kc, :], kc == 0, kc == DC - 1, reads=[lwd, x_d], out_dep=pbd)
                s.op("act", lambda e: e.activation(out=t1[0:n1, :], in_=pb[0:n1, :], func=func1), reads=[pbd], writes=[t1d])
                if ncols > 128:
                    pb2, pb2d = self.bank(5)
                    for kc in range(DC):
                        s.mm(pb2[0:32, :], wa[:, kc, 128:160], x_t[:, kc, :], kc == 0, kc == DC - 1, reads=[lwd, x_d], out_dep=pb2d)
                    s.op("act", lambda e: e.activation(out=t1b[0:32, :], in_=pb2[0:32, :], func=func1), reads=[pb2d], writes=[t1d])
                for m in range(DC):
                    po, pod = self.bank(m % 4)
                    s.mm(po, wb[0:n1, m * 128:(m + 1) * 128], t1[0:n1, :], True, wb2 is None, reads=[lwd, t1d], out_dep=pod)
                    if wb2 is not None:
                        s.mm(po, wb2[0:32, m * 128:(m + 1) * 128], t1b[0:32, :], False, True, reads=[lwd, t1d], out_dep=pod)
                    bias = None if m_bias_name is None else self.P(m_bias_name, m, m + 1)
                    if dst is None:
                        s.op("act", lambda e: e.activation(out=vgt[:, m, :], in_=po, func=func2, bias=bias), reads=[pod, self.ptd], writes=[vgd])
                    else:
                        emit_out(po, pod, func2, bias, dst, m, tb)

            for n in range(6):
                if n in (0, 2, 3):
                    wt, wd = bst.get()
                for tb in range(4):
                    x_t, x_d = make_xn(n, tb)
                    if n == 0:
                        for m in range(DC):
                            pb, pbd = big_proj(x_t, x_d, wt, wd, m)
                            emit_out(pb, pbd, AF.Copy, None, scr["r"], m, tb)
                    elif n == 2:
                        for m in range(DC):
                            pb, pbd = big_proj(x_t, x_d, wt, wd, m)
                            emit_out(pb, pbd, AF.Copy, None, scr["k"], m, tb)
                    elif n == 3:
                        if vres:
                            lora(x_t, x_d, v1, 32, AF.Copy, v2, "v0", AF.Sigmoid, None, tb)
                        for m in range(DC):
                            pb, pbd = big_proj(x_t, x_d, wt, wd, m)
                            if not vres:
                                emit_out(pb, pbd, AF.Copy, None, scr["vf"], m, tb)
                            else:
                                vf, vfd = vft[m % 2]
                                s.dma("sp", vf[:, :], fmv(scr["vf"])[:, m, tb * 512:(tb + 1) * 512], writes=[vfd])

                                def post(st, std, m=m, vf=vf, vfd=vfd):
                                    s.op("dve", lambda e: e.tensor_tensor(out=vf[:, :], in0=vf[:, :], in1=st[:, :], op=ALU.subtract),
                                         reads=[vfd, std], writes=[vfd])
                                    s.op("pool", lambda e: e.tensor_tensor(out=vf[:, :], in0=vf[:, :], in1=vgt[:, m, :], op=ALU.mult),
                                         reads=[vfd, vgd], writes=[vfd])
                                    s.op("dve", lambda e: e.tensor_tensor(out=st[:, :], in0=st[:, :], in1=vf[:, :], op=ALU.add),
                                         reads=[vfd, std], writes=[std])
                                emit_out(pb, pbd, AF.Copy, None, scr["v"], m, tb, post=post)
                    elif n == 1:
                        lora(x_t, x_d, w1, 64, AF.Tanh, w2, f"w0{j}", AF.Sigmoid, scr["sg"], tb)
                    elif n == 4:
                        lora(x_t, x_d, a1, 64, AF.Copy, a2, f"a0{j}", AF.Sigmoid, scr["a"], tb)
                    else:
                        lora(x_t, x_d, g1, 160, AF.Sigmoid, g2a, None, AF.Copy, scr["g"], tb, wb2=g2b)

    def rwkv_pass3(self, i, j):
        s, d = self.s, self.d
        scr = self.rw_scratch()
        gm = self.modc(i, 2)
        with self.phase() as alloc:
            YG = alloc("w_YG", [128, DC, S], BF16)
            YGd = [Dep() for _ in range(4)]
            src = scr["yg"].rearrange("(c p) n -> p c n", p=128)
            for tb in range(4):
                s.dma("sp", YG[:, :, tb * 512:(tb + 1) * 512], src[:, :, tb * 512:(tb + 1) * 512], writes=[YGd[tb]])
            self.out_proj(alloc, YG, YGd, d["rw_w_o"][j], gm)

    Prog.rw_scratch = rw_scratch
    Prog.rwkv_layer = rwkv_layer
    Prog.rwkv_pass1 = rwkv_pass1
    Prog.rwkv_pass3 = rwkv_pass3


_rwkv_methods()


def _rwkv2_methods():
    def rwkv_pass2(self, i, j):
        s, d = self.s, self.d
        scr = self.rw_scratch()
        fmv = lambda ap: ap.rearrange("(c p) n -> p c n", p=128)
        vsrc = scr["v"] if j > 0 else scr["vf"]
        xs = fmv(scr["xs"])
        for c in range(DC):
            s.dma("sp", xs[:, c, :], self.X[:, c, :], reads=self.Xd[c])
        s.barrier()
        xptr = [SB_BASE]

        def xalloc(name, shape, dt):
            nbytes = (int(np.prod(shape[1:])) * (2 if dt == BF16 else 4) + 31) // 32 * 32
            off = xptr[0]
            if off + nbytes > SB_BASE + DC * S * 4:
                return self.salloc(name, shape, dt)
            xptr[0] += nbytes
            return self.salloc(name, shape, dt, at=off)

        with self.phase() as alloc:
            rmask = alloc("p_rmask", [128, 8, 64], F32)
            cd = Dep()
            s.op("dve", lambda e: e.memset(rmask[:, :, :], 1.0), writes=[cd])
            s.op("dve", lambda e: e.memset(rmask[:, :, 0:1], 0.0), writes=[cd])
            omka = alloc("p_omka", [128, 8], F32)
            s.op("dve", lambda e: e.tensor_scalar(out=omka[:, :], in0=self.P(f"k_a{j}"), scalar1=-1.0, scalar2=1.0, op0=ALU.mult, op1=ALU.add),
                 reads=[self.ptd], writes=[cd])
            tiny = alloc("p_tiny", [128, 1], F32)
            s.op("dve", lambda e: e.memset(tiny[:, :], 1e-24), writes=[cd])
            Sf = alloc("p_Sf", [64, 16, 64], F32)
            Sb = alloc("p_Sb", [64, 16, 64], BF16)
            Sfd = [Dep() for _ in range(4)]
            Sbd = [Dep() for _ in range(4)]
            s.op("dve", lambda e: e.memset(Sf[:, :, :], 0.0), writes=Sfd)
            s.op("dve", lambda e: e.memset(Sb[:, :, :], 0.0), writes=Sbd)
            bc8 = lambda nm: self.P(nm).unsqueeze(2).broadcast_to([128, 8, 64])
            names = ("r", "k", "v", "sg", "a", "g")
            srcs = {"r": scr["r"], "k": scr["k"], "v": vsrc, "sg": scr["sg"], "a": scr["a"], "g": scr["g"]}
            Lb = [{nm: xalloc(f"p_L{nm}{k}", [128, 8, 128], F32) for nm in names} for k in range(2)]
            Ld = [Dep(), Dep()]

            def load_sb(sb, bufs, dep):
                for nm in names:
                    s.dma("sp", bufs[nm][:, :, :], fmv(srcs[nm])[:, :, sb * 128:(sb + 1) * 128], writes=[dep])
            lst = Stream([(Lb[0], Ld[0]), (Lb[1], Ld[1])], list(range(16)), load_sb)
            def T(nm, dt=F32, shape=(128, 8, 64)):
                return xalloc("p_" + nm, list(shape), dt)
            prep = []
            inter_names = ("lw", "cum", "cumE", "e1", "e2", "e3", "e4", "kkr", "sqk", "rn", "kk", "ta", "k2", "beta", "rk")
            inter = {nm: T(nm) for nm in inter_names}
            inter_d = {nm: Dep() for nm in inter_names}
            for k in range(2):
                pt_ = dict(inter)
                pt_.update({nm: T(f"{nm}{k}") for nm in ("bonus", "dg")})
                pt_["PL"] = xalloc(f"p_PL{k}", [128, 8], F32)
                pt_["PLs"] = xalloc(f"p_PLs{k}", [128, 8, 2], F32)
                pt_["PLh"] = xalloc(f"p_PLh{k}", [64, 16], F32)
                s.op("dve", lambda e: e.memset(pt_["PLs"][:, :, :], 0.0), writes=[cd])
                pt_["AR"] = T(f"AR{k}", BF16, (128, 8, 128))
                for nm in ("Bt", "Kt", "Bh", "Kh", "vb"):
                    pt_[nm] = T(f"{nm}{k}", BF16)
                pt_["d"] = {nm: Dep() for nm in ("bonus", "dg", "PL", "PLs", "PLh", "AR", "Bt", "Kt", "Bh", "Kh", "vb")}
                pt_["d"].update(inter_d)
                prep.append(pt_)
            tmaj = []
            for k in range(2):
                tm = {"AX": alloc(f"p_AX{k}", [64, 16, 128], BF16), "B": alloc(f"p_tmB{k}", [64, 1024], BF16),
                      "K": alloc(f"p_tmK{k}", [64, 1024], BF16), "V": alloc(f"p_tmV{k}", [64, 1024], BF16)}
                tm["d"] = {"AXa": Dep(), "AXx": [Dep() for _ in range(4)], "B": Dep(), "K": Dep(), "V": Dep()}
                tmaj.append(tm)
            grp = []
            for k in range(2):
                gb = {"Mbm": alloc(f"p_Mbm{k}", [64, 4, 128], F32), "Mbr": alloc(f"p_Mbr{k}", [64, 4, 64], BF16),
                      "Mkb": alloc(f"p_Mkb{k}", [64, 4, 128], BF16), "ATt": alloc(f"p_ATt{k}", [64, 4, 64], BF16),
                      "AT": alloc(f"p_AT{k}", [64, 4, 128], BF16), "Tb": alloc(f"p_Tb{k}", [64, 4, 64], BF16),
                      "AU": alloc(f"p_AU{k}", [64, 4, 128], BF16), "Rh": alloc(f"p_Rh{k}", [64, 4, 64], BF16),
                      "Phi": alloc(f"p_Phi{k}", [64, 4, 64], F32), "Sn": alloc(f"p_Sn{k}", [64, 4, 64], F32)}
                gb["d"] = {nm: Dep() for nm in ("Mbm", "Mbr", "Mkb", "ATt", "AT", "Tb", "AU", "Rh", "Phi", "Sn")}
                grp.append(gb)
            Yall = [(alloc(f"p_Y{k}", [64, 16, 64], F32), [Dep() for _ in range(4)]) for k in range(2)]
            Yc = alloc("p_Yc", [64, 16, 64], F32)
            Ysq = alloc("p_Ysq", [64, 16, 64], F32)
            yst = alloc("p_yst", [64, 64], F32)
            ycd, ysqd, ystd = Dep(), Dep(), Dep()
            yf = alloc("p_yf", [128, 8, 64], F32)
            yfd = Dep()
            ygs = [(alloc(f"p_ygs{k}", [128, 8, 256], BF16), Dep()) for k in range(2)]
            gnb = alloc("p_gnb", [64, 1], F32)
            s.op("dve", lambda e: e.memset(gnb[:, :], 64e-5), writes=[cd])
            rrb = [4]

            def nb():
                b = rrb[0]
                rrb[0] = 4 + (rrb[0] - 4 + 1) % 4
                return self.bank(b)
            ident = self.C("ident")
            mask_ar = self.C("mask_ar", rows=(0, 64)).unsqueeze(1).broadcast_to([64, 4, 128])
            maskT = self.C("maskT", rows=(0, 64)).unsqueeze(1).broadcast_to([64, 4, 64])
            id64b = self.C("ident", 0, 64, rows=(0, 64)).unsqueeze(1).broadcast_to([64, 4, 64])
            identh_b = self.C("identh").unsqueeze(1).broadcast_to([128, 8, 64])
            v4 = lambda ap, n: ap.rearrange("p (h c) -> p h c", c=n)

            Lcur = [None]
            Rodd2 = [(alloc(f"p_Rodd{k}", [64, 8, 64], F32), Dep()) for k in range(2)]
            gq2 = [(alloc(f"p_gq{k}", [128, 8, 64], F32), Dep()) for k in range(2)]
            nch = getattr(self, "dbg_nch", 32)

            def serial_pre(c):
                if False:
                    yield
                sbk, ch = c // 2, c % 2
                if ch == 0:
                    Lcur[0] = lst.get()
                L, Ldep = Lcur[0]
                csl = slice(ch * 64, (ch + 1) * 64)
                Rodd_c, Roddd_c = Rodd2[c % 2]
                gq_c, gqd_c = gq2[c % 2]
                P_ = prep[c % 2]
                pd = P_["d"]
                Lr, Lk, Lv, Lsg, La, Lg = (L[nm][:, :, csl] for nm in names)

                def op(eng, fn, reads, writes):
                    s.op(eng, fn, reads=[(pd[x] if isinstance(x, str) else x) for x in reads],
                         writes=[(pd[x] if isinstance(x, str) else x) for x in writes])
                op("dve", lambda e: e.tensor_scalar(out=P_["lw"][:, :, :], in0=Lsg, scalar1=-0.6065306597126334, scalar2=None, op0=ALU.mult),
                   [Ldep], ["lw"])
                op("dve", lambda e: e.tensor_tensor_scan(P_["cum"][:, :, :].rearrange("p a b -> p (a b)"), rmask[:, :, :].rearrange("p a b -> p (a b)"),
                                                         P_["lw"][:, :, :].rearrange("p a b -> p (a b)"), 0.0, ALU.mult, ALU.add),
                   ["lw", cd], ["cum"])
                op("pool", lambda e: e.tensor_tensor(out=P_["cumE"][:, :, :], in0=P_["cum"][:, :, :], in1=P_["lw"][:, :, :], op=ALU.subtract),
                   ["cum", "lw"], ["cumE"])
                op("act", lambda e: e.activation(out=P_["e1"][:, :, :], in_=P_["cumE"][:, :, :], func=AF.Exp), ["cumE"], ["e1"])
                op("act", lambda e: e.activation(out=P_["e2"][:, :, :], in_=P_["cum"][:, :, :], func=AF.Exp), ["cum"], ["e2"])
                op("act", lambda e: e.activation(out=P_["e3"][:, :, :], in_=P_["cum"][:, :, :], func=AF.Exp, scale=-1.0), ["cum"], ["e3"])
                op("dve", lambda e: e.tensor_tensor(out=P_["e4"][:, :, :], in0=P_["cum"][:, :, :],
                                                    in1=P_["cum"][:, :, 63:64].broadcast_to([128, 8, 64]), op=ALU.subtract), ["cum"], ["e4"])
                op("act", lambda e: e.activation(out=P_["e4"][:, :, :], in_=P_["e4"][:, :, :], func=AF.Exp, scale=-1.0), ["e4"], ["e4"])
                op("act", lambda e: e.activation(out=P_["PL"][:, :].unsqueeze(2), in_=P_["cum"][:, :, 63:64], func=AF.Exp), ["cum"], ["PL"])
                op("pool", lambda e: e.tensor_copy(out=P_["PLs"][0:64, :, 0:1], in_=P_["PL"][0:64, :].unsqueeze(2)), ["PL", cd], ["PLs"])
                op("pool", lambda e: e.tensor_copy(out=P_["PLs"][64:128, :, 1:2], in_=P_["PL"][64:128, :].unsqueeze(2)), ["PL", cd], ["PLs"])
                bpl, bpld = self.bank(3)
                s.mm(bpl[0:64, 0:16], self.C("identh"), P_["PLs"][:, :, :].rearrange("p a b -> p (a b)"), True, True, reads=[pd["PLs"], self.ctd], out_dep=bpld)
                op("dve", lambda e: e.tensor_copy(out=P_["PLh"][:, :], in_=bpl[0:64, 0:16]), [bpld], ["PLh"])
                yield
                op("dve", lambda e: e.tensor_tensor(out=P_["kkr"][:, :, :], in0=Lk, in1=bc8(f"k_k{j}"), op=ALU.mult), [Ldep, self.ptd], ["kkr"])
                op("act", lambda e: e.activation(out=P_["sqk"][:, :, :], in_=P_["kkr"][:, :, :], func=AF.Square), ["kkr"], ["sqk"])
                yield
                bs, bsd = self.bank(2)
                for hp in range(8):
                    s.mm(bs[:, hp * 64:(hp + 1) * 64], self.C("bd"), P_["sqk"][:, hp, :], True, hp == 7, reads=[pd["sqk"], self.ctd], out_dep=bsd)
                op("act", lambda e: e.activation(out=P_["rn"][:, :, :], in_=v4(bs, 64), func=AF.Sqrt, bias=tiny[:, 0:1]), [bsd, cd], ["rn"])
                op("dve", lambda e: e.reciprocal(out=P_["rn"][:, :, :], in_=P_["rn"][:, :, :]), ["rn"], ["rn"])
                op("dve", lambda e: e.tensor_tensor(out=P_["kk"][:, :, :], in0=P_["kkr"][:, :, :], in1=P_["rn"][:, :, :], op=ALU.mult), ["kkr", "rn"], ["kk"])
                yield
                op("pool", lambda e: e.tensor_tensor(out=P_["ta"][:, :, :], in0=La, in1=bc8(f"k_a{j}"), op=ALU.mult), [Ldep, self.ptd], ["ta"])
                op("pool", lambda e: e.tensor_tensor(out=P_["ta"][:, :, :], in0=P_["ta"][:, :, :], in1=omka[:, :].unsqueeze(2).broadcast_to([128, 8, 64]),
                                                     op=ALU.add), ["ta", cd], ["ta"])
                op("dve", lambda e: e.tensor_tensor(out=P_["k2"][:, :, :], in0=Lk, in1=P_["ta"][:, :, :], op=ALU.mult), [Ldep, "ta"], ["k2"])
                op("pool", lambda e: e.tensor_tensor(out=P_["beta"][:, :, :], in0=P_["kk"][:, :, :], in1=La, op=ALU.mult), ["kk", Ldep], ["beta"])
                yield
                op("dve", lambda e: e.scalar_tensor_tensor(out=P_["AR"][:, :, 0:64], in0=P_["kk"][:, :, :], scalar=-1.0, in1=P_["e1"][:, :, :],
                                                           op0=ALU.mult, op1=ALU.mult), ["kk", "e1"], ["AR"])
                op("pool", lambda e: e.tensor_tensor(out=P_["AR"][:, :, 64:128], in0=Lr, in1=P_["e2"][:, :, :], op=ALU.mult), [Ldep, "e2"], ["AR"])
                op("dve", lambda e: e.tensor_tensor(out=P_["Bt"][:, :, :], in0=P_["beta"][:, :, :], in1=P_["e3"][:, :, :], op=ALU.mult), ["beta", "e3"], ["Bt"])
                op("pool", lambda e: e.tensor_tensor(out=P_["Kt"][:, :, :], in0=P_["k2"][:, :, :], in1=P_["e3"][:, :, :], op=ALU.mult), ["k2", "e3"], ["Kt"])
                op("dve", lambda e: e.tensor_tensor(out=P_["Bh"][:, :, :], in0=P_["beta"][:, :, :], in1=P_["e4"][:, :, :], op=ALU.mult), ["beta", "e4"], ["Bh"])
                op("pool", lambda e: e.tensor_tensor(out=P_["Kh"][:, :, :], in0=P_["k2"][:, :, :], in1=P_["e4"][:, :, :], op=ALU.mult), ["k2", "e4"], ["Kh"])
                yield
                s.op("act", lambda e: e.activation(out=gq_c[:, :, :], in_=Lg, func=AF.Copy), reads=[Ldep], writes=[gqd_c])
                op("act", lambda e: e.activation(out=P_["vb"][:, :, :], in_=Lv, func=AF.Copy), [Ldep], ["vb"])
                op("pool", lambda e: e.tensor_tensor(out=P_["rk"][:, :, :], in0=Lr, in1=P_["k2"][:, :, :], op=ALU.mult), [Ldep, "k2"], ["rk"])
                op("pool", lambda e: e.tensor_tensor(out=P_["rk"][:, :, :], in0=P_["rk"][:, :, :], in1=bc8(f"r_k{j}"), op=ALU.mult), ["rk", self.ptd], ["rk"])
                yield
                bb, bbd = self.bank(3)
                for hp in range(8):
                    s.mm(bb[:, hp * 64:(hp + 1) * 64], self.C("bd"), P_["rk"][:, hp, :], True, hp == 7, reads=[pd["rk"], self.ctd], out_dep=bbd)
                op("dve", lambda e: e.tensor_tensor(out=P_["bonus"][:, :, :], in0=v4(bb, 64), in1=Lv, op=ALU.mult), [bbd, Ldep], ["bonus"])
                tm = tmaj[c % 2]
                td_ = tm["d"]
                for ti, (srcf, sdep, dst, ddeps) in enumerate((
                        (lambda hp: P_["AR"][:, hp, 0:64], "AR", tm["AX"][:, :, 0:64], [td_["AXa"]]),
                        (lambda hp: P_["Bh"][:, hp, :], "Bh", tm["B"][:, :].rearrange("p (h c) -> p h c", c=64), [td_["B"]]),
                        (lambda hp: P_["Kh"][:, hp, :], "Kh", tm["K"][:, :].rearrange("p (h c) -> p h c", c=64), [td_["K"]]),
                        (lambda hp: P_["vb"][:, hp, :], "vb", tm["V"][:, :].rearrange("p (h c) -> p h c", c=64), [td_["V"]]))):
                    yield
                    half = 1024
                    b0 = 2
                    for hp in range(8):
                        s.mm(self.psA[0:64, half + hp * 128:half + (hp + 1) * 128], srcf(hp), self.identb[:, :], True, hp % 4 == 3,
                             reads=[pd[sdep], self.cbd], out_dep=self.bd_[b0 + hp // 4])
                    src = self.psA[0:64, half:half + 1024].rearrange("p (h c) -> p h c", c=64)
                    if ti % 2 == 0:
                        s.op("act", lambda e: e.activation(out=dst, in_=src, func=AF.Copy), reads=[self.bd_[b0], self.bd_[b0 + 1]], writes=ddeps)
                    else:
                        s.op("dve", lambda e: e.tensor_copy(out=dst, in_=src), reads=[self.bd_[b0], self.bd_[b0 + 1]], writes=ddeps)
                yield
                bro, brod = self.bank(2)
                for hp in range(8):
                    s.mm(bro[0:64, hp * 64:(hp + 1) * 64], self.identb[64:128, 64:128], P_["AR"][64:128, hp, 64:128], True, hp == 7,
                         reads=[self.cbd, pd["AR"]], out_dep=brod)
                s.op("act", lambda e: e.activation(out=Rodd_c[:, :, :], in_=v4(bro[0:64, :], 64), func=AF.Copy), reads=[brod], writes=[Roddd_c])

            def make_groups(c):
                sbk, ch = c // 2, c % 2
                P_ = prep[c % 2]
                pd = P_["d"]
                tm = tmaj[c % 2]
                td_ = tm["d"]
                Yt, Ytd = Yall[c % 2]

                def op(eng, fn, reads, writes):
                    s.op(eng, fn, reads=[(pd[x] if isinstance(x, str) else x) for x in reads],
                         writes=[(pd[x] if isinstance(x, str) else x) for x in writes])
                def gsteps(g, mybanks):
                    rr_ = [0]

                    def nb():
                        b_ = mybanks[rr_[0] % len(mybanks)]
                        rr_[0] += 1
                        return self.bank(b_)
                    G = grp[g % 2]
                    gd = G["d"]
                    hb = (g // 2) * 8 + (g % 2)
                    heads = [(hb + 2 * hh, (hb + 2 * hh) // 2, hb % 2) for hh in range(4)]
                    hs = slice(hb, hb + 7, 2)
                    hpsl = slice(hb // 2, hb // 2 + 4)
                    gpar = hb % 2
                    b1, b1d = nb()
                    b2, b2d = nb()
                    b3, b3d = nb()
                    for hh, (h, hp, par) in enumerate(heads):
                        ps = slice(par * 64, (par + 1) * 64)
                        s.mm(b1[0:64, hh * 128:(hh + 1) * 128], P_["Bt"][ps, hp, :], P_["AR"][ps, hp, :], True, hh == 3, reads=[pd["Bt"], pd["AR"]], out_dep=b1d)
                    for hh, (h, hp, par) in enumerate(heads):
                        ps = slice(par * 64, (par + 1) * 64)
                        s.mm(b2[0:64, hh * 128:(hh + 1) * 128], P_["Kt"][ps, hp, :], P_["AR"][ps, hp, :], True, hh == 3, reads=[pd["Kt"], pd["AR"]], out_dep=b2d)
                    for hh, (h, hp, par) in enumerate(heads):
                        ps = slice(par * 64, (par + 1) * 64)
                        s.mm(b3[0:64, hh * 64:(hh + 1) * 64], P_["AR"][ps, hp, 0:64], P_["Bt"][ps, hp, :], True, hh == 3, reads=[pd["Bt"], pd["AR"]], out_dep=b3d)
                    if getattr(self, 'dbg_m', 9) < 1:
                        return
                    s.op("dve", lambda e: e.tensor_tensor(out=G["Mbm"][:, :, :], in0=v4(b1[0:64, :], 128), in1=mask_ar, op=ALU.mult),
                         reads=[b1d, self.ctd], writes=[gd["Mbm"]])
                    s.op("dve", lambda e: e.tensor_tensor(out=G["Mkb"][:, :, :], in0=v4(b2[0:64, :], 128), in1=mask_ar, op=ALU.mult),
                         reads=[b2d, self.ctd], writes=[gd["Mkb"]])
                    s.op("dve", lambda e: e.tensor_tensor(out=G["ATt"][:, :, :], in0=v4(b3[0:64, 0:256], 64), in1=maskT, op=ALU.mult),
                         reads=[b3d, self.ctd], writes=[gd["ATt"]])
                    if getattr(self, 'dbg_m', 9) < 2:
                        return
                    s.op("act", lambda e: e.activation(out=G["AT"][:, :, 0:64], in_=G["Mbm"][:, :, 0:64], func=AF.Copy), reads=[gd["Mbm"]], writes=[gd["AT"]])
                    s.op("pool", lambda e: e.tensor_tensor(out=G["AT"][:, :, 64:128], in0=G["Mbm"][:, :, 0:64], in1=id64b, op=ALU.add),
                         reads=[gd["Mbm"], self.ctd], writes=[gd["AT"]])
                    s.op("act", lambda e: e.activation(out=G["Mbr"][:, :, :], in_=G["Mbm"][:, :, 64:128], func=AF.Copy), reads=[gd["Mbm"]], writes=[gd["Mbr"]])
                    if getattr(self, 'dbg_sub', 9) < 2:
                        return
                    yield
                    for rnd in range(6):
                        if rnd > 0:
                            yield
                        bA, bAd = nb()
                        if rnd == 0:
                            bB, bBd = nb()
                            for hh in range(4):
                                s.mm(bA[0:64, hh * 64:(hh + 1) * 64], G["ATt"][:, hh, :], G["AT"][:, hh, 0:64], True, hh == 3, reads=[gd["ATt"], gd["AT"]], out_dep=bAd)
                            for hh in range(4):
                                s.mm(bB[0:64, hh * 64:(hh + 1) * 64], G["AT"][:, hh, 0:64], G["ATt"][:, hh, :], True, hh == 3, reads=[gd["ATt"], gd["AT"]], out_dep=bBd)
                            s.op("act", lambda e: e.activation(out=G["AT"][:, :, 0:64], in_=v4(bA[0:64, 0:256], 64), func=AF.Copy), reads=[bAd], writes=[gd["AT"]])
                            s.op("act", lambda e: e.activation(out=G["ATt"][:, :, :], in_=v4(bB[0:64, 0:256], 64), func=AF.Copy), reads=[bBd], writes=[gd["ATt"]])
                        elif rnd < 5:
                            bB, bBd = nb()
                            for hh in range(4):
                                s.mm(bA[0:64, hh * 128:(hh + 1) * 128], G["ATt"][:, hh, :], G["AT"][:, hh, :], True, hh == 3, reads=[gd["ATt"], gd["AT"]], out_dep=bAd)
                            for hh in range(4):
                                s.mm(bB[0:64, hh * 64:(hh + 1) * 64], G["AT"][:, hh, 0:64], G["ATt"][:, hh, :], True, hh == 3, reads=[gd["ATt"], gd["AT"]], out_dep=bBd)
                            bA4 = v4(bA[0:64, :], 128)
                            s.op("dve", lambda e: e.tensor_tensor(out=G["AT"][:, :, 64:128], in0=G["AT"][:, :, 64:128], in1=bA4[:, :, 64:128], op=ALU.add),
                                 reads=[bAd, gd["AT"]], writes=[gd["AT"]])
                            s.op("dve", lambda e: e.tensor_copy(out=G["AT"][:, :, 0:64], in_=bA4[:, :, 0:64]), reads=[bAd, gd["AT"]], writes=[gd["AT"]])
                            s.op("act", lambda e: e.activation(out=G["ATt"][:, :, :], in_=v4(bB[0:64, 0:256], 64), func=AF.Copy), reads=[bBd], writes=[gd["ATt"]])
                        else:
                            for hh in range(4):
                                s.mm(bA[0:64, hh * 64:(hh + 1) * 64], G["ATt"][:, hh, :], G["AT"][:, hh, 64:128], True, hh == 3, reads=[gd["ATt"], gd["AT"]], out_dep=bAd)
                            s.op("dve", lambda e: e.tensor_tensor(out=G["Tb"][:, :, :], in0=G["AT"][:, :, 64:128], in1=v4(bA[0:64, 0:256], 64), op=ALU.add),
                                 reads=[bAd, gd["AT"]], writes=[gd["Tb"]])
                    if getattr(self, 'dbg_sub', 9) < 3:
                        return
                    yield
                    bx, bxd = nb()
                    for hh, (h, hp, par) in enumerate(heads):
                        s.mm(bx[0:64, hh * 64:(hh + 1) * 64], G["Mkb"][:, hh, 0:64], tm["V"][:, h * 64:(h + 1) * 64], True, hh == 3,
                             reads=[gd["Mkb"], td_["V"]], out_dep=bxd)
                    s.op("act", lambda e: e.activation(out=tm["AX"][:, hs, 64:128], in_=v4(bx[0:64, 0:256], 64), func=AF.Copy),
                         reads=[bxd], writes=[td_["AXx"][g]])
                    yield
                    bu, bud = nb()
                    for hh, (h, hp, par) in enumerate(heads):
                        s.mm(bu[0:64, hh * 128:(hh + 1) * 128], G["Tb"][:, hh, :], tm["AX"][:, h, :], True, hh == 3,
                             reads=[gd["Tb"], td_["AXa"], td_["AXx"][g]], out_dep=bud)
                    s.op("act", lambda e: e.activation(out=G["AU"][:, :, :], in_=v4(bu[0:64, :], 128), func=AF.Copy), reads=[bud], writes=[gd["AU"]])
                    if getattr(self, 'dbg_sub', 9) < 4:
                        return
                    yield
                    br_, brd = nb()
                    bp, bpd = nb()
                    for hh, (h, hp, par) in enumerate(heads):
                        ps = slice(par * 64, (par + 1) * 64)
                        s.mm(br_[0:64, hh * 64:(hh + 1) * 64], G["AU"][:, hh, 0:64], G["Mbr"][:, hh, :], True, hh == 3, reads=[gd["AU"], gd["Mbr"]], out_dep=brd)
                    for hh, (h, hp, par) in enumerate(heads):
                        ps = slice(par * 64, (par + 1) * 64)
                        s.mm(bp[0:64, hh * 64:(hh + 1) * 64], G["AU"][:, hh, 0:64], tm["B"][:, h * 64:(h + 1) * 64], True, hh == 3, reads=[gd["AU"], td_["B"]], out_dep=bpd)
                    rsrc = P_["AR"][0:64, hpsl, 64:128] if gpar == 0 else Rodd2[c % 2][0][:, hpsl, :]
                    s.op("dve", lambda e: e.tensor_tensor(out=G["Rh"][:, :, :], in0=v4(br_[0:64, 0:256], 64), in1=rsrc, op=ALU.add),
                         reads=[brd, pd["AR"], Rodd2[c % 2][1]], writes=[gd["Rh"]])
                    s.op("pool", lambda e: e.tensor_tensor(out=G["Phi"][:, :, :], in0=id64b, in1=P_["PLh"][:, hs].unsqueeze(2).broadcast_to([64, 4, 64]),
                                                           op=ALU.mult), reads=[pd["PLh"], self.ctd], writes=[gd["Phi"]])
                    s.op("dve", lambda e: e.tensor_tensor(out=G["Phi"][:, :, :], in0=G["Phi"][:, :, :], in1=v4(bp[0:64, 0:256], 64), op=ALU.add),
                         reads=[bpd, gd["Phi"]], writes=[gd["Phi"]])
                    if getattr(self, 'dbg_sub', 9) < 5:
                        return
                    yield
                    by, byd = nb()
                    for hh, (h, hp, par) in enumerate(heads):
                        o_ = by[0:64, hh * 64:(hh + 1) * 64]
                        s.mm(o_, G["Mbr"][:, hh, :], G["AU"][:, hh, 64:128], True, False, reads=[gd["Mbr"], gd["AU"]], out_dep=byd)
                        s.mm(o_, G["Mkb"][:, hh, 64:128], tm["V"][:, h * 64:(h + 1) * 64], False, False, reads=[gd["Mkb"], td_["V"]], out_dep=byd)
                        s.mm(o_, G["Rh"][:, hh, :], Sb[:, h, :], False, hh == 3, reads=[gd["Rh"], Sbd[g]], out_dep=byd)
                    s.op("act", lambda e: e.activation(out=Yt[:, hs, :], in_=v4(by[0:64, 0:256], 64), func=AF.Copy), reads=[byd], writes=[Ytd[g]])
                    if getattr(self, 'dbg_sub', 9) < 6:
                        return
                    yield
                    bn, bnd = nb()
                    bn2, bn2d = nb()
                    for hh, (h, hp, par) in enumerate(heads):
                        o_ = bn[0:64, hh * 64:(hh + 1) * 64]
                        s.mm(o_, tm["B"][:, h * 64:(h + 1) * 64], G["AU"][:, hh, 64:128], True, False, reads=[td_["B"], gd["AU"]], out_dep=bnd)
                        s.mm(o_, tm["K"][:, h * 64:(h + 1) * 64], tm["V"][:, h * 64:(h + 1) * 64], False, hh == 3, reads=[td_["K"], td_["V"]], out_dep=bnd)
                    for hh, (h, hp, par) in enumerate(heads):
                        s.mm(bn2[0:64, hh * 64:(hh + 1) * 64], G["Phi"][:, hh, :], Sf[:, h, :], True, hh == 3, reads=[gd["Phi"], Sfd[g]], out_dep=bn2d)
                    s.op("act", lambda e: e.activation(out=G["Sn"][:, :, :], in_=v4(bn[0:64, 0:256], 64), func=AF.Copy), reads=[bnd], writes=[gd["Sn"]])
                    s.op("dve", lambda e: e.tensor_tensor(out=Sf[:, hs, :], in0=G["Sn"][:, :, :], in1=v4(bn2[0:64, 0:256], 64), op=ALU.add),
                         reads=[bn2d, gd["Sn"]], writes=[Sfd[g]])
                    s.op("act", lambda e: e.activation(out=Sb[:, hs, :], in_=Sf[:, hs, :], func=AF.Copy), reads=[Sfd[g]], writes=[Sbd[g]])

                return gsteps

            def serial_out(c):
                if False:
                    yield
                sbk, ch = c // 2, c % 2
                P_ = prep[c % 2]
                pd = P_["d"]
                tm = tmaj[c % 2]
                td_ = tm["d"]
                Yt, Ytd = Yall[c % 2]

                def op(eng, fn, reads, writes):
                    s.op(eng, fn, reads=[(pd[x] if isinstance(x, str) else x) for x in reads],
                         writes=[(pd[x] if isinstance(x, str) else x) for x in writes])
                s.op("dve", lambda e: e.reduce_sum(out=yst[:, 0:16], in_=Yt[:, :, :], axis=AX.X), reads=Ytd, writes=[ystd])
                s.op("dve", lambda e: e.tensor_scalar(out=yst[:, 0:16], in0=yst[:, 0:16], scalar1=1.0 / 64.0, scalar2=None, op0=ALU.mult), reads=[ystd], writes=[ystd])
                s.op("pool", lambda e: e.tensor_tensor(out=Yc[:, :, :], in0=Yt[:, :, :], in1=yst[:, 0:16].unsqueeze(2).broadcast_to([64, 16, 64]), op=ALU.subtract),
                     reads=Ytd + [ystd], writes=[ycd])
                yield
                s.op("act", lambda e: e.activation(out=Ysq[:, :, :], in_=Yc[:, :, :], func=AF.Square), reads=[ycd], writes=[ysqd])
                s.op("dve", lambda e: e.reduce_sum(out=yst[:, 16:32], in_=Ysq[:, :, :], axis=AX.X), reads=[ysqd, ystd], writes=[ystd])
                s.op("act", lambda e: e.activation(out=yst[:, 16:32], in_=yst[:, 16:32], func=AF.Sqrt, bias=gnb[:, 0:1], scale=1.0 / 64.0), reads=[ystd, cd], writes=[ystd])
                s.op("dve", lambda e: e.reciprocal(out=yst[:, 16:32], in_=yst[:, 16:32]), reads=[ystd], writes=[ystd])
                s.op("pool", lambda e: e.tensor_tensor(out=Yc[:, :, :], in0=Yc[:, :, :], in1=yst[:, 16:32].unsqueeze(2).broadcast_to([64, 16, 64]), op=ALU.mult),
                     reads=[ycd, ystd, ysqd], writes=[ycd])
                yield
                bo, bod = self.bank(2)
                Ycf = Yc[:, :, :].rearrange("p a b -> p (a b)")
                for hp in range(8):
                    s.mm(bo[:, hp * 64:(hp + 1) * 64], Ycf[:, hp * 128:(hp + 1) * 128], ident[0:64, 0:64], True, hp == 7, reads=[ycd, self.ctd], out_dep=bod)
                s.op("dve", lambda e: e.tensor_tensor(out=yf[:, :, :], in0=v4(bo, 64), in1=bc8(f"lnx_g{j}"), op=ALU.mult), reads=[bod, self.ptd], writes=[yfd])
                s.op("pool", lambda e: e.tensor_tensor(out=yf[:, :, :], in0=yf[:, :, :], in1=bc8(f"lnx_b{j}"), op=ALU.add), reads=[yfd, self.ptd], writes=[yfd])
                s.op("pool", lambda e: e.tensor_tensor(out=yf[:, :, :], in0=yf[:, :, :], in1=P_["bonus"][:, :, :], op=ALU.add), reads=[yfd, pd["bonus"]], writes=[yfd])
                yield
                yg_t, yg_d = ygs[(c // 4) % 2]
                s.op("dve", lambda e: e.tensor_tensor(out=yg_t[:, :, (c % 4) * 64:(c % 4 + 1) * 64], in0=yf[:, :, :], in1=gq2[c % 2][0][:, :, :], op=ALU.mult),
                     reads=[yfd, gq2[c % 2][1]], writes=[yg_d])
                if c % 4 == 3:
                    c0 = (c // 4) * 256
                    s.dma("sp", fmv(scr["yg"])[:, :, c0:c0 + 256], yg_t[:, :, :], reads=[yg_d])

            def chain(*gens):
                for g_ in gens:
                    if g_ is not None:
                        yield from g_

            for _ in serial_pre(0):
                pass
            for c in range(nch):
                ser = chain(serial_out(c - 1) if c > 0 else None, serial_pre(c + 1) if c + 1 < nch else None)
                gsteps = make_groups(c)
                ser_live = True
                for pair in ((0, 1), (2, 3)):
                    live = [gsteps(pair[0], [4, 5, 6]), gsteps(pair[1], [7, 0, 1])]
                    while live:
                        for gg in list(live):
                            try:
                                next(gg)
                            except StopIteration:
                                live.remove(gg)
                        if ser_live:
                            try:
                                next(ser)
                            except StopIteration:
                                ser_live = False
                if ser_live:
                    for _ in ser:
                        pass
            for _ in serial_out(nch - 1):
                pass
        self.Xd = [[Dep() for _ in range(4)] for _ in range(DC)]
        for c in range(DC):
            s.dma("sp", self.X[:, c, :], xs[:, c, :], writes=self.Xd[c])

    Prog.rwkv_pass2 = rwkv_pass2


_rwkv2_methods()
```

```python
import contextlib
import math
import numpy as np
import concourse.bass as bass
import concourse.mybir as mybir
from concourse.bass_utils import run_bass_kernel_spmd

F32 = mybir.dt.float32
BF16 = mybir.dt.bfloat16
I32 = mybir.dt.int32
AF = mybir.ActivationFunctionType
ALU = mybir.AluOpType
AX = mybir.AxisListType

S = 2048
D = 1024
DC = 8
NL = 4
DFF = 2816
FC = 22
NE = 8
EPS = 1e-6
NEG = -30000.0
SB_BASE = 16512
SB_TOP = 229344


class Dep:
    __slots__ = ("w", "r")

    def __init__(self):
        self.w = None
        self.r = {}


class Sched:
    def __init__(self, nc, n_dma_sems=24):
        self.nc = nc
        self.eng = {"pe": nc.tensor, "dve": nc.vector, "act": nc.scalar,
                    "pool": nc.gpsimd, "sp": nc.sync}
        self.sem = {e: nc.alloc_semaphore(name=f"s_{e}") for e in self.eng}
        self.cnt = {e: 0 for e in self.eng}
        self.dsem = [nc.alloc_semaphore(name=f"d_{i}") for i in range(n_dma_sems)]
        self.dcnt = [0] * n_dma_sems
        self.dlast = [None] * n_dma_sems
        self.dnext = 0
        self.seen = {e: {} for e in self.eng}
        self.nins = 0

    def _wait(self, e, key, val):
        if self.seen[e].get(key, 0) >= val:
            return
        sem = self.sem[key[1]] if key[0] == "e" else self.dsem[key[1]]
        self.eng[e].wait_ge(sem, val)
        self.seen[e][key] = val

    def _deps(self, e, reads, writes):
        need = {}
        for d in reads:
            if d.w is not None:
                k, v = d.w
                if need.get(k, 0) < v:
                    need[k] = v
        for d in writes:
            if d.w is not None:
                k, v = d.w
                if need.get(k, 0) < v:
                    need[k] = v
            for k, v in d.r.items():
                if need.get(k, 0) < v:
                    need[k] = v
        for k, v in need.items():
            self._wait(e, k, v)

    def _commit(self, tok, reads, writes):
        k, v = tok
        for d in reads:
            if d.r.get(k, 0) < v:
                d.r[k] = v
        for d in writes:
            d.w = tok
            d.r = {}

    def op(self, e, fn, reads=(), writes=()):
        self._deps(e, reads, writes)
        ins = fn(self.eng[e])
        self.cnt[e] += 1
        ins.then_inc(self.sem[e], 1)
        tok = (("e", e), self.cnt[e])
        self._commit(tok, reads, writes)
        self.nins += 1
        return tok

    def mm(self, out, lhsT, rhs, start, stop, reads=(), out_dep=None, inc=None):
        e = "pe"
        if inc is None:
            inc = stop
        self._deps(e, reads, [])
        if start and out_dep is not None:
            need = dict(out_dep.r)
            if out_dep.w is not None and out_dep.w[0] != ("e", "pe"):
                k, v = out_dep.w
                if need.get(k, 0) < v:
                    need[k] = v
            for k, v in need.items():
                self._wait(e, k, v)
        ins = self.eng[e].matmul(out, lhsT, rhs, start=start, stop=stop)
        if inc:
            self.cnt[e] += 1
            ins.then_inc(self.sem[e], 1)
            tok = (("e", e), self.cnt[e])
        else:
            tok = (("e", e), self.cnt[e] + 1)
        self._commit(tok, reads, [out_dep] if stop else [])
        self.nins += 1
        return tok

    def dma(self, q, out, in_, reads=(), writes=(), **kw):
        i = self.dnext
        self.dnext = (i + 1) % len(self.dsem)
        self._deps(q, reads, writes)
        if self.dlast[i] is not None:
            self._wait(q, *self.dlast[i])
        ins = self.eng[q].dma_start(out=out, in_=in_, **kw)
        self.dcnt[i] += 16
        ins.then_inc(self.dsem[i], 16)
        tok = (("d", i), self.dcnt[i])
        self.dlast[i] = tok
        self._commit(tok, reads, writes)
        self.nins += 1
        return tok

    def barrier(self, engines=None):
        engines = engines or list(self.eng)
        for e in engines:
            for f in self.eng:
                if self.cnt[f] > 0:
                    self._wait(e, ("e", f), self.cnt[f])
            for t in self.dlast:
                if t is not None:
                    self._wait(e, *t)


class Stream:
    def __init__(self, bufs, items, load_fn):
        self.bufs = bufs
        self.items = items
        self.load_fn = load_fn
        self.n = 0
        self.issued = 0

    def get(self):
        nb = len(self.bufs)
        while self.issued < len(self.items) and self.issued < self.n + nb:
            t, d = self.bufs[self.issued % nb]
            self.load_fn(self.items[self.issued], t, d)
            self.issued += 1
        t, d = self.bufs[self.n % nb]
        self.n += 1
        return t, d


def _fm(v):
    v = np.asarray(v, np.float32).reshape(-1)
    n = v.size // 128
    return np.ascontiguousarray(v.reshape(n, 128).T)


class Table:
    def __init__(self):
        self.cols = {}
        self.parts = []
        self.n = 0

    def add(self, name, arr):
        arr = np.asarray(arr, np.float32)
        assert arr.shape[0] == 128, (name, arr.shape)
        self.cols[name] = (self.n, arr.shape[1])
        self.parts.append(arr)
        self.n += arr.shape[1]

    def build(self):
        return np.ascontiguousarray(np.concatenate(self.parts, axis=1))


def _pad128(a):
    out = np.zeros((128, a.shape[1]), np.float32)
    out[: a.shape[0]] = a
    return out


def const_table():
    t = Table()
    p = np.arange(128)
    t.add("ident", np.eye(128, dtype=np.float32))
    t.add("ones", np.ones((128, 128), np.float32))
    t.add("bd", (p[:, None] // 64 == p[None, :] // 64).astype(np.float32))
    t.add("identh", (p[:, None] % 64 == np.arange(64)[None, :]).astype(np.float32))
    j = np.arange(64)[:, None]
    tt = np.arange(64)[None, :]
    t.add("mask_ar", _pad128(np.concatenate([(j < tt), (j <= tt)], axis=1).astype(np.float32)))
    t.add("maskT", _pad128((tt < j).astype(np.float32)))
    qi = p[:, None]
    kj = p[None, :]
    t.add("cmask", np.where(kj <= qi, 0.0, NEG).astype(np.float32))
    t.add("mmask", np.where((kj // 64) <= (qi // 64), 0.0, NEG).astype(np.float32))
    t.add("invf", (10000.0 ** (-(p % 16).astype(np.float64) / 16.0)).astype(np.float32)[:, None])
    sel8 = np.zeros((128, 8 * 128), np.float32)
    for e in range(8):
        sel8[e, e * 128:(e + 1) * 128] = 1.0
    t.add("sel8", sel8)
    sel16 = np.zeros((128, 16 * 128), np.float32)
    for e in range(16):
        sel16[e, e * 128:(e + 1) * 128] = 1.0
    t.add("sel16", sel16)
    return t


def param_table(inp, b):
    t = Table()
    t.add("c", _fm(inp["c"][b]))
    for i in range(NL):
        t.add(f"adab{i}", _fm(inp["ada_b"][i]))
        t.add(f"nmg{i}", _fm(inp["norm_mix_g"][i]))
        t.add(f"nfg{i}", _fm(inp["norm_ffn_g"][i]))
    t.add("fng", _fm(inp["final_norm_g"]))
    for j in range(2):
        t.add(f"mu{j}", _fm(inp["rw_mu"][j]))
        for nm in ("w0", "a0", "k_k", "k_a", "r_k", "lnx_g", "lnx_b"):
            t.add(f"{nm}{j}", _fm(inp["rw_" + nm][j]))
    t.add("v0", _fm(inp["rw_v0"][0]))
    t.add("mqg", _fm(inp["mla_q_norm_g"][0]))
    t.add("mkvg", _fm(inp["mla_kv_norm_g"][0]))
    t.add("fbf", _pad128(np.asarray(inp["fox_b_f"][0], np.float32).reshape(16, 1)))
    t.add("fqg", np.tile(np.asarray(inp["fox_q_norm_g"][0], np.float32).reshape(64, 1), (2, 1)))
    t.add("fkg", np.tile(np.asarray(inp["fox_k_norm_g"][0], np.float32).reshape(64, 1), (2, 1)))
    for j in range(2):
        t.add(f"mbr{j}", np.tile(np.asarray(inp["moe_b_router"][j], np.float32).reshape(1, 8), (128, 1)))
        wr = np.asarray(inp["moe_w_router"][j], np.float32)
        t.add(f"mwr{j}", np.ascontiguousarray(wr.reshape(8, 128, 8).transpose(1, 0, 2).reshape(128, 64)))
    return t


_CT = const_table()


W_SHAPES = {
    "ada_w": [4, 1024, 6144],
    "rw_w_rkv": [2, 3, 1024, 1024], "rw_w_o": [2, 1024, 1024],
    "rw_w1": [2, 1024, 64], "rw_w2": [2, 64, 1024],
    "rw_a1": [2, 1024, 64], "rw_a2": [2, 64, 1024],
    "rw_g1": [2, 1024, 160], "rw_g2": [2, 160, 1024],
    "rw_v1": [1, 1024, 32], "rw_v2": [1, 32, 1024],
    "mla_w_down": [1, 1024, 1056], "mla_w_uq": [1, 768, 1536],
    "mla_w_ukv": [1, 256, 2048], "mla_w_o": [1, 1024, 1024],
    "fox_w_in": [1, 1024, 4112], "fox_w_o": [1, 1024, 1024],
    "ffn_w_gate_up": [2, 1024, 5632], "ffn_w_down": [2, 2816, 1024],
    "moe_w_gate_up": [2, 8, 1024, 5632], "moe_w_down": [2, 8, 2816, 1024],
}


class Prog:
    def __init__(self, pcols, npcols, plan=None, x_in_dbg=False):
        self.plan = plan
        nc = bass.Bass("TRN2", target_bir_lowering=False)
        self.nc = nc
        self.s = Sched(nc)
        self.pcols = pcols
        self.ccols = _CT.cols
        d = {}
        d["xT"] = nc.dram_tensor("xT", [D, S], F32, kind="ExternalInput").ap()
        d["pos"] = nc.dram_tensor("pos", [1, S], I32, kind="ExternalInput").ap()
        d["ptab"] = nc.dram_tensor("ptab", [128, npcols], F32, kind="ExternalInput").ap()
        d["ctab"] = nc.dram_tensor("ctab", [128, _CT.n], F32, kind="ExternalInput").ap()
        for k, shp in W_SHAPES.items():
            d[k] = nc.dram_tensor(k, shp, F32, kind="ExternalInput").ap()
        d["outT"] = nc.dram_tensor("outT", [D, S], F32, kind="ExternalOutput").ap()
        self.d = d
        self.sb_ptr = SB_BASE
        self.uid = 0
        self.X = self.salloc("X", [128, DC, S], F32)
        self.Xd = [[Dep() for _ in range(4)] for _ in range(DC)]
        self.pt = self.salloc("pt", [128, npcols], F32)
        self.ptd = Dep()
        nct = self.ccols["sel8"][0]
        self.ct = self.salloc("ct", [128, nct], F32)
        self.ctd = Dep()
        self.identb = self.salloc("identb", [128, 128], BF16)
        self.cmaskb = self.salloc("cmaskb", [128, 128], BF16)
        self.onesb = self.salloc("onesb", [128, 128], BF16)
        self.mmaskb = self.salloc("mmaskb", [128, 128], BF16)
        self.cbd = Dep()
        self.mod = self.salloc("mod", [128, NL * 48], F32)
        self.modd = Dep()
        self.amod = self.salloc("amod", [128, NL * 16 + 8], F32)
        self.amodd = Dep()
        self.cond = self.salloc("cond", [128, 8], F32)
        self.epsc = self.salloc("epsc", [128, 4], F32)
        self.s.op("dve", lambda e: e.memset(self.epsc[:, 0:1], EPS))
        self.s.op("dve", lambda e: e.memset(self.epsc[:, 1:2], 64e-5))
        self.s.op("dve", lambda e: e.memset(self.epsc[:, 2:3], 1.0))
        self.s.op("dve", lambda e: e.memset(self.epsc[:, 3:4], 0.0))
        self.condd = Dep()
        self.psA = nc.alloc_psum_tensor("psA", [128, 2048], F32)
        self.psB = [nc.alloc_psum_tensor(f"psB{i}", [128, 512], F32) for i in range(4)]
        self.bd_ = [Dep() for _ in range(8)]
        self.rr = 0

    def bank(self, i):
        if i < 4:
            return self.psA[:, i * 512:(i + 1) * 512], self.bd_[i]
        return self.psB[i - 4][:, :], self.bd_[i]

    def P(self, name, a=0, b=None):
        st, n = self.pcols[name]
        b = n if b is None else b
        return self.pt[:, st + a:st + b]

    def C(self, name, a=0, b=None, rows=None):
        st, n = self.ccols[name]
        b = n if b is None else b
        if rows is None:
            return self.ct[:, st + a:st + b]
        return self.ct[rows[0]:rows[1], st + a:st + b]

    def modc(self, i, w, c0=0, c1=8):
        return self.mod[:, i * 48 + w * 8 + c0:i * 48 + w * 8 + c1]

    def salloc(self, name, shape, dt, at=None):
        nbytes = int(np.prod(shape[1:])) * (2 if dt == BF16 else 4)
        nbytes = (nbytes + 31) // 32 * 32
        if at is None:
            off = self.sb_ptr
            self.sb_ptr += nbytes
            assert self.sb_ptr <= SB_TOP, f"SBUF overflow allocating {name}: {self.sb_ptr} > {SB_TOP}"
        else:
            off = at
        self.uid += 1
        t = self.nc.alloc_sbuf_tensor_at(f"{name}_{self.uid}", list(shape), dt, offset=off)
        return t

    @contextlib.contextmanager
    def phase(self):
        mark = self.sb_ptr
        yield self.salloc
        self.s.barrier()
        self.sb_ptr = mark

    def prelude(self, layers):
        s, nc, d = self.s, self.nc, self.d
        s.dma("sp", self.pt[:, :], d["ptab"][:, :], writes=[self.ptd])
        s.dma("sp", self.ct[:, :], d["ctab"][:, 0:self.ccols["sel8"][0]], writes=[self.ctd])
        xv = d["xT"].rearrange("(c p) s -> p c s", p=128)
        for c in range(DC):
            s.dma("sp" if c % 2 == 0 else "act", self.X[:, c, :], xv[:, c, :], writes=self.Xd[c])
        s.op("dve", lambda e: e.tensor_copy(self.identb[:, :], self.C("ident")), reads=[self.ctd], writes=[self.cbd])
        s.op("dve", lambda e: e.tensor_copy(self.cmaskb[:, :], self.C("cmask")), reads=[self.ctd], writes=[self.cbd])
        s.op("dve", lambda e: e.tensor_copy(self.mmaskb[:, :], self.C("mmask")), reads=[self.ctd], writes=[self.cbd])
        s.op("dve", lambda e: e.memset(self.onesb[:, :], 1.0), writes=[self.cbd])
        s.op("act", lambda e: e.activation(out=self.cond[:, :], in_=self.P("c"), func=AF.Silu),
             reads=[self.ptd], writes=[self.condd])
        with self.phase() as alloc:
            NB = 768
            bufs = [(alloc(f"adaw{i}", [128, 8, NB], F32), Dep()) for i in range(2)]
            items = [(i, cb) for i in layers for cb in range(8)]

            def load(it, t, dep):
                i, cb = it
                src = d["ada_w"][i].rearrange("(kc p) n -> p kc n", p=128)
                s.dma("sp", t[:, :, :], src[:, :, cb * NB:(cb + 1) * NB], writes=[dep])
            st = Stream(bufs, items, load)
            pb, pbd = self.bank(4)
            for i in layers:
                for cb in range(8):
                    t, dep = st.get()
                    for oc in range(6):
                        col = cb * 6 + oc
                        for kc in range(8):
                            s.mm(pb[:, col:col + 1], t[:, kc, oc * 128:(oc + 1) * 128], self.cond[:, kc:kc + 1],
                                 kc == 0, kc == 7, reads=[dep, self.condd], out_dep=pbd)
                s.op("dve", lambda e: e.tensor_tensor(out=self.mod[:, i * 48:(i + 1) * 48], in0=pb[:, 0:48],
                                                      in1=self.P(f"adab{i}"), op=ALU.add),
                     reads=[pbd, self.ptd], writes=[self.modd])
                s.op("dve", lambda e: e.scalar_tensor_tensor(out=self.amod[:, i * 16:i * 16 + 8], in0=self.modc(i, 1), scalar=1.0,
                                                             in1=self.P(f"nmg{i}"), op0=ALU.add, op1=ALU.mult),
                     reads=[self.modd, self.ptd], writes=[self.amodd])
                s.op("dve", lambda e: e.scalar_tensor_tensor(out=self.amod[:, i * 16 + 8:i * 16 + 16], in0=self.modc(i, 4), scalar=1.0,
                                                             in1=self.P(f"nfg{i}"), op0=ALU.add, op1=ALU.mult),
                     reads=[self.modd, self.ptd], writes=[self.amodd])
            s.op("dve", lambda e: e.memset(self.amod[:, NL * 16:NL * 16 + 8], 0.0), writes=[self.amodd])

    def norm_tmps(self, alloc):
        return {"sq": [(alloc(f"sq{i}", [128, 512], BF16), Dep()) for i in range(2)],
                "tmp": [(alloc(f"nt{i}", [128, 512], F32), Dep()) for i in range(2)],
                "rstd": (alloc("rstd", [128, 512], F32), Dep())}

    def norm_mod(self, nt, A, B, t0, nblk, out_fn):
        s = self.s
        sq, tmp = nt["sq"], nt["tmp"]
        rstd, rstdd = nt["rstd"]
        pb, pbd = self.bank(7)
        for tb in range(nblk):
            tsl = slice(t0 + tb * 512, t0 + (tb + 1) * 512)
            xb = (t0 // 512) + tb
            for c in range(DC):
                q, qd = sq[c % 2]
                s.op("act", lambda e: e.activation(out=q[:, :], in_=self.X[:, c, tsl], func=AF.Square, scale=1.0 / 32.0),
                     reads=[self.Xd[c][xb]], writes=[qd])
                s.mm(pb, self.onesb[:, :], q[:, :], c == 0, c == DC - 1, reads=[qd, self.cbd], out_dep=pbd, inc=True)
            s.op("act", lambda e: e.activation(out=rstd[:, :], in_=pb, func=AF.Sqrt, bias=self.epsc[:, 0:1]),
                 reads=[pbd], writes=[rstdd])
            s.op("dve", lambda e: e.reciprocal(out=rstd[:, :], in_=rstd[:, :]), reads=[rstdd], writes=[rstdd])
            for c in range(DC):
                t, td = tmp[c % 2]
                s.op("dve", lambda e: e.tensor_tensor(out=t[:, :], in0=self.X[:, c, tsl], in1=rstd[:, :], op=ALU.mult),
                     reads=[self.Xd[c][xb], rstdd], writes=[td])
                out_fn(c, tb, t, td, A[:, c:c + 1], B[:, c:c + 1])

    def ffn_expert(self, hT, hTd, act, actd, gu_stream, wd_stream, gf, t0, sgb, cb=None):
        s = self.s
        k = 0
        for g in range(FC // 2):
            wt, wdep = gu_stream.get()
            for j in range(2):
                fc = g * 2 + j
                for sb in range(2):
                    bg, bgd = self.bank(0 + (k % 2))
                    bu, bud = self.bank(2 + (k % 2))
                    tsl = slice(sb * 512, (sb + 1) * 512)
                    for kc in range(DC):
                        s.mm(bg, wt[:, kc, 0, j * 128:(j + 1) * 128], hT[:, kc, tsl], kc == 0, kc == DC - 1,
                             reads=[wdep, hTd[sb]], out_dep=bgd)
                    for kc in range(DC):
                        s.mm(bu, wt[:, kc, 1, j * 128:(j + 1) * 128], hT[:, kc, tsl], kc == 0, kc == DC - 1,
                             reads=[wdep, hTd[sb]], out_dep=bud)
                    sg, sgd = sgb[k % 2]
                    s.op("act", lambda e: e.activation(out=sg[:, :], in_=bg, func=AF.Silu), reads=[bgd], writes=[sgd])
                    if cb is not None:
                        cbt, cbdep = cb[sb]
                        s.op("pool", lambda e: e.tensor_tensor(out=sg[:, :], in0=sg[:, :], in1=cbt[:, :], op=ALU.mult),
                             reads=[cbdep, sgd], writes=[sgd])
                    s.op("dve", lambda e: e.tensor_tensor(out=act[:, fc, tsl], in0=sg[:, :], in1=bu, op=ALU.mult),
                         reads=[sgd, bud], writes=[actd[fc][sb]])
                    k += 1
        for dc in range(DC):
            wdt, wddep = wd_stream.get()
            for sb in range(2):
                bo, bod = self.bank(4 + ((dc * 2 + sb) % 2))
                tsl = slice(sb * 512, (sb + 1) * 512)
                for fc in range(FC):
                    s.mm(bo, wdt[:, fc, :], act[:, fc, tsl], fc == 0, fc == FC - 1,
                         reads=[wddep, actd[fc][sb]], out_dep=bod)
                xsl = slice(t0 + sb * 512, t0 + (sb + 1) * 512)
                xb = (t0 // 512) + sb
                s.op("dve", lambda e: e.scalar_tensor_tensor(out=self.X[:, dc, xsl], in0=bo, scalar=gf[:, dc:dc + 1],
                                                             in1=self.X[:, dc, xsl], op0=ALU.mult, op1=ALU.add),
                     reads=[bod, self.modd, self.Xd[dc][xb]], writes=[self.Xd[dc][xb]])

    def ffn_layer(self, i):
        s, d = self.s, self.d
        moe = (i % 2 == 1)
        li = i // 2
        A = self.amod[:, i * 16 + 8:i * 16 + 16]
        B = self.modc(i, 3)
        gf = self.modc(i, 5)
        with self.phase() as alloc:
            hT = alloc("f_hT", [128, DC, 1024], BF16)
            hTd = [Dep(), Dep()]
            act_off = self.sb_ptr
            act = alloc("f_act", [128, FC, 1024], BF16)
            nt = self.norm_tmps(alloc)
            actd = [[Dep(), Dep()] for _ in range(FC)]
            gub = [(alloc(f"f_gu{k}", [128, DC, 2, 256], BF16), Dep()) for k in range(3)]
            wdb = [(alloc(f"f_wd{k}", [128, FC, 128], BF16), Dep()) for k in range(2)]
            sgb = [(alloc(f"f_sg{k}", [128, 512], F32), Dep()) for k in range(2)]
            experts = list(range(NE)) if moe else [None]

            def wgu_ap(e):
                return d["moe_w_gate_up"][li, e] if moe else d["ffn_w_gate_up"][li]

            def wd_ap(e):
                return d["moe_w_down"][li, e] if moe else d["ffn_w_down"][li]

            def load_gu(it, t, dep):
                e, g = it
                src = wgu_ap(e).rearrange("(kc p) n -> p kc n", p=128)
                s.dma("pool", t[:, :, 0, :], src[:, :, g * 256:(g + 1) * 256], writes=[dep])
                s.dma("pool", t[:, :, 1, :], src[:, :, DFF + g * 256:DFF + (g + 1) * 256], writes=[dep])

            def load_wd(it, t, dep):
                e, dc = it
                src = wd_ap(e).rearrange("(fc p) n -> p fc n", p=128)
                s.dma("pool", t[:, :, :], src[:, :, dc * 128:(dc + 1) * 128], writes=[dep])

            gu_items = [(e, g) for _ in range(2) for e in experts for g in range(FC // 2)]
            wd_items = [(e, dc) for _ in range(2) for e in experts for dc in range(DC)]
            gu_stream = Stream(gub, gu_items, load_gu)
            wd_stream = Stream(wdb, wd_items, load_wd)
            if moe:
                h32 = self.salloc("f_h32", [128, DC, 1024], F32, at=act_off)
                h32d = Dep()
                combT = alloc("f_combT", [8, 1024], F32)
                combTd = Dep()
                sel8 = alloc("f_sel8", [8, 8 * 128], F32)
                sel8d = Dep()
                st8 = self.ccols["sel8"][0]
                s.dma("sp", sel8[:, :], d["ctab"][0:8, st8:st8 + 1024], writes=[sel8d])
                cbb = [(alloc(f"f_cb{k}", [128, 512], F32), Dep()) for k in range(4)]
                rt = {nm: (alloc(f"f_rt_{nm}", [128, 8, 8], F32), Dep()) for nm in ("lg", "z", "m", "z2", "ez")}
                rs = {nm: (alloc(f"f_rs_{nm}", [128, 8], F32), Dep()) for nm in ("m1", "m2", "ss")}
            for ps_ in range(2):
                t0 = ps_ * 1024

                def out_fn(c, tb, t, td, Ac, Bc):
                    s.op("act", lambda e: e.activation(out=hT[:, c, tb * 512:(tb + 1) * 512], in_=t[:, :], func=AF.Identity,
                                                       bias=Bc, scale=Ac),
                         reads=[td, self.amodd, self.modd], writes=[hTd[tb]])
                    if moe:
                        s.op("act", lambda e: e.activation(out=h32[:, c, tb * 512:(tb + 1) * 512], in_=t[:, :], func=AF.Identity, bias=Bc, scale=Ac),
                             reads=[td, self.amodd, self.modd], writes=[h32d])
                if moe:
                    s.barrier()
                self.norm_mod(nt, A, B, t0, 2, out_fn)
                if moe:
                    self.router(li, h32, h32d, combT, combTd, rt, rs)
                    s.barrier()
                for e in experts:
                    cb = None
                    if moe:
                        cb = []
                        for sb in range(2):
                            cbt, cbdep = cbb[(e * 2 + sb) % 4]
                            pb, pbd = self.bank(6)
                            s.mm(pb, sel8[0:8, e * 128:(e + 1) * 128], combT[0:8, sb * 512:(sb + 1) * 512], True, True,
                                 reads=[sel8d, combTd], out_dep=pbd)
                            s.op("act", lambda e_: e_.activation(out=cbt[:, :], in_=pb, func=AF.Copy), reads=[pbd], writes=[cbdep])
                            cb.append((cbt, cbdep))
                    self.ffn_expert(hT, hTd, act, actd, gu_stream, wd_stream, gf, t0, sgb, cb)

    def router(self, li, h32, h32d, combT, combTd, rt, rs):
        s = self.s
        G = 8
        pb, pbd = self.bank(6)
        mwr = self.P(f"mwr{li}")
        lg, lgd = rt["lg"]; z, zd = rt["z"]; m, md = rt["m"]; z2, z2d = rt["z2"]; ez, ezd = rt["ez"]
        m1, m1d = rs["m1"]; m2, m2d = rs["m2"]; ss, ssd = rs["ss"]
        for g in range(G):
            for c in range(DC):
                s.mm(pb[:, g * 8:(g + 1) * 8], h32[:, c, g * 128:(g + 1) * 128], mwr[:, c * 8:(c + 1) * 8], c == 0, c == DC - 1,
                     reads=[h32d, self.ptd], out_dep=pbd)
        bc = lambda t: t[:, :].unsqueeze(2).broadcast_to([128, G, 8])
        pv = pb[:, 0:G * 8].rearrange("p (g e) -> p g e", e=8)
        s.op("dve", lambda e: e.tensor_tensor(out=lg[:, :, :], in0=pv, in1=self.P(f"mbr{li}").unsqueeze(1).broadcast_to([128, G, 8]), op=ALU.add),
             reads=[pbd, self.ptd], writes=[lgd])
        s.op("dve", lambda e: e.reduce_max(out=m1[:, :], in_=lg[:, :, :], axis=AX.X), reads=[lgd], writes=[m1d])
        s.op("dve", lambda e: e.tensor_tensor(out=z[:, :, :], in0=lg[:, :, :], in1=bc(m1), op=ALU.subtract), reads=[lgd, m1d], writes=[zd])
        s.op("dve", lambda e: e.tensor_single_scalar(out=m[:, :, :], in_=z[:, :, :], scalar=0.0, op=ALU.is_ge), reads=[zd], writes=[md])
        s.op("dve", lambda e: e.scalar_tensor_tensor(out=z2[:, :, :], in0=m[:, :, :], scalar=-1e30, in1=z[:, :, :], op0=ALU.mult, op1=ALU.add),
             reads=[md, zd], writes=[z2d])
        s.op("dve", lambda e: e.reduce_max(out=m2[:, :], in_=z2[:, :, :], axis=AX.X), reads=[z2d], writes=[m2d])
        s.op("dve", lambda e: e.tensor_tensor(out=m[:, :, :], in0=z[:, :, :], in1=bc(m2), op=ALU.is_ge), reads=[zd, m2d, md], writes=[md])
        s.op("act", lambda e: e.activation(out=ez[:, :, :], in_=z[:, :, :], func=AF.Exp), reads=[zd], writes=[ezd])
        s.op("dve", lambda e: e.tensor_tensor(out=ez[:, :, :], in0=ez[:, :, :], in1=m[:, :, :], op=ALU.mult), reads=[ezd, md], writes=[ezd])
        s.op("dve", lambda e: e.reduce_sum(out=ss[:, :], in_=ez[:, :, :], axis=AX.X), reads=[ezd], writes=[ssd])
        s.op("dve", lambda e: e.reciprocal(out=ss[:, :], in_=ss[:, :]), reads=[ssd], writes=[ssd])
        s.op("dve", lambda e: e.tensor_tensor(out=ez[:, :, :], in0=ez[:, :, :], in1=bc(ss), op=ALU.mult), reads=[ezd, ssd], writes=[ezd])
        for half in range(2):
            pt_, ptd_ = self.bank(7 if half == 0 else 5)
            for gg in range(4):
                g = half * 4 + gg
                s.mm(pt_[0:8, gg * 128:(gg + 1) * 128], ez[:, g, :], self.C("ident"), True, gg == 3, reads=[ezd, self.ctd], out_dep=ptd_)
            s.op("dve", lambda e: e.tensor_copy(out=combT[0:8, half * 512:(half + 1) * 512], in_=pt_[0:8, 0:512]), reads=[ptd_], writes=[combTd])

    def final(self, do_norm=True):
        s, d = self.s, self.d
        ov = d["outT"].rearrange("(c p) s -> p c s", p=128)
        with self.phase() as alloc:
            if not do_norm:
                for c in range(DC):
                    s.dma("sp", ov[:, c, :], self.X[:, c, :], reads=self.Xd[c])
                return
            ob = [(alloc(f"ob{k}", [128, 512], F32), Dep()) for k in range(3)]
            cnt = [0]

            def out_fn(c, tb, t, td, Ac, Bc):
                o, od = ob[cnt[0] % 3]
                cnt[0] += 1
                s.op("act", lambda e: e.activation(out=o[:, :], in_=t[:, :], func=AF.Identity, bias=Bc, scale=Ac),
                     reads=[td, self.amodd, self.ptd], writes=[od])
                s.dma("sp", ov[:, c, tb * 512:(tb + 1) * 512], o[:, :], reads=[od])
            self.norm_mod(self.norm_tmps(alloc), self.P("fng"), self.amod[:, NL * 16:NL * 16 + 8], 0, 4, out_fn)

    def run_plan(self, plan, final_norm=True):
        layers = sorted({i for _, i in plan})
        self.prelude(layers)
        for kind, i in plan:
            if kind == "ffn":
                self.ffn_layer(i)
            elif kind == "mix":
                self.mix_layer(i)
        self.final(final_norm)
        self.s.barrier(["sp"])

    def mix_layer(self, i):
        kind = i % 3
        if kind == 0:
            self.rwkv_layer(i)
        elif kind == 1:
            self.mla_layer(i)
        else:
            self.fox_layer(i)


FULL_PLAN = [(k, i) for i in range(NL) for k in ("mix", "ffn")]


def build(pcols, npcols, plan=None, final_norm=True):
    plan = FULL_PLAN if plan is None else plan
    p = Prog(pcols, npcols)
    p.run_plan(plan, final_norm)
    return p.nc


def make_in_maps(inp, x_override=None):
    ctab = _CT.build()
    maps = []
    pcols = None
    wts = {k: np.ascontiguousarray(np.asarray(inp[k], np.float32)) for k in W_SHAPES}
    for b in range(8):
        pt = param_table(inp, b)
        pcols = pt.cols
        x = np.asarray(inp["x"][b] if x_override is None else x_override[b], np.float32)
        m = {"xT": np.ascontiguousarray(x.T),
             "pos": np.ascontiguousarray(np.asarray(inp["positions"][b], np.int32).reshape(1, S)),
             "ptab": pt.build(), "ctab": ctab}
        m.update(wts)
        maps.append(m)
    return maps, pcols, maps[0]["ptab"].shape[1]


def kernel(**inputs):
    maps, pcols, npc = make_in_maps(inputs)
    nc = build(pcols, npc)
    res = run_bass_kernel_spmd(nc, maps, core_ids=list(range(8)))
    out = np.stack([np.ascontiguousarray(r["outT"].T) for r in res.results], axis=0)
    return out.astype(np.float32)


def _attn_methods():
    def out_proj(self, alloc, oT, oTd, w_ap, gm):
        s = self.s
        wob = [(alloc(f"wo{k}", [128, DC, 128], BF16), Dep()) for k in range(2)]
        src = w_ap.rearrange("(kc p) n -> p kc n", p=128)

        def load(m, t, dep):
            s.dma("pool", t[:, :, :], src[:, :, m * 128:(m + 1) * 128], writes=[dep])
        st = Stream(wob, list(range(DC)), load)
        k = 0
        for m in range(DC):
            wt, wd = st.get()
            for tb in range(4):
                pb, pbd = self.bank(4 + k % 2)
                k += 1
                tsl = slice(tb * 512, (tb + 1) * 512)
                for kc in range(DC):
                    s.mm(pb, wt[:, kc, :], oT[:, kc, tsl], kc == 0, kc == DC - 1, reads=[wd, oTd[tb]], out_dep=pbd)
                s.op("dve", lambda e: e.scalar_tensor_tensor(out=self.X[:, m, tsl], in0=pb, scalar=gm[:, m:m + 1],
                                                             in1=self.X[:, m, tsl], op0=ALU.mult, op1=ALU.add),
                     reads=[pbd, self.modd, self.Xd[m][tb]], writes=[self.Xd[m][tb]])

    def attn_bufs(self, alloc, nP=1, nPT=1):
        return {"Pb": [(alloc(f"a_P{k}", [128, S], BF16), Dep()) for k in range(nP)] * (2 // nP),
                "PT": [(alloc(f"a_PT{k}", [128, 16, 128], BF16), Dep()) for k in range(nPT)] * (2 // nPT),
                "dg": [(alloc(f"a_dg{k}", [128, 128], BF16), Dep()) for k in range(2)],
                "st": [(alloc(f"a_st{k}", [128, 8], F32), Dep()) for k in range(2)],
                "raw": [(alloc(f"a_raw{k}", [128, S], F32), Dep()) for k in range(2)]}

    def attention_pair(self, ab, score_fn, Vt, Vtd, maskb, o_evac):
        s = self.s
        items = [(qb, par) for qb in range(16) for par in range(2)]
        segrr = [0]

        seg_banks = {}

        def a_pe(it):
            qb, par = items[it]
            nk = (qb + 1) * 128
            nseg = (nk + 511) // 512
            seg_banks[it] = []
            for sg in range(nseg):
                k0 = sg * 512
                n = min(512, nk - k0)
                bk = segrr[0]
                segrr[0] = (segrr[0] + 1) % 4
                pb, pbd = self.bank(bk)
                score_fn(par, qb, k0, n, pb[:, 0:n], pbd)
                if sg == nseg - 1:
                    s.mm(pb[:, n - 128:n], self.identb[:, :], maskb[:, :], False, True, reads=[self.cbd], out_dep=pbd)
                else:
                    pbd.w = (("e", "pe"), s.cnt["pe"])
                    pbd.r = {}
                seg_banks[it].append((pb, pbd, k0, n))

        def a_ev(it):
            raw, rawd = ab["raw"][it % 2]
            st, std = ab["st"][it % 2]
            for sg, (pb, pbd, k0, n) in enumerate(seg_banks.pop(it)):
                s.op("act", lambda e: e.activation(out=raw[:, k0:k0 + n], in_=pb[:, 0:n], func=AF.Copy), reads=[pbd], writes=[rawd])
                s.op("dve", lambda e: e.reduce_max(out=st[:, sg:sg + 1], in_=raw[:, k0:k0 + n], axis=AX.X), reads=[rawd], writes=[std])

        def b_soft(it):
            qb, par = items[it]
            nk = (qb + 1) * 128
            nseg = (nk + 511) // 512
            raw, rawd = ab["raw"][it % 2]
            st, std = ab["st"][it % 2]
            Pb, Pbd = ab["Pb"][it % 2]
            dg, dgd = ab["dg"][it % 2]
            if nseg > 1:
                s.op("dve", lambda e: e.reduce_max(out=st[:, 4:5], in_=st[:, 0:nseg], axis=AX.X), reads=[std], writes=[std])
                mcol = st[:, 4:5]
            else:
                mcol = st[:, 0:1]
            s.op("dve", lambda e: e.tensor_scalar(out=st[:, 5:6], in0=mcol, scalar1=-1.0, scalar2=None, op0=ALU.mult), reads=[std], writes=[std])
            s.op("act", lambda e: e.activation(out=Pb[:, 0:nk], in_=raw[:, 0:nk], func=AF.Exp, bias=st[:, 5:6], accum_out=st[:, 6:7]),
                 reads=[rawd, std], writes=[Pbd, std])
            s.op("dve", lambda e: e.reciprocal(out=st[:, 7:8], in_=st[:, 6:7]), reads=[std], writes=[std])
            s.op("dve", lambda e: e.tensor_scalar(out=dg[:, :], in0=self.C("ident"), scalar1=st[:, 7:8], scalar2=None, op0=ALU.mult),
                 reads=[std, self.ctd], writes=[dgd])

        def b_pe(it):
            qb, par = items[it]
            nkb = qb + 1
            Pb, Pbd = ab["Pb"][it % 2]
            PT, PTd = ab["PT"][it % 2]
            dg, dgd = ab["dg"][it % 2]
            for g4 in range((nkb + 3) // 4):
                pb, pbd = self.bank(4 + g4 % 2)
                nj = min(4, nkb - g4 * 4)
                for j in range(nj):
                    kb = g4 * 4 + j
                    s.mm(pb[:, j * 128:(j + 1) * 128], Pb[:, kb * 128:(kb + 1) * 128], dg[:, :], True, j == nj - 1,
                         reads=[Pbd, dgd], out_dep=pbd)
                src = pb[:, 0:nj * 128]
                dst = PT[:, g4 * 4:g4 * 4 + nj, :]
                s.op("dve", lambda e: e.tensor_copy(out=dst, in_=src), reads=[pbd], writes=[PTd])
            ob, obd = self.bank(6 + (it % 2))
            for kb in range(nkb):
                s.mm(ob[:, 0:128], Vt[:, kb, :], PT[:, kb, :], kb == 0, kb == nkb - 1, reads=[Vtd, PTd], out_dep=obd)
            o_evac(par, qb, ob[par * 64:(par + 1) * 64, 0:128], obd)

        a_pe(0)
        a_ev(0)
        for it in range(len(items)):
            if it + 1 < len(items):
                a_pe(it + 1)
            b_soft(it)
            if it + 1 < len(items):
                a_ev(it + 1)
            b_pe(it)

    def rope_tables(self, cosT, sinT, tabd):
        s, d = self.s, self.d
        with self.phase() as alloc:
            posi = alloc("r_posi", [64, S], I32)
            ang = alloc("r_ang", [64, S], F32)
            y = alloc("r_y", [64, S], F32)
            yi = alloc("r_yi", [64, S], I32)
            r = alloc("r_r", [64, S], F32)
            mk = alloc("r_mk", [64, S], F32)
            dd = Dep()
            s.dma("sp", posi[:, :], d["pos"][0:1, :].partition_broadcast(64), writes=[dd])
            s.op("dve", lambda e: e.tensor_copy(out=ang[:, :], in_=posi[:, :]), reads=[dd], writes=[dd])
            s.op("dve", lambda e: e.tensor_scalar(out=ang[:, :], in0=ang[:, :], scalar1=self.C("invf", rows=(0, 64)), scalar2=None,
                                                  op0=ALU.mult), reads=[dd, self.ctd], writes=[dd])
            TWO_PI = 2.0 * math.pi
            for tab, shift in ((sinT, 0.0), (cosT, math.pi / 2.0)):
                s.op("dve", lambda e: e.tensor_scalar(out=y[:, :], in0=ang[:, :], scalar1=shift, scalar2=1.0 / TWO_PI,
                                                      op0=ALU.add, op1=ALU.mult), reads=[dd], writes=[dd])
                s.op("dve", lambda e: e.tensor_copy(out=yi[:, :], in_=y[:, :]), reads=[dd], writes=[dd])
                s.op("dve", lambda e: e.tensor_copy(out=y[:, :], in_=yi[:, :]), reads=[dd], writes=[dd])
                s.op("dve", lambda e: e.scalar_tensor_tensor(out=r[:, :], in0=y[:, :], scalar=-TWO_PI, in1=ang[:, :],
                                                             op0=ALU.mult, op1=ALU.add), reads=[dd], writes=[dd])
                if shift != 0.0:
                    s.op("dve", lambda e: e.tensor_scalar(out=r[:, :], in0=r[:, :], scalar1=shift, scalar2=None, op0=ALU.add),
                         reads=[dd], writes=[dd])
                s.op("dve", lambda e: e.tensor_single_scalar(out=mk[:, :], in_=r[:, :], scalar=math.pi, op=ALU.is_gt), reads=[dd], writes=[dd])
                s.op("dve", lambda e: e.scalar_tensor_tensor(out=r[:, :], in0=mk[:, :], scalar=-TWO_PI, in1=r[:, :],
                                                             op0=ALU.mult, op1=ALU.add), reads=[dd], writes=[dd])
                s.op("dve", lambda e: e.tensor_single_scalar(out=mk[:, :], in_=r[:, :], scalar=-math.pi, op=ALU.is_lt), reads=[dd], writes=[dd])
                s.op("dve", lambda e: e.scalar_tensor_tensor(out=r[:, :], in0=mk[:, :], scalar=TWO_PI, in1=r[:, :],
                                                             op0=ALU.mult, op1=ALU.add), reads=[dd], writes=[dd])
                s.op("dve", lambda e: e.tensor_scalar(out=r[:, :], in0=r[:, :], scalar1=3.14159, scalar2=-3.14159, op0=ALU.min, op1=ALU.max),
                     reads=[dd], writes=[dd])
                s.op("act", lambda e: e.activation(out=tab[:, :], in_=r[:, :], func=AF.Sin), reads=[dd], writes=[tabd])

    def mla_layer(self, i):
        s, d = self.s, self.d
        A = self.amod[:, i * 16:i * 16 + 8]
        B = self.modc(i, 0)
        gm = self.modc(i, 2)
        SC = float((64 + 32) ** -0.5)
        wdown = d["mla_w_down"][0].rearrange("(kc p) n -> p kc n", p=128)
        wuq = d["mla_w_uq"][0].rearrange("(kc p) (h dd) -> p kc h dd", p=128, dd=96)
        wukv = d["mla_w_ukv"][0].rearrange("(kc p) (h dd) -> p kc h dd", p=128, dd=128)
        with self.phase() as alloc:
            cq = alloc("m_cq", [128, 6, S], BF16)
            ckv = alloc("m_ckv", [128, 2, S], BF16)
            kr = alloc("m_kr", [64, S], BF16)
            cqd = [Dep() for _ in range(4)]
            ckvd = [Dep() for _ in range(4)]
            krd = [Dep() for _ in range(4)]
            cosT = alloc("m_cos", [64, S], F32)
            sinT = alloc("m_sin", [64, S], F32)
            tabd = Dep()
            self.rope_tables(cosT, sinT, tabd)
            with self.phase() as a1:
                hT = a1("m_hT", [128, DC, 1024], BF16)
                hTd = [Dep(), Dep()]
                nt = self.norm_tmps(a1)
                wdn = a1("m_wdn", [128, DC, 1024], BF16)
                wdnd = Dep()
                wkr = a1("m_wkr", [128, DC, 128], BF16)
                wkrd = Dep()
                raw = a1("m_raw", [128, 8, 256], F32)
                rawd = [Dep() for _ in range(8)]
                sqt = [(a1(f"m_sq{k}", [128, 256], F32), Dep()) for k in range(2)]
                rq = a1("m_rq", [128, 256], F32)
                rkv = a1("m_rkv", [128, 256], F32)
                rqd, rkvd = Dep(), Dep()
                t1 = a1("m_t1", [64, 256], F32)
                t2 = a1("m_t2", [64, 256], F32)
                t1d, t2d = Dep(), Dep()
                for h2 in range(2):
                    s.dma("pool", wdn[:, :, h2 * 512:(h2 + 1) * 512], wdown[:, :, h2 * 512:(h2 + 1) * 512], writes=[wdnd])
                s.dma("pool", wkr[:, :, 0:32], wdown[:, :, 1024:1056], writes=[wkrd])
                s.dma("pool", wkr[:, :, 32:64], wdown[:, :, 1024:1056], writes=[wkrd])
                s.op("dve", lambda e: e.tensor_scalar(out=wkr[:, :, 64:80], in0=wkr[:, :, 16:32], scalar1=-1.0, scalar2=None, op0=ALU.mult),
                     reads=[wkrd], writes=[wkrd])
                s.op("dve", lambda e: e.tensor_copy(out=wkr[:, :, 80:96], in_=wkr[:, :, 0:16]), reads=[wkrd], writes=[wkrd])
                s.op("dve", lambda e: e.tensor_copy(out=wkr[:, :, 96:128], in_=wkr[:, :, 64:96]), reads=[wkrd], writes=[wkrd])
                for ps_ in range(2):
                    def out_fn(c, tb, t, td, Ac, Bc):
                        s.op("act", lambda e: e.activation(out=hT[:, c, tb * 512:(tb + 1) * 512], in_=t[:, :], func=AF.Identity,
                                                           bias=Bc, scale=Ac),
                             reads=[td, self.amodd, self.modd], writes=[hTd[tb]])
                    self.norm_mod(nt, A, B, ps_ * 1024, 2, out_fn)
                    for blk in range(4):
                        tok0 = ps_ * 1024 + blk * 256
                        tsl = slice(tok0, tok0 + 256)
                        hsl = slice(blk * 256, (blk + 1) * 256)
                        hd = hTd[blk // 2]
                        dq = tok0 // 512
                        bq, bqd = self.bank(2)
                        bkv, bkvd = self.bank(3)
                        for m in range(8):
                            pb, pbd = self.bank(m % 2)
                            for kc in range(DC):
                                s.mm(pb[:, 0:256], wdn[:, kc, m * 128:(m + 1) * 128], hT[:, kc, hsl], kc == 0, kc == DC - 1,
                                     reads=[wdnd, hd], out_dep=pbd)
                            s.op("act", lambda e: e.activation(out=raw[:, m, :], in_=pb[:, 0:256], func=AF.Copy), reads=[pbd], writes=[rawd[m]])
                            q, qd = sqt[m % 2]
                            s.op("pool", lambda e: e.tensor_tensor(out=q[:, :], in0=raw[:, m, :], in1=raw[:, m, :], op=ALU.mult),
                                 reads=[rawd[m]], writes=[qd])
                            if m < 6:
                                s.mm(bq[:, 0:256], self.C("ones"), q[:, :], m == 0, m == 5, reads=[qd, self.ctd], out_dep=bqd, inc=True)
                            else:
                                s.mm(bkv[:, 0:256], self.C("ones"), q[:, :], m == 6, m == 7, reads=[qd, self.ctd], out_dep=bkvd, inc=True)
                        s.op("act", lambda e: e.activation(out=rq[:, :], in_=bq[:, 0:256], func=AF.Sqrt, bias=self.epsc[:, 0:1], scale=1.0 / 768.0),
                             reads=[bqd], writes=[rqd])
                        s.op("dve", lambda e: e.reciprocal(out=rq[:, :], in_=rq[:, :]), reads=[rqd], writes=[rqd])
                        s.op("act", lambda e: e.activation(out=rkv[:, :], in_=bkv[:, 0:256], func=AF.Sqrt, bias=self.epsc[:, 0:1], scale=1.0 / 256.0),
                             reads=[bkvd], writes=[rkvd])
                        s.op("dve", lambda e: e.reciprocal(out=rkv[:, :], in_=rkv[:, :]), reads=[rkvd], writes=[rkvd])
                        for m in range(8):
                            if m < 6:
                                s.op("dve", lambda e: e.scalar_tensor_tensor(out=cq[:, m, tsl], in0=raw[:, m, :], scalar=self.P("mqg", m, m + 1),
                                                                             in1=rq[:, :], op0=ALU.mult, op1=ALU.mult),
                                     reads=[rawd[m], rqd, self.ptd], writes=[cqd[dq]])
                            else:
                                s.op("dve", lambda e: e.scalar_tensor_tensor(out=ckv[:, m - 6, tsl], in0=raw[:, m, :], scalar=self.P("mkvg", m - 6, m - 5),
                                                                             in1=rkv[:, :], op0=ALU.mult, op1=ALU.mult),
                                     reads=[rawd[m], rkvd, self.ptd], writes=[ckvd[dq]])
                        bx, bxd = self.bank(4)
                        br, brd = self.bank(5)
                        for kc in range(DC):
                            s.mm(bx[0:64, 0:256], wkr[:, kc, 0:64], hT[:, kc, hsl], kc == 0, kc == DC - 1, reads=[wkrd, hd], out_dep=bxd)
                        for kc in range(DC):
                            s.mm(br[0:64, 0:256], wkr[:, kc, 64:128], hT[:, kc, hsl], kc == 0, kc == DC - 1, reads=[wkrd, hd], out_dep=brd)
                        s.op("dve", lambda e: e.tensor_tensor(out=t1[:, :], in0=bx[0:64, 0:256], in1=cosT[:, tsl], op=ALU.mult),
                             reads=[bxd, tabd], writes=[t1d])
                        s.op("dve", lambda e: e.tensor_tensor(out=t2[:, :], in0=br[0:64, 0:256], in1=sinT[:, tsl], op=ALU.mult),
                             reads=[brd, tabd], writes=[t2d])
                        s.op("pool", lambda e: e.tensor_tensor(out=kr[:, tsl], in0=t1[:, :], in1=t2[:, :], op=ALU.add),
                             reads=[t1d, t2d], writes=[krd[dq]])
            with self.phase() as a2:
                oT = a2("m_oT", [128, DC, S], BF16)
                oTd = [Dep() for _ in range(4)]
                with self.phase() as a3:
                    ab = self.attn_bufs(a3)
                    wqn = [(a3(f"m_wqn{k}", [128, 6, 128], BF16), Dep()) for k in range(1)]
                    wqr = [(a3(f"m_wqr{k}", [128, 6, 64], BF16), Dep()) for k in range(1)]
                    wqo = [(a3(f"m_wqo{k}", [128, 6, 64], BF16), Dep()) for k in range(1)]
                    wkn = [(a3(f"m_wkn{k}", [128, 2, 128], BF16), Dep()) for k in range(1)]
                    wv = [(a3(f"m_wv{k}", [128, 2, 128], BF16), Dep()) for k in range(1)]
                    qn = a3("m_qn", [128, S], BF16)
                    kn = a3("m_kn", [128, S], BF16)
                    qr = a3("m_qr", [64, S], BF16)
                    Vt = a3("m_Vt", [128, 16, 128], BF16)
                    qnd, knd, qrd, Vtd = Dep(), Dep(), Dep(), Dep()
                    t1 = a3("m_u1", [64, 512], F32)
                    t2 = a3("m_u2", [64, 512], F32)
                    t1d, t2d = Dep(), Dep()
                    for hp in range(8):
                        k2 = 0
                        (wqn_t, wqn_d), (wqr_t, wqr_d), (wqo_t, wqo_d) = wqn[k2], wqr[k2], wqo[k2]
                        (wkn_t, wkn_d), (wv_t, wv_d) = wkn[k2], wv[k2]
                        for hh in range(2):
                            s.dma("pool", wqn_t[:, :, hh * 64:(hh + 1) * 64], wuq[:, :, 2 * hp + hh, 0:64], writes=[wqn_d])
                            s.dma("pool", wqr_t[:, :, hh * 32:(hh + 1) * 32], wuq[:, :, 2 * hp + hh, 64:96], writes=[wqr_d])
                            s.dma("pool", wkn_t[:, :, hh * 64:(hh + 1) * 64], wukv[:, :, 2 * hp + hh, 0:64], writes=[wkn_d])
                            s.dma("pool", wv_t[:, :, hh * 64:(hh + 1) * 64], wukv[:, :, 2 * hp + hh, 64:128], writes=[wv_d])
                        wr4 = wqr_t[:, :, :].rearrange("p k (h e) -> p k h e", h=2)
                        wo4 = wqo_t[:, :, :].rearrange("p k (h e) -> p k h e", h=2)
                        s.op("dve", lambda e: e.tensor_scalar(out=wo4[:, :, :, 0:16], in0=wr4[:, :, :, 16:32], scalar1=-1.0, scalar2=None, op0=ALU.mult),
                             reads=[wqr_d], writes=[wqo_d])
                        s.op("dve", lambda e: e.tensor_copy(out=wo4[:, :, :, 16:32], in_=wr4[:, :, :, 0:16]), reads=[wqr_d], writes=[wqo_d])
                        for tb in range(4):
                            tsl = slice(tb * 512, (tb + 1) * 512)
                            pb, pbd = self.bank(4)
                            for kc in range(6):
                                s.mm(pb, wqn_t[:, kc, :], cq[:, kc, tsl], kc == 0, kc == 5, reads=[wqn_d, cqd[tb]], out_dep=pbd)
                            s.op("act", lambda e: e.activation(out=qn[:, tsl], in_=pb, func=AF.Copy, scale=SC), reads=[pbd], writes=[qnd])
                            pk, pkd = self.bank(5)
                            for kc in range(2):
                                s.mm(pk, wkn_t[:, kc, :], ckv[:, kc, tsl], kc == 0, kc == 1, reads=[wkn_d, ckvd[tb]], out_dep=pkd)
                            s.op("act", lambda e: e.activation(out=kn[:, tsl], in_=pk, func=AF.Copy), reads=[pkd], writes=[knd])
                            bx, bxd = self.bank(6)
                            br, brd = self.bank(7)
                            for kc in range(6):
                                s.mm(bx[0:64, :], wqr_t[:, kc, :], cq[:, kc, tsl], kc == 0, kc == 5, reads=[wqr_d, cqd[tb]], out_dep=bxd)
                            for kc in range(6):
                                s.mm(br[0:64, :], wqo_t[:, kc, :], cq[:, kc, tsl], kc == 0, kc == 5, reads=[wqo_d, cqd[tb]], out_dep=brd)
                            s.op("dve", lambda e: e.scalar_tensor_tensor(out=t1[:, :], in0=bx[0:64, :], scalar=SC, in1=cosT[:, tsl],
                                                                         op0=ALU.mult, op1=ALU.mult), reads=[bxd, tabd], writes=[t1d])
                            s.op("dve", lambda e: e.scalar_tensor_tensor(out=t2[:, :], in0=br[0:64, :], scalar=SC, in1=sinT[:, tsl],
                                                                         op0=ALU.mult, op1=ALU.mult), reads=[brd, tabd], writes=[t2d])
                            s.op("pool", lambda e: e.tensor_tensor(out=qr[:, tsl], in0=t1[:, :], in1=t2[:, :], op=ALU.add),
                                 reads=[t1d, t2d], writes=[qrd])
                        for g in range(4):
                            pb, pbd = self.bank(4 + g % 2)
                            for j in range(4):
                                t16 = g * 4 + j
                                for kc in range(2):
                                    s.mm(pb[:, j * 128:(j + 1) * 128], ckv[:, kc, t16 * 128:(t16 + 1) * 128], wv_t[:, kc, :],
                                         kc == 0, (kc == 1 and j == 3), reads=[wv_d, ckvd[t16 // 4]], out_dep=pbd)
                            s.op("act", lambda e: e.activation(out=Vt[:, g * 4:(g + 1) * 4, :], in_=pb, func=AF.Copy), reads=[pbd], writes=[Vtd])

                        def score_fn(par, qb, k0, n, out_ap, od):
                            ps = slice(par * 64, (par + 1) * 64)
                            rs = slice(par * 32, (par + 1) * 32)
                            qsl = slice(qb * 128, (qb + 1) * 128)
                            s.mm(out_ap, qn[ps, qsl], kn[ps, k0:k0 + n], True, False, reads=[qnd, knd], out_dep=od)
                            s.mm(out_ap, qr[rs, qsl], kr[rs, k0:k0 + n], False, False, reads=[qrd] + krd, out_dep=od, inc=True)

                        def o_evac(par, qb, src, srcd):
                            ps = slice(par * 64, (par + 1) * 64)
                            s.op("act", lambda e: e.activation(out=oT[ps, hp, qb * 128:(qb + 1) * 128], in_=src, func=AF.Copy),
                                 reads=[srcd], writes=[oTd[qb // 4]])
                        self.attention_pair(ab, score_fn, Vt, Vtd, self.mmaskb, o_evac)
                self.out_proj(a2, oT, oTd, d["mla_w_o"][0], gm)

    Prog.out_proj = out_proj
    Prog.attn_bufs = attn_bufs
    Prog.attention_pair = attention_pair
    Prog.rope_tables = rope_tables
    Prog.mla_layer = mla_layer


_attn_methods()


def _fox_methods():
    def fox_layer(self, i):
        s, d = self.s, self.d
        A = self.amod[:, i * 16:i * 16 + 8]
        B = self.modc(i, 0)
        gm = self.modc(i, 2)
        FSC = float(64 ** -0.5)
        win = d["fox_w_in"][0].rearrange("(kc p) n -> p kc n", p=128)
        with self.phase() as alloc:
            hT = alloc("x_hT", [128, DC, S], BF16)
            hTd = [Dep() for _ in range(4)]
            oT = alloc("x_oT", [128, DC, S], BF16)
            oTd = [Dep() for _ in range(4)]
            nFh = alloc("x_nFh", [16, S], BF16)
            nFl = alloc("x_nFl", [16, S], BF16)
            negFd = Dep()
            gsc = alloc("x_gsc", [128, 2], F32)
            gscd = Dep()
            s.op("dve", lambda e: e.tensor_scalar(out=gsc[:, 0:1], in0=self.P("fqg"), scalar1=FSC, scalar2=None, op0=ALU.mult),
                 reads=[self.ptd], writes=[gscd])
            s.op("dve", lambda e: e.tensor_copy(out=gsc[:, 1:2], in_=self.P("fkg")), reads=[self.ptd], writes=[gscd])
            with self.phase() as a1:
                nt = self.norm_tmps(a1)

                def out_fn(c, tb, t, td, Ac, Bc):
                    s.op("act", lambda e: e.activation(out=hT[:, c, tb * 512:(tb + 1) * 512], in_=t[:, :], func=AF.Identity,
                                                       bias=Bc, scale=Ac),
                         reads=[td, self.amodd, self.modd], writes=[hTd[tb]])
                self.norm_mod(nt, A, B, 0, 4, out_fn)
                wf = a1("x_wf", [128, DC, 16], BF16)
                wfd = Dep()
                s.dma("pool", wf[:, :, :], win[:, :, 3072:3088], writes=[wfd])
                z = a1("x_z", [16, 512], F32)
                az = a1("x_az", [16, 512], F32)
                lf = a1("x_lf", [16, 512], F32)
                ones16 = a1("x_ones", [16, 512], F32)
                Fc = a1("x_F", [16, S], F32)
                zd = Dep()
                s.op("dve", lambda e: e.memset(ones16[:, :], 1.0), writes=[zd])
                for tb in range(4):
                    tsl = slice(tb * 512, (tb + 1) * 512)
                    pb, pbd = self.bank(4)
                    for kc in range(DC):
                        s.mm(pb[0:16, :], wf[:, kc, :], hT[:, kc, tsl], kc == 0, kc == DC - 1, reads=[wfd, hTd[tb]], out_dep=pbd)
                    s.op("act", lambda e: e.activation(out=z[:, :], in_=pb[0:16, :], func=AF.Identity, bias=self.P("fbf")[0:16, :]),
                         reads=[pbd, self.ptd, zd], writes=[zd])
                    s.op("act", lambda e: e.activation(out=az[:, :], in_=z[:, :], func=AF.Abs), reads=[zd], writes=[zd])
                    s.op("act", lambda e: e.activation(out=az[:, :], in_=az[:, :], func=AF.Exp, scale=-1.0), reads=[zd], writes=[zd])
                    s.op("act", lambda e: e.activation(out=az[:, :], in_=az[:, :], func=AF.Ln, bias=self.epsc[0:16, 2:3]), reads=[zd], writes=[zd])
                    s.op("dve", lambda e: e.tensor_scalar(out=lf[:, :], in0=z[:, :], scalar1=0.0, scalar2=None, op0=ALU.min), reads=[zd], writes=[zd])
                    s.op("dve", lambda e: e.tensor_tensor(out=lf[:, :], in0=lf[:, :], in1=az[:, :], op=ALU.subtract), reads=[zd], writes=[zd])
                    init = 0.0 if tb == 0 else Fc[:, tb * 512 - 1:tb * 512]
                    s.op("dve", lambda e: e.tensor_tensor_scan(Fc[:, tsl], ones16[:, :], lf[:, :], init, ALU.mult, ALU.add),
                         reads=[zd], writes=[zd])
                negF = a1("x_negF", [16, S], F32)
                nF32 = a1("x_nF32", [16, S], F32)
                s.op("dve", lambda e: e.tensor_scalar(out=negF[:, :], in0=Fc[:, :], scalar1=-1.0, scalar2=None, op0=ALU.mult),
                     reads=[zd], writes=[zd])
                s.op("dve", lambda e: e.tensor_copy(out=nFh[:, :], in_=negF[:, :]), reads=[zd], writes=[negFd])
                s.op("dve", lambda e: e.tensor_copy(out=nF32[:, :], in_=nFh[:, :]), reads=[negFd, zd], writes=[zd])
                s.op("dve", lambda e: e.tensor_tensor(out=nFl[:, :], in0=negF[:, :], in1=nF32[:, :], op=ALU.subtract), reads=[zd], writes=[negFd])
            with self.phase() as a3:
                ab = self.attn_bufs(a3)
                wq = [(a3(f"x_wq{k}", [128, DC, 128], BF16), Dep()) for k in range(1)]
                wk = [(a3(f"x_wk{k}", [128, DC, 128], BF16), Dep()) for k in range(1)]
                wv = [(a3(f"x_wv{k}", [128, DC, 128], BF16), Dep()) for k in range(1)]
                wg = [(a3(f"x_wg{k}", [128, DC, 128], BF16), Dep()) for k in range(1)]
                selp = [(a3(f"x_sel{k}", [16, 256], BF16), Dep()) for k in range(2)]
                qn = a3("x_qn", [128, S], BF16)
                kn = a3("x_kn", [128, S], BF16)
                og = a3("x_og", [128, S], BF16)
                Vt = a3("x_Vt", [128, 16, 128], BF16)
                qnd, knd, ogd, Vtd = Dep(), Dep(), Dep(), Dep()
                raw = [(a3(f"x_raw{k}", [128, 512], F32), Dep()) for k in range(2)]
                sq = [(a3(f"x_sq{k}", [128, 512], F32), Dep()) for k in range(2)]
                rs_ = [(a3(f"x_rs{k}", [128, 512], F32), Dep()) for k in range(2)]
                st16 = self.ccols["sel16"][0]
                for hp in range(8):
                    k2 = 0
                    (wq_t, wq_d), (wk_t, wk_d), (wv_t, wv_d), (wg_t, wg_d) = wq[k2], wk[k2], wv[k2], wg[k2]
                    sel_t, sel_d = selp[hp % 2]
                    s.dma("pool", wq_t[:, :, :], win[:, :, hp * 128:(hp + 1) * 128], writes=[wq_d])
                    s.dma("pool", wk_t[:, :, :], win[:, :, 1024 + hp * 128:1024 + (hp + 1) * 128], writes=[wk_d])
                    s.dma("pool", wv_t[:, :, :], win[:, :, 2048 + hp * 128:2048 + (hp + 1) * 128], writes=[wv_d])
                    s.dma("pool", wg_t[:, :, :], win[:, :, 3088 + hp * 128:3088 + (hp + 1) * 128], writes=[wg_d])
                    s.dma("pool", sel_t[:, :], d["ctab"][0:16, st16 + hp * 256:st16 + (hp + 1) * 256], writes=[sel_d])
                    for tb in range(4):
                        tsl = slice(tb * 512, (tb + 1) * 512)
                        for which, (w_t, w_d), dst, dstd, gcol in ((0, (wq_t, wq_d), qn, qnd, 0), (1, (wk_t, wk_d), kn, knd, 1)):
                            pb, pbd = self.bank(4 + which)
                            for kc in range(DC):
                                s.mm(pb, w_t[:, kc, :], hT[:, kc, tsl], kc == 0, kc == DC - 1, reads=[w_d, hTd[tb]], out_dep=pbd)
                            r_t, r_d = raw[which]
                            q_t, q_d = sq[which]
                            rr_t, rr_d = rs_[which]
                            s.op("act", lambda e: e.activation(out=r_t[:, :], in_=pb, func=AF.Copy), reads=[pbd], writes=[r_d])
                            s.op("pool", lambda e: e.tensor_tensor(out=q_t[:, :], in0=r_t[:, :], in1=r_t[:, :], op=ALU.mult), reads=[r_d], writes=[q_d])
                            p2, p2d = self.bank(6 + which)
                            s.mm(p2, self.C("bd"), q_t[:, :], True, True, reads=[q_d, self.ctd], out_dep=p2d)
                            s.op("act", lambda e: e.activation(out=rr_t[:, :], in_=p2, func=AF.Sqrt, bias=self.epsc[:, 0:1], scale=1.0 / 64.0),
                                 reads=[p2d], writes=[rr_d])
                            s.op("dve", lambda e: e.reciprocal(out=rr_t[:, :], in_=rr_t[:, :]), reads=[rr_d], writes=[rr_d])
                            s.op("dve", lambda e: e.scalar_tensor_tensor(out=dst[:, tsl], in0=r_t[:, :], scalar=gsc[:, gcol:gcol + 1],
                                                                         in1=rr_t[:, :], op0=ALU.mult, op1=ALU.mult),
                                 reads=[r_d, rr_d, gscd], writes=[dstd])
                        pg, pgd = self.bank(4)
                        for kc in range(DC):
                            s.mm(pg, wg_t[:, kc, :], hT[:, kc, tsl], kc == 0, kc == DC - 1, reads=[wg_d, hTd[tb]], out_dep=pgd)
                        s.op("act", lambda e: e.activation(out=og[:, tsl], in_=pg, func=AF.Sigmoid), reads=[pgd], writes=[ogd])
                    for g in range(4):
                        pb, pbd = self.bank(4 + g % 2)
                        for j in range(4):
                            t16 = g * 4 + j
                            for kc in range(DC):
                                s.mm(pb[:, j * 128:(j + 1) * 128], hT[:, kc, t16 * 128:(t16 + 1) * 128], wv_t[:, kc, :],
                                     kc == 0, (kc == DC - 1 and j == 3), reads=[wv_d, hTd[t16 // 4]], out_dep=pbd)
                        s.op("act", lambda e: e.activation(out=Vt[:, g * 4:(g + 1) * 4, :], in_=pb, func=AF.Copy), reads=[pbd], writes=[Vtd])

                    def score_fn(par, qb, k0, n, out_ap, od):
                        ps = slice(par * 64, (par + 1) * 64)
                        qsl = slice(qb * 128, (qb + 1) * 128)
                        s.mm(out_ap, qn[ps, qsl], kn[ps, k0:k0 + n], True, False, reads=[qnd, knd], out_dep=od)
                        s.mm(out_ap, sel_t[0:16, par * 128:(par + 1) * 128], nFh[0:16, k0:k0 + n], False, False,
                             reads=[sel_d, negFd], out_dep=od)
                        s.mm(out_ap, sel_t[0:16, par * 128:(par + 1) * 128], nFl[0:16, k0:k0 + n], False, False,
                             reads=[sel_d, negFd], out_dep=od, inc=True)

                    def o_evac(par, qb, src, srcd):
                        ps = slice(par * 64, (par + 1) * 64)
                        qsl = slice(qb * 128, (qb + 1) * 128)
                        s.op("dve", lambda e: e.tensor_tensor(out=oT[ps, hp, qsl], in0=src, in1=og[ps, qsl], op=ALU.mult),
                             reads=[srcd, ogd], writes=[oTd[qb // 4]])
                    self.attention_pair(ab, score_fn, Vt, Vtd, self.cmaskb, o_evac)
            self.out_proj(alloc, oT, oTd, d["fox_w_o"][0], gm)

    Prog.fox_layer = fox_layer


_fox_methods()


def _rwkv_methods():
    def rw_scratch(self):
        if not hasattr(self, "_scr"):
            nc = self.nc
            self._scr = {nm: nc.dram_tensor(f"scr_{nm}", [D, S], F32).ap() for nm in ("r", "k", "v", "sg", "a", "g", "vf", "xs")}
            self._scr["yg"] = nc.dram_tensor("scr_yg", [D, S], BF16).ap()
        return self._scr

    def rwkv_layer(self, i):
        j = i // 3
        lvl = getattr(self, "dbg_rw", 3)
        self.rwkv_pass1(i, j)
        if lvl >= 2:
            self.rwkv_pass2(i, j)
        if lvl >= 3:
            self.rwkv_pass3(i, j)

    def rwkv_pass1(self, i, j):
        s, d = self.s, self.d
        scr = self.rw_scratch()
        A = self.amod[:, i * 16:i * 16 + 8]
        B = self.modc(i, 0)
        fmv = lambda ap: ap.rearrange("(c p) n -> p c n", p=128)
        with self.phase() as alloc:
            hT = alloc("w_hT", [128, DC, S + 1], BF16)
            hTd = [Dep() for _ in range(4)]
            h0d = Dep()
            s.op("dve", lambda e: e.memset(hT[:, :, 0:1], 0.0), writes=[h0d])
            nt = self.norm_tmps(alloc)

            def out_fn(c, tb, t, td, Ac, Bc):
                s.op("act", lambda e: e.activation(out=hT[:, c, 1 + tb * 512:1 + (tb + 1) * 512], in_=t[:, :], func=AF.Identity,
                                                   bias=Bc, scale=Ac),
                     reads=[td, self.amodd, self.modd], writes=[hTd[tb]])
            self.norm_mod(nt, A, B, 0, 4, out_fn)
            omu = alloc("w_omu", [128, 48], F32)
            omud = Dep()
            s.op("dve", lambda e: e.tensor_scalar(out=omu[:, :], in0=self.P(f"mu{j}"), scalar1=-1.0, scalar2=1.0, op0=ALU.mult, op1=ALU.add),
                 reads=[self.ptd], writes=[omud])
            xn = [(alloc(f"w_xn{k}", [128, DC, 512], BF16), Dep()) for k in range(2)]
            tmpx = [(alloc(f"w_tx{k}", [128, 512], F32), Dep()) for k in range(4)]
            wbig = [(alloc(f"w_big{k}", [128, DC, 1024], BF16), Dep()) for k in range(2)]
            w1 = alloc("w_w1", [128, DC, 64], BF16)
            a1 = alloc("w_a1", [128, DC, 64], BF16)
            g1 = alloc("w_g1", [128, DC, 160], BF16)
            w2 = alloc("w_w2", [64, D], BF16)
            a2 = alloc("w_a2", [64, D], BF16)
            g2a = alloc("w_g2a", [128, D], BF16)
            g2b = alloc("w_g2b", [32, D], BF16)
            lwd = Dep()
            kcv = lambda ap: ap.rearrange("(kc p) n -> p kc n", p=128)
            s.dma("pool", w1[:, :, :], kcv(d["rw_w1"][j]), writes=[lwd])
            s.dma("pool", a1[:, :, :], kcv(d["rw_a1"][j]), writes=[lwd])
            s.dma("pool", g1[:, :, :], kcv(d["rw_g1"][j]), writes=[lwd])
            s.dma("pool", w2[:, :], d["rw_w2"][j], writes=[lwd])
            s.dma("pool", a2[:, :], d["rw_a2"][j], writes=[lwd])
            s.dma("pool", g2a[:, :], d["rw_g2"][j][0:128, :], writes=[lwd])
            s.dma("pool", g2b[:, :], d["rw_g2"][j][128:160, :], writes=[lwd])
            vres = (j > 0)
            if vres:
                v1 = alloc("w_v1", [128, DC, 32], BF16)
                v2 = alloc("w_v2", [32, D], BF16)
                s.dma("pool", v1[:, :, :], kcv(d["rw_v1"][j - 1]), writes=[lwd])
                s.dma("pool", v2[:, :], d["rw_v2"][j - 1], writes=[lwd])
                vgt = alloc("w_vgt", [128, DC, 512], BF16)
                vgd = Dep()
                vft = [(alloc(f"w_vf{k}", [128, 512], F32), Dep()) for k in range(2)]
            stage = [(alloc(f"w_st{k}", [128, 512], F32), Dep()) for k in range(3)]
            t1 = alloc("w_t1", [128, 512], BF16)
            t1b = alloc("w_t1b", [32, 512], BF16)
            t1d = Dep()
            stc = [0]

            def emit_out(pb, pbd, func, bias, dst, m, tb, post=None):
                st, std = stage[stc[0] % 3]
                stc[0] += 1
                kw = {} if bias is None else {"bias": bias}
                s.op("act", lambda e: e.activation(out=st[:, :], in_=pb, func=func, **kw), reads=[pbd, self.ptd], writes=[std])
                if post is not None:
                    post(st, std)
                s.dma("sp", fmv(dst)[:, m, tb * 512:(tb + 1) * 512], st[:, :], reads=[std])

            big_items = [0, 1, 2]

            def load_big(which, t, dep):
                src = kcv(d["rw_w_rkv"][j, which])
                for h2 in range(2):
                    s.dma("pool", t[:, :, h2 * 512:(h2 + 1) * 512], src[:, :, h2 * 512:(h2 + 1) * 512], writes=[dep])
            bst = Stream(wbig, big_items, load_big)
            xc = [0]

            def make_xn(n, tb):
                x_t, x_d = xn[xc[0] % 2]
                xc[0] += 1
                for c in range(DC):
                    tx, txd = tmpx[c % 4]
                    col = n * 8 + c
                    s.op("act", lambda e: e.activation(out=tx[:, :], in_=hT[:, c, tb * 512:tb * 512 + 512], func=AF.Copy,
                                                       scale=self.P(f"mu{j}", col, col + 1)),
                         reads=[hTd[tb], hTd[max(tb - 1, 0)], h0d, self.ptd], writes=[txd])
                    s.op("dve", lambda e: e.scalar_tensor_tensor(out=x_t[:, c, :], in0=hT[:, c, 1 + tb * 512:1 + tb * 512 + 512],
                                                               scalar=omu[:, col:col + 1], in1=tx[:, :], op0=ALU.mult, op1=ALU.add),
                         reads=[hTd[tb], omud, txd], writes=[x_d])
                return x_t, x_d

            def big_proj(x_t, x_d, wt, wd, m):
                pb, pbd = self.bank(m % 4)
                for kc in range(DC):
                    s.mm(pb, wt[:, kc, m * 128:(m + 1) * 128], x_t[:, kc, :], kc == 0, kc == DC - 1, reads=[wd, x_d], out_dep=pbd)
                return pb, pbd

            def lora(x_t, x_d, wa, ncols, func1, wb, m_bias_name, func2, dst, tb, wb2=None):
                pb, pbd = self.bank(4)
                n1 = min(ncols, 128)
                for kc in range(DC):
                    s.mm(pb[0:n1, :], wa[:, kc, 0:n1], x_t[:, kc, :], kc == 0, kc == DC - 1, reads=[lwd, x_d], out_dep=pbd)
                s.op("act", lambda e: e.activation(out=t1[0:n1, :], in_=pb[0:n1, :], func=func1), reads=[pbd], writes=[t1d])
                if ncols > 128:
                    pb2, pb2d = self.bank(5)
                    for kc in range(DC):
                        s.mm(pb2[0:32, :], wa[:, kc, 128:160], x_t[:, kc, :], kc == 0, kc == DC - 1, reads=[lwd, x_d], out_dep=pb2d)
                    s.op("act", lambda e: e.activation(out=t1b[0:32, :], in_=pb2[0:32, :], func=func1), reads=[pb2d], writes=[t1d])
                for m in range(DC):
                    po, pod = self.bank(m % 4)
                    s.mm(po, wb[0:n1, m * 128:(m + 1) * 128], t1[0:n1, :], True, wb2 is None, reads=[lwd, t1d], out_dep=pod)
                    if wb2 is not None:
                        s.mm(po, wb2[0:32, m * 128:(m + 1) * 128], t1b[0:32, :], False, True, reads=[lwd, t1d], out_dep=pod)
                    bias = None if m_bias_name is None else self.P(m_bias_name, m, m + 1)
                    if dst is None:
                        s.op("act", lambda e: e.activation(out=vgt[:, m, :], in_=po, func=func2, bias=bias), reads=[pod, self.ptd], writes=[vgd])
                    else:
                        emit_out(po, pod, func2, bias, dst, m, tb)

            for n in range(6):
                if n in (0, 2, 3):
                    wt, wd = bst.get()
                for tb in range(4):
                    x_t, x_d = make_xn(n, tb)
                    if n == 0:
                        for m in range(DC):
                            pb, pbd = big_proj(x_t, x_d, wt, wd, m)
                            emit_out(pb, pbd, AF.Copy, None, scr["r"], m, tb)
                    elif n == 2:
                        for m in range(DC):
                            pb, pbd = big_proj(x_t, x_d, wt, wd, m)
                            emit_out(pb, pbd, AF.Copy, None, scr["k"], m, tb)
                    elif n == 3:
                        if vres:
                            lora(x_t, x_d, v1, 32, AF.Copy, v2, "v0", AF.Sigmoid, None, tb)
                        for m in range(DC):
                            pb, pbd = big_proj(x_t, x_d, wt, wd, m)
                            if not vres:
                                emit_out(pb, pbd, AF.Copy, None, scr["vf"], m, tb)
                            else:
                                vf, vfd = vft[m % 2]
                                s.dma("sp", vf[:, :], fmv(scr["vf"])[:, m, tb * 512:(tb + 1) * 512], writes=[vfd])

                                def post(st, std, m=m, vf=vf, vfd=vfd):
                                    s.op("dve", lambda e: e.tensor_tensor(out=vf[:, :], in0=vf[:, :], in1=st[:, :], op=ALU.subtract),
                                         reads=[vfd, std], writes=[vfd])
                                    s.op("pool", lambda e: e.tensor_tensor(out=vf[:, :], in0=vf[:, :], in1=vgt[:, m, :], op=ALU.mult),
                                         reads=[vfd, vgd], writes=[vfd])
                                    s.op("dve", lambda e: e.tensor_tensor(out=st[:, :], in0=st[:, :], in1=vf[:, :], op=ALU.add),
                                         reads=[vfd, std], writes=[std])
                                emit_out(pb, pbd, AF.Copy, None, scr["v"], m, tb, post=post)
                    elif n == 1:
                        lora(x_t, x_d, w1, 64, AF.Tanh, w2, f"w0{j}", AF.Sigmoid, scr["sg"], tb)
                    elif n == 4:
                        lora(x_t, x_d, a1, 64, AF.Copy, a2, f"a0{j}", AF.Sigmoid, scr["a"], tb)
                    else:
                        lora(x_t, x_d, g1, 160, AF.Sigmoid, g2a, None, AF.Copy, scr["g"], tb, wb2=g2b)

    def rwkv_pass3(self, i, j):
        s, d = self.s, self.d
        scr = self.rw_scratch()
        gm = self.modc(i, 2)
        with self.phase() as alloc:
            YG = alloc("w_YG", [128, DC, S], BF16)
            YGd = [Dep() for _ in range(4)]
            src = scr["yg"].rearrange("(c p) n -> p c n", p=128)
            for tb in range(4):
                s.dma("sp", YG[:, :, tb * 512:(tb + 1) * 512], src[:, :, tb * 512:(tb + 1) * 512], writes=[YGd[tb]])
            self.out_proj(alloc, YG, YGd, d["rw_w_o"][j], gm)

    Prog.rw_scratch = rw_scratch
    Prog.rwkv_layer = rwkv_layer
    Prog.rwkv_pass1 = rwkv_pass1
    Prog.rwkv_pass3 = rwkv_pass3


_rwkv_methods()


def _rwkv2_methods():
    def rwkv_pass2(self, i, j):
        s, d = self.s, self.d
        scr = self.rw_scratch()
        fmv = lambda ap: ap.rearrange("(c p) n -> p c n", p=128)
        vsrc = scr["v"] if j > 0 else scr["vf"]
        xs = fmv(scr["xs"])
        for c in range(DC):
            s.dma("sp", xs[:, c, :], self.X[:, c, :], reads=self.Xd[c])
        s.barrier()
        xptr = [SB_BASE]

        def xalloc(name, shape, dt):
            nbytes = (int(np.prod(shape[1:])) * (2 if dt == BF16 else 4) + 31) // 32 * 32
            off = xptr[0]
            if off + nbytes > SB_BASE + DC * S * 4:
                return self.salloc(name, shape, dt)
            xptr[0] += nbytes
            return self.salloc(name, shape, dt, at=off)

        with self.phase() as alloc:
            rmask = alloc("p_rmask", [128, 8, 64], F32)
            cd = Dep()
            s.op("dve", lambda e: e.memset(rmask[:, :, :], 1.0), writes=[cd])
            s.op("dve", lambda e: e.memset(rmask[:, :, 0:1], 0.0), writes=[cd])
            omka = alloc("p_omka", [128, 8], F32)
            s.op("dve", lambda e: e.tensor_scalar(out=omka[:, :], in0=self.P(f"k_a{j}"), scalar1=-1.0, scalar2=1.0, op0=ALU.mult, op1=ALU.add),
                 reads=[self.ptd], writes=[cd])
            tiny = alloc("p_tiny", [128, 1], F32)
            s.op("dve", lambda e: e.memset(tiny[:, :], 1e-24), writes=[cd])
            Sf = alloc("p_Sf", [64, 16, 64], F32)
            Sb = alloc("p_Sb", [64, 16, 64], BF16)
            Sfd = [Dep() for _ in range(4)]
            Sbd = [Dep() for _ in range(4)]
            s.op("dve", lambda e: e.memset(Sf[:, :, :], 0.0), writes=Sfd)
            s.op("dve", lambda e: e.memset(Sb[:, :, :], 0.0), writes=Sbd)
            bc8 = lambda nm: self.P(nm).unsqueeze(2).broadcast_to([128, 8, 64])
            names = ("r", "k", "v", "sg", "a", "g")
            srcs = {"r": scr["r"], "k": scr["k"], "v": vsrc, "sg": scr["sg"], "a": scr["a"], "g": scr["g"]}
            Lb = [{nm: xalloc(f"p_L{nm}{k}", [128, 8, 128], F32) for nm in names} for k in range(2)]
            Ld = [Dep(), Dep()]

            def load_sb(sb, bufs, dep):
                for nm in names:
                    s.dma("sp", bufs[nm][:, :, :], fmv(srcs[nm])[:, :, sb * 128:(sb + 1) * 128], writes=[dep])
            lst = Stream([(Lb[0], Ld[0]), (Lb[1], Ld[1])], list(range(16)), load_sb)
            def T(nm, dt=F32, shape=(128, 8, 64)):
                return xalloc("p_" + nm, list(shape), dt)
            prep = []
            inter_names = ("lw", "cum", "cumE", "e1", "e2", "e3", "e4", "kkr", "sqk", "rn", "kk", "ta", "k2", "beta", "rk")
            inter = {nm: T(nm) for nm in inter_names}
            inter_d = {nm: Dep() for nm in inter_names}
            for k in range(2):
                pt_ = dict(inter)
                pt_.update({nm: T(f"{nm}{k}") for nm in ("bonus", "dg")})
                pt_["PL"] = xalloc(f"p_PL{k}", [128, 8], F32)
                pt_["PLs"] = xalloc(f"p_PLs{k}", [128, 8, 2], F32)
                pt_["PLh"] = xalloc(f"p_PLh{k}", [64, 16], F32)
                s.op("dve", lambda e: e.memset(pt_["PLs"][:, :, :], 0.0), writes=[cd])
                pt_["AR"] = T(f"AR{k}", BF16, (128, 8, 128))
                for nm in ("Bt", "Kt", "Bh", "Kh", "vb"):
                    pt_[nm] = T(f"{nm}{k}", BF16)
                pt_["d"] = {nm: Dep() for nm in ("bonus", "dg", "PL", "PLs", "PLh", "AR", "Bt", "Kt", "Bh", "Kh", "vb")}
                pt_["d"].update(inter_d)
                prep.append(pt_)
            tmaj = []
            for k in range(2):
                tm = {"AX": alloc(f"p_AX{k}", [64, 16, 128], BF16), "B": alloc(f"p_tmB{k}", [64, 1024], BF16),
                      "K": alloc(f"p_tmK{k}", [64, 1024], BF16), "V": alloc(f"p_tmV{k}", [64, 1024], BF16)}
                tm["d"] = {"AXa": Dep(), "AXx": [Dep() for _ in range(4)], "B": Dep(), "K": Dep(), "V": Dep()}
                tmaj.append(tm)
            grp = []
            for k in range(2):
                gb = {"Mbm": alloc(f"p_Mbm{k}", [64, 4, 128], F32), "Mbr": alloc(f"p_Mbr{k}", [64, 4, 64], BF16),
                      "Mkb": alloc(f"p_Mkb{k}", [64, 4, 128], BF16), "ATt": alloc(f"p_ATt{k}", [64, 4, 64], BF16),
                      "AT": alloc(f"p_AT{k}", [64, 4, 128], BF16), "Tb": alloc(f"p_Tb{k}", [64, 4, 64], BF16),
                      "AU": alloc(f"p_AU{k}", [64, 4, 128], BF16), "Rh": alloc(f"p_Rh{k}", [64, 4, 64], BF16),
                      "Phi": alloc(f"p_Phi{k}", [64, 4, 64], F32), "Sn": alloc(f"p_Sn{k}", [64, 4, 64], F32)}
                gb["d"] = {nm: Dep() for nm in ("Mbm", "Mbr", "Mkb", "ATt", "AT", "Tb", "AU", "Rh", "Phi", "Sn")}
                grp.append(gb)
            Yall = [(alloc(f"p_Y{k}", [64, 16, 64], F32), [Dep() for _ in range(4)]) for k in range(2)]
            Yc = alloc("p_Yc", [64, 16, 64], F32)
            Ysq = alloc("p_Ysq", [64, 16, 64], F32)
            yst = alloc("p_yst", [64, 64], F32)
            ycd, ysqd, ystd = Dep(), Dep(), Dep()
            yf = alloc("p_yf", [128, 8, 64], F32)
            yfd = Dep()
            ygs = [(alloc(f"p_ygs{k}", [128, 8, 256], BF16), Dep()) for k in range(2)]
            gnb = alloc("p_gnb", [64, 1], F32)
            s.op("dve", lambda e: e.memset(gnb[:, :], 64e-5), writes=[cd])
            rrb = [4]

            def nb():
                b = rrb[0]
                rrb[0] = 4 + (rrb[0] - 4 + 1) % 4
                return self.bank(b)
            ident = self.C("ident")
            mask_ar = self.C("mask_ar", rows=(0, 64)).unsqueeze(1).broadcast_to([64, 4, 128])
            maskT = self.C("maskT", rows=(0, 64)).unsqueeze(1).broadcast_to([64, 4, 64])
            id64b = self.C("ident", 0, 64, rows=(0, 64)).unsqueeze(1).broadcast_to([64, 4, 64])
            identh_b = self.C("identh").unsqueeze(1).broadcast_to([128, 8, 64])
            v4 = lambda ap, n: ap.rearrange("p (h c) -> p h c", c=n)

            Lcur = [None]
            Rodd2 = [(alloc(f"p_Rodd{k}", [64, 8, 64], F32), Dep()) for k in range(2)]
            gq2 = [(alloc(f"p_gq{k}", [128, 8, 64], F32), Dep()) for k in range(2)]
            nch = getattr(self, "dbg_nch", 32)

            def serial_pre(c):
                if False:
                    yield
                sbk, ch = c // 2, c % 2
                if ch == 0:
                    Lcur[0] = lst.get()
                L, Ldep = Lcur[0]
                csl = slice(ch * 64, (ch + 1) * 64)
                Rodd_c, Roddd_c = Rodd2[c % 2]
                gq_c, gqd_c = gq2[c % 2]
                P_ = prep[c % 2]
                pd = P_["d"]
                Lr, Lk, Lv, Lsg, La, Lg = (L[nm][:, :, csl] for nm in names)

                def op(eng, fn, reads, writes):
                    s.op(eng, fn, reads=[(pd[x] if isinstance(x, str) else x) for x in reads],
                         writes=[(pd[x] if isinstance(x, str) else x) for x in writes])
                op("dve", lambda e: e.tensor_scalar(out=P_["lw"][:, :, :], in0=Lsg, scalar1=-0.6065306597126334, scalar2=None, op0=ALU.mult),
                   [Ldep], ["lw"])
                op("dve", lambda e: e.tensor_tensor_scan(P_["cum"][:, :, :].rearrange("p a b -> p (a b)"), rmask[:, :, :].rearrange("p a b -> p (a b)"),
                                                         P_["lw"][:, :, :].rearrange("p a b -> p (a b)"), 0.0, ALU.mult, ALU.add),
                   ["lw", cd], ["cum"])
                op("pool", lambda e: e.tensor_tensor(out=P_["cumE"][:, :, :], in0=P_["cum"][:, :, :], in1=P_["lw"][:, :, :], op=ALU.subtract),
                   ["cum", "lw"], ["cumE"])
                op("act", lambda e: e.activation(out=P_["e1"][:, :, :], in_=P_["cumE"][:, :, :], func=AF.Exp), ["cumE"], ["e1"])
                op("act", lambda e: e.activation(out=P_["e2"][:, :, :], in_=P_["cum"][:, :, :], func=AF.Exp), ["cum"], ["e2"])
                op("act", lambda e: e.activation(out=P_["e3"][:, :, :], in_=P_["cum"][:, :, :], func=AF.Exp, scale=-1.0), ["cum"], ["e3"])
                op("dve", lambda e: e.tensor_tensor(out=P_["e4"][:, :, :], in0=P_["cum"][:, :, :],
                                                    in1=P_["cum"][:, :, 63:64].broadcast_to([128, 8, 64]), op=ALU.subtract), ["cum"], ["e4"])
                op("act", lambda e: e.activation(out=P_["e4"][:, :, :], in_=P_["e4"][:, :, :], func=AF.Exp, scale=-1.0), ["e4"], ["e4"])
                op("act", lambda e: e.activation(out=P_["PL"][:, :].unsqueeze(2), in_=P_["cum"][:, :, 63:64], func=AF.Exp), ["cum"], ["PL"])
                op("pool", lambda e: e.tensor_copy(out=P_["PLs"][0:64, :, 0:1], in_=P_["PL"][0:64, :].unsqueeze(2)), ["PL", cd], ["PLs"])
                op("pool", lambda e: e.tensor_copy(out=P_["PLs"][64:128, :, 1:2], in_=P_["PL"][64:128, :].unsqueeze(2)), ["PL", cd], ["PLs"])
                bpl, bpld = self.bank(3)
                s.mm(bpl[0:64, 0:16], self.C("identh"), P_["PLs"][:, :, :].rearrange("p a b -> p (a b)"), True, True, reads=[pd["PLs"], self.ctd], out_dep=bpld)
                op("dve", lambda e: e.tensor_copy(out=P_["PLh"][:, :], in_=bpl[0:64, 0:16]), [bpld], ["PLh"])
                yield
                op("dve", lambda e: e.tensor_tensor(out=P_["kkr"][:, :, :], in0=Lk, in1=bc8(f"k_k{j}"), op=ALU.mult), [Ldep, self.ptd], ["kkr"])
                op("act", lambda e: e.activation(out=P_["sqk"][:, :, :], in_=P_["kkr"][:, :, :], func=AF.Square), ["kkr"], ["sqk"])
                yield
                bs, bsd = self.bank(2)
                for hp in range(8):
                    s.mm(bs[:, hp * 64:(hp + 1) * 64], self.C("bd"), P_["sqk"][:, hp, :], True, hp == 7, reads=[pd["sqk"], self.ctd], out_dep=bsd)
                op("act", lambda e: e.activation(out=P_["rn"][:, :, :], in_=v4(bs, 64), func=AF.Sqrt, bias=tiny[:, 0:1]), [bsd, cd], ["rn"])
                op("dve", lambda e: e.reciprocal(out=P_["rn"][:, :, :], in_=P_["rn"][:, :, :]), ["rn"], ["rn"])
                op("dve", lambda e: e.tensor_tensor(out=P_["kk"][:, :, :], in0=P_["kkr"][:, :, :], in1=P_["rn"][:, :, :], op=ALU.mult), ["kkr", "rn"], ["kk"])
                yield
                op("pool", lambda e: e.tensor_tensor(out=P_["ta"][:, :, :], in0=La, in1=bc8(f"k_a{j}"), op=ALU.mult), [Ldep, self.ptd], ["ta"])
                op("pool", lambda e: e.tensor_tensor(out=P_["ta"][:, :, :], in0=P_["ta"][:, :, :], in1=omka[:, :].unsqueeze(2).broadcast_to([128, 8, 64]),
                                                     op=ALU.add), ["ta", cd], ["ta"])
                op("dve", lambda e: e.tensor_tensor(out=P_["k2"][:, :, :], in0=Lk, in1=P_["ta"][:, :, :], op=ALU.mult), [Ldep, "ta"], ["k2"])
                op("pool", lambda e: e.tensor_tensor(out=P_["beta"][:, :, :], in0=P_["kk"][:, :, :], in1=La, op=ALU.mult), ["kk", Ldep], ["beta"])
                yield
                op("dve", lambda e: e.scalar_tensor_tensor(out=P_["AR"][:, :, 0:64], in0=P_["kk"][:, :, :], scalar=-1.0, in1=P_["e1"][:, :, :],
                                                           op0=ALU.mult, op1=ALU.mult), ["kk", "e1"], ["AR"])
                op("pool", lambda e: e.tensor_tensor(out=P_["AR"][:, :, 64:128], in0=Lr, in1=P_["e2"][:, :, :], op=ALU.mult), [Ldep, "e2"], ["AR"])
                op("dve", lambda e: e.tensor_tensor(out=P_["Bt"][:, :, :], in0=P_["beta"][:, :, :], in1=P_["e3"][:, :, :], op=ALU.mult), ["beta", "e3"], ["Bt"])
                op("pool", lambda e: e.tensor_tensor(out=P_["Kt"][:, :, :], in0=P_["k2"][:, :, :], in1=P_["e3"][:, :, :], op=ALU.mult), ["k2", "e3"], ["Kt"])
                op("dve", lambda e: e.tensor_tensor(out=P_["Bh"][:, :, :], in0=P_["beta"][:, :, :], in1=P_["e4"][:, :, :], op=ALU.mult), ["beta", "e4"], ["Bh"])
                op("pool", lambda e: e.tensor_tensor(out=P_["Kh"][:, :, :], in0=P_["k2"][:, :, :], in1=P_["e4"][:, :, :], op=ALU.mult), ["k2", "e4"], ["Kh"])
                yield
                s.op("act", lambda e: e.activation(out=gq_c[:, :, :], in_=Lg, func=AF.Copy), reads=[Ldep], writes=[gqd_c])
                op("act", lambda e: e.activation(out=P_["vb"][:, :, :], in_=Lv, func=AF.Copy), [Ldep], ["vb"])
                op("pool", lambda e: e.tensor_tensor(out=P_["rk"][:, :, :], in0=Lr, in1=P_["k2"][:, :, :], op=ALU.mult), [Ldep, "k2"], ["rk"])
                op("pool", lambda e: e.tensor_tensor(out=P_["rk"][:, :, :], in0=P_["rk"][:, :, :], in1=bc8(f"r_k{j}"), op=ALU.mult), ["rk", self.ptd], ["rk"])
                yield
                bb, bbd = self.bank(3)
                for hp in range(8):
                    s.mm(bb[:, hp * 64:(hp + 1) * 64], self.C("bd"), P_["rk"][:, hp, :], True, hp == 7, reads=[pd["rk"], self.ctd], out_dep=bbd)
                op("dve", lambda e: e.tensor_tensor(out=P_["bonus"][:, :, :], in0=v4(bb, 64), in1=Lv, op=ALU.mult), [bbd, Ldep], ["bonus"])
                tm = tmaj[c % 2]
                td_ = tm["d"]
                for ti, (srcf, sdep, dst, ddeps) in enumerate((
                        (lambda hp: P_["AR"][:, hp, 0:64], "AR", tm["AX"][:, :, 0:64], [td_["AXa"]]),
                        (lambda hp: P_["Bh"][:, hp, :], "Bh", tm["B"][:, :].rearrange("p (h c) -> p h c", c=64), [td_["B"]]),
                        (lambda hp: P_["Kh"][:, hp, :], "Kh", tm["K"][:, :].rearrange("p (h c) -> p h c", c=64), [td_["K"]]),
                        (lambda hp: P_["vb"][:, hp, :], "vb", tm["V"][:, :].rearrange("p (h c) -> p h c", c=64), [td_["V"]]))):
                    yield
                    half = 1024
                    b0 = 2
                    for hp in range(8):
                        s.mm(self.psA[0:64, half + hp * 128:half + (hp + 1) * 128], srcf(hp), self.identb[:, :], True, hp % 4 == 3,
                             reads=[pd[sdep], self.cbd], out_dep=self.bd_[b0 + hp // 4])
                    src = self.psA[0:64, half:half + 1024].rearrange("p (h c) -> p h c", c=64)
                    if ti % 2 == 0:
                        s.op("act", lambda e: e.activation(out=dst, in_=src, func=AF.Copy), reads=[self.bd_[b0], self.bd_[b0 + 1]], writes=ddeps)
                    else:
                        s.op("dve", lambda e: e.tensor_copy(out=dst, in_=src), reads=[self.bd_[b0], self.bd_[b0 + 1]], writes=ddeps)
                yield
                bro, brod = self.bank(2)
                for hp in range(8):
                    s.mm(bro[0:64, hp * 64:(hp + 1) * 64], self.identb[64:128, 64:128], P_["AR"][64:128, hp, 64:128], True, hp == 7,
                         reads=[self.cbd, pd["AR"]], out_dep=brod)
                s.op("act", lambda e: e.activation(out=Rodd_c[:, :, :], in_=v4(bro[0:64, :], 64), func=AF.Copy), reads=[brod], writes=[Roddd_c])

            def make_groups(c):
                sbk, ch = c // 2, c % 2
                P_ = prep[c % 2]
                pd = P_["d"]
                tm = tmaj[c % 2]
                td_ = tm["d"]
                Yt, Ytd = Yall[c % 2]

                def op(eng, fn, reads, writes):
                    s.op(eng, fn, reads=[(pd[x] if isinstance(x, str) else x) for x in reads],
                         writes=[(pd[x] if isinstance(x, str) else x) for x in writes])
                def gsteps(g, mybanks):
                    rr_ = [0]

                    def nb():
                        b_ = mybanks[rr_[0] % len(mybanks)]
                        rr_[0] += 1
                        return self.bank(b_)
                    G = grp[g % 2]
                    gd = G["d"]
                    hb = (g // 2) * 8 + (g % 2)
                    heads = [(hb + 2 * hh, (hb + 2 * hh) // 2, hb % 2) for hh in range(4)]
                    hs = slice(hb, hb + 7, 2)
                    hpsl = slice(hb // 2, hb // 2 + 4)
                    gpar = hb % 2
                    b1, b1d = nb()
                    b2, b2d = nb()
                    b3, b3d = nb()
                    for hh, (h, hp, par) in enumerate(heads):
                        ps = slice(par * 64, (par + 1) * 64)
                        s.mm(b1[0:64, hh * 128:(hh + 1) * 128], P_["Bt"][ps, hp, :], P_["AR"][ps, hp, :], True, hh == 3, reads=[pd["Bt"], pd["AR"]], out_dep=b1d)
                    for hh, (h, hp, par) in enumerate(heads):
                        ps = slice(par * 64, (par + 1) * 64)
                        s.mm(b2[0:64, hh * 128:(hh + 1) * 128], P_["Kt"][ps, hp, :], P_["AR"][ps, hp, :], True, hh == 3, reads=[pd["Kt"], pd["AR"]], out_dep=b2d)
                    for hh, (h, hp, par) in enumerate(heads):
                        ps = slice(par * 64, (par + 1) * 64)
                        s.mm(b3[0:64, hh * 64:(hh + 1) * 64], P_["AR"][ps, hp, 0:64], P_["Bt"][ps, hp, :], True, hh == 3, reads=[pd["Bt"], pd["AR"]], out_dep=b3d)
                    if getattr(self, 'dbg_m', 9) < 1:
                        return
                    s.op("dve", lambda e: e.tensor_tensor(out=G["Mbm"][:, :, :], in0=v4(b1[0:64, :], 128), in1=mask_ar, op=ALU.mult),
                         reads=[b1d, self.ctd], writes=[gd["Mbm"]])
                    s.op("dve", lambda e: e.tensor_tensor(out=G["Mkb"][:, :, :], in0=v4(b2[0:64, :], 128), in1=mask_ar, op=ALU.mult),
                         reads=[b2d, self.ctd], writes=[gd["Mkb"]])
                    s.op("dve", lambda e: e.tensor_tensor(out=G["ATt"][:, :, :], in0=v4(b3[0:64, 0:256], 64), in1=maskT, op=ALU.mult),
                         reads=[b3d, self.ctd], writes=[gd["ATt"]])
                    if getattr(self, 'dbg_m', 9) < 2:
                        return
                    s.op("act", lambda e: e.activation(out=G["AT"][:, :, 0:64], in_=G["Mbm"][:, :, 0:64], func=AF.Copy), reads=[gd["Mbm"]], writes=[gd["AT"]])
                    s.op("pool", lambda e: e.tensor_tensor(out=G["AT"][:, :, 64:128], in0=G["Mbm"][:, :, 0:64], in1=id64b, op=ALU.add),
                         reads=[gd["Mbm"], self.ctd], writes=[gd["AT"]])
                    s.op("act", lambda e: e.activation(out=G["Mbr"][:, :, :], in_=G["Mbm"][:, :, 64:128], func=AF.Copy), reads=[gd["Mbm"]], writes=[gd["Mbr"]])
                    if getattr(self, 'dbg_sub', 9) < 2:
                        return
                    yield
                    for rnd in range(6):
                        if rnd > 0:
                            yield
                        bA, bAd = nb()
                        if rnd == 0:
                            bB, bBd = nb()
                            for hh in range(4):
                                s.mm(bA[0:64, hh * 64:(hh + 1) * 64], G["ATt"][:, hh, :], G["AT"][:, hh, 0:64], True, hh == 3, reads=[gd["ATt"], gd["AT"]], out_dep=bAd)
                            for hh in range(4):
                                s.mm(bB[0:64, hh * 64:(hh + 1) * 64], G["AT"][:, hh, 0:64], G["ATt"][:, hh, :], True, hh == 3, reads=[gd["ATt"], gd["AT"]], out_dep=bBd)
                            s.op("act", lambda e: e.activation(out=G["AT"][:, :, 0:64], in_=v4(bA[0:64, 0:256], 64), func=AF.Copy), reads=[bAd], writes=[gd["AT"]])
                            s.op("act", lambda e: e.activation(out=G["ATt"][:, :, :], in_=v4(bB[0:64, 0:256], 64), func=AF.Copy), reads=[bBd], writes=[gd["ATt"]])
                        elif rnd < 5:
                            bB, bBd = nb()
                            for hh in range(4):
                                s.mm(bA[0:64, hh * 128:(hh + 1) * 128], G["ATt"][:, hh, :], G["AT"][:, hh, :], True, hh == 3, reads=[gd["ATt"], gd["AT"]], out_dep=bAd)
                            for hh in range(4):
                                s.mm(bB[0:64, hh * 64:(hh + 1) * 64], G["AT"][:, hh, 0:64], G["ATt"][:, hh, :], True, hh == 3, reads=[gd["ATt"], gd["AT"]], out_dep=bBd)
                            bA4 = v4(bA[0:64, :], 128)
                            s.op("dve", lambda e: e.tensor_tensor(out=G["AT"][:, :, 64:128], in0=G["AT"][:, :, 64:128], in1=bA4[:, :, 64:128], op=ALU.add),
                                 reads=[bAd, gd["AT"]], writes=[gd["AT"]])
                            s.op("dve", lambda e: e.tensor_copy(out=G["AT"][:, :, 0:64], in_=bA4[:, :, 0:64]), reads=[bAd, gd["AT"]], writes=[gd["AT"]])
                            s.op("act", lambda e: e.activation(out=G["ATt"][:, :, :], in_=v4(bB[0:64, 0:256], 64), func=AF.Copy), reads=[bBd], writes=[gd["ATt"]])
                        else:
                            for hh in range(4):
                                s.mm(bA[0:64, hh * 64:(hh + 1) * 64], G["ATt"][:, hh, :], G["AT"][:, hh, 64:128], True, hh == 3, reads=[gd["ATt"], gd["AT"]], out_dep=bAd)
                            s.op("dve", lambda e: e.tensor_tensor(out=G["Tb"][:, :, :], in0=G["AT"][:, :, 64:128], in1=v4(bA[0:64, 0:256], 64), op=ALU.add),
                                 reads=[bAd, gd["AT"]], writes=[gd["Tb"]])
                    if getattr(self, 'dbg_sub', 9) < 3:
                        return
                    yield
                    bx, bxd = nb()
                    for hh, (h, hp, par) in enumerate(heads):
                        s.mm(bx[0:64, hh * 64:(hh + 1) * 64], G["Mkb"][:, hh, 0:64], tm["V"][:, h * 64:(h + 1) * 64], True, hh == 3,
                             reads=[gd["Mkb"], td_["V"]], out_dep=bxd)
                    s.op("act", lambda e: e.activation(out=tm["AX"][:, hs, 64:128], in_=v4(bx[0:64, 0:256], 64), func=AF.Copy),
                         reads=[bxd], writes=[td_["AXx"][g]])
                    yield
                    bu, bud = nb()
                    for hh, (h, hp, par) in enumerate(heads):
                        s.mm(bu[0:64, hh * 128:(hh + 1) * 128], G["Tb"][:, hh, :], tm["AX"][:, h, :], True, hh == 3,
                             reads=[gd["Tb"], td_["AXa"], td_["AXx"][g]], out_dep=bud)
                    s.op("act", lambda e: e.activation(out=G["AU"][:, :, :], in_=v4(bu[0:64, :], 128), func=AF.Copy), reads=[bud], writes=[gd["AU"]])
                    if getattr(self, 'dbg_sub', 9) < 4:
                        return
                    yield
                    br_, brd = nb()
                    bp, bpd = nb()
                    for hh, (h, hp, par) in enumerate(heads):
                        ps = slice(par * 64, (par + 1) * 64)
                        s.mm(br_[0:64, hh * 64:(hh + 1) * 64], G["AU"][:, hh, 0:64], G["Mbr"][:, hh, :], True, hh == 3, reads=[gd["AU"], gd["Mbr"]], out_dep=brd)
                    for hh, (h, hp, par) in enumerate(heads):
                        ps = slice(par * 64, (par + 1) * 64)
                        s.mm(bp[0:64, hh * 64:(hh + 1) * 64], G["AU"][:, hh, 0:64], tm["B"][:, h * 64:(h + 1) * 64], True, hh == 3, reads=[gd["AU"], td_["B"]], out_dep=bpd)
                    rsrc = P_["AR"][0:64, hpsl, 64:128] if gpar == 0 else Rodd2[c % 2][0][:, hpsl, :]
                    s.op("dve", lambda e: e.tensor_tensor(out=G["Rh"][:, :, :], in0=v4(br_[0:64, 0:256], 64), in1=rsrc, op=ALU.add),
                         reads=[brd, pd["AR"], Rodd2[c % 2][1]], writes=[gd["Rh"]])
                    s.op("pool", lambda e: e.tensor_tensor(out=G["Phi"][:, :, :], in0=id64b, in1=P_["PLh"][:, hs].unsqueeze(2).broadcast_to([64, 4, 64]),
                                                           op=ALU.mult), reads=[pd["PLh"], self.ctd], writes=[gd["Phi"]])
                    s.op("dve", lambda e: e.tensor_tensor(out=G["Phi"][:, :, :], in0=G["Phi"][:, :, :], in1=v4(bp[0:64, 0:256], 64), op=ALU.add),
                         reads=[bpd, gd["Phi"]], writes=[gd["Phi"]])
                    if getattr(self, 'dbg_sub', 9) < 5:
                        return
                    yield
                    by, byd = nb()
                    for hh, (h, hp, par) in enumerate(heads):
                        o_ = by[0:64, hh * 64:(hh + 1) * 64]
                        s.mm(o_, G["Mbr"][:, hh, :], G["AU"][:, hh, 64:128], True, False, reads=[gd["Mbr"], gd["AU"]], out_dep=byd)
                        s.mm(o_, G["Mkb"][:, hh, 64:128], tm["V"][:, h * 64:(h + 1) * 64], False, False, reads=[gd["Mkb"], td_["V"]], out_dep=byd)
                        s.mm(o_, G["Rh"][:, hh, :], Sb[:, h, :], False, hh == 3, reads=[gd["Rh"], Sbd[g]], out_dep=byd)
                    s.op("act", lambda e: e.activation(out=Yt[:, hs, :], in_=v4(by[0:64, 0:256], 64), func=AF.Copy), reads=[byd], writes=[Ytd[g]])
                    if getattr(self, 'dbg_sub', 9) < 6:
                        return
                    yield
                    bn, bnd = nb()
                    bn2, bn2d = nb()
                    for hh, (h, hp, par) in enumerate(heads):
                        o_ = bn[0:64, hh * 64:(hh + 1) * 64]
                        s.mm(o_, tm["B"][:, h * 64:(h + 1) * 64], G["AU"][:, hh, 64:128], True, False, reads=[td_["B"], gd["AU"]], out_dep=bnd)
                        s.mm(o_, tm["K"][:, h * 64:(h + 1) * 64], tm["V"][:, h * 64:(h + 1) * 64], False, hh == 3, reads=[td_["K"], td_["V"]], out_dep=bnd)
                    for hh, (h, hp, par) in enumerate(heads):
                        s.mm(bn2[0:64, hh * 64:(hh + 1) * 64], G["Phi"][:, hh, :], Sf[:, h, :], True, hh == 3, reads=[gd["Phi"], Sfd[g]], out_dep=bn2d)
                    s.op("act", lambda e: e.activation(out=G["Sn"][:, :, :], in_=v4(bn[0:64, 0:256], 64), func=AF.Copy), reads=[bnd], writes=[gd["Sn"]])
                    s.op("dve", lambda e: e.tensor_tensor(out=Sf[:, hs, :], in0=G["Sn"][:, :, :], in1=v4(bn2[0:64, 0:256], 64), op=ALU.add),
                         reads=[bn2d, gd["Sn"]], writes=[Sfd[g]])
                    s.op("act", lambda e: e.activation(out=Sb[:, hs, :], in_=Sf[:, hs, :], func=AF.Copy), reads=[Sfd[g]], writes=[Sbd[g]])

                return gsteps

            def serial_out(c):
                if False:
                    yield
                sbk, ch = c // 2, c % 2
                P_ = prep[c % 2]
                pd = P_["d"]
                tm = tmaj[c % 2]
                td_ = tm["d"]
                Yt, Ytd = Yall[c % 2]

                def op(eng, fn, reads, writes):
                    s.op(eng, fn, reads=[(pd[x] if isinstance(x, str) else x) for x in reads],
                         writes=[(pd[x] if isinstance(x, str) else x) for x in writes])
                s.op("dve", lambda e: e.reduce_sum(out=yst[:, 0:16], in_=Yt[:, :, :], axis=AX.X), reads=Ytd, writes=[ystd])
                s.op("dve", lambda e: e.tensor_scalar(out=yst[:, 0:16], in0=yst[:, 0:16], scalar1=1.0 / 64.0, scalar2=None, op0=ALU.mult), reads=[ystd], writes=[ystd])
                s.op("pool", lambda e: e.tensor_tensor(out=Yc[:, :, :], in0=Yt[:, :, :], in1=yst[:, 0:16].unsqueeze(2).broadcast_to([64, 16, 64]), op=ALU.subtract),
                     reads=Ytd + [ystd], writes=[ycd])
                yield
                s.op("act", lambda e: e.activation(out=Ysq[:, :, :], in_=Yc[:, :, :], func=AF.Square), reads=[ycd], writes=[ysqd])
                s.op("dve", lambda e: e.reduce_sum(out=yst[:, 16:32], in_=Ysq[:, :, :], axis=AX.X), reads=[ysqd, ystd], writes=[ystd])
                s.op("act", lambda e: e.activation(out=yst[:, 16:32], in_=yst[:, 16:32], func=AF.Sqrt, bias=gnb[:, 0:1], scale=1.0 / 64.0), reads=[ystd, cd], writes=[ystd])
                s.op("dve", lambda e: e.reciprocal(out=yst[:, 16:32], in_=yst[:, 16:32]), reads=[ystd], writes=[ystd])
                s.op("pool", lambda e: e.tensor_tensor(out=Yc[:, :, :], in0=Yc[:, :, :], in1=yst[:, 16:32].unsqueeze(2).broadcast_to([64, 16, 64]), op=ALU.mult),
                     reads=[ycd, ystd, ysqd], writes=[ycd])
                yield
                bo, bod = self.bank(2)
                Ycf = Yc[:, :, :].rearrange("p a b -> p (a b)")
                for hp in range(8):
                    s.mm(bo[:, hp * 64:(hp + 1) * 64], Ycf[:, hp * 128:(hp + 1) * 128], ident[0:64, 0:64], True, hp == 7, reads=[ycd, self.ctd], out_dep=bod)
                s.op("dve", lambda e: e.tensor_tensor(out=yf[:, :, :], in0=v4(bo, 64), in1=bc8(f"lnx_g{j}"), op=ALU.mult), reads=[bod, self.ptd], writes=[yfd])
                s.op("pool", lambda e: e.tensor_tensor(out=yf[:, :, :], in0=yf[:, :, :], in1=bc8(f"lnx_b{j}"), op=ALU.add), reads=[yfd, self.ptd], writes=[yfd])
                s.op("pool", lambda e: e.tensor_tensor(out=yf[:, :, :], in0=yf[:, :, :], in1=P_["bonus"][:, :, :], op=ALU.add), reads=[yfd, pd["bonus"]], writes=[yfd])
                yield
                yg_t, yg_d = ygs[(c // 4) % 2]
                s.op("dve", lambda e: e.tensor_tensor(out=yg_t[:, :, (c % 4) * 64:(c % 4 + 1) * 64], in0=yf[:, :, :], in1=gq2[c % 2][0][:, :, :], op=ALU.mult),
                     reads=[yfd, gq2[c % 2][1]], writes=[yg_d])
                if c % 4 == 3:
                    c0 = (c // 4) * 256
                    s.dma("sp", fmv(scr["yg"])[:, :, c0:c0 + 256], yg_t[:, :, :], reads=[yg_d])

            def chain(*gens):
                for g_ in gens:
                    if g_ is not None:
                        yield from g_

            for _ in serial_pre(0):
                pass
            for c in range(nch):
                ser = chain(serial_out(c - 1) if c > 0 else None, serial_pre(c + 1) if c + 1 < nch else None)
                gsteps = make_groups(c)
                ser_live = True
                for pair in ((0, 1), (2, 3)):
                    live = [gsteps(pair[0], [4, 5, 6]), gsteps(pair[1], [7, 0, 1])]
                    while live:
                        for gg in list(live):
                            try:
                                next(gg)
                            except StopIteration:
                                live.remove(gg)
                        if ser_live:
                            try:
                                next(ser)
                            except StopIteration:
                                ser_live = False
                if ser_live:
                    for _ in ser:
                        pass
            for _ in serial_out(nch - 1):
                pass
        self.Xd = [[Dep() for _ in range(4)] for _ in range(DC)]
        for c in range(DC):
            s.dma("sp", self.X[:, c, :], xs[:, c, :], writes=self.Xd[c])

    Prog.rwkv_pass2 = rwkv_pass2


_rwkv2_methods()
```

```python
import contextlib
import math
import numpy as np
import concourse.bass as bass
import concourse.mybir as mybir
from concourse.bass_utils import run_bass_kernel_spmd

F32 = mybir.dt.float32
BF16 = mybir.dt.bfloat16
I32 = mybir.dt.int32
AF = mybir.ActivationFunctionType
ALU = mybir.AluOpType
AX = mybir.AxisListType

S = 2048
D = 1024
DC = 8
NL = 4
DFF = 2816
FC = 22
NE = 8
EPS = 1e-6
NEG = -30000.0
SB_BASE = 16512
SB_TOP = 229344


class Dep:
    __slots__ = ("w", "r")

    def __init__(self):
        self.w = None
        self.r = {}


class Sched:
    def __init__(self, nc, n_dma_sems=24):
        self.nc = nc
        self.eng = {"pe": nc.tensor, "dve": nc.vector, "act": nc.scalar,
                    "pool": nc.gpsimd, "sp": nc.sync}
        self.sem = {e: nc.alloc_semaphore(name=f"s_{e}") for e in self.eng}
        self.cnt = {e: 0 for e in self.eng}
        self.dsem = [nc.alloc_semaphore(name=f"d_{i}") for i in range(n_dma_sems)]
        self.dcnt = [0] * n_dma_sems
        self.dlast = [None] * n_dma_sems
        self.dnext = 0
        self.seen = {e: {} for e in self.eng}
        self.nins = 0

    def _wait(self, e, key, val):
        if self.seen[e].get(key, 0) >= val:
            return
        sem = self.sem[key[1]] if key[0] == "e" else self.dsem[key[1]]
        self.eng[e].wait_ge(sem, val)
        self.seen[e][key] = val

    def _deps(self, e, reads, writes):
        need = {}
        for d in reads:
            if d.w is not None:
                k, v = d.w
                if need.get(k, 0) < v:
                    need[k] = v
        for d in writes:
            if d.w is not None:
                k, v = d.w
                if need.get(k, 0) < v:
                    need[k] = v
            for k, v in d.r.items():
                if need.get(k, 0) < v:
                    need[k] = v
        for k, v in need.items():
            self._wait(e, k, v)

    def _commit(self, tok, reads, writes):
        k, v = tok
        for d in reads:
            if d.r.get(k, 0) < v:
                d.r[k] = v
        for d in writes:
            d.w = tok
            d.r = {}

    def op(self, e, fn, reads=(), writes=()):
        self._deps(e, reads, writes)
        ins = fn(self.eng[e])
        self.cnt[e] += 1
        ins.then_inc(self.sem[e], 1)
        tok = (("e", e), self.cnt[e])
        self._commit(tok, reads, writes)
        self.nins += 1
        return tok

    def mm(self, out, lhsT, rhs, start, stop, reads=(), out_dep=None, inc=None):
        e = "pe"
        if inc is None:
            inc = stop
        self._deps(e, reads, [])
        if start and out_dep is not None:
            need = dict(out_dep.r)
            if out_dep.w is not None and out_dep.w[0] != ("e", "pe"):
                k, v = out_dep.w
                if need.get(k, 0) < v:
                    need[k] = v
            for k, v in need.items():
                self._wait(e, k, v)
        ins = self.eng[e].matmul(out, lhsT, rhs, start=start, stop=stop)
        if inc:
            self.cnt[e] += 1
            ins.then_inc(self.sem[e], 1)
            tok = (("e", e), self.cnt[e])
        else:
            tok = (("e", e), self.cnt[e] + 1)
        self._commit(tok, reads, [out_dep] if stop else [])
        self.nins += 1
        return tok

    def dma(self, q, out, in_, reads=(), writes=(), **kw):
        i = self.dnext
        self.dnext = (i + 1) % len(self.dsem)
        self._deps(q, reads, writes)
        if self.dlast[i] is not None:
            self._wait(q, *self.dlast[i])
        ins = self.eng[q].dma_start(out=out, in_=in_, **kw)
        self.dcnt[i] += 16
        ins.then_inc(self.dsem[i], 16)
        tok = (("d", i), self.dcnt[i])
        self.dlast[i] = tok
        self._commit(tok, reads, writes)
        self.nins += 1
        return tok

    def barrier(self, engines=None):
        engines = engines or list(self.eng)
        for e in engines:
            for f in self.eng:
                if self.cnt[f] > 0:
                    self._wait(e, ("e", f), self.cnt[f])
            for t in self.dlast:
                if t is not None:
                    self._wait(e, *t)


class Stream:
    def __init__(self, bufs, items, load_fn):
        self.bufs = bufs
        self.items = items
        self.load_fn = load_fn
        self.n = 0
        self.issued = 0

    def get(self):
        nb = len(self.bufs)
        while self.issued < len(self.items) and self.issued < self.n + nb:
            t, d = self.bufs[self.issued % nb]
            self.load_fn(self.items[self.issued], t, d)
            self.issued += 1
        t, d = self.bufs[self.n % nb]
        self.n += 1
        return t, d


def _fm(v):
    v = np.asarray(v, np.float32).reshape(-1)
    n = v.size // 128
    return np.ascontiguousarray(v.reshape(n, 128).T)


class Table:
    def __init__(self):
        self.cols = {}
        self.parts = []
        self.n = 0

    def add(self, name, arr):
        arr = np.asarray(arr, np.float32)
        assert arr.shape[0] == 128, (name, arr.shape)
        self.cols[name] = (self.n, arr.shape[1])
        self.parts.append(arr)
        self.n += arr.shape[1]

    def build(self):
        return np.ascontiguousarray(np.concatenate(self.parts, axis=1))


def _pad128(a):
    out = np.zeros((128, a.shape[1]), np.float32)
    out[: a.shape[0]] = a
    return out


def const_table():
    t = Table()
    p = np.arange(128)
    t.add("ident", np.eye(128, dtype=np.float32))
    t.add("ones", np.ones((128, 128), np.float32))
    t.add("bd", (p[:, None] // 64 == p[None, :] // 64).astype(np.float32))
    t.add("identh", (p[:, None] % 64 == np.arange(64)[None, :]).astype(np.float32))
    j = np.arange(64)[:, None]
    tt = np.arange(64)[None, :]
    t.add("mask_ar", _pad128(np.concatenate([(j < tt), (j <= tt)], axis=1).astype(np.float32)))
    t.add("maskT", _pad128((tt < j).astype(np.float32)))
    qi = p[:, None]
    kj = p[None, :]
    t.add("cmask", np.where(kj <= qi, 0.0, NEG).astype(np.float32))
    t.add("mmask", np.where((kj // 64) <= (qi // 64), 0.0, NEG).astype(np.float32))
    t.add("invf", (10000.0 ** (-(p % 16).astype(np.float64) / 16.0)).astype(np.float32)[:, None])
    sel8 = np.zeros((128, 8 * 128), np.float32)
    for e in range(8):
        sel8[e, e * 128:(e + 1) * 128] = 1.0
    t.add("sel8", sel8)
    sel16 = np.zeros((128, 16 * 128), np.float32)
    for e in range(16):
        sel16[e, e * 128:(e + 1) * 128] = 1.0
    t.add("sel16", sel16)
    return t


def param_table(inp, b):
    t = Table()
    t.add("c", _fm(inp["c"][b]))
    for i in range(NL):
        t.add(f"adab{i}", _fm(inp["ada_b"][i]))
        t.add(f"nmg{i}", _fm(inp["norm_mix_g"][i]))
        t.add(f"nfg{i}", _fm(inp["norm_ffn_g"][i]))
    t.add("fng", _fm(inp["final_norm_g"]))
    for j in range(2):
        t.add(f"mu{j}", _fm(inp["rw_mu"][j]))
        for nm in ("w0", "a0", "k_k", "k_a", "r_k", "lnx_g", "lnx_b"):
            t.add(f"{nm}{j}", _fm(inp["rw_" + nm][j]))
    t.add("v0", _fm(inp["rw_v0"][0]))
    t.add("mqg", _fm(inp["mla_q_norm_g"][0]))
    t.add("mkvg", _fm(inp["mla_kv_norm_g"][0]))
    t.add("fbf", _pad128(np.asarray(inp["fox_b_f"][0], np.float32).reshape(16, 1)))
    t.add("fqg", np.tile(np.asarray(inp["fox_q_norm_g"][0], np.float32).reshape(64, 1), (2, 1)))
    t.add("fkg", np.tile(np.asarray(inp["fox_k_norm_g"][0], np.float32).reshape(64, 1), (2, 1)))
    for j in range(2):
        t.add(f"mbr{j}", np.tile(np.asarray(inp["moe_b_router"][j], np.float32).reshape(1, 8), (128, 1)))
        wr = np.asarray(inp["moe_w_router"][j], np.float32)
        t.add(f"mwr{j}", np.ascontiguousarray(wr.reshape(8, 128, 8).transpose(1, 0, 2).reshape(128, 64)))
    return t


_CT = const_table()


W_SHAPES = {
    "ada_w": [4, 1024, 6144],
    "rw_w_rkv": [2, 3, 1024, 1024], "rw_w_o": [2, 1024, 1024],
    "rw_w1": [2, 1024, 64], "rw_w2": [2, 64, 1024],
    "rw_a1": [2, 1024, 64], "rw_a2": [2, 64, 1024],
    "rw_g1": [2, 1024, 160], "rw_g2": [2, 160, 1024],
    "rw_v1": [1, 1024, 32], "rw_v2": [1, 32, 1024],
    "mla_w_down": [1, 1024, 1056], "mla_w_uq": [1, 768, 1536],
    "mla_w_ukv": [1, 256, 2048], "mla_w_o": [1, 1024, 1024],
    "fox_w_in": [1, 1024, 4112], "fox_w_o": [1, 1024, 1024],
    "ffn_w_gate_up": [2, 1024, 5632], "ffn_w_down": [2, 2816, 1024],
    "moe_w_gate_up": [2, 8, 1024, 5632], "moe_w_down": [2, 8, 2816, 1024],
}


class Prog:
    def __init__(self, pcols, npcols, plan=None, x_in_dbg=False):
        self.plan = plan
        nc = bass.Bass("TRN2", target_bir_lowering=False)
        self.nc = nc
        self.s = Sched(nc)
        self.pcols = pcols
        self.ccols = _CT.cols
        d = {}
        d["xT"] = nc.dram_tensor("xT", [D, S], F32, kind="ExternalInput").ap()
        d["pos"] = nc.dram_tensor("pos", [1, S], I32, kind="ExternalInput").ap()
        d["ptab"] = nc.dram_tensor("ptab", [128, npcols], F32, kind="ExternalInput").ap()
        d["ctab"] = nc.dram_tensor("ctab", [128, _CT.n], F32, kind="ExternalInput").ap()
        for k, shp in W_SHAPES.items():
            d[k] = nc.dram_tensor(k, shp, F32, kind="ExternalInput").ap()
        d["outT"] = nc.dram_tensor("outT", [D, S], F32, kind="ExternalOutput").ap()
        self.d = d
        self.sb_ptr = SB_BASE
        self.uid = 0
        self.X = self.salloc("X", [128, DC, S], F32)
        self.Xd = [[Dep() for _ in range(4)] for _ in range(DC)]
        self.pt = self.salloc("pt", [128, npcols], F32)
        self.ptd = Dep()
        nct = self.ccols["sel8"][0]
        self.ct = self.salloc("ct", [128, nct], F32)
        self.ctd = Dep()
        self.identb = self.salloc("identb", [128, 128], BF16)
        self.cmaskb = self.salloc("cmaskb", [128, 128], BF16)
        self.onesb = self.salloc("onesb", [128, 128], BF16)
        self.bdb = self.salloc("bdb", [128, 128], BF16)
        self.mmaskb = self.salloc("mmaskb", [128, 128], BF16)
        self.cbd = Dep()
        self.mod = self.salloc("mod", [128, NL * 48], F32)
        self.modd = Dep()
        self.amod = self.salloc("amod", [128, NL * 16 + 8], F32)
        self.amodd = Dep()
        self.cond = self.salloc("cond", [128, 8], F32)
        self.epsc = self.salloc("epsc", [128, 4], F32)
        self.s.op("dve", lambda e: e.memset(self.epsc[:, 0:1], EPS))
        self.s.op("dve", lambda e: e.memset(self.epsc[:, 1:2], 64e-5))
        self.s.op("dve", lambda e: e.memset(self.epsc[:, 2:3], 1.0))
        self.s.op("dve", lambda e: e.memset(self.epsc[:, 3:4], 0.0))
        self.condd = Dep()
        self.psA = nc.alloc_psum_tensor("psA", [128, 2048], F32)
        self.psB = [nc.alloc_psum_tensor(f"psB{i}", [128, 512], F32) for i in range(4)]
        self.bd_ = [Dep() for _ in range(8)]
        self.rr = 0

    def bank(self, i):
        if i < 4:
            return self.psA[:, i * 512:(i + 1) * 512], self.bd_[i]
        return self.psB[i - 4][:, :], self.bd_[i]

    def P(self, name, a=0, b=None):
        st, n = self.pcols[name]
        b = n if b is None else b
        return self.pt[:, st + a:st + b]

    def C(self, name, a=0, b=None, rows=None):
        st, n = self.ccols[name]
        b = n if b is None else b
        if rows is None:
            return self.ct[:, st + a:st + b]
        return self.ct[rows[0]:rows[1], st + a:st + b]

    def modc(self, i, w, c0=0, c1=8):
        return self.mod[:, i * 48 + w * 8 + c0:i * 48 + w * 8 + c1]

    def salloc(self, name, shape, dt, at=None):
        nbytes = int(np.prod(shape[1:])) * (2 if dt == BF16 else 4)
        nbytes = (nbytes + 31) // 32 * 32
        if at is None:
            off = self.sb_ptr
            self.sb_ptr += nbytes
            assert self.sb_ptr <= SB_TOP, f"SBUF overflow allocating {name}: {self.sb_ptr} > {SB_TOP}"
        else:
            off = at
        self.uid += 1
        t = self.nc.alloc_sbuf_tensor_at(f"{name}_{self.uid}", list(shape), dt, offset=off)
        return t

    @contextlib.contextmanager
    def phase(self):
        mark = self.sb_ptr
        yield self.salloc
        self.s.barrier()
        self.sb_ptr = mark

    def prelude(self, layers):
        s, nc, d = self.s, self.nc, self.d
        s.dma("sp", self.pt[:, :], d["ptab"][:, :], writes=[self.ptd])
        s.dma("sp", self.ct[:, :], d["ctab"][:, 0:self.ccols["sel8"][0]], writes=[self.ctd])
        xv = d["xT"].rearrange("(c p) s -> p c s", p=128)
        for c in range(DC):
            s.dma("sp" if c % 2 == 0 else "act", self.X[:, c, :], xv[:, c, :], writes=self.Xd[c])
        s.op("dve", lambda e: e.tensor_copy(self.identb[:, :], self.C("ident")), reads=[self.ctd], writes=[self.cbd])
        s.op("dve", lambda e: e.tensor_copy(self.cmaskb[:, :], self.C("cmask")), reads=[self.ctd], writes=[self.cbd])
        s.op("dve", lambda e: e.tensor_copy(self.mmaskb[:, :], self.C("mmask")), reads=[self.ctd], writes=[self.cbd])
        s.op("dve", lambda e: e.memset(self.onesb[:, :], 1.0), writes=[self.cbd])
        s.op("dve", lambda e: e.tensor_copy(self.bdb[:, :], self.C("bd")), reads=[self.ctd], writes=[self.cbd])
        s.op("act", lambda e: e.activation(out=self.cond[:, :], in_=self.P("c"), func=AF.Silu),
             reads=[self.ptd], writes=[self.condd])
        with self.phase() as alloc:
            NB = 768
            bufs = [(alloc(f"adaw{i}", [128, 8, NB], F32), Dep()) for i in range(2)]
            items = [(i, cb) for i in layers for cb in range(8)]

            def load(it, t, dep):
                i, cb = it
                src = d["ada_w"][i].rearrange("(kc p) n -> p kc n", p=128)
                s.dma("sp", t[:, :, :], src[:, :, cb * NB:(cb + 1) * NB], writes=[dep])
            st = Stream(bufs, items, load)
            pb, pbd = self.bank(4)
            for i in layers:
                for cb in range(8):
                    t, dep = st.get()
                    for oc in range(6):
                        col = cb * 6 + oc
                        for kc in range(8):
                            s.mm(pb[:, col:col + 1], t[:, kc, oc * 128:(oc + 1) * 128], self.cond[:, kc:kc + 1],
                                 kc == 0, kc == 7, reads=[dep, self.condd], out_dep=pbd)
                s.op("dve", lambda e: e.tensor_tensor(out=self.mod[:, i * 48:(i + 1) * 48], in0=pb[:, 0:48],
                                                      in1=self.P(f"adab{i}"), op=ALU.add),
                     reads=[pbd, self.ptd], writes=[self.modd])
                s.op("dve", lambda e: e.scalar_tensor_tensor(out=self.amod[:, i * 16:i * 16 + 8], in0=self.modc(i, 1), scalar=1.0,
                                                             in1=self.P(f"nmg{i}"), op0=ALU.add, op1=ALU.mult),
                     reads=[self.modd, self.ptd], writes=[self.amodd])
                s.op("dve", lambda e: e.scalar_tensor_tensor(out=self.amod[:, i * 16 + 8:i * 16 + 16], in0=self.modc(i, 4), scalar=1.0,
                                                             in1=self.P(f"nfg{i}"), op0=ALU.add, op1=ALU.mult),
                     reads=[self.modd, self.ptd], writes=[self.amodd])
            s.op("dve", lambda e: e.memset(self.amod[:, NL * 16:NL * 16 + 8], 0.0), writes=[self.amodd])

    def norm_tmps(self, alloc):
        return {"sq": [(alloc(f"sq{i}", [128, 512], BF16), Dep()) for i in range(2)],
                "tmp": [(alloc(f"nt{i}", [128, 512], F32), Dep()) for i in range(2)],
                "rstd": (alloc("rstd", [128, 512], F32), Dep())}

    def norm_mod(self, nt, A, B, t0, nblk, out_fn):
        s = self.s
        sq, tmp = nt["sq"], nt["tmp"]
        rstd, rstdd = nt["rstd"]
        pb, pbd = self.bank(7)
        for tb in range(nblk):
            tsl = slice(t0 + tb * 512, t0 + (tb + 1) * 512)
            xb = (t0 // 512) + tb
            for c in range(DC):
                q, qd = sq[c % 2]
                s.op("act", lambda e: e.activation(out=q[:, :], in_=self.X[:, c, tsl], func=AF.Square, scale=1.0 / 32.0),
                     reads=[self.Xd[c][xb]], writes=[qd])
                s.mm(pb, self.onesb[:, :], q[:, :], c == 0, c == DC - 1, reads=[qd, self.cbd], out_dep=pbd, inc=True)
            s.op("act", lambda e: e.activation(out=rstd[:, :], in_=pb, func=AF.Sqrt, bias=self.epsc[:, 0:1]),
                 reads=[pbd], writes=[rstdd])
            s.op("dve", lambda e: e.reciprocal(out=rstd[:, :], in_=rstd[:, :]), reads=[rstdd], writes=[rstdd])
            for c in range(DC):
                t, td = tmp[c % 2]
                s.op("dve", lambda e: e.tensor_tensor(out=t[:, :], in0=self.X[:, c, tsl], in1=rstd[:, :], op=ALU.mult),
                     reads=[self.Xd[c][xb], rstdd], writes=[td])
                out_fn(c, tb, t, td, A[:, c:c + 1], B[:, c:c + 1])

    def ffn_expert(self, hT, hTd, act, actd, gu_stream, wd_stream, gf, t0, sgb, cb=None):
        s = self.s
        k = 0
        for g in range(FC // 2):
            wt, wdep = gu_stream.get()
            for j in range(2):
                fc = g * 2 + j
                for sb in range(2):
                    bg, bgd = self.bank(0 + (k % 2))
                    bu, bud = self.bank(2 + (k % 2))
                    tsl = slice(sb * 512, (sb + 1) * 512)
                    for kc in range(DC):
                        s.mm(bg, wt[:, kc, 0, j * 128:(j + 1) * 128], hT[:, kc, tsl], kc == 0, kc == DC - 1,
                             reads=[wdep, hTd[sb]], out_dep=bgd)
                    for kc in range(DC):
                        s.mm(bu, wt[:, kc, 1, j * 128:(j + 1) * 128], hT[:, kc, tsl], kc == 0, kc == DC - 1,
                             reads=[wdep, hTd[sb]], out_dep=bud)
                    sg, sgd = sgb[k % 2]
                    s.op("act", lambda e: e.activation(out=sg[:, :], in_=bg, func=AF.Silu), reads=[bgd], writes=[sgd])
                    if cb is not None:
                        cbt, cbdep = cb[sb]
                        s.op("pool", lambda e: e.tensor_tensor(out=sg[:, :], in0=sg[:, :], in1=cbt[:, :], op=ALU.mult),
                             reads=[cbdep, sgd], writes=[sgd])
                    s.op("dve", lambda e: e.tensor_tensor(out=act[:, fc, tsl], in0=sg[:, :], in1=bu, op=ALU.mult),
                         reads=[sgd, bud], writes=[actd[fc][sb]])
                    k += 1
        for dc in range(DC):
            wdt, wddep = wd_stream.get()
            for sb in range(2):
                bo, bod = self.bank(4 + ((dc * 2 + sb) % 2))
                tsl = slice(sb * 512, (sb + 1) * 512)
                for fc in range(FC):
                    s.mm(bo, wdt[:, fc, :], act[:, fc, tsl], fc == 0, fc == FC - 1,
                         reads=[wddep, actd[fc][sb]], out_dep=bod)
                xsl = slice(t0 + sb * 512, t0 + (sb + 1) * 512)
                xb = (t0 // 512) + sb
                s.op("dve", lambda e: e.scalar_tensor_tensor(out=self.X[:, dc, xsl], in0=bo, scalar=gf[:, dc:dc + 1],
                                                             in1=self.X[:, dc, xsl], op0=ALU.mult, op1=ALU.add),
                     reads=[bod, self.modd, self.Xd[dc][xb]], writes=[self.Xd[dc][xb]])

    def ffn_layer(self, i):
        s, d = self.s, self.d
        moe = (i % 2 == 1)
        li = i // 2
        A = self.amod[:, i * 16 + 8:i * 16 + 16]
        B = self.modc(i, 3)
        gf = self.modc(i, 5)
        with self.phase() as alloc:
            hT = alloc("f_hT", [128, DC, 1024], BF16)
            hTd = [Dep(), Dep()]
            act_off = self.sb_ptr
            act = alloc("f_act", [128, FC, 1024], BF16)
            nt = self.norm_tmps(alloc)
            actd = [[Dep(), Dep()] for _ in range(FC)]
            gub = [(alloc(f"f_gu{k}", [128, DC, 2, 256], BF16), Dep()) for k in range(3)]
            wdb = [(alloc(f"f_wd{k}", [128, FC, 128], BF16), Dep()) for k in range(2)]
            sgb = [(alloc(f"f_sg{k}", [128, 512], F32), Dep()) for k in range(2)]
            experts = list(range(NE)) if moe else [None]

            def wgu_ap(e):
                return d["moe_w_gate_up"][li, e] if moe else d["ffn_w_gate_up"][li]

            def wd_ap(e):
                return d["moe_w_down"][li, e] if moe else d["ffn_w_down"][li]

            def load_gu(it, t, dep):
                e, g = it
                src = wgu_ap(e).rearrange("(kc p) n -> p kc n", p=128)
                s.dma("pool", t[:, :, 0, :], src[:, :, g * 256:(g + 1) * 256], writes=[dep])
                s.dma("pool", t[:, :, 1, :], src[:, :, DFF + g * 256:DFF + (g + 1) * 256], writes=[dep])

            def load_wd(it, t, dep):
                e, dc = it
                src = wd_ap(e).rearrange("(fc p) n -> p fc n", p=128)
                s.dma("pool", t[:, :, :], src[:, :, dc * 128:(dc + 1) * 128], writes=[dep])

            gu_items = [(e, g) for _ in range(2) for e in experts for g in range(FC // 2)]
            wd_items = [(e, dc) for _ in range(2) for e in experts for dc in range(DC)]
            gu_stream = Stream(gub, gu_items, load_gu)
            wd_stream = Stream(wdb, wd_items, load_wd)
            if moe:
                h32 = self.salloc("f_h32", [128, DC, 1024], F32, at=act_off)
                h32d = Dep()
                combT = alloc("f_combT", [8, 1024], F32)
                combTd = Dep()
                sel8 = alloc("f_sel8", [8, 8 * 128], F32)
                sel8d = Dep()
                st8 = self.ccols["sel8"][0]
                s.dma("sp", sel8[:, :], d["ctab"][0:8, st8:st8 + 1024], writes=[sel8d])
                cbb = [(alloc(f"f_cb{k}", [128, 512], F32), Dep()) for k in range(4)]
                rt = {nm: (alloc(f"f_rt_{nm}", [128, 8, 8], F32), Dep()) for nm in ("lg", "z", "m", "z2", "ez")}
                rs = {nm: (alloc(f"f_rs_{nm}", [128, 8], F32), Dep()) for nm in ("m1", "m2", "ss")}
            for ps_ in range(2):
                t0 = ps_ * 1024

                def out_fn(c, tb, t, td, Ac, Bc):
                    s.op("act", lambda e: e.activation(out=hT[:, c, tb * 512:(tb + 1) * 512], in_=t[:, :], func=AF.Identity,
                                                       bias=Bc, scale=Ac),
                         reads=[td, self.amodd, self.modd], writes=[hTd[tb]])
                    if moe:
                        s.op("act", lambda e: e.activation(out=h32[:, c, tb * 512:(tb + 1) * 512], in_=t[:, :], func=AF.Identity, bias=Bc, scale=Ac),
                             reads=[td, self.amodd, self.modd], writes=[h32d])
                if moe:
                    s.barrier()
                self.norm_mod(nt, A, B, t0, 2, out_fn)
                if moe:
                    self.router(li, h32, h32d, combT, combTd, rt, rs)
                    s.barrier()
                for e in experts:
                    cb = None
                    if moe:
                        cb = []
                        for sb in range(2):
                            cbt, cbdep = cbb[(e * 2 + sb) % 4]
                            pb, pbd = self.bank(6)
                            s.mm(pb, sel8[0:8, e * 128:(e + 1) * 128], combT[0:8, sb * 512:(sb + 1) * 512], True, True,
                                 reads=[sel8d, combTd], out_dep=pbd)
                            s.op("act", lambda e_: e_.activation(out=cbt[:, :], in_=pb, func=AF.Copy), reads=[pbd], writes=[cbdep])
                            cb.append((cbt, cbdep))
                    self.ffn_expert(hT, hTd, act, actd, gu_stream, wd_stream, gf, t0, sgb, cb)

    def router(self, li, h32, h32d, combT, combTd, rt, rs):
        s = self.s
        G = 8
        pb, pbd = self.bank(6)
        mwr = self.P(f"mwr{li}")
        lg, lgd = rt["lg"]; z, zd = rt["z"]; m, md = rt["m"]; z2, z2d = rt["z2"]; ez, ezd = rt["ez"]
        m1, m1d = rs["m1"]; m2, m2d = rs["m2"]; ss, ssd = rs["ss"]
        for g in range(G):
            for c in range(DC):
                s.mm(pb[:, g * 8:(g + 1) * 8], h32[:, c, g * 128:(g + 1) * 128], mwr[:, c * 8:(c + 1) * 8], c == 0, c == DC - 1,
                     reads=[h32d, self.ptd], out_dep=pbd)
        bc = lambda t: t[:, :].unsqueeze(2).broadcast_to([128, G, 8])
        pv = pb[:, 0:G * 8].rearrange("p (g e) -> p g e", e=8)
        s.op("dve", lambda e: e.tensor_tensor(out=lg[:, :, :], in0=pv, in1=self.P(f"mbr{li}").unsqueeze(1).broadcast_to([128, G, 8]), op=ALU.add),
             reads=[pbd, self.ptd], writes=[lgd])
        s.op("dve", lambda e: e.reduce_max(out=m1[:, :], in_=lg[:, :, :], axis=AX.X), reads=[lgd], writes=[m1d])
        s.op("dve", lambda e: e.tensor_tensor(out=z[:, :, :], in0=lg[:, :, :], in1=bc(m1), op=ALU.subtract), reads=[lgd, m1d], writes=[zd])
        s.op("dve", lambda e: e.tensor_single_scalar(out=m[:, :, :], in_=z[:, :, :], scalar=0.0, op=ALU.is_ge), reads=[zd], writes=[md])
        s.op("dve", lambda e: e.scalar_tensor_tensor(out=z2[:, :, :], in0=m[:, :, :], scalar=-1e30, in1=z[:, :, :], op0=ALU.mult, op1=ALU.add),
             reads=[md, zd], writes=[z2d])
        s.op("dve", lambda e: e.reduce_max(out=m2[:, :], in_=z2[:, :, :], axis=AX.X), reads=[z2d], writes=[m2d])
        s.op("dve", lambda e: e.tensor_tensor(out=m[:, :, :], in0=z[:, :, :], in1=bc(m2), op=ALU.is_ge), reads=[zd, m2d, md], writes=[md])
        s.op("act", lambda e: e.activation(out=ez[:, :, :], in_=z[:, :, :], func=AF.Exp), reads=[zd], writes=[ezd])
        s.op("dve", lambda e: e.tensor_tensor(out=ez[:, :, :], in0=ez[:, :, :], in1=m[:, :, :], op=ALU.mult), reads=[ezd, md], writes=[ezd])
        s.op("dve", lambda e: e.reduce_sum(out=ss[:, :], in_=ez[:, :, :], axis=AX.X), reads=[ezd], writes=[ssd])
        s.op("dve", lambda e: e.reciprocal(out=ss[:, :], in_=ss[:, :]), reads=[ssd], writes=[ssd])
        s.op("dve", lambda e: e.tensor_tensor(out=ez[:, :, :], in0=ez[:, :, :], in1=bc(ss), op=ALU.mult), reads=[ezd, ssd], writes=[ezd])
        for half in range(2):
            pt_, ptd_ = self.bank(7 if half == 0 else 5)
            for gg in range(4):
                g = half * 4 + gg
                s.mm(pt_[0:8, gg * 128:(gg + 1) * 128], ez[:, g, :], self.C("ident"), True, gg == 3, reads=[ezd, self.ctd], out_dep=ptd_)
            s.op("dve", lambda e: e.tensor_copy(out=combT[0:8, half * 512:(half + 1) * 512], in_=pt_[0:8, 0:512]), reads=[ptd_], writes=[combTd])

    def final(self, do_norm=True):
        s, d = self.s, self.d
        ov = d["outT"].rearrange("(c p) s -> p c s", p=128)
        with self.phase() as alloc:
            if not do_norm:
                for c in range(DC):
                    s.dma("sp", ov[:, c, :], self.X[:, c, :], reads=self.Xd[c])
                return
            ob = [(alloc(f"ob{k}", [128, 512], F32), Dep()) for k in range(3)]
            cnt = [0]

            def out_fn(c, tb, t, td, Ac, Bc):
                o, od = ob[cnt[0] % 3]
                cnt[0] += 1
                s.op("act", lambda e: e.activation(out=o[:, :], in_=t[:, :], func=AF.Identity, bias=Bc, scale=Ac),
                     reads=[td, self.amodd, self.ptd], writes=[od])
                s.dma("sp", ov[:, c, tb * 512:(tb + 1) * 512], o[:, :], reads=[od])
            self.norm_mod(self.norm_tmps(alloc), self.P("fng"), self.amod[:, NL * 16:NL * 16 + 8], 0, 4, out_fn)

    def run_plan(self, plan, final_norm=True):
        layers = sorted({i for _, i in plan})
        self.prelude(layers)
        for kind, i in plan:
            if kind == "ffn":
                self.ffn_layer(i)
            elif kind == "mix":
                self.mix_layer(i)
        self.final(final_norm)
        self.s.barrier(["sp"])

    def mix_layer(self, i):
        kind = i % 3
        if kind == 0:
            self.rwkv_layer(i)
        elif kind == 1:
            self.mla_layer(i)
        else:
            self.fox_layer(i)


FULL_PLAN = [(k, i) for i in range(NL) for k in ("mix", "ffn")]


def build(pcols, npcols, plan=None, final_norm=True):
    plan = FULL_PLAN if plan is None else plan
    p = Prog(pcols, npcols)
    p.run_plan(plan, final_norm)
    return p.nc


def make_in_maps(inp, x_override=None):
    ctab = _CT.build()
    maps = []
    pcols = None
    wts = {k: np.ascontiguousarray(np.asarray(inp[k], np.float32)) for k in W_SHAPES}
    for b in range(8):
        pt = param_table(inp, b)
        pcols = pt.cols
        x = np.asarray(inp["x"][b] if x_override is None else x_override[b], np.float32)
        m = {"xT": np.ascontiguousarray(x.T),
             "pos": np.ascontiguousarray(np.asarray(inp["positions"][b], np.int32).reshape(1, S)),
             "ptab": pt.build(), "ctab": ctab}
        m.update(wts)
        maps.append(m)
    return maps, pcols, maps[0]["ptab"].shape[1]


def kernel(**inputs):
    maps, pcols, npc = make_in_maps(inputs)
    nc = build(pcols, npc)
    res = run_bass_kernel_spmd(nc, maps, core_ids=list(range(8)))
    out = np.stack([np.ascontiguousarray(r["outT"].T) for r in res.results], axis=0)
    return out.astype(np.float32)


def _attn_methods():
    def out_proj(self, alloc, oT, oTd, w_ap, gm):
        s = self.s
        wob = [(alloc(f"wo{k}", [128, DC, 128], BF16), Dep()) for k in range(2)]
        src = w_ap.rearrange("(kc p) n -> p kc n", p=128)

        def load(m, t, dep):
            s.dma("pool", t[:, :, :], src[:, :, m * 128:(m + 1) * 128], writes=[dep])
        st = Stream(wob, list(range(DC)), load)
        k = 0
        for m in range(DC):
            wt, wd = st.get()
            for tb in range(4):
                pb, pbd = self.bank(4 + k % 2)
                k += 1
                tsl = slice(tb * 512, (tb + 1) * 512)
                for kc in range(DC):
                    s.mm(pb, wt[:, kc, :], oT[:, kc, tsl], kc == 0, kc == DC - 1, reads=[wd, oTd[tb]], out_dep=pbd)
                s.op("dve", lambda e: e.scalar_tensor_tensor(out=self.X[:, m, tsl], in0=pb, scalar=gm[:, m:m + 1],
                                                             in1=self.X[:, m, tsl], op0=ALU.mult, op1=ALU.add),
                     reads=[pbd, self.modd, self.Xd[m][tb]], writes=[self.Xd[m][tb]])

    def attn_bufs(self, alloc, nP=1, nPT=1):
        return {"Pb": [(alloc(f"a_P{k}", [128, S], BF16), Dep()) for k in range(nP)] * (2 // nP),
                "PT": [(alloc(f"a_PT{k}", [128, 16, 128], BF16), Dep()) for k in range(nPT)] * (2 // nPT),
                "dg": [(alloc(f"a_dg{k}", [128, 128], BF16), Dep()) for k in range(2)],
                "st": [(alloc(f"a_st{k}", [128, 8], F32), Dep()) for k in range(2)],
                "raw": [(alloc(f"a_raw{k}", [128, S], F32), Dep()) for k in range(2)]}

    def attention_pair(self, ab, score_fn, Vt, Vtd, maskb, o_evac):
        s = self.s
        items = [(qb, par) for qb in range(16) for par in range(2)]
        segrr = [0]

        seg_banks = {}

        def a_pe(it):
            qb, par = items[it]
            nk = (qb + 1) * 128
            nseg = (nk + 511) // 512
            seg_banks[it] = []
            for sg in range(nseg):
                k0 = sg * 512
                n = min(512, nk - k0)
                bk = segrr[0]
                segrr[0] = (segrr[0] + 1) % 4
                pb, pbd = self.bank(bk)
                score_fn(par, qb, k0, n, pb[:, 0:n], pbd)
                if sg == nseg - 1:
                    s.mm(pb[:, n - 128:n], self.identb[:, :], maskb[:, :], False, True, reads=[self.cbd], out_dep=pbd)
                else:
                    pbd.w = (("e", "pe"), s.cnt["pe"])
                    pbd.r = {}
                seg_banks[it].append((pb, pbd, k0, n))

        def a_ev(it):
            raw, rawd = ab["raw"][it % 2]
            st, std = ab["st"][it % 2]
            for sg, (pb, pbd, k0, n) in enumerate(seg_banks.pop(it)):
                s.op("act", lambda e: e.activation(out=raw[:, k0:k0 + n], in_=pb[:, 0:n], func=AF.Copy), reads=[pbd], writes=[rawd])
                s.op("dve", lambda e: e.reduce_max(out=st[:, sg:sg + 1], in_=raw[:, k0:k0 + n], axis=AX.X), reads=[rawd], writes=[std])

        def b_soft(it):
            qb, par = items[it]
            nk = (qb + 1) * 128
            nseg = (nk + 511) // 512
            raw, rawd = ab["raw"][it % 2]
            st, std = ab["st"][it % 2]
            Pb, Pbd = ab["Pb"][it % 2]
            dg, dgd = ab["dg"][it % 2]
            if nseg > 1:
                s.op("dve", lambda e: e.reduce_max(out=st[:, 4:5], in_=st[:, 0:nseg], axis=AX.X), reads=[std], writes=[std])
                mcol = st[:, 4:5]
            else:
                mcol = st[:, 0:1]
            s.op("dve", lambda e: e.tensor_scalar(out=st[:, 5:6], in0=mcol, scalar1=-1.0, scalar2=None, op0=ALU.mult), reads=[std], writes=[std])
            s.op("act", lambda e: e.activation(out=Pb[:, 0:nk], in_=raw[:, 0:nk], func=AF.Exp, bias=st[:, 5:6], accum_out=st[:, 6:7]),
                 reads=[rawd, std], writes=[Pbd, std])
            s.op("dve", lambda e: e.reciprocal(out=st[:, 7:8], in_=st[:, 6:7]), reads=[std], writes=[std])
            s.op("dve", lambda e: e.tensor_scalar(out=dg[:, :], in0=self.C("ident"), scalar1=st[:, 7:8], scalar2=None, op0=ALU.mult),
                 reads=[std, self.ctd], writes=[dgd])

        def b_pe(it):
            qb, par = items[it]
            nkb = qb + 1
            Pb, Pbd = ab["Pb"][it % 2]
            PT, PTd = ab["PT"][it % 2]
            dg, dgd = ab["dg"][it % 2]
            for g4 in range((nkb + 3) // 4):
                pb, pbd = self.bank(4 + g4 % 2)
                nj = min(4, nkb - g4 * 4)
                for j in range(nj):
                    kb = g4 * 4 + j
                    s.mm(pb[:, j * 128:(j + 1) * 128], Pb[:, kb * 128:(kb + 1) * 128], dg[:, :], True, j == nj - 1,
                         reads=[Pbd, dgd], out_dep=pbd)
                src = pb[:, 0:nj * 128]
                dst = PT[:, g4 * 4:g4 * 4 + nj, :]
                s.op("dve", lambda e: e.tensor_copy(out=dst, in_=src), reads=[pbd], writes=[PTd])
            ob, obd = self.bank(6 + (it % 2))
            for kb in range(nkb):
                s.mm(ob[:, 0:128], Vt[:, kb, :], PT[:, kb, :], kb == 0, kb == nkb - 1, reads=[Vtd, PTd], out_dep=obd)
            o_evac(par, qb, ob[par * 64:(par + 1) * 64, 0:128], obd)

        a_pe(0)
        a_ev(0)
        for it in range(len(items)):
            if it + 1 < len(items):
                a_pe(it + 1)
            b_soft(it)
            if it + 1 < len(items):
                a_ev(it + 1)
            b_pe(it)

    def rope_tables(self, cosT, sinT, tabd):
        s, d = self.s, self.d
        with self.phase() as alloc:
            posi = alloc("r_posi", [64, S], I32)
            ang = alloc("r_ang", [64, S], F32)
            y = alloc("r_y", [64, S], F32)
            yi = alloc("r_yi", [64, S], I32)
            r = alloc("r_r", [64, S], F32)
            mk = alloc("r_mk", [64, S], F32)
            dd = Dep()
            s.dma("sp", posi[:, :], d["pos"][0:1, :].partition_broadcast(64), writes=[dd])
            s.op("dve", lambda e: e.tensor_copy(out=ang[:, :], in_=posi[:, :]), reads=[dd], writes=[dd])
            s.op("dve", lambda e: e.tensor_scalar(out=ang[:, :], in0=ang[:, :], scalar1=self.C("invf", rows=(0, 64)), scalar2=None,
                                                  op0=ALU.mult), reads=[dd, self.ctd], writes=[dd])
            TWO_PI = 2.0 * math.pi
            for tab, shift in ((sinT, 0.0), (cosT, math.pi / 2.0)):
                s.op("dve", lambda e: e.tensor_scalar(out=y[:, :], in0=ang[:, :], scalar1=shift, scalar2=1.0 / TWO_PI,
                                                      op0=ALU.add, op1=ALU.mult), reads=[dd], writes=[dd])
                s.op("dve", lambda e: e.tensor_copy(out=yi[:, :], in_=y[:, :]), reads=[dd], writes=[dd])
                s.op("dve", lambda e: e.tensor_copy(out=y[:, :], in_=yi[:, :]), reads=[dd], writes=[dd])
                s.op("dve", lambda e: e.scalar_tensor_tensor(out=r[:, :], in0=y[:, :], scalar=-TWO_PI, in1=ang[:, :],
                                                             op0=ALU.mult, op1=ALU.add), reads=[dd], writes=[dd])
                if shift != 0.0:
                    s.op("dve", lambda e: e.tensor_scalar(out=r[:, :], in0=r[:, :], scalar1=shift, scalar2=None, op0=ALU.add),
                         reads=[dd], writes=[dd])
                s.op("dve", lambda e: e.tensor_single_scalar(out=mk[:, :], in_=r[:, :], scalar=math.pi, op=ALU.is_gt), reads=[dd], writes=[dd])
                s.op("dve", lambda e: e.scalar_tensor_tensor(out=r[:, :], in0=mk[:, :], scalar=-TWO_PI, in1=r[:, :],
                                                             op0=ALU.mult, op1=ALU.add), reads=[dd], writes=[dd])
                s.op("dve", lambda e: e.tensor_single_scalar(out=mk[:, :], in_=r[:, :], scalar=-math.pi, op=ALU.is_lt), reads=[dd], writes=[dd])
                s.op("dve", lambda e: e.scalar_tensor_tensor(out=r[:, :], in0=mk[:, :], scalar=TWO_PI, in1=r[:, :],
                                                             op0=ALU.mult, op1=ALU.add), reads=[dd], writes=[dd])
                s.op("dve", lambda e: e.tensor_scalar(out=r[:, :], in0=r[:, :], scalar1=3.14159, scalar2=-3.14159, op0=ALU.min, op1=ALU.max),
                     reads=[dd], writes=[dd])
                s.op("act", lambda e: e.activation(out=tab[:, :], in_=r[:, :], func=AF.Sin), reads=[dd], writes=[tabd])

    def mla_layer(self, i):
        s, d = self.s, self.d
        A = self.amod[:, i * 16:i * 16 + 8]
        B = self.modc(i, 0)
        gm = self.modc(i, 2)
        SC = float((64 + 32) ** -0.5)
        wdown = d["mla_w_down"][0].rearrange("(kc p) n -> p kc n", p=128)
        wuq = d["mla_w_uq"][0].rearrange("(kc p) (h dd) -> p kc h dd", p=128, dd=96)
        wukv = d["mla_w_ukv"][0].rearrange("(kc p) (h dd) -> p kc h dd", p=128, dd=128)
        with self.phase() as alloc:
            cq = alloc("m_cq", [128, 6, S], BF16)
            ckv = alloc("m_ckv", [128, 2, S], BF16)
            kr = alloc("m_kr", [64, S], BF16)
            cqd = [Dep() for _ in range(4)]
            ckvd = [Dep() for _ in range(4)]
            krd = [Dep() for _ in range(4)]
            cosT = alloc("m_cos", [64, S], F32)
            sinT = alloc("m_sin", [64, S], F32)
            tabd = Dep()
            self.rope_tables(cosT, sinT, tabd)
            with self.phase() as a1:
                hT = a1("m_hT", [128, DC, 1024], BF16)
                hTd = [Dep(), Dep()]
                nt = self.norm_tmps(a1)
                wdn = a1("m_wdn", [128, DC, 1024], BF16)
                wdnd = Dep()
                wkr = a1("m_wkr", [128, DC, 128], BF16)
                wkrd = Dep()
                raw = a1("m_raw", [128, 8, 256], F32)
                rawd = [Dep() for _ in range(8)]
                sqt = [(a1(f"m_sq{k}", [128, 256], F32), Dep()) for k in range(2)]
                rq = a1("m_rq", [128, 256], F32)
                rkv = a1("m_rkv", [128, 256], F32)
                rqd, rkvd = Dep(), Dep()
                t1 = a1("m_t1", [64, 256], F32)
                t2 = a1("m_t2", [64, 256], F32)
                t1d, t2d = Dep(), Dep()
                for h2 in range(2):
                    s.dma("pool", wdn[:, :, h2 * 512:(h2 + 1) * 512], wdown[:, :, h2 * 512:(h2 + 1) * 512], writes=[wdnd])
                s.dma("pool", wkr[:, :, 0:32], wdown[:, :, 1024:1056], writes=[wkrd])
                s.dma("pool", wkr[:, :, 32:64], wdown[:, :, 1024:1056], writes=[wkrd])
                s.op("dve", lambda e: e.tensor_scalar(out=wkr[:, :, 64:80], in0=wkr[:, :, 16:32], scalar1=-1.0, scalar2=None, op0=ALU.mult),
                     reads=[wkrd], writes=[wkrd])
                s.op("dve", lambda e: e.tensor_copy(out=wkr[:, :, 80:96], in_=wkr[:, :, 0:16]), reads=[wkrd], writes=[wkrd])
                s.op("dve", lambda e: e.tensor_copy(out=wkr[:, :, 96:128], in_=wkr[:, :, 64:96]), reads=[wkrd], writes=[wkrd])
                for ps_ in range(2):
                    def out_fn(c, tb, t, td, Ac, Bc):
                        s.op("act", lambda e: e.activation(out=hT[:, c, tb * 512:(tb + 1) * 512], in_=t[:, :], func=AF.Identity,
                                                           bias=Bc, scale=Ac),
                             reads=[td, self.amodd, self.modd], writes=[hTd[tb]])
                    self.norm_mod(nt, A, B, ps_ * 1024, 2, out_fn)
                    for blk in range(4):
                        tok0 = ps_ * 1024 + blk * 256
                        tsl = slice(tok0, tok0 + 256)
                        hsl = slice(blk * 256, (blk + 1) * 256)
                        hd = hTd[blk // 2]
                        dq = tok0 // 512
                        bq, bqd = self.bank(2)
                        bkv, bkvd = self.bank(3)
                        for m in range(8):
                            pb, pbd = self.bank(m % 2)
                            for kc in range(DC):
                                s.mm(pb[:, 0:256], wdn[:, kc, m * 128:(m + 1) * 128], hT[:, kc, hsl], kc == 0, kc == DC - 1,
                                     reads=[wdnd, hd], out_dep=pbd)
                            s.op("act", lambda e: e.activation(out=raw[:, m, :], in_=pb[:, 0:256], func=AF.Copy), reads=[pbd], writes=[rawd[m]])
                            q, qd = sqt[m % 2]
                            s.op("pool", lambda e: e.tensor_tensor(out=q[:, :], in0=raw[:, m, :], in1=raw[:, m, :], op=ALU.mult),
                                 reads=[rawd[m]], writes=[qd])
                            if m < 6:
                                s.mm(bq[:, 0:256], self.C("ones"), q[:, :], m == 0, m == 5, reads=[qd, self.ctd], out_dep=bqd, inc=True)
                            else:
                                s.mm(bkv[:, 0:256], self.C("ones"), q[:, :], m == 6, m == 7, reads=[qd, self.ctd], out_dep=bkvd, inc=True)
                        s.op("act", lambda e: e.activation(out=rq[:, :], in_=bq[:, 0:256], func=AF.Sqrt, bias=self.epsc[:, 0:1], scale=1.0 / 768.0),
                             reads=[bqd], writes=[rqd])
                        s.op("dve", lambda e: e.reciprocal(out=rq[:, :], in_=rq[:, :]), reads=[rqd], writes=[rqd])
                        s.op("act", lambda e: e.activation(out=rkv[:, :], in_=bkv[:, 0:256], func=AF.Sqrt, bias=self.epsc[:, 0:1], scale=1.0 / 256.0),
                             reads=[bkvd], writes=[rkvd])
                        s.op("dve", lambda e: e.reciprocal(out=rkv[:, :], in_=rkv[:, :]), reads=[rkvd], writes=[rkvd])
                        for m in range(8):
                            if m < 6:
                                s.op("dve", lambda e: e.scalar_tensor_tensor(out=cq[:, m, tsl], in0=raw[:, m, :], scalar=self.P("mqg", m, m + 1),
                                                                             in1=rq[:, :], op0=ALU.mult, op1=ALU.mult),
                                     reads=[rawd[m], rqd, self.ptd], writes=[cqd[dq]])
                            else:
                                s.op("dve", lambda e: e.scalar_tensor_tensor(out=ckv[:, m - 6, tsl], in0=raw[:, m, :], scalar=self.P("mkvg", m - 6, m - 5),
                                                                             in1=rkv[:, :], op0=ALU.mult, op1=ALU.mult),
                                     reads=[rawd[m], rkvd, self.ptd], writes=[ckvd[dq]])
                        bx, bxd = self.bank(4)
                        br, brd = self.bank(5)
                        for kc in range(DC):
                            s.mm(bx[0:64, 0:256], wkr[:, kc, 0:64], hT[:, kc, hsl], kc == 0, kc == DC - 1, reads=[wkrd, hd], out_dep=bxd)
                        for kc in range(DC):
                            s.mm(br[0:64, 0:256], wkr[:, kc, 64:128], hT[:, kc, hsl], kc == 0, kc == DC - 1, reads=[wkrd, hd], out_dep=brd)
                        s.op("dve", lambda e: e.tensor_tensor(out=t1[:, :], in0=bx[0:64, 0:256], in1=cosT[:, tsl], op=ALU.mult),
                             reads=[bxd, tabd], writes=[t1d])
                        s.op("dve", lambda e: e.tensor_tensor(out=t2[:, :], in0=br[0:64, 0:256], in1=sinT[:, tsl], op=ALU.mult),
                             reads=[brd, tabd], writes=[t2d])
                        s.op("pool", lambda e: e.tensor_tensor(out=kr[:, tsl], in0=t1[:, :], in1=t2[:, :], op=ALU.add),
                             reads=[t1d, t2d], writes=[krd[dq]])
            with self.phase() as a2:
                oT = a2("m_oT", [128, DC, S], BF16)
                oTd = [Dep() for _ in range(4)]
                with self.phase() as a3:
                    ab = self.attn_bufs(a3)
                    wqn = [(a3(f"m_wqn{k}", [128, 6, 128], BF16), Dep()) for k in range(1)]
                    wqr = [(a3(f"m_wqr{k}", [128, 6, 64], BF16), Dep()) for k in range(1)]
                    wqo = [(a3(f"m_wqo{k}", [128, 6, 64], BF16), Dep()) for k in range(1)]
                    wkn = [(a3(f"m_wkn{k}", [128, 2, 128], BF16), Dep()) for k in range(1)]
                    wv = [(a3(f"m_wv{k}", [128, 2, 128], BF16), Dep()) for k in range(1)]
                    qn = a3("m_qn", [128, S], BF16)
                    kn = a3("m_kn", [128, S], BF16)
                    qr = a3("m_qr", [64, S], BF16)
                    Vt = a3("m_Vt", [128, 16, 128], BF16)
                    qnd, knd, qrd, Vtd = Dep(), Dep(), Dep(), Dep()
                    t1 = a3("m_u1", [64, 512], F32)
                    t2 = a3("m_u2", [64, 512], F32)
                    t1d, t2d = Dep(), Dep()
                    for hp in range(8):
                        k2 = 0
                        (wqn_t, wqn_d), (wqr_t, wqr_d), (wqo_t, wqo_d) = wqn[k2], wqr[k2], wqo[k2]
                        (wkn_t, wkn_d), (wv_t, wv_d) = wkn[k2], wv[k2]
                        for hh in range(2):
                            s.dma("pool", wqn_t[:, :, hh * 64:(hh + 1) * 64], wuq[:, :, 2 * hp + hh, 0:64], writes=[wqn_d])
                            s.dma("pool", wqr_t[:, :, hh * 32:(hh + 1) * 32], wuq[:, :, 2 * hp + hh, 64:96], writes=[wqr_d])
                            s.dma("pool", wkn_t[:, :, hh * 64:(hh + 1) * 64], wukv[:, :, 2 * hp + hh, 0:64], writes=[wkn_d])
                            s.dma("pool", wv_t[:, :, hh * 64:(hh + 1) * 64], wukv[:, :, 2 * hp + hh, 64:128], writes=[wv_d])
                        wr4 = wqr_t[:, :, :].rearrange("p k (h e) -> p k h e", h=2)
                        wo4 = wqo_t[:, :, :].rearrange("p k (h e) -> p k h e", h=2)
                        s.op("dve", lambda e: e.tensor_scalar(out=wo4[:, :, :, 0:16], in0=wr4[:, :, :, 16:32], scalar1=-1.0, scalar2=None, op0=ALU.mult),
                             reads=[wqr_d], writes=[wqo_d])
                        s.op("dve", lambda e: e.tensor_copy(out=wo4[:, :, :, 16:32], in_=wr4[:, :, :, 0:16]), reads=[wqr_d], writes=[wqo_d])
                        for tb in range(4):
                            tsl = slice(tb * 512, (tb + 1) * 512)
                            pb, pbd = self.bank(4)
                            for kc in range(6):
                                s.mm(pb, wqn_t[:, kc, :], cq[:, kc, tsl], kc == 0, kc == 5, reads=[wqn_d, cqd[tb]], out_dep=pbd)
                            s.op("act", lambda e: e.activation(out=qn[:, tsl], in_=pb, func=AF.Copy, scale=SC), reads=[pbd], writes=[qnd])
                            pk, pkd = self.bank(5)
                            for kc in range(2):
                                s.mm(pk, wkn_t[:, kc, :], ckv[:, kc, tsl], kc == 0, kc == 1, reads=[wkn_d, ckvd[tb]], out_dep=pkd)
                            s.op("act", lambda e: e.activation(out=kn[:, tsl], in_=pk, func=AF.Copy), reads=[pkd], writes=[knd])
                            bx, bxd = self.bank(6)
                            br, brd = self.bank(7)
                            for kc in range(6):
                                s.mm(bx[0:64, :], wqr_t[:, kc, :], cq[:, kc, tsl], kc == 0, kc == 5, reads=[wqr_d, cqd[tb]], out_dep=bxd)
                            for kc in range(6):
                                s.mm(br[0:64, :], wqo_t[:, kc, :], cq[:, kc, tsl], kc == 0, kc == 5, reads=[wqo_d, cqd[tb]], out_dep=brd)
                            s.op("dve", lambda e: e.scalar_tensor_tensor(out=t1[:, :], in0=bx[0:64, :], scalar=SC, in1=cosT[:, tsl],
                                                                         op0=ALU.mult, op1=ALU.mult), reads=[bxd, tabd], writes=[t1d])
                            s.op("dve", lambda e: e.scalar_tensor_tensor(out=t2[:, :], in0=br[0:64, :], scalar=SC, in1=sinT[:, tsl],
                                                                         op0=ALU.mult, op1=ALU.mult), reads=[brd, tabd], writes=[t2d])
                            s.op("pool", lambda e: e.tensor_tensor(out=qr[:, tsl], in0=t1[:, :], in1=t2[:, :], op=ALU.add),
                                 reads=[t1d, t2d], writes=[qrd])
                        for g in range(4):
                            pb, pbd = self.bank(4 + g % 2)
                            for j in range(4):
                                t16 = g * 4 + j
                                for kc in range(2):
                                    s.mm(pb[:, j * 128:(j + 1) * 128], ckv[:, kc, t16 * 128:(t16 + 1) * 128], wv_t[:, kc, :],
                                         kc == 0, (kc == 1 and j == 3), reads=[wv_d, ckvd[t16 // 4]], out_dep=pbd)
                            s.op("act", lambda e: e.activation(out=Vt[:, g * 4:(g + 1) * 4, :], in_=pb, func=AF.Copy), reads=[pbd], writes=[Vtd])

                        def score_fn(par, qb, k0, n, out_ap, od):
                            ps = slice(par * 64, (par + 1) * 64)
                            rs = slice(par * 32, (par + 1) * 32)
                            qsl = slice(qb * 128, (qb + 1) * 128)
                            s.mm(out_ap, qn[ps, qsl], kn[ps, k0:k0 + n], True, False, reads=[qnd, knd], out_dep=od)
                            s.mm(out_ap, qr[rs, qsl], kr[rs, k0:k0 + n], False, False, reads=[qrd] + krd, out_dep=od, inc=True)

                        def o_evac(par, qb, src, srcd):
                            ps = slice(par * 64, (par + 1) * 64)
                            s.op("act", lambda e: e.activation(out=oT[ps, hp, qb * 128:(qb + 1) * 128], in_=src, func=AF.Copy),
                                 reads=[srcd], writes=[oTd[qb // 4]])
                        self.attention_pair(ab, score_fn, Vt, Vtd, self.mmaskb, o_evac)
                self.out_proj(a2, oT, oTd, d["mla_w_o"][0], gm)

    Prog.out_proj = out_proj
    Prog.attn_bufs = attn_bufs
    Prog.attention_pair = attention_pair
    Prog.rope_tables = rope_tables
    Prog.mla_layer = mla_layer


_attn_methods()


def _fox_methods():
    def fox_layer(self, i):
        s, d = self.s, self.d
        A = self.amod[:, i * 16:i * 16 + 8]
        B = self.modc(i, 0)
        gm = self.modc(i, 2)
        FSC = float(64 ** -0.5)
        win = d["fox_w_in"][0].rearrange("(kc p) n -> p kc n", p=128)
        with self.phase() as alloc:
            hT = alloc("x_hT", [128, DC, S], BF16)
            hTd = [Dep() for _ in range(4)]
            oT = alloc("x_oT", [128, DC, S], BF16)
            oTd = [Dep() for _ in range(4)]
            nFh = alloc("x_nFh", [16, S], BF16)
            nFl = alloc("x_nFl", [16, S], BF16)
            negFd = Dep()
            gsc = alloc("x_gsc", [128, 2], F32)
            gscd = Dep()
            s.op("dve", lambda e: e.tensor_scalar(out=gsc[:, 0:1], in0=self.P("fqg"), scalar1=FSC, scalar2=None, op0=ALU.mult),
                 reads=[self.ptd], writes=[gscd])
            s.op("dve", lambda e: e.tensor_copy(out=gsc[:, 1:2], in_=self.P("fkg")), reads=[self.ptd], writes=[gscd])
            with self.phase() as a1:
                nt = self.norm_tmps(a1)

                def out_fn(c, tb, t, td, Ac, Bc):
                    s.op("act", lambda e: e.activation(out=hT[:, c, tb * 512:(tb + 1) * 512], in_=t[:, :], func=AF.Identity,
                                                       bias=Bc, scale=Ac),
                         reads=[td, self.amodd, self.modd], writes=[hTd[tb]])
                self.norm_mod(nt, A, B, 0, 4, out_fn)
                wf = a1("x_wf", [128, DC, 16], BF16)
                wfd = Dep()
                s.dma("pool", wf[:, :, :], win[:, :, 3072:3088], writes=[wfd])
                z = a1("x_z", [16, 512], F32)
                az = a1("x_az", [16, 512], F32)
                lf = a1("x_lf", [16, 512], F32)
                ones16 = a1("x_ones", [16, 512], F32)
                Fc = a1("x_F", [16, S], F32)
                zd = Dep()
                s.op("dve", lambda e: e.memset(ones16[:, :], 1.0), writes=[zd])
                for tb in range(4):
                    tsl = slice(tb * 512, (tb + 1) * 512)
                    pb, pbd = self.bank(4)
                    for kc in range(DC):
                        s.mm(pb[0:16, :], wf[:, kc, :], hT[:, kc, tsl], kc == 0, kc == DC - 1, reads=[wfd, hTd[tb]], out_dep=pbd)
                    s.op("act", lambda e: e.activation(out=z[:, :], in_=pb[0:16, :], func=AF.Identity, bias=self.P("fbf")[0:16, :]),
                         reads=[pbd, self.ptd, zd], writes=[zd])
                    s.op("act", lambda e: e.activation(out=az[:, :], in_=z[:, :], func=AF.Abs), reads=[zd], writes=[zd])
                    s.op("act", lambda e: e.activation(out=az[:, :], in_=az[:, :], func=AF.Exp, scale=-1.0), reads=[zd], writes=[zd])
                    s.op("act", lambda e: e.activation(out=az[:, :], in_=az[:, :], func=AF.Ln, bias=self.epsc[0:16, 2:3]), reads=[zd], writes=[zd])
                    s.op("dve", lambda e: e.tensor_scalar(out=lf[:, :], in0=z[:, :], scalar1=0.0, scalar2=None, op0=ALU.min), reads=[zd], writes=[zd])
                    s.op("dve", lambda e: e.tensor_tensor(out=lf[:, :], in0=lf[:, :], in1=az[:, :], op=ALU.subtract), reads=[zd], writes=[zd])
                    init = 0.0 if tb == 0 else Fc[:, tb * 512 - 1:tb * 512]
                    s.op("dve", lambda e: e.tensor_tensor_scan(Fc[:, tsl], ones16[:, :], lf[:, :], init, ALU.mult, ALU.add),
                         reads=[zd], writes=[zd])
                negF = a1("x_negF", [16, S], F32)
                nF32 = a1("x_nF32", [16, S], F32)
                s.op("dve", lambda e: e.tensor_scalar(out=negF[:, :], in0=Fc[:, :], scalar1=-1.0, scalar2=None, op0=ALU.mult),
                     reads=[zd], writes=[zd])
                s.op("dve", lambda e: e.tensor_copy(out=nFh[:, :], in_=negF[:, :]), reads=[zd], writes=[negFd])
                s.op("dve", lambda e: e.tensor_copy(out=nF32[:, :], in_=nFh[:, :]), reads=[negFd, zd], writes=[zd])
                s.op("dve", lambda e: e.tensor_tensor(out=nFl[:, :], in0=negF[:, :], in1=nF32[:, :], op=ALU.subtract), reads=[zd], writes=[negFd])
            with self.phase() as a3:
                ab = self.attn_bufs(a3)
                wq = [(a3(f"x_wq{k}", [128, DC, 128], BF16), Dep()) for k in range(1)]
                wk = [(a3(f"x_wk{k}", [128, DC, 128], BF16), Dep()) for k in range(1)]
                wv = [(a3(f"x_wv{k}", [128, DC, 128], BF16), Dep()) for k in range(1)]
                wg = [(a3(f"x_wg{k}", [128, DC, 128], BF16), Dep()) for k in range(1)]
                selp = [(a3(f"x_sel{k}", [16, 256], BF16), Dep()) for k in range(2)]
                qn = a3("x_qn", [128, S], BF16)
                kn = a3("x_kn", [128, S], BF16)
                og = a3("x_og", [128, S], BF16)
                Vt = a3("x_Vt", [128, 16, 128], BF16)
                qnd, knd, ogd, Vtd = Dep(), Dep(), Dep(), Dep()
                raw = [(a3(f"x_raw{k}", [128, 512], F32), Dep()) for k in range(2)]
                sq = [(a3(f"x_sq{k}", [128, 512], F32), Dep()) for k in range(2)]
                rs_ = [(a3(f"x_rs{k}", [128, 512], F32), Dep()) for k in range(2)]
                st16 = self.ccols["sel16"][0]
                for hp in range(8):
                    k2 = 0
                    (wq_t, wq_d), (wk_t, wk_d), (wv_t, wv_d), (wg_t, wg_d) = wq[k2], wk[k2], wv[k2], wg[k2]
                    sel_t, sel_d = selp[hp % 2]
                    s.dma("pool", wq_t[:, :, :], win[:, :, hp * 128:(hp + 1) * 128], writes=[wq_d])
                    s.dma("pool", wk_t[:, :, :], win[:, :, 1024 + hp * 128:1024 + (hp + 1) * 128], writes=[wk_d])
                    s.dma("pool", wv_t[:, :, :], win[:, :, 2048 + hp * 128:2048 + (hp + 1) * 128], writes=[wv_d])
                    s.dma("pool", wg_t[:, :, :], win[:, :, 3088 + hp * 128:3088 + (hp + 1) * 128], writes=[wg_d])
                    s.dma("pool", sel_t[:, :], d["ctab"][0:16, st16 + hp * 256:st16 + (hp + 1) * 256], writes=[sel_d])
                    for tb in range(4):
                        tsl = slice(tb * 512, (tb + 1) * 512)
                        for which, (w_t, w_d), dst, dstd, gcol in ((0, (wq_t, wq_d), qn, qnd, 0), (1, (wk_t, wk_d), kn, knd, 1)):
                            pb, pbd = self.bank(4 + which)
                            for kc in range(DC):
                                s.mm(pb, w_t[:, kc, :], hT[:, kc, tsl], kc == 0, kc == DC - 1, reads=[w_d, hTd[tb]], out_dep=pbd)
                            r_t, r_d = raw[which]
                            q_t, q_d = sq[which]
                            rr_t, rr_d = rs_[which]
                            s.op("act", lambda e: e.activation(out=r_t[:, :], in_=pb, func=AF.Copy), reads=[pbd], writes=[r_d])
                            s.op("pool", lambda e: e.tensor_tensor(out=q_t[:, :], in0=r_t[:, :], in1=r_t[:, :], op=ALU.mult), reads=[r_d], writes=[q_d])
                            p2, p2d = self.bank(6 + which)
                            s.mm(p2, self.C("bd"), q_t[:, :], True, True, reads=[q_d, self.ctd], out_dep=p2d)
                            s.op("act", lambda e: e.activation(out=rr_t[:, :], in_=p2, func=AF.Sqrt, bias=self.epsc[:, 0:1], scale=1.0 / 64.0),
                                 reads=[p2d], writes=[rr_d])
                            s.op("dve", lambda e: e.reciprocal(out=rr_t[:, :], in_=rr_t[:, :]), reads=[rr_d], writes=[rr_d])
                            s.op("dve", lambda e: e.scalar_tensor_tensor(out=dst[:, tsl], in0=r_t[:, :], scalar=gsc[:, gcol:gcol + 1],
                                                                         in1=rr_t[:, :], op0=ALU.mult, op1=ALU.mult),
                                 reads=[r_d, rr_d, gscd], writes=[dstd])
                        pg, pgd = self.bank(4)
                        for kc in range(DC):
                            s.mm(pg, wg_t[:, kc, :], hT[:, kc, tsl], kc == 0, kc == DC - 1, reads=[wg_d, hTd[tb]], out_dep=pgd)
                        s.op("act", lambda e: e.activation(out=og[:, tsl], in_=pg, func=AF.Sigmoid), reads=[pgd], writes=[ogd])
                    for g in range(4):
                        pb, pbd = self.bank(4 + g % 2)
                        for j in range(4):
                            t16 = g * 4 + j
                            for kc in range(DC):
                                s.mm(pb[:, j * 128:(j + 1) * 128], hT[:, kc, t16 * 128:(t16 + 1) * 128], wv_t[:, kc, :],
                                     kc == 0, (kc == DC - 1 and j == 3), reads=[wv_d, hTd[t16 // 4]], out_dep=pbd)
                        s.op("act", lambda e: e.activation(out=Vt[:, g * 4:(g + 1) * 4, :], in_=pb, func=AF.Copy), reads=[pbd], writes=[Vtd])

                    def score_fn(par, qb, k0, n, out_ap, od):
                        ps = slice(par * 64, (par + 1) * 64)
                        qsl = slice(qb * 128, (qb + 1) * 128)
                        s.mm(out_ap, qn[ps, qsl], kn[ps, k0:k0 + n], True, False, reads=[qnd, knd], out_dep=od)
                        s.mm(out_ap, sel_t[0:16, par * 128:(par + 1) * 128], nFh[0:16, k0:k0 + n], False, False,
                             reads=[sel_d, negFd], out_dep=od)
                        s.mm(out_ap, sel_t[0:16, par * 128:(par + 1) * 128], nFl[0:16, k0:k0 + n], False, False,
                             reads=[sel_d, negFd], out_dep=od, inc=True)

                    def o_evac(par, qb, src, srcd):
                        ps = slice(par * 64, (par + 1) * 64)
                        qsl = slice(qb * 128, (qb + 1) * 128)
                        s.op("dve", lambda e: e.tensor_tensor(out=oT[ps, hp, qsl], in0=src, in1=og[ps, qsl], op=ALU.mult),
                             reads=[srcd, ogd], writes=[oTd[qb // 4]])
                    self.attention_pair(ab, score_fn, Vt, Vtd, self.cmaskb, o_evac)
            self.out_proj(alloc, oT, oTd, d["fox_w_o"][0], gm)

    Prog.fox_layer = fox_layer


_fox_methods()


def _rwkv_methods():
    def rw_scratch(self):
        if not hasattr(self, "_scr"):
            nc = self.nc
            self._scr = {nm: nc.dram_tensor(f"scr_{nm}", [D, S], F32).ap() for nm in ("r", "k", "v", "sg", "a", "g", "vf", "xs")}
            self._scr["yg"] = nc.dram_tensor("scr_yg", [D, S], BF16).ap()
        return self._scr

    def rwkv_layer(self, i):
        j = i // 3
        lvl = getattr(self, "dbg_rw", 3)
        self.rwkv_pass1(i, j)
        if lvl >= 2:
            self.rwkv_pass2(i, j)
        if lvl >= 3:
            self.rwkv_pass3(i, j)

    def rwkv_pass1(self, i, j):
        s, d = self.s, self.d
        scr = self.rw_scratch()
        A = self.amod[:, i * 16:i * 16 + 8]
        B = self.modc(i, 0)
        fmv = lambda ap: ap.rearrange("(c p) n -> p c n", p=128)
        with self.phase() as alloc:
            hT = alloc("w_hT", [128, DC, S + 1], BF16)
            hTd = [Dep() for _ in range(4)]
            h0d = Dep()
            s.op("dve", lambda e: e.memset(hT[:, :, 0:1], 0.0), writes=[h0d])
            nt = self.norm_tmps(alloc)

            def out_fn(c, tb, t, td, Ac, Bc):
                s.op("act", lambda e: e.activation(out=hT[:, c, 1 + tb * 512:1 + (tb + 1) * 512], in_=t[:, :], func=AF.Identity,
                                                   bias=Bc, scale=Ac),
                     reads=[td, self.amodd, self.modd], writes=[hTd[tb]])
            self.norm_mod(nt, A, B, 0, 4, out_fn)
            omu = alloc("w_omu", [128, 48], F32)
            omud = Dep()
            s.op("dve", lambda e: e.tensor_scalar(out=omu[:, :], in0=self.P(f"mu{j}"), scalar1=-1.0, scalar2=1.0, op0=ALU.mult, op1=ALU.add),
                 reads=[self.ptd], writes=[omud])
            xn = [(alloc(f"w_xn{k}", [128, DC, 512], BF16), Dep()) for k in range(2)]
            tmpx = [(alloc(f"w_tx{k}", [128, 512], F32), Dep()) for k in range(4)]
            wbig = [(alloc(f"w_big{k}", [128, DC, 1024], BF16), Dep()) for k in range(2)]
            w1 = alloc("w_w1", [128, DC, 64], BF16)
            a1 = alloc("w_a1", [128, DC, 64], BF16)
            g1 = alloc("w_g1", [128, DC, 160], BF16)
            w2 = alloc("w_w2", [64, D], BF16)
            a2 = alloc("w_a2", [64, D], BF16)
            g2a = alloc("w_g2a", [128, D], BF16)
            g2b = alloc("w_g2b", [32, D], BF16)
            lwd = Dep()
            kcv = lambda ap: ap.rearrange("(kc p) n -> p kc n", p=128)
            s.dma("pool", w1[:, :, :], kcv(d["rw_w1"][j]), writes=[lwd])
            s.dma("pool", a1[:, :, :], kcv(d["rw_a1"][j]), writes=[lwd])
            s.dma("pool", g1[:, :, :], kcv(d["rw_g1"][j]), writes=[lwd])
            s.dma("pool", w2[:, :], d["rw_w2"][j], writes=[lwd])
            s.dma("pool", a2[:, :], d["rw_a2"][j], writes=[lwd])
            s.dma("pool", g2a[:, :], d["rw_g2"][j][0:128, :], writes=[lwd])
            s.dma("pool", g2b[:, :], d["rw_g2"][j][128:160, :], writes=[lwd])
            vres = (j > 0)
            if vres:
                v1 = alloc("w_v1", [128, DC, 32], BF16)
                v2 = alloc("w_v2", [32, D], BF16)
                s.dma("pool", v1[:, :, :], kcv(d["rw_v1"][j - 1]), writes=[lwd])
                s.dma("pool", v2[:, :], d["rw_v2"][j - 1], writes=[lwd])
                vgt = alloc("w_vgt", [128, DC, 512], BF16)
                vgd = Dep()
                vft = [(alloc(f"w_vf{k}", [128, 512], F32), Dep()) for k in range(2)]
            stage = [(alloc(f"w_st{k}", [128, 512], F32), Dep()) for k in range(3)]
            t1 = alloc("w_t1", [128, 512], BF16)
            t1b = alloc("w_t1b", [32, 512], BF16)
            t1d = Dep()
            stc = [0]

            def emit_out(pb, pbd, func, bias, dst, m, tb, post=None):
                st, std = stage[stc[0] % 3]
                stc[0] += 1
                kw = {} if bias is None else {"bias": bias}
                s.op("act", lambda e: e.activation(out=st[:, :], in_=pb, func=func, **kw), reads=[pbd, self.ptd], writes=[std])
                if post is not None:
                    post(st, std)
                s.dma("sp", fmv(dst)[:, m, tb * 512:(tb + 1) * 512], st[:, :], reads=[std])

            big_items = [0, 1, 2]

            def load_big(which, t, dep):
                src = kcv(d["rw_w_rkv"][j, which])
                for h2 in range(2):
                    s.dma("pool", t[:, :, h2 * 512:(h2 + 1) * 512], src[:, :, h2 * 512:(h2 + 1) * 512], writes=[dep])
            bst = Stream(wbig, big_items, load_big)
            xc = [0]

            def make_xn(n, tb):
                x_t, x_d = xn[xc[0] % 2]
                xc[0] += 1
                for c in range(DC):
                    tx, txd = tmpx[c % 4]
                    col = n * 8 + c
                    s.op("act", lambda e: e.activation(out=tx[:, :], in_=hT[:, c, tb * 512:tb * 512 + 512], func=AF.Copy,
                                                       scale=self.P(f"mu{j}", col, col + 1)),
                         reads=[hTd[tb], hTd[max(tb - 1, 0)], h0d, self.ptd], writes=[txd])
                    s.op("dve", lambda e: e.scalar_tensor_tensor(out=x_t[:, c, :], in0=hT[:, c, 1 + tb * 512:1 + tb * 512 + 512],
                                                               scalar=omu[:, col:col + 1], in1=tx[:, :], op0=ALU.mult, op1=ALU.add),
                         reads=[hTd[tb], omud, txd], writes=[x_d])
                return x_t, x_d

            def big_proj(x_t, x_d, wt, wd, m):
                pb, pbd = self.bank(m % 4)
                for kc in range(DC):
                    s.mm(pb, wt[:, kc, m * 128:(m + 1) * 128], x_t[:, kc, :], kc == 0, kc == DC - 1, reads=[wd, x_d], out_dep=pbd)
                return pb, pbd

            def lora(x_t, x_d, wa, ncols, func1, wb, m_bias_name, func2, dst, tb, wb2=None):
                pb, pbd = self.bank(4)
                n1 = min(ncols, 128)
                for kc in range(DC):
                    s.mm(pb[0:n1, :], wa[:, kc, 0:n1], x_t[:, kc, :], kc == 0, kc == DC - 1, reads=[lwd, x_d], out_dep=pbd)
                s.op("act", lambda e: e.activation(out=t1[0:n1, :], in_=pb[0:n1, :], func=func1), reads=[pbd], writes=[t1d])
                if ncols > 128:
                    pb2, pb2d = self.bank(5)
                    for kc in range(DC):
                        s.mm(pb2[0:32, :], wa[:, kc, 128:160], x_t[:, kc, :], kc == 0, kc == DC - 1, reads=[lwd, x_d], out_dep=pb2d)
                    s.op("act", lambda e: e.activation(out=t1b[0:32, :], in_=pb2[0:32, :], func=func1), reads=[pb2d], writes=[t1d])
                for m in range(DC):
                    po, pod = self.bank(m % 4)
                    s.mm(po, wb[0:n1, m * 128:(m + 1) * 128], t1[0:n1, :], True, wb2 is None, reads=[lwd, t1d], out_dep=pod)
                    if wb2 is not None:
                        s.mm(po, wb2[0:32, m * 128:(m + 1) * 128], t1b[0:32, :], False, True, reads=[lwd, t1d], out_dep=pod)
                    bias = None if m_bias_name is None else self.P(m_bias_name, m, m + 1)
                    if dst is None:
                        s.op("act", lambda e: e.activation(out=vgt[:, m, :], in_=po, func=func2, bias=bias), reads=[pod, self.ptd], writes=[vgd])
                    else:
                        emit_out(po, pod, func2, bias, dst, m, tb)

            for n in range(6):
                if n in (0, 2, 3):
                    wt, wd = bst.get()
                for tb in range(4):
                    x_t, x_d = make_xn(n, tb)
                    if n == 0:
                        for m in range(DC):
                            pb, pbd = big_proj(x_t, x_d, wt, wd, m)
                            emit_out(pb, pbd, AF.Copy, None, scr["r"], m, tb)
                    elif n == 2:
                        for m in range(DC):
                            pb, pbd = big_proj(x_t, x_d, wt, wd, m)
                            emit_out(pb, pbd, AF.Copy, None, scr["k"], m, tb)
                    elif n == 3:
                        if vres:
                            lora(x_t, x_d, v1, 32, AF.Copy, v2, "v0", AF.Sigmoid, None, tb)
                        for m in range(DC):
                            pb, pbd = big_proj(x_t, x_d, wt, wd, m)
                            if not vres:
                                emit_out(pb, pbd, AF.Copy, None, scr["vf"], m, tb)
                            else:
                                vf, vfd = vft[m % 2]
                                s.dma("sp", vf[:, :], fmv(scr["vf"])[:, m, tb * 512:(tb + 1) * 512], writes=[vfd])

                                def post(st, std, m=m, vf=vf, vfd=vfd):
                                    s.op("dve", lambda e: e.tensor_tensor(out=vf[:, :], in0=vf[:, :], in1=st[:, :], op=ALU.subtract),
                                         reads=[vfd, std], writes=[vfd])
                                    s.op("pool", lambda e: e.tensor_tensor(out=vf[:, :], in0=vf[:, :], in1=vgt[:, m, :], op=ALU.mult),
                                         reads=[vfd, vgd], writes=[vfd])
                                    s.op("dve", lambda e: e.tensor_tensor(out=st[:, :], in0=st[:, :], in1=vf[:, :], op=ALU.add),
                                         reads=[vfd, std], writes=[std])
                                emit_out(pb, pbd, AF.Copy, None, scr["v"], m, tb, post=post)
                    elif n == 1:
                        lora(x_t, x_d, w1, 64, AF.Tanh, w2, f"w0{j}", AF.Sigmoid, scr["sg"], tb)
                    elif n == 4:
                        lora(x_t, x_d, a1, 64, AF.Copy, a2, f"a0{j}", AF.Sigmoid, scr["a"], tb)
                    else:
                        lora(x_t, x_d, g1, 160, AF.Sigmoid, g2a, None, AF.Copy, scr["g"], tb, wb2=g2b)

    def rwkv_pass3(self, i, j):
        s, d = self.s, self.d
        scr = self.rw_scratch()
        gm = self.modc(i, 2)
        with self.phase() as alloc:
            YG = alloc("w_YG", [128, DC, S], BF16)
            YGd = [Dep() for _ in range(4)]
            src = scr["yg"].rearrange("(c p) n -> p c n", p=128)
            for tb in range(4):
                s.dma("sp", YG[:, :, tb * 512:(tb + 1) * 512], src[:, :, tb * 512:(tb + 1) * 512], writes=[YGd[tb]])
            self.out_proj(alloc, YG, YGd, d["rw_w_o"][j], gm)

    Prog.rw_scratch = rw_scratch
    Prog.rwkv_layer = rwkv_layer
    Prog.rwkv_pass1 = rwkv_pass1
    Prog.rwkv_pass3 = rwkv_pass3


_rwkv_methods()


def _rwkv2_methods():
    def rwkv_pass2(self, i, j):
        s, d = self.s, self.d
        scr = self.rw_scratch()
        fmv = lambda ap: ap.rearrange("(c p) n -> p c n", p=128)
        vsrc = scr["v"] if j > 0 else scr["vf"]
        xs = fmv(scr["xs"])
        for c in range(DC):
            s.dma("sp", xs[:, c, :], self.X[:, c, :], reads=self.Xd[c])
        s.barrier()
        xptr = [SB_BASE]

        def xalloc(name, shape, dt):
            nbytes = (int(np.prod(shape[1:])) * (2 if dt == BF16 else 4) + 31) // 32 * 32
            off = xptr[0]
            if off + nbytes > SB_BASE + DC * S * 4:
                return self.salloc(name, shape, dt)
            xptr[0] += nbytes
            return self.salloc(name, shape, dt, at=off)

        with self.phase() as alloc:
            rmask = alloc("p_rmask", [128, 8, 64], F32)
            cd = Dep()
            s.op("dve", lambda e: e.memset(rmask[:, :, :], 1.0), writes=[cd])
            s.op("dve", lambda e: e.memset(rmask[:, :, 0:1], 0.0), writes=[cd])
            omka = alloc("p_omka", [128, 8], F32)
            s.op("dve", lambda e: e.tensor_scalar(out=omka[:, :], in0=self.P(f"k_a{j}"), scalar1=-1.0, scalar2=1.0, op0=ALU.mult, op1=ALU.add),
                 reads=[self.ptd], writes=[cd])
            tiny = alloc("p_tiny", [128, 1], F32)
            s.op("dve", lambda e: e.memset(tiny[:, :], 1e-24), writes=[cd])
            Sf = alloc("p_Sf", [64, 16, 64], F32)
            Sb = alloc("p_Sb", [64, 16, 64], BF16)
            Sfd = [Dep() for _ in range(4)]
            Sbd = [Dep() for _ in range(4)]
            s.op("dve", lambda e: e.memset(Sf[:, :, :], 0.0), writes=Sfd)
            s.op("dve", lambda e: e.memset(Sb[:, :, :], 0.0), writes=Sbd)
            bc8 = lambda nm: self.P(nm).unsqueeze(2).broadcast_to([128, 8, 64])
            names = ("r", "k", "v", "sg", "a", "g")
            srcs = {"r": scr["r"], "k": scr["k"], "v": vsrc, "sg": scr["sg"], "a": scr["a"], "g": scr["g"]}
            Lb = [{nm: xalloc(f"p_L{nm}{k}", [128, 8, 128], F32) for nm in names} for k in range(2)]
            Ld = [Dep(), Dep()]

            def load_sb(sb, bufs, dep):
                for nm in names:
                    s.dma("sp", bufs[nm][:, :, :], fmv(srcs[nm])[:, :, sb * 128:(sb + 1) * 128], writes=[dep])
            lst = Stream([(Lb[0], Ld[0]), (Lb[1], Ld[1])], list(range(16)), load_sb)
            def T(nm, dt=F32, shape=(128, 8, 64)):
                return xalloc("p_" + nm, list(shape), dt)
            prep = []
            inter_names = ("lw", "cum", "cumE", "e1", "e2", "e3", "e4", "kkr", "sqk", "rn", "kk", "ta", "k2", "beta", "rk")
            inter = {nm: T(nm, BF16 if nm in ("sqk", "rk") else F32) for nm in inter_names}
            inter_d = {nm: Dep() for nm in inter_names}
            for k in range(2):
                pt_ = dict(inter)
                pt_.update({nm: T(f"{nm}{k}") for nm in ("bonus", "dg")})
                pt_["PL"] = xalloc(f"p_PL{k}", [128, 8], F32)
                pt_["PLs"] = xalloc(f"p_PLs{k}", [128, 8, 2], F32)
                pt_["PLh"] = xalloc(f"p_PLh{k}", [64, 16], F32)
                s.op("dve", lambda e: e.memset(pt_["PLs"][:, :, :], 0.0), writes=[cd])
                pt_["AR"] = T(f"AR{k}", BF16, (128, 8, 128))
                for nm in ("Bt", "Kt", "Bh", "Kh", "vb"):
                    pt_[nm] = T(f"{nm}{k}", BF16)
                pt_["d"] = {nm: Dep() for nm in ("bonus", "dg", "PL", "PLs", "PLh", "AR", "Bt", "Kt", "Bh", "Kh", "vb")}
                pt_["d"].update(inter_d)
                prep.append(pt_)
            tmaj = []
            for k in range(2):
                tm = {"AX": alloc(f"p_AX{k}", [64, 16, 128], BF16), "B": alloc(f"p_tmB{k}", [64, 1024], BF16),
                      "K": alloc(f"p_tmK{k}", [64, 1024], BF16), "V": alloc(f"p_tmV{k}", [64, 1024], BF16)}
                tm["d"] = {"AXa": Dep(), "AXx": [Dep() for _ in range(4)], "B": Dep(), "K": Dep(), "V": Dep()}
                tmaj.append(tm)
            grp = []
            for k in range(2):
                gb = {"Mbm": alloc(f"p_Mbm{k}", [64, 4, 128], F32), "Mbr": alloc(f"p_Mbr{k}", [64, 4, 64], BF16),
                      "Mkb": alloc(f"p_Mkb{k}", [64, 4, 128], BF16), "ATt": alloc(f"p_ATt{k}", [64, 4, 64], BF16),
                      "AT": alloc(f"p_AT{k}", [64, 4, 128], BF16), "Tb": alloc(f"p_Tb{k}", [64, 4, 64], BF16),
                      "AU": alloc(f"p_AU{k}", [64, 4, 128], BF16), "Rh": alloc(f"p_Rh{k}", [64, 4, 64], BF16),
                      "Phi": alloc(f"p_Phi{k}", [64, 4, 64], F32), "Sn": alloc(f"p_Sn{k}", [64, 4, 64], F32)}
                gb["d"] = {nm: Dep() for nm in ("Mbm", "Mbr", "Mkb", "ATt", "AT", "Tb", "AU", "Rh", "Phi", "Sn")}
                grp.append(gb)
            Yall = [(alloc(f"p_Y{k}", [64, 16, 64], F32), [Dep() for _ in range(4)]) for k in range(2)]
            Yc = alloc("p_Yc", [64, 16, 64], F32)
            Ysq = alloc("p_Ysq", [64, 16, 64], F32)
            Ycb = alloc("p_Ycb", [64, 16, 64], BF16)
            ycbd = Dep()
            yst = alloc("p_yst", [64, 64], F32)
            ycd, ysqd, ystd = Dep(), Dep(), Dep()
            yf = alloc("p_yf", [128, 8, 64], F32)
            yfd = Dep()
            ygs = [(alloc(f"p_ygs{k}", [128, 8, 256], BF16), Dep()) for k in range(2)]
            gnb = alloc("p_gnb", [64, 1], F32)
            s.op("dve", lambda e: e.memset(gnb[:, :], 64e-5), writes=[cd])
            rrb = [4]

            def nb():
                b = rrb[0]
                rrb[0] = 4 + (rrb[0] - 4 + 1) % 4
                return self.bank(b)
            ident = self.C("ident")
            mask_ar = self.C("mask_ar", rows=(0, 64)).unsqueeze(1).broadcast_to([64, 4, 128])
            maskT = self.C("maskT", rows=(0, 64)).unsqueeze(1).broadcast_to([64, 4, 64])
            id64b = self.C("ident", 0, 64, rows=(0, 64)).unsqueeze(1).broadcast_to([64, 4, 64])
            identh_b = self.C("identh").unsqueeze(1).broadcast_to([128, 8, 64])
            v4 = lambda ap, n: ap.rearrange("p (h c) -> p h c", c=n)

            Lcur = [None]
            Rodd2 = [(alloc(f"p_Rodd{k}", [64, 8, 64], F32), Dep()) for k in range(2)]
            gq2 = [(alloc(f"p_gq{k}", [128, 8, 64], F32), Dep()) for k in range(2)]
            nch = getattr(self, "dbg_nch", 32)

            def serial_pre(c):
                if False:
                    yield
                sbk, ch = c // 2, c % 2
                if ch == 0:
                    Lcur[0] = lst.get()
                L, Ldep = Lcur[0]
                csl = slice(ch * 64, (ch + 1) * 64)
                Rodd_c, Roddd_c = Rodd2[c % 2]
                gq_c, gqd_c = gq2[c % 2]
                P_ = prep[c % 2]
                pd = P_["d"]
                Lr, Lk, Lv, Lsg, La, Lg = (L[nm][:, :, csl] for nm in names)

                def op(eng, fn, reads, writes):
                    s.op(eng, fn, reads=[(pd[x] if isinstance(x, str) else x) for x in reads],
                         writes=[(pd[x] if isinstance(x, str) else x) for x in writes])
                op("dve", lambda e: e.tensor_scalar(out=P_["lw"][:, :, :], in0=Lsg, scalar1=-0.6065306597126334, scalar2=None, op0=ALU.mult),
                   [Ldep], ["lw"])
                op("dve", lambda e: e.tensor_tensor_scan(P_["cum"][:, :, :].rearrange("p a b -> p (a b)"), rmask[:, :, :].rearrange("p a b -> p (a b)"),
                                                         P_["lw"][:, :, :].rearrange("p a b -> p (a b)"), 0.0, ALU.mult, ALU.add),
                   ["lw", cd], ["cum"])
                op("pool", lambda e: e.tensor_tensor(out=P_["cumE"][:, :, :], in0=P_["cum"][:, :, :], in1=P_["lw"][:, :, :], op=ALU.subtract),
                   ["cum", "lw"], ["cumE"])
                op("act", lambda e: e.activation(out=P_["e1"][:, :, :], in_=P_["cumE"][:, :, :], func=AF.Exp), ["cumE"], ["e1"])
                op("act", lambda e: e.activation(out=P_["e2"][:, :, :], in_=P_["cum"][:, :, :], func=AF.Exp), ["cum"], ["e2"])
                op("act", lambda e: e.activation(out=P_["e3"][:, :, :], in_=P_["cum"][:, :, :], func=AF.Exp, scale=-1.0), ["cum"], ["e3"])
                op("dve", lambda e: e.tensor_tensor(out=P_["e4"][:, :, :], in0=P_["cum"][:, :, :],
                                                    in1=P_["cum"][:, :, 63:64].broadcast_to([128, 8, 64]), op=ALU.subtract), ["cum"], ["e4"])
                op("act", lambda e: e.activation(out=P_["e4"][:, :, :], in_=P_["e4"][:, :, :], func=AF.Exp, scale=-1.0), ["e4"], ["e4"])
                op("act", lambda e: e.activation(out=P_["PL"][:, :].unsqueeze(2), in_=P_["cum"][:, :, 63:64], func=AF.Exp), ["cum"], ["PL"])
                op("pool", lambda e: e.tensor_copy(out=P_["PLs"][0:64, :, 0:1], in_=P_["PL"][0:64, :].unsqueeze(2)), ["PL", cd], ["PLs"])
                op("pool", lambda e: e.tensor_copy(out=P_["PLs"][64:128, :, 1:2], in_=P_["PL"][64:128, :].unsqueeze(2)), ["PL", cd], ["PLs"])
                bpl, bpld = self.bank(3)
                s.mm(bpl[0:64, 0:16], self.C("identh"), P_["PLs"][:, :, :].rearrange("p a b -> p (a b)"), True, True, reads=[pd["PLs"], self.ctd], out_dep=bpld)
                op("dve", lambda e: e.tensor_copy(out=P_["PLh"][:, :], in_=bpl[0:64, 0:16]), [bpld], ["PLh"])
                yield
                op("dve", lambda e: e.tensor_tensor(out=P_["kkr"][:, :, :], in0=Lk, in1=bc8(f"k_k{j}"), op=ALU.mult), [Ldep, self.ptd], ["kkr"])
                op("act", lambda e: e.activation(out=P_["sqk"][:, :, :], in_=P_["kkr"][:, :, :], func=AF.Square), ["kkr"], ["sqk"])
                yield
                bs, bsd = self.bank(2)
                for hp in range(8):
                    s.mm(bs[:, hp * 64:(hp + 1) * 64], self.bdb[:, :], P_["sqk"][:, hp, :], True, hp == 7, reads=[pd["sqk"], self.cbd], out_dep=bsd)
                op("act", lambda e: e.activation(out=P_["rn"][:, :, :], in_=v4(bs, 64), func=AF.Sqrt, bias=tiny[:, 0:1]), [bsd, cd], ["rn"])
                op("dve", lambda e: e.reciprocal(out=P_["rn"][:, :, :], in_=P_["rn"][:, :, :]), ["rn"], ["rn"])
                op("dve", lambda e: e.tensor_tensor(out=P_["kk"][:, :, :], in0=P_["kkr"][:, :, :], in1=P_["rn"][:, :, :], op=ALU.mult), ["kkr", "rn"], ["kk"])
                yield
                op("pool", lambda e: e.tensor_tensor(out=P_["ta"][:, :, :], in0=La, in1=bc8(f"k_a{j}"), op=ALU.mult), [Ldep, self.ptd], ["ta"])
                op("pool", lambda e: e.tensor_tensor(out=P_["ta"][:, :, :], in0=P_["ta"][:, :, :], in1=omka[:, :].unsqueeze(2).broadcast_to([128, 8, 64]),
                                                     op=ALU.add), ["ta", cd], ["ta"])
                op("dve", lambda e: e.tensor_tensor(out=P_["k2"][:, :, :], in0=Lk, in1=P_["ta"][:, :, :], op=ALU.mult), [Ldep, "ta"], ["k2"])
                op("pool", lambda e: e.tensor_tensor(out=P_["beta"][:, :, :], in0=P_["kk"][:, :, :], in1=La, op=ALU.mult), ["kk", Ldep], ["beta"])
                yield
                op("dve", lambda e: e.scalar_tensor_tensor(out=P_["AR"][:, :, 0:64], in0=P_["kk"][:, :, :], scalar=-1.0, in1=P_["e1"][:, :, :],
                                                           op0=ALU.mult, op1=ALU.mult), ["kk", "e1"], ["AR"])
                op("pool", lambda e: e.tensor_tensor(out=P_["AR"][:, :, 64:128], in0=Lr, in1=P_["e2"][:, :, :], op=ALU.mult), [Ldep, "e2"], ["AR"])
                op("dve", lambda e: e.tensor_tensor(out=P_["Bt"][:, :, :], in0=P_["beta"][:, :, :], in1=P_["e3"][:, :, :], op=ALU.mult), ["beta", "e3"], ["Bt"])
                op("pool", lambda e: e.tensor_tensor(out=P_["Kt"][:, :, :], in0=P_["k2"][:, :, :], in1=P_["e3"][:, :, :], op=ALU.mult), ["k2", "e3"], ["Kt"])
                op("dve", lambda e: e.tensor_tensor(out=P_["Bh"][:, :, :], in0=P_["beta"][:, :, :], in1=P_["e4"][:, :, :], op=ALU.mult), ["beta", "e4"], ["Bh"])
                op("pool", lambda e: e.tensor_tensor(out=P_["Kh"][:, :, :], in0=P_["k2"][:, :, :], in1=P_["e4"][:, :, :], op=ALU.mult), ["k2", "e4"], ["Kh"])
                yield
                s.op("act", lambda e: e.activation(out=gq_c[:, :, :], in_=Lg, func=AF.Copy), reads=[Ldep], writes=[gqd_c])
                op("act", lambda e: e.activation(out=P_["vb"][:, :, :], in_=Lv, func=AF.Copy), [Ldep], ["vb"])
                op("pool", lambda e: e.tensor_tensor(out=P_["rk"][:, :, :], in0=Lr, in1=P_["k2"][:, :, :], op=ALU.mult), [Ldep, "k2"], ["rk"])
                op("pool", lambda e: e.tensor_tensor(out=P_["rk"][:, :, :], in0=P_["rk"][:, :, :], in1=bc8(f"r_k{j}"), op=ALU.mult), ["rk", self.ptd], ["rk"])
                yield
                bb, bbd = self.bank(3)
                for hp in range(8):
                    s.mm(bb[:, hp * 64:(hp + 1) * 64], self.bdb[:, :], P_["rk"][:, hp, :], True, hp == 7, reads=[pd["rk"], self.cbd], out_dep=bbd)
                op("dve", lambda e: e.tensor_tensor(out=P_["bonus"][:, :, :], in0=v4(bb, 64), in1=Lv, op=ALU.mult), [bbd, Ldep], ["bonus"])
                tm = tmaj[c % 2]
                td_ = tm["d"]
                for ti, (srcf, sdep, dst, ddeps) in enumerate((
                        (lambda hp: P_["AR"][:, hp, 0:64], "AR", tm["AX"][:, :, 0:64], [td_["AXa"]]),
                        (lambda hp: P_["Bh"][:, hp, :], "Bh", tm["B"][:, :].rearrange("p (h c) -> p h c", c=64), [td_["B"]]),
                        (lambda hp: P_["Kh"][:, hp, :], "Kh", tm["K"][:, :].rearrange("p (h c) -> p h c", c=64), [td_["K"]]),
                        (lambda hp: P_["vb"][:, hp, :], "vb", tm["V"][:, :].rearrange("p (h c) -> p h c", c=64), [td_["V"]]))):
                    yield
                    half = 1024
                    b0 = 2
                    for hp in range(8):
                        s.mm(self.psA[0:64, half + hp * 128:half + (hp + 1) * 128], srcf(hp), self.identb[:, :], True, hp % 4 == 3,
                             reads=[pd[sdep], self.cbd], out_dep=self.bd_[b0 + hp // 4])
                    src = self.psA[0:64, half:half + 1024].rearrange("p (h c) -> p h c", c=64)
                    if ti % 2 == 0:
                        s.op("act", lambda e: e.activation(out=dst, in_=src, func=AF.Copy), reads=[self.bd_[b0], self.bd_[b0 + 1]], writes=ddeps)
                    else:
                        s.op("dve", lambda e: e.tensor_copy(out=dst, in_=src), reads=[self.bd_[b0], self.bd_[b0 + 1]], writes=ddeps)
                yield
                bro, brod = self.bank(2)
                for hp in range(8):
                    s.mm(bro[0:64, hp * 64:(hp + 1) * 64], self.identb[64:128, 64:128], P_["AR"][64:128, hp, 64:128], True, hp == 7,
                         reads=[self.cbd, pd["AR"]], out_dep=brod)
                s.op("act", lambda e: e.activation(out=Rodd_c[:, :, :], in_=v4(bro[0:64, :], 64), func=AF.Copy), reads=[brod], writes=[Roddd_c])

            def make_groups(c):
                sbk, ch = c // 2, c % 2
                P_ = prep[c % 2]
                pd = P_["d"]
                tm = tmaj[c % 2]
                td_ = tm["d"]
                Yt, Ytd = Yall[c % 2]

                def op(eng, fn, reads, writes):
                    s.op(eng, fn, reads=[(pd[x] if isinstance(x, str) else x) for x in reads],
                         writes=[(pd[x] if isinstance(x, str) else x) for x in writes])
                def gsteps(g, mybanks):
                    rr_ = [0]

                    def nb():
                        b_ = mybanks[rr_[0] % len(mybanks)]
                        rr_[0] += 1
                        return self.bank(b_)
                    G = grp[g % 2]
                    gd = G["d"]
                    hb = (g // 2) * 8 + (g % 2)
                    heads = [(hb + 2 * hh, (hb + 2 * hh) // 2, hb % 2) for hh in range(4)]
                    hs = slice(hb, hb + 7, 2)
                    hpsl = slice(hb // 2, hb // 2 + 4)
                    gpar = hb % 2
                    b1, b1d = nb()
                    b2, b2d = nb()
                    b3, b3d = nb()
                    for hh, (h, hp, par) in enumerate(heads):
                        ps = slice(par * 64, (par + 1) * 64)
                        s.mm(b1[0:64, hh * 128:(hh + 1) * 128], P_["Bt"][ps, hp, :], P_["AR"][ps, hp, :], True, hh == 3, reads=[pd["Bt"], pd["AR"]], out_dep=b1d)
                    for hh, (h, hp, par) in enumerate(heads):
                        ps = slice(par * 64, (par + 1) * 64)
                        s.mm(b2[0:64, hh * 128:(hh + 1) * 128], P_["Kt"][ps, hp, :], P_["AR"][ps, hp, :], True, hh == 3, reads=[pd["Kt"], pd["AR"]], out_dep=b2d)
                    for hh, (h, hp, par) in enumerate(heads):
                        ps = slice(par * 64, (par + 1) * 64)
                        s.mm(b3[0:64, hh * 64:(hh + 1) * 64], P_["AR"][ps, hp, 0:64], P_["Bt"][ps, hp, :], True, hh == 3, reads=[pd["Bt"], pd["AR"]], out_dep=b3d)
                    if getattr(self, 'dbg_m', 9) < 1:
                        return
                    s.op("dve", lambda e: e.tensor_tensor(out=G["Mbm"][:, :, :], in0=v4(b1[0:64, :], 128), in1=mask_ar, op=ALU.mult),
                         reads=[b1d, self.ctd], writes=[gd["Mbm"]])
                    s.op("dve", lambda e: e.tensor_tensor(out=G["Mkb"][:, :, :], in0=v4(b2[0:64, :], 128), in1=mask_ar, op=ALU.mult),
                         reads=[b2d, self.ctd], writes=[gd["Mkb"]])
                    s.op("dve", lambda e: e.tensor_tensor(out=G["ATt"][:, :, :], in0=v4(b3[0:64, 0:256], 64), in1=maskT, op=ALU.mult),
                         reads=[b3d, self.ctd], writes=[gd["ATt"]])
                    if getattr(self, 'dbg_m', 9) < 2:
                        return
                    s.op("act", lambda e: e.activation(out=G["AT"][:, :, 0:64], in_=G["Mbm"][:, :, 0:64], func=AF.Copy), reads=[gd["Mbm"]], writes=[gd["AT"]])
                    s.op("pool", lambda e: e.tensor_tensor(out=G["AT"][:, :, 64:128], in0=G["Mbm"][:, :, 0:64], in1=id64b, op=ALU.add),
                         reads=[gd["Mbm"], self.ctd], writes=[gd["AT"]])
                    s.op("act", lambda e: e.activation(out=G["Mbr"][:, :, :], in_=G["Mbm"][:, :, 64:128], func=AF.Copy), reads=[gd["Mbm"]], writes=[gd["Mbr"]])
                    if getattr(self, 'dbg_sub', 9) < 2:
                        return
                    yield
                    for rnd in range(6):
                        if rnd > 0:
                            yield
                        bA, bAd = nb()
                        if rnd == 0:
                            bB, bBd = nb()
                            for hh in range(4):
                                s.mm(bA[0:64, hh * 64:(hh + 1) * 64], G["ATt"][:, hh, :], G["AT"][:, hh, 0:64], True, hh == 3, reads=[gd["ATt"], gd["AT"]], out_dep=bAd)
                            for hh in range(4):
                                s.mm(bB[0:64, hh * 64:(hh + 1) * 64], G["AT"][:, hh, 0:64], G["ATt"][:, hh, :], True, hh == 3, reads=[gd["ATt"], gd["AT"]], out_dep=bBd)
                            s.op("act", lambda e: e.activation(out=G["AT"][:, :, 0:64], in_=v4(bA[0:64, 0:256], 64), func=AF.Copy), reads=[bAd], writes=[gd["AT"]])
                            s.op("act", lambda e: e.activation(out=G["ATt"][:, :, :], in_=v4(bB[0:64, 0:256], 64), func=AF.Copy), reads=[bBd], writes=[gd["ATt"]])
                        elif rnd < 5:
                            bB, bBd = nb()
                            for hh in range(4):
                                s.mm(bA[0:64, hh * 128:(hh + 1) * 128], G["ATt"][:, hh, :], G["AT"][:, hh, :], True, hh == 3, reads=[gd["ATt"], gd["AT"]], out_dep=bAd)
                            for hh in range(4):
                                s.mm(bB[0:64, hh * 64:(hh + 1) * 64], G["AT"][:, hh, 0:64], G["ATt"][:, hh, :], True, hh == 3, reads=[gd["ATt"], gd["AT"]], out_dep=bBd)
                            bA4 = v4(bA[0:64, :], 128)
                            s.op("dve", lambda e: e.tensor_tensor(out=G["AT"][:, :, 64:128], in0=G["AT"][:, :, 64:128], in1=bA4[:, :, 64:128], op=ALU.add),
                                 reads=[bAd, gd["AT"]], writes=[gd["AT"]])
                            s.op("dve", lambda e: e.tensor_copy(out=G["AT"][:, :, 0:64], in_=bA4[:, :, 0:64]), reads=[bAd, gd["AT"]], writes=[gd["AT"]])
                            s.op("act", lambda e: e.activation(out=G["ATt"][:, :, :], in_=v4(bB[0:64, 0:256], 64), func=AF.Copy), reads=[bBd], writes=[gd["ATt"]])
                        else:
                            for hh in range(4):
                                s.mm(bA[0:64, hh * 64:(hh + 1) * 64], G["ATt"][:, hh, :], G["AT"][:, hh, 64:128], True, hh == 3, reads=[gd["ATt"], gd["AT"]], out_dep=bAd)
                            s.op("dve", lambda e: e.tensor_tensor(out=G["Tb"][:, :, :], in0=G["AT"][:, :, 64:128], in1=v4(bA[0:64, 0:256], 64), op=ALU.add),
                                 reads=[bAd, gd["AT"]], writes=[gd["Tb"]])
                    if getattr(self, 'dbg_sub', 9) < 3:
                        return
                    yield
                    bx, bxd = nb()
                    for hh, (h, hp, par) in enumerate(heads):
                        s.mm(bx[0:64, hh * 64:(hh + 1) * 64], G["Mkb"][:, hh, 0:64], tm["V"][:, h * 64:(h + 1) * 64], True, hh == 3,
                             reads=[gd["Mkb"], td_["V"]], out_dep=bxd)
                    s.op("act", lambda e: e.activation(out=tm["AX"][:, hs, 64:128], in_=v4(bx[0:64, 0:256], 64), func=AF.Copy),
                         reads=[bxd], writes=[td_["AXx"][g]])
                    yield
                    bu, bud = nb()
                    for hh, (h, hp, par) in enumerate(heads):
                        s.mm(bu[0:64, hh * 128:(hh + 1) * 128], G["Tb"][:, hh, :], tm["AX"][:, h, :], True, hh == 3,
                             reads=[gd["Tb"], td_["AXa"], td_["AXx"][g]], out_dep=bud)
                    s.op("act", lambda e: e.activation(out=G["AU"][:, :, :], in_=v4(bu[0:64, :], 128), func=AF.Copy), reads=[bud], writes=[gd["AU"]])
                    if getattr(self, 'dbg_sub', 9) < 4:
                        return
                    yield
                    br_, brd = nb()
                    bp, bpd = nb()
                    for hh, (h, hp, par) in enumerate(heads):
                        ps = slice(par * 64, (par + 1) * 64)
                        s.mm(br_[0:64, hh * 64:(hh + 1) * 64], G["AU"][:, hh, 0:64], G["Mbr"][:, hh, :], True, hh == 3, reads=[gd["AU"], gd["Mbr"]], out_dep=brd)
                    for hh, (h, hp, par) in enumerate(heads):
                        ps = slice(par * 64, (par + 1) * 64)
                        s.mm(bp[0:64, hh * 64:(hh + 1) * 64], G["AU"][:, hh, 0:64], tm["B"][:, h * 64:(h + 1) * 64], True, hh == 3, reads=[gd["AU"], td_["B"]], out_dep=bpd)
                    rsrc = P_["AR"][0:64, hpsl, 64:128] if gpar == 0 else Rodd2[c % 2][0][:, hpsl, :]
                    s.op("dve", lambda e: e.tensor_tensor(out=G["Rh"][:, :, :], in0=v4(br_[0:64, 0:256], 64), in1=rsrc, op=ALU.add),
                         reads=[brd, pd["AR"], Rodd2[c % 2][1]], writes=[gd["Rh"]])
                    s.op("pool", lambda e: e.tensor_tensor(out=G["Phi"][:, :, :], in0=id64b, in1=P_["PLh"][:, hs].unsqueeze(2).broadcast_to([64, 4, 64]),
                                                           op=ALU.mult), reads=[pd["PLh"], self.ctd], writes=[gd["Phi"]])
                    s.op("dve", lambda e: e.tensor_tensor(out=G["Phi"][:, :, :], in0=G["Phi"][:, :, :], in1=v4(bp[0:64, 0:256], 64), op=ALU.add),
                         reads=[bpd, gd["Phi"]], writes=[gd["Phi"]])
                    if getattr(self, 'dbg_sub', 9) < 5:
                        return
                    yield
                    by, byd = nb()
                    for hh, (h, hp, par) in enumerate(heads):
                        o_ = by[0:64, hh * 64:(hh + 1) * 64]
                        s.mm(o_, G["Mbr"][:, hh, :], G["AU"][:, hh, 64:128], True, False, reads=[gd["Mbr"], gd["AU"]], out_dep=byd)
                        s.mm(o_, G["Mkb"][:, hh, 64:128], tm["V"][:, h * 64:(h + 1) * 64], False, False, reads=[gd["Mkb"], td_["V"]], out_dep=byd)
                        s.mm(o_, G["Rh"][:, hh, :], Sb[:, h, :], False, hh == 3, reads=[gd["Rh"], Sbd[g]], out_dep=byd)
                    s.op("act", lambda e: e.activation(out=Yt[:, hs, :], in_=v4(by[0:64, 0:256], 64), func=AF.Copy), reads=[byd], writes=[Ytd[g]])
                    if getattr(self, 'dbg_sub', 9) < 6:
                        return
                    yield
                    bn, bnd = nb()
                    bn2, bn2d = nb()
                    for hh, (h, hp, par) in enumerate(heads):
                        o_ = bn[0:64, hh * 64:(hh + 1) * 64]
                        s.mm(o_, tm["B"][:, h * 64:(h + 1) * 64], G["AU"][:, hh, 64:128], True, False, reads=[td_["B"], gd["AU"]], out_dep=bnd)
                        s.mm(o_, tm["K"][:, h * 64:(h + 1) * 64], tm["V"][:, h * 64:(h + 1) * 64], False, hh == 3, reads=[td_["K"], td_["V"]], out_dep=bnd)
                    for hh, (h, hp, par) in enumerate(heads):
                        s.mm(bn2[0:64, hh * 64:(hh + 1) * 64], G["Phi"][:, hh, :], Sf[:, h, :], True, hh == 3, reads=[gd["Phi"], Sfd[g]], out_dep=bn2d)
                    s.op("act", lambda e: e.activation(out=G["Sn"][:, :, :], in_=v4(bn[0:64, 0:256], 64), func=AF.Copy), reads=[bnd], writes=[gd["Sn"]])
                    s.op("dve", lambda e: e.tensor_tensor(out=Sf[:, hs, :], in0=G["Sn"][:, :, :], in1=v4(bn2[0:64, 0:256], 64), op=ALU.add),
                         reads=[bn2d, gd["Sn"]], writes=[Sfd[g]])
                    s.op("act", lambda e: e.activation(out=Sb[:, hs, :], in_=Sf[:, hs, :], func=AF.Copy), reads=[Sfd[g]], writes=[Sbd[g]])

                return gsteps

            def serial_out(c):
                if False:
                    yield
                sbk, ch = c // 2, c % 2
                P_ = prep[c % 2]
                pd = P_["d"]
                tm = tmaj[c % 2]
                td_ = tm["d"]
                Yt, Ytd = Yall[c % 2]

                def op(eng, fn, reads, writes):
                    s.op(eng, fn, reads=[(pd[x] if isinstance(x, str) else x) for x in reads],
                         writes=[(pd[x] if isinstance(x, str) else x) for x in writes])
                s.op("dve", lambda e: e.reduce_sum(out=yst[:, 0:16], in_=Yt[:, :, :], axis=AX.X), reads=Ytd, writes=[ystd])
                s.op("dve", lambda e: e.tensor_scalar(out=yst[:, 0:16], in0=yst[:, 0:16], scalar1=1.0 / 64.0, scalar2=None, op0=ALU.mult), reads=[ystd], writes=[ystd])
                s.op("pool", lambda e: e.tensor_tensor(out=Yc[:, :, :], in0=Yt[:, :, :], in1=yst[:, 0:16].unsqueeze(2).broadcast_to([64, 16, 64]), op=ALU.subtract),
                     reads=Ytd + [ystd], writes=[ycd])
                yield
                s.op("act", lambda e: e.activation(out=Ysq[:, :, :], in_=Yc[:, :, :], func=AF.Square), reads=[ycd], writes=[ysqd])
                s.op("dve", lambda e: e.reduce_sum(out=yst[:, 16:32], in_=Ysq[:, :, :], axis=AX.X), reads=[ysqd, ystd], writes=[ystd])
                s.op("act", lambda e: e.activation(out=yst[:, 16:32], in_=yst[:, 16:32], func=AF.Sqrt, bias=gnb[:, 0:1], scale=1.0 / 64.0), reads=[ystd, cd], writes=[ystd])
                s.op("dve", lambda e: e.reciprocal(out=yst[:, 16:32], in_=yst[:, 16:32]), reads=[ystd], writes=[ystd])
                s.op("pool", lambda e: e.tensor_tensor(out=Ycb[:, :, :], in0=Yc[:, :, :], in1=yst[:, 16:32].unsqueeze(2).broadcast_to([64, 16, 64]), op=ALU.mult),
                     reads=[ycd, ystd, ysqd], writes=[ycbd])
                yield
                bo, bod = self.bank(2)
                Ycf = Ycb[:, :, :].rearrange("p a b -> p (a b)")
                for hp in range(8):
                    s.mm(bo[:, hp * 64:(hp + 1) * 64], Ycf[:, hp * 128:(hp + 1) * 128], self.identb[0:64, 0:64], True, hp == 7, reads=[ycbd, self.cbd], out_dep=bod)
                s.op("dve", lambda e: e.tensor_tensor(out=yf[:, :, :], in0=v4(bo, 64), in1=bc8(f"lnx_g{j}"), op=ALU.mult), reads=[bod, self.ptd], writes=[yfd])
                s.op("pool", lambda e: e.tensor_tensor(out=yf[:, :, :], in0=yf[:, :, :], in1=bc8(f"lnx_b{j}"), op=ALU.add), reads=[yfd, self.ptd], writes=[yfd])
                s.op("pool", lambda e: e.tensor_tensor(out=yf[:, :, :], in0=yf[:, :, :], in1=P_["bonus"][:, :, :], op=ALU.add), reads=[yfd, pd["bonus"]], writes=[yfd])
                yield
                yg_t, yg_d = ygs[(c // 4) % 2]
                s.op("dve", lambda e: e.tensor_tensor(out=yg_t[:, :, (c % 4) * 64:(c % 4 + 1) * 64], in0=yf[:, :, :], in1=gq2[c % 2][0][:, :, :], op=ALU.mult),
                     reads=[yfd, gq2[c % 2][1]], writes=[yg_d])
                if c % 4 == 3:
                    c0 = (c // 4) * 256
                    s.dma("sp", fmv(scr["yg"])[:, :, c0:c0 + 256], yg_t[:, :, :], reads=[yg_d])

            def chain(*gens):
                for g_ in gens:
                    if g_ is not None:
                        yield from g_

            for _ in serial_pre(0):
                pass
            for c in range(nch):
                ser = chain(serial_out(c - 1) if c > 0 else None, serial_pre(c + 1) if c + 1 < nch else None)
                gsteps = make_groups(c)
                ser_live = True
                for pair in ((0, 1), (2, 3)):
                    live = [gsteps(pair[0], [4, 5, 6]), gsteps(pair[1], [7, 0, 1])]
                    while live:
                        for gg in list(live):
                            try:
                                next(gg)
                            except StopIteration:
                                live.remove(gg)
                        if ser_live:
                            try:
                                next(ser)
                            except StopIteration:
                                ser_live = False
                if ser_live:
                    for _ in ser:
                        pass
            for _ in serial_out(nch - 1):
                pass
        self.Xd = [[Dep() for _ in range(4)] for _ in range(DC)]
        for c in range(DC):
            s.dma("sp", self.X[:, c, :], xs[:, c, :], writes=self.Xd[c])

    Prog.rwkv_pass2 = rwkv_pass2


_rwkv2_methods()
```
